# Optimizing a Trainium2 kernel written in Bass

```python
import math
import jax, jax.numpy as jnp
from jax import lax
import numpy as np

D_MODEL = 2048
BATCH = 16
SEQ = 2048
DEPTH = 4

CHUNK = 64
Q_BLOCK = 128
D_MIX = D_MODEL
GROUP_W = D_MIX // 4

FOX_HD = 64
FOX_HEADS = GROUP_W // FOX_HD

MLA_HEADS = 4
MLA_NOPE = 128
MLA_ROPE = 64
MLA_VD = GROUP_W // MLA_HEADS
MLA_Q_RANK = 384
MLA_KV_RANK = 128
ROPE_THETA = 10000.0

RWKV_HD = 64
RWKV_HEADS = GROUP_W // RWKV_HD
RWKV_W_RANK = 64
RWKV_A_RANK = 64
RWKV_G_RANK = 128
RWKV_GN_EPS = 64e-5

GDN_HD = 128
GDN_HEADS = GROUP_W // GDN_HD
GDN_CONV = 4

N_GROUPS = 4
EXP_PER_GROUP = 8
N_EXPERTS = N_GROUPS * EXP_PER_GROUP
TOP_K = 2
D_EXPERT = 512
MOE_BLOCK = 128

ALPHA = (2 * DEPTH) ** 0.25
BETA = (8 * DEPTH) ** -0.25
LN_EPS = 1e-5
RMS_EPS = 1e-6

FOX_COLS = (GROUP_W, GROUP_W, GROUP_W, FOX_HEADS)
MLA_COLS = (MLA_Q_RANK, MLA_KV_RANK, MLA_ROPE)
RWKV_COLS = (GROUP_W, GROUP_W, GROUP_W, RWKV_W_RANK, RWKV_A_RANK, RWKV_G_RANK)
GDN_COLS = (GROUP_W, GROUP_W, GROUP_W, GROUP_W, GDN_HEADS, GDN_HEADS)
GROUP_IN_COLS = (sum(FOX_COLS), sum(MLA_COLS), sum(RWKV_COLS), sum(GDN_COLS))
N_IN = sum(GROUP_IN_COLS)

kernel_name = 'hybrid_chunk_causal_hier_moe_trunk'


def _split(u, sizes):
    cuts = np.cumsum(np.asarray(sizes))[:-1].tolist()
    return jnp.split(u, cuts, axis=-1)


def layer_norm(x, g, b):
    xf = x.astype(jnp.float32)
    mu = jnp.mean(xf, axis=-1, keepdims=True)
    var = jnp.mean(jnp.square(xf - mu), axis=-1, keepdims=True)
    return ((xf - mu) * lax.rsqrt(var + LN_EPS) * g + b).astype(x.dtype)


def rms_norm(x, g):
    xf = x.astype(jnp.float32)
    return (xf * lax.rsqrt(jnp.mean(xf * xf, axis=-1, keepdims=True) + RMS_EPS) * g).astype(x.dtype)


def l2_normalize(x):
    xf = x.astype(jnp.float32)
    return (xf * lax.rsqrt(jnp.sum(xf * xf, axis=-1, keepdims=True) + 1e-6)).astype(x.dtype)


def time_shift(u):
    return jnp.pad(u, ((0, 0), (1, 0), (0, 0)))[:, :-1]


def causal_depthwise_conv(u, w):
    K = w.shape[0]
    S = u.shape[1]
    up = jnp.pad(u, ((0, 0), (K - 1, 0), (0, 0)))
    return sum(up[:, j:j + S] * w[j] for j in range(K))


def rope(x, positions):
    half = x.shape[-1] // 2
    inv_freq = ROPE_THETA ** (-jnp.arange(half, dtype=jnp.float32) / half)
    ang = positions.astype(jnp.float32)[..., None] * inv_freq
    cos = jnp.cos(ang)[:, :, None, :]
    sin = jnp.sin(ang)[:, :, None, :]
    x1 = x[..., :half].astype(jnp.float32)
    x2 = x[..., half:].astype(jnp.float32)
    return jnp.concatenate([x1 * cos - x2 * sin, x2 * cos + x1 * sin], axis=-1).astype(x.dtype)


def block_causal_attention(q, k, v, chunk, log_fgate_cum=None):
    B, H, S, Dk = q.shape
    nb = S // Q_BLOCK
    scale = Dk ** -0.5
    key_chunk = jnp.arange(S) // chunk
    qb = jnp.moveaxis(q.reshape(B, H, nb, Q_BLOCK, Dk), 2, 0)
    use_decay = log_fgate_cum is not None

    def attend(args):
        i, qi = args
        s = jnp.einsum('bhqd,bhkd->bhqk', qi, k, preferred_element_type=jnp.float32) * scale
        if use_decay:
            Fi = lax.dynamic_slice_in_dim(log_fgate_cum, i * Q_BLOCK, Q_BLOCK, axis=2)
            s = s + (Fi[..., :, None] - log_fgate_cum[..., None, :])
        q_chunk = (i * Q_BLOCK + jnp.arange(Q_BLOCK)) // chunk
        allowed = key_chunk[None, :] <= q_chunk[:, None]
        s = jnp.where(allowed, s, -1e30)
        p = jax.nn.softmax(s, axis=-1)
        return jnp.einsum('bhqk,bhkd->bhqd', p.astype(v.dtype), v)

    out = lax.map(attend, (jnp.arange(nb), qb))
    return jnp.moveaxis(out, 0, 2).reshape(B, H, S, v.shape[-1])


def fox_mixer(u, b_f, out_g):
    B, S, _ = u.shape
    q, k, v, f = _split(u, FOX_COLS)
    heads = lambda t: t.reshape(B, S, FOX_HEADS, FOX_HD).transpose(0, 2, 1, 3)
    log_f = jax.nn.log_sigmoid(f.astype(jnp.float32) + b_f)
    F = jnp.cumsum(log_f, axis=1).transpose(0, 2, 1)
    o = block_causal_attention(heads(q), heads(k), heads(v), 1, F)
    o = o.transpose(0, 2, 1, 3).reshape(B, S, GROUP_W)
    return rms_norm(o, out_g).astype(u.dtype)


def mla_mixer(u, positions, q_norm_g, kv_norm_g, w_uq, w_ukv, out_g):
    B, S, _ = u.shape
    c_q, c_kv, k_pe = _split(u, MLA_COLS)
    q = (rms_norm(c_q, q_norm_g) @ w_uq).reshape(B, S, MLA_HEADS, MLA_NOPE + MLA_ROPE)
    q_nope = q[..., :MLA_NOPE]
    q_pe = rope(q[..., MLA_NOPE:], positions)
    kv = (rms_norm(c_kv, kv_norm_g) @ w_ukv).reshape(B, S, MLA_HEADS, MLA_NOPE + MLA_VD)
    k_nope = kv[..., :MLA_NOPE]
    v = kv[..., MLA_NOPE:]
    k_pe = rope(k_pe[:, :, None, :], positions)
    q_full = jnp.concatenate([q_nope, q_pe], axis=-1)
    k_full = jnp.concatenate([k_nope, jnp.broadcast_to(k_pe, (B, S, MLA_HEADS, MLA_ROPE))], axis=-1)
    o = block_causal_attention(q_full.transpose(0, 2, 1, 3), k_full.transpose(0, 2, 1, 3),
                               v.transpose(0, 2, 1, 3), CHUNK)
    o = o.transpose(0, 2, 1, 3).reshape(B, S, GROUP_W)
    return rms_norm(o, out_g).astype(u.dtype)


def rwkv7_recurrence(r, decay, k, v, a, b):
    B, S, H, N = r.shape

    def step(state, inp):
        r_t, w_t, k_t, v_t, a_t, b_t = inp
        sa = jnp.einsum('bhij,bhj->bhi', state, a_t)
        state = (state * w_t[:, :, None, :] + sa[..., None] * b_t[:, :, None, :]
                 + v_t[..., None] * k_t[:, :, None, :])
        return state, jnp.einsum('bhij,bhj->bhi', state, r_t)

    xs = tuple(jnp.moveaxis(t.astype(jnp.float32), 1, 0) for t in (r, decay, k, v, a, b))
    _, y = lax.scan(step, jnp.zeros((B, H, N, N), jnp.float32), xs)
    return jnp.moveaxis(y, 0, 1)


def rwkv7_mixer(u, mu, w0, w2, a0, a2, g2, k_k, k_a, r_k, ln_g, ln_b):
    B, S, _ = u.shape
    u = u + mu * (time_shift(u) - u)
    r, k, v, w_lo, a_lo, g_lo = _split(u, RWKV_COLS)
    w = -jax.nn.softplus(-(w0 + jnp.tanh(w_lo) @ w2)) - 0.5
    decay = jnp.exp(-jnp.exp(w.astype(jnp.float32)))
    a = jax.nn.sigmoid(a0 + a_lo @ a2)
    g = jax.nn.sigmoid(g_lo) @ g2
    heads = lambda t: t.reshape(B, S, RWKV_HEADS, RWKV_HD)
    kk = l2_normalize(heads(k * k_k))
    k = k * (1.0 + (a - 1.0) * k_a)
    r_h, k_h, v_h, a_h = heads(r), heads(k), heads(v), heads(a)
    y = rwkv7_recurrence(r_h, heads(decay), k_h, v_h, -kk, kk * a_h)
    m = jnp.mean(y, axis=-1, keepdims=True)
    var = jnp.mean(jnp.square(y - m), axis=-1, keepdims=True)
    y = ((y - m) * lax.rsqrt(var + RWKV_GN_EPS)).reshape(B, S, GROUP_W) * ln_g + ln_b
    bonus = jnp.sum(r_h * k_h * r_k.reshape(RWKV_HEADS, RWKV_HD), axis=-1, keepdims=True) * v_h
    return ((y + bonus.reshape(B, S, GROUP_W)) * g).astype(u.dtype)


def gated_delta_chunked(q, k, v, g, beta):
    in_dtype = q.dtype
    B, H, S, Dk = q.shape
    Dv = v.shape[-1]
    n = S // CHUNK
    q, k, v, g, beta = (t.astype(jnp.float32) for t in (q, k, v, g, beta))
    q = q.reshape(B, H, n, CHUNK, Dk)
    k = k.reshape(B, H, n, CHUNK, Dk)
    v = v.reshape(B, H, n, CHUNK, Dv)
    beta = beta.reshape(B, H, n, CHUNK)
    gc = jnp.cumsum(g.reshape(B, H, n, CHUNK), axis=-1)
    lower = jnp.tril(jnp.ones((CHUNK, CHUNK), bool))
    strict = jnp.tril(jnp.ones((CHUNK, CHUNK), bool), -1)
    decay = jnp.exp(jnp.where(lower, gc[..., :, None] - gc[..., None, :], -jnp.inf))
    kb = k * beta[..., None]
    vb = v * beta[..., None]
    L = jnp.where(strict, jnp.einsum('bhncd,bhnsd->bhncs', kb, k) * decay, 0.0)
    rhs = jnp.concatenate([vb, kb * jnp.exp(gc)[..., None]], axis=-1)
    sol = lax.linalg.triangular_solve(L, rhs, left_side=True, lower=True, unit_diagonal=True)
    u_val, w_cum = sol[..., :Dv], sol[..., Dv:]
    attn = jnp.einsum('bhncd,bhnsd->bhncs', q, k) * decay
    q_dec = q * jnp.exp(gc)[..., None]
    k_dec = k * jnp.exp(gc[..., -1:] - gc)[..., None]
    g_last = jnp.exp(gc[..., -1])

    def step(state, inp):
        u_i, w_i, attn_i, qd_i, kd_i, gl_i = inp
        v_new = u_i - jnp.einsum('bhcd,bhde->bhce', w_i, state)
        o = jnp.einsum('bhcd,bhde->bhce', qd_i, state) + jnp.einsum('bhcs,bhse->bhce', attn_i, v_new)
        state = state * gl_i[..., None, None] + jnp.einsum('bhcd,bhce->bhde', kd_i, v_new)
        return state, o

    xs = tuple(jnp.moveaxis(t, 2, 0) for t in (u_val, w_cum, attn, q_dec, k_dec, g_last))
    _, o = lax.scan(step, jnp.zeros((B, H, Dk, Dv), jnp.float32), xs)
    return jnp.moveaxis(o, 0, 2).reshape(B, H, S, Dv).astype(in_dtype)


def gdn_mixer(u, conv_w, a_log, dt_bias, norm_g):
    B, S, _ = u.shape
    q, k, v, z, b, a = _split(u, GDN_COLS)
    qkv = jax.nn.silu(causal_depthwise_conv(jnp.concatenate([q, k, v], axis=-1), conv_w))
    q, k, v = jnp.split(qkv, 3, axis=-1)
    heads = lambda t: t.reshape(B, S, GDN_HEADS, GDN_HD).transpose(0, 2, 1, 3)
    q = l2_normalize(heads(q)) * (GDN_HD ** -0.5)
    k = l2_normalize(heads(k))
    beta = jax.nn.sigmoid(b.astype(jnp.float32)).transpose(0, 2, 1)
    g = (-jnp.exp(a_log.astype(jnp.float32)) * jax.nn.softplus(a.astype(jnp.float32) + dt_bias)).transpose(0, 2, 1)
    o = gated_delta_chunked(q, k, heads(v), g, beta)
    o = rms_norm(o, norm_g) * jax.nn.silu(heads(z))
    return o.transpose(0, 2, 1, 3).reshape(B, S, GROUP_W).astype(u.dtype)


def hier_moe(x, w_grp, b_grp, w_exp, b_exp, w_gate, w_up, w_down):
    B, S, D = x.shape
    T = B * S
    xf = x.reshape(T, D)
    grp_prob = jax.nn.softmax((xf @ w_grp).astype(jnp.float32) + b_grp, axis=-1)
    p_grp, grp = lax.top_k(grp_prob, 1)
    exp_logits = ((xf @ w_exp).astype(jnp.float32) + b_exp).reshape(T, N_GROUPS, EXP_PER_GROUP)
    in_grp = exp_logits[jnp.arange(T), grp[:, 0]]
    top_logit, top_local = lax.top_k(in_grp, TOP_K)
    gate = p_grp * jax.nn.softmax(top_logit, axis=-1)
    expert = grp * EXP_PER_GROUP + top_local

    A = T * TOP_K
    n_slots = ((A + N_EXPERTS * (MOE_BLOCK - 1) + MOE_BLOCK - 1) // MOE_BLOCK) * MOE_BLOCK
    n_blocks = n_slots // MOE_BLOCK
    flat_e = expert.reshape(A)
    order = jnp.argsort(flat_e)
    e_sorted = flat_e[order]
    tok_sorted = (order // TOP_K).astype(jnp.int32)
    w_sorted = gate.reshape(A)[order]
    counts = jnp.bincount(flat_e, length=N_EXPERTS)
    padded = (counts + MOE_BLOCK - 1) // MOE_BLOCK * MOE_BLOCK
    start = jnp.cumsum(counts) - counts
    pend = jnp.cumsum(padded)
    pstart = pend - padded
    dest = pstart[e_sorted] + jnp.arange(A) - start[e_sorted]
    token_of_slot = jnp.full((n_slots,), T, jnp.int32).at[dest].set(tok_sorted)
    weight_of_slot = jnp.zeros((n_slots,), jnp.float32).at[dest].set(w_sorted)
    block_expert = jnp.minimum(jnp.searchsorted(pend, jnp.arange(n_blocks) * MOE_BLOCK, side='right'),
                               N_EXPERTS - 1)
    x_pad = jnp.concatenate([xf, jnp.zeros((1, D), xf.dtype)], axis=0)

    def expert_block(args):
        tok, e = args
        xb = x_pad[tok]
        h = jax.nn.silu(xb @ w_gate[e]) * (xb @ w_up[e])
        return h @ w_down[e]

    y_slots = lax.map(expert_block, (token_of_slot.reshape(n_blocks, MOE_BLOCK), block_expert))
    y_slots = y_slots.reshape(n_slots, D) * weight_of_slot[:, None].astype(x.dtype)
    y = jax.ops.segment_sum(y_slots, token_of_slot, num_segments=T + 1)[:T]
    return y.reshape(B, S, D).astype(x.dtype)


def setup_inputs(seed: int = 0) -> dict:
    key = jax.random.key(seed)
    ks = iter(jax.random.split(key, 64))
    L = DEPTH
    nrm = lambda shape, scale: jax.random.normal(next(ks), shape, jnp.float32) * scale
    gain = lambda shape: 1.0 + nrm(shape, 0.02)
    unif = lambda shape, lo, hi: jax.random.uniform(next(ks), shape, jnp.float32, lo, hi)
    x = nrm((BATCH, SEQ, D_MODEL), 1.0)
    start = jax.random.randint(next(ks), (BATCH, 1), 0, 4096, dtype=jnp.int32)
    positions = start + jnp.arange(SEQ, dtype=jnp.int32)[None, :]
    dt = jnp.exp(unif((L, GDN_HEADS), math.log(1e-3), math.log(1e-1)))
    return {
        'x': x,
        'positions': positions,
        'w_in': nrm((L, D_MODEL, N_IN), D_MODEL ** -0.5),
        'fox_b_f': unif((L, FOX_HEADS), 1.0, 5.0),
        'fox_out_g': gain((L, GROUP_W)),
        'mla_q_norm_g': gain((L, MLA_Q_RANK)),
        'mla_kv_norm_g': gain((L, MLA_KV_RANK)),
        'mla_w_uq': nrm((L, MLA_Q_RANK, MLA_HEADS * (MLA_NOPE + MLA_ROPE)), MLA_Q_RANK ** -0.5),
        'mla_w_ukv': nrm((L, MLA_KV_RANK, MLA_HEADS * (MLA_NOPE + MLA_VD)), MLA_KV_RANK ** -0.5),
        'mla_out_g': gain((L, GROUP_W)),
        'rwkv_mu': unif((L, sum(RWKV_COLS)), 0.0, 1.0),
        'rwkv_w0': unif((L, GROUP_W), -6.0, -1.0),
        'rwkv_w2': nrm((L, RWKV_W_RANK, GROUP_W), 0.5 * RWKV_W_RANK ** -0.5),
        'rwkv_a0': nrm((L, GROUP_W), 0.1),
        'rwkv_a2': nrm((L, RWKV_A_RANK, GROUP_W), 0.5 * RWKV_A_RANK ** -0.5),
        'rwkv_g2': nrm((L, RWKV_G_RANK, GROUP_W), RWKV_G_RANK ** -0.5),
        'rwkv_k_k': 0.85 + nrm((L, GROUP_W), 0.05),
        'rwkv_k_a': gain((L, GROUP_W)),
        'rwkv_r_k': nrm((L, GROUP_W), 0.1),
        'rwkv_ln_g': gain((L, GROUP_W)),
        'rwkv_ln_b': nrm((L, GROUP_W), 0.02),
        'gdn_conv_w': nrm((L, GDN_CONV, 3 * GROUP_W), GDN_CONV ** -0.5),
        'gdn_a_log': jnp.log(unif((L, GDN_HEADS), 1.0, 16.0)),
        'gdn_dt_bias': jnp.log(jnp.expm1(dt)),
        'gdn_norm_g': gain((L, GDN_HD)),
        'w_out': nrm((L, D_MIX, D_MODEL), BETA * D_MIX ** -0.5),
        'ln1_g': gain((L, D_MODEL)),
        'ln1_b': nrm((L, D_MODEL), 0.02),
        'moe_w_grp': nrm((L, D_MODEL, N_GROUPS), D_MODEL ** -0.5),
        'moe_b_grp': nrm((L, N_GROUPS), 0.01),
        'moe_w_exp': nrm((L, D_MODEL, N_EXPERTS), D_MODEL ** -0.5),
        'moe_b_exp': nrm((L, N_EXPERTS), 0.01),
        'moe_w_gate': nrm((L, N_EXPERTS, D_MODEL, D_EXPERT), D_MODEL ** -0.5),
        'moe_w_up': nrm((L, N_EXPERTS, D_MODEL, D_EXPERT), D_MODEL ** -0.5),
        'moe_w_down': nrm((L, N_EXPERTS, D_EXPERT, D_MODEL), BETA * D_EXPERT ** -0.5),
        'ln2_g': gain((L, D_MODEL)),
        'ln2_b': nrm((L, D_MODEL), 0.02),
    }


def reference(x, positions, w_in, fox_b_f, fox_out_g, mla_q_norm_g, mla_kv_norm_g, mla_w_uq,
              mla_w_ukv, mla_out_g, rwkv_mu, rwkv_w0, rwkv_w2, rwkv_a0, rwkv_a2, rwkv_g2, rwkv_k_k,
              rwkv_k_a, rwkv_r_k, rwkv_ln_g, rwkv_ln_b, gdn_conv_w, gdn_a_log, gdn_dt_bias, gdn_norm_g,
              w_out, ln1_g, ln1_b, moe_w_grp, moe_b_grp, moe_w_exp, moe_b_exp, moe_w_gate, moe_w_up,
              moe_w_down, ln2_g, ln2_b):
    for i in range(DEPTH):
        u = x @ w_in[i]
        u_fox, u_mla, u_rwkv, u_gdn = _split(u, GROUP_IN_COLS)
        y_fox = fox_mixer(u_fox, fox_b_f[i], fox_out_g[i])
        y_mla = mla_mixer(u_mla, positions, mla_q_norm_g[i], mla_kv_norm_g[i], mla_w_uq[i],
                          mla_w_ukv[i], mla_out_g[i])
        y_rwkv = rwkv7_mixer(u_rwkv, rwkv_mu[i], rwkv_w0[i], rwkv_w2[i], rwkv_a0[i], rwkv_a2[i],
                             rwkv_g2[i], rwkv_k_k[i], rwkv_k_a[i], rwkv_r_k[i], rwkv_ln_g[i], rwkv_ln_b[i])
        y_gdn = gdn_mixer(u_gdn, gdn_conv_w[i], gdn_a_log[i], gdn_dt_bias[i], gdn_norm_g[i])
        mixed = jnp.concatenate([y_fox, y_mla, y_rwkv, y_gdn], axis=-1)
        x = layer_norm(ALPHA * x + mixed @ w_out[i], ln1_g[i], ln1_b[i])
        moe_out = hier_moe(x, moe_w_grp[i], moe_b_grp[i], moe_w_exp[i], moe_b_exp[i],
                           moe_w_gate[i], moe_w_up[i], moe_w_down[i])
        x = layer_norm(ALPHA * x + moe_out, ln2_g[i], ln2_b[i])
    return x
```

```python
import numpy as np
import concourse.bass as bass
import concourse.mybir as mybir
from concourse.bass_utils import run_bass_kernel_spmd

F32 = mybir.dt.float32
BF16 = mybir.dt.bfloat16
I32 = mybir.dt.int32
U32 = mybir.dt.uint32
AF = mybir.ActivationFunctionType
ALU = mybir.AluOpType
AX = mybir.AxisListType

D = 2048
GW = 512
N_IN = 5968
C_FOX, C_MLA, C_RWKV, C_GDN = 0, 1544, 2120, 3912
ALPHA = (2 * 4) ** 0.25
LN_EPS = 1e-5
RMS_EPS = 1e-6
NEG = -30000.0


class _Op:
    __slots__ = ("stream", "fn", "deps", "sig", "ticket", "dma", "dma_idx", "idx", "sw")


class Prog:
    STREAMS = ("pe", "act", "dve", "pool", "sp")
    NDMA = 48

    def __init__(self, nc):
        self.nc = nc
        self.ops = []
        self.by_stream = {s: [] for s in self.STREAMS}
        self.last_w = {}
        self.readers = {}
        self.n_dma = 0
        self.dma_ops = []
        self.barrier_deps = {s: [] for s in self.STREAMS}

    POOLQ = "pool"
    NSW = 40

    def add(self, stream, fn, r=(), w=(), dma=False, sw=False):
        op = _Op()
        op.stream, op.fn, op.dma, op.sig, op.ticket = stream, fn, dma, False, 0
        op.sw = sw
        op.idx = len(self.ops)
        pk = [k for k in r if isinstance(k, tuple) and k and k[0] == "ps"]
        if pk:
            r = [k for k in r if not (isinstance(k, tuple) and k and k[0] == "ps")]
            w = list(w) + pk
        deps = set()
        for k in r:
            d = self.last_w.get(k)
            if d is not None:
                deps.add(d)
        for k in w:
            d = self.last_w.get(k)
            if d is not None:
                deps.add(d)
            for rd in self.readers.get(k, ()):
                deps.add(rd)
        for k in r:
            self.readers.setdefault(k, []).append(op.idx)
        for k in w:
            self.last_w[k] = op.idx
            self.readers[k] = []
        bd = self.barrier_deps[stream]
        if bd:
            deps.update(bd)
            self.barrier_deps[stream] = []
        if dma:
            op.dma_idx = self.n_dma
            self.n_dma += 1
            if op.dma_idx >= self.NDMA:
                deps.add(self.dma_ops[op.dma_idx - self.NDMA])
            self.dma_ops.append(op.idx)
        deps.discard(op.idx)
        fin = []
        for d in deps:
            o = self.ops[d]
            if o.stream == stream and not o.dma and stream == "pe":
                continue
            fin.append(d)
            if not o.dma:
                o.sig = True
        op.deps = fin
        self.ops.append(op)
        self.by_stream[stream].append(op)
        return op

    def barrier(self):
        deps = []
        for s in self.STREAMS:
            for o in reversed(self.by_stream[s]):
                if not o.dma:
                    deps.append(o.idx)
                    break
        deps.extend(self.dma_ops[-self.NDMA:])
        for s in self.STREAMS:
            self.barrier_deps[s] = list(deps)
        self.last_w = {}
        self.readers = {}

    def pe(self, fn, r=(), w=()):
        return self.add("pe", fn, r, w)

    def act(self, fn, r=(), w=()):
        return self.add("act", fn, r, w)

    def dve(self, fn, r=(), w=()):
        return self.add("dve", fn, r, w)

    def pool(self, fn, r=(), w=()):
        return self.add("pool", fn, r, w)

    def dma(self, out, in_, r=(), w=(), q="sp", slow=False):
        if q == "pool":
            q = self.POOLQ
        if slow:
            return self.add(q, lambda e: e.dma_start(out=out, in_=in_, allow_slow_non_contiguous=True), r, w, dma=True)
        return self.add(q, lambda e: e.dma_start(out=out, in_=in_), r, w, dma=True)

    def emit(self, final_wait=True):
        nc = self.nc
        for s in self.STREAMS:
            t = 0
            for o in self.by_stream[s]:
                if not o.dma and o.sig:
                    t += 1
                    o.ticket = t
        import contextlib
        with contextlib.ExitStack() as es:
            esem = {s: es.enter_context(nc.semaphore("e_" + s)) for s in self.STREAMS}
            dsem = [es.enter_context(nc.semaphore("d%d" % i)) for i in range(self.NDMA)]
            nsw = sum(1 for o in self.ops if o.sw)
            SWBASE = 215
            swsems = [es.enter_context(nc.semaphore("sw%d" % i, num=SWBASE + i)) for i in range(min(nsw, self.NSW))]
            swctr = [0, 1]
            block = es.enter_context(nc.Block())
            ops = self.ops
            NDMA = self.NDMA

            def completion(o):
                if o.dma:
                    return ("d", o.dma_idx % NDMA), dsem[o.dma_idx % NDMA], 16 * (o.dma_idx // NDMA + 1)
                return ("e", o.stream), esem[o.stream], o.ticket

            def run_stream(s, eng):
                waited = {}
                for o in self.by_stream[s]:
                    need = {}
                    for d in o.deps:
                        key, sem, val = completion(ops[d])
                        if waited.get(key, 0) >= val:
                            continue
                        if key not in need or need[key][1] < val:
                            need[key] = (sem, val)
                    for key, (sem, val) in need.items():
                        eng.wait_ge(sem, val)
                        waited[key] = val
                    inst = o.fn(eng)
                    if o.sw:
                        if swctr[0] == len(swsems):
                            eng.dma_reset(range(SWBASE, SWBASE + len(swsems)))
                            swctr[0] = 0
                            swctr[1] += 1
                        sw_ = swsems[swctr[0]]
                        swctr[0] += 1
                        inst.then_inc(sw_, 16)
                        eng.wait_ge(sw_, 16 * swctr[1])
                        if o.sig:
                            eng.nop().then_inc(esem[s], 1)
                    elif o.dma:
                        inst.then_inc(dsem[o.dma_idx % NDMA], 16)
                    elif o.sig:
                        inst.then_inc(esem[s], 1)
                if s == "sp" and final_wait:
                    for i in range(min(NDMA, self.n_dma)):
                        last = i + ((self.n_dma - 1 - i) // NDMA) * NDMA
                        val = 16 * (last // NDMA + 1)
                        if waited.get(("d", i), 0) < val:
                            eng.wait_ge(dsem[i], val)

            @block.tensor
            def _(e):
                run_stream("pe", e)

            @block.scalar
            def _(e):
                run_stream("act", e)

            @block.vector
            def _(e):
                run_stream("dve", e)

            @block.gpsimd
            def _(e):
                run_stream("pool", e)

            @block.sync
            def _(e):
                run_stream("sp", e)


class SB:
    def __init__(self, nc, base=16512, cap=229344):
        self.nc, self.base, self.cap, self.top = nc, base, cap, base
        self.n = 0

    def mark(self):
        return self.top

    def reset(self, m):
        self.top = m

    def alloc(self, shape, dtype, name="t"):
        esz = {F32: 4, BF16: 2, I32: 4, U32: 4}[dtype]
        per = esz
        for s in shape[1:]:
            per *= s
        per = (per + 31) // 32 * 32
        off = self.top
        assert off + per <= self.cap, "SBUF overflow %s %d+%d" % (name, off, per)
        self.top += per
        self.n += 1
        return self.nc.alloc_sbuf_tensor_at("%s_%d" % (name, self.n), list(shape), dtype, offset=off)


class Ctx:
    def __init__(self, nc, S, NB, io=None):
        self.nc = nc
        self.S = S
        self.NB = NB
        self.T = S * NB
        self.P = Prog(nc)
        self.sb = SB(nc)
        self.io = io or {}
        self.dram = {}
        self.uid = 0

    def dt(self, name, shape, dtype, kind=None):
        k = self.io.get(name, kind or "Internal")
        t = self.nc.dram_tensor(name, list(shape), dtype, kind=k).ap()
        self.dram[name] = t
        return t

    def key(self, base):
        self.uid += 1
        return (base, self.uid)


def make_consts(cx):
    nc, P, sb = cx.nc, cx.P, cx.sb
    c = {}
    ident = sb.alloc([128, 128], F32, "ident")
    P.pool(lambda e: e.memset(ident[:], 0.0), w=["ident"])
    P.pool(lambda e: e.affine_select(out=ident[:], in_=ident[:], pattern=[[-1, 128]],
                                     compare_op=ALU.not_equal, fill=1.0, base=0, channel_multiplier=1),
           r=["ident"], w=["ident"])
    identb = sb.alloc([128, 128], BF16, "identb")
    P.dve(lambda e: e.tensor_copy(identb[:], ident[:]), r=["ident"], w=["identb"])
    c["ident"], c["identb"] = ident, identb
    cx.ps = [nc.alloc_psum_tensor("psb%d" % i, [128, 512], F32) for i in range(8)]
    cx.c = c
    dplr_consts(cx)
    SU = sb.alloc([128, 128], F32, "SU")
    SUb = sb.alloc([128, 128], BF16, "SUb")
    P.pool(lambda e: e.memset(SU[:], 1.0), w=["SU"])
    P.pool(lambda e: e.affine_select(out=SU[:], in_=SU[:], pattern=[[1, 128]], compare_op=ALU.is_ge, fill=0.0, base=-1, channel_multiplier=-1),
           r=["SU"], w=["SU"])
    P.pool(lambda e: e.tensor_copy(SUb[:], SU[:]), r=["SU"], w=["SUb"])
    eoff = sb.alloc([128, 32], F32, "eoff")
    P.pool(lambda e: e.iota(eoff[:], pattern=[[MOE_CAP, 32]], base=0, channel_multiplier=0, allow_small_or_imprecise_dtypes=True), w=["eoff"])
    trash = sb.alloc([128, 1], F32, "trash")
    P.pool(lambda e: e.iota(trash[:], pattern=[[0, 1]], base=32 * MOE_CAP, channel_multiplier=1, allow_small_or_imprecise_dtypes=True), w=["trash"])
    c.update(SUb=SUb, eoff=eoff, trash=trash)
    c["masks_causal"] = build_masks(cx, "causal")
    c["masks_chunk64"] = build_masks(cx, "chunk64")
    P.barrier()
    return c


def evac(P, i, out, in_, r, w):
    if i % 2 == 0:
        P.act(lambda e: e.copy(out, in_), r, w)
    else:
        P.dve(lambda e: e.tensor_copy(out, in_), r, w)


def build_xT(cx, src_dram, b, xT, ps_banks, xrow_bufs, tag):
    P, c = cx.P, cx.c
    S = cx.S
    nt = S // 128
    for tt in range(nt):
        xr = xrow_bufs[tt % len(xrow_bufs)]
        kx = ("xrow", tag, tt % len(xrow_bufs))
        r0 = b * S + tt * 128
        P.dma(xr[:], src_dram[r0:r0 + 128, :], w=[kx])
        for g in range(4):
            ps = ps_banks[(tt * 4 + g) % len(ps_banks)]
            kp = ("psT", tag, (tt * 4 + g) % len(ps_banks))
            for j in range(4):
                kc = g * 4 + j
                P.pe(lambda e, ps=ps, xr=xr, kc=kc, j=j: e.transpose(ps[:, j * 128:(j + 1) * 128],
                                                                   xr[:, kc * 128:(kc + 1) * 128], c["ident"][:]),
                     r=[kx, "ident"], w=[kp])
            o = xT[:, g * 4:(g + 1) * 4, tt * 128:(tt + 1) * 128]
            i_ = ps[:].rearrange("p (j t) -> p j t", j=4)
            evac(P, tt * 4 + g, o, i_, r=[kp], w=[("xT", tag, tt)])


def stage_inproj(cx, x_dram, w_in, u_tok, qkT, fT):
    nc, P, sb, c = cx.nc, cx.P, cx.sb, cx.c
    S, NB = cx.S, cx.NB
    nt = S // 128
    m = sb.mark()
    xT = sb.alloc([128, 16, S], BF16, "xT")
    xrows = [sb.alloc([128, 2048], F32, "xrow") for _ in range(2)]
    wst = [sb.alloc([128, 16, 512], F32, "wst") for _ in range(2)]
    wbf = [sb.alloc([128, 16, 512], BF16, "wbf") for _ in range(2)]
    ost = [sb.alloc([128, 512], F32, "ost") for _ in range(4)]
    obf = [sb.alloc([128, 512], BF16, "obf") for _ in range(2)]
    psb = cx.ps
    wv = w_in.rearrange("(kc p) n -> p kc n", p=128)
    blocks = [(c0, min(512, N_IN - c0)) for c0 in range(0, N_IN, 512)]
    nev = 0
    for b in range(NB):
        build_xT(cx, x_dram, b, xT, psb[0:4], xrows, "A%d" % b)
        xkeys = [("xT", "A%d" % b, tt) for tt in range(nt)]
        for bi, (c0, cw) in enumerate(blocks):
            it = b * len(blocks) + bi
            ws, wb = wst[it % 2], wbf[it % 2]
            kws, kwb = ("wst", it % 2), ("wbf", it % 2)
            P.dma(ws[:, :, 0:cw], wv[:, :, c0:c0 + cw], w=[kws])
            P.pool(lambda e, wb=wb, ws=ws, cw=cw: e.tensor_copy(wb[:, :, 0:cw], ws[:, :, 0:cw]), r=[kws], w=[kwb])
            for tt in range(nt):
                if c0 in (0, 512):
                    break
                pi = 4 + (nev % 4)
                ps, kp = psb[pi], ("psA", pi)
                for kc in range(16):
                    P.pe(lambda e, ps=ps, kc=kc, tt=tt, wb=wb, cw=cw: e.matmul(
                        ps[:, 0:cw], xT[:, kc, tt * 128:(tt + 1) * 128], wb[:, kc, 0:cw],
                        start=(kc == 0), stop=(kc == 15)), r=[xkeys[tt], kwb], w=[kp])
                o, ko = ost[nev % 4], ("ost", nev % 4)
                evac(P, nev, o[:, 0:cw], ps[:, 0:cw], r=[kp], w=[ko])
                r0 = b * S + tt * 128
                P.dma(u_tok[r0:r0 + 128, c0:c0 + cw], o[:, 0:cw], r=[ko], q="pool")
                nev += 1
            fm = []
            if c0 in (0, 512):
                fm = [(c0 + j * 128, 128) for j in range(4)]
            elif c0 == 1536:
                fm = [(1536, 8)]
            for (f0, fw) in fm:
                for tb in range(S // 512):
                    pi = 4 + (nev % 4)
                    ps, kp = psb[pi], ("psA", pi)
                    for kc in range(16):
                        P.pe(lambda e, ps=ps, kc=kc, tb=tb, wb=wb, f0=f0, fw=fw, c0=c0: e.matmul(
                            ps[0:fw, :], wb[:, kc, f0 - c0:f0 - c0 + fw], xT[:, kc, tb * 512:(tb + 1) * 512],
                            start=(kc == 0), stop=(kc == 15)), r=xkeys[tb * 4:tb * 4 + 4] + [kwb], w=[kp])
                    t0 = b * S + tb * 512
                    if fw == 128:
                        o, ko = obf[nev % 2], ("obf", nev % 2)
                        evac(P, nev, o[:], ps[:], r=[kp], w=[ko])
                        P.dma(qkT[f0:f0 + 128, t0:t0 + 512], o[:], r=[ko], q="pool")
                    else:
                        o, ko = ost[nev % 4], ("ost", nev % 4)
                        evac(P, nev, o[0:8, :], ps[0:8, :], r=[kp], w=[ko])
                        P.dma(fT[0:8, t0:t0 + 512], o[0:8, :], r=[ko], q="pool")
                    nev += 1
    P.barrier()
    sb.reset(m)


def bcast_load(cx, dst, src_row, key, q="sp"):
    cx.P.dma(dst, src_row.partition_broadcast(128), w=[key], q=q)


def rstd_from_ssq(P, out, ssq, n, eps, r, w):
    P.dve(lambda e: e.tensor_scalar(out, ssq, 1.0 / n, eps, ALU.mult, ALU.add), r=r, w=w)
    P.act(lambda e: e.activation(out, out, AF.Sqrt), r=w, w=w)
    P.dve(lambda e: e.reciprocal(out, out), r=w, w=w)


def layer_norm_tile(cx, pre, y, g_b, b_b, stats, mv, rstd, kpre, ky, kst):
    P = cx.P
    for j in range(4):
        P.dve(lambda e, j=j: e.bn_stats(stats[:, j, :], pre[:, j * 512:(j + 1) * 512]), r=[kpre], w=[kst])
    P.dve(lambda e: e.bn_aggr(mv[:], stats[:].rearrange("p a b -> p (a b)")), r=[kst], w=[kst])
    P.dve(lambda e: e.tensor_scalar_add(rstd[:], mv[:, 1:2], LN_EPS), r=[kst], w=[kst])
    P.act(lambda e: e.activation(rstd[:], rstd[:], AF.Sqrt), r=[kst], w=[kst])
    P.dve(lambda e: e.reciprocal(rstd[:], rstd[:]), r=[kst], w=[kst])
    P.dve(lambda e: e.tensor_scalar(y, pre, mv[:, 0:1], rstd[:], ALU.subtract, ALU.mult), r=[kst, kpre], w=[ky])
    P.pool(lambda e: e.tensor_mul(y, y, g_b), r=[ky, "lnp"], w=[ky])
    P.pool(lambda e: e.tensor_add(y, y, b_b), r=[ky, "lnp"], w=[ky])


def stage_outproj_ln(cx, mixed, w_out, x_res, ln_g, ln_b, h_out):
    nc, P, sb, c = cx.nc, cx.P, cx.sb, cx.c
    T = cx.T
    m = sb.mark()
    wbf = sb.alloc([128, 16, 2048], BF16, "woutbf")
    wst = [sb.alloc([128, 16, 256], F32, "wst") for _ in range(2)]
    mrow = [sb.alloc([128, 2048], F32, "mrow") for _ in range(2)]
    xrow = [sb.alloc([128, 2048], F32, "xrow") for _ in range(2)]
    mT = [sb.alloc([128, 16, 128], BF16, "mT") for _ in range(2)]
    pre = [sb.alloc([128, 2048], F32, "pre") for _ in range(2)]
    g_b = sb.alloc([128, 2048], F32, "g_b")
    b_b = sb.alloc([128, 2048], F32, "b_b")
    stats = sb.alloc([128, 4, 6], F32, "stats")
    mv = sb.alloc([128, 2], F32, "mv")
    rstd = sb.alloc([128, 1], F32, "rstd")
    psb = cx.ps
    bcast_load(cx, g_b[:], ln_g, "lnp")
    bcast_load(cx, b_b[:], ln_b, "lnp")
    wv = w_out.rearrange("(kc p) n -> p kc n", p=128)
    for j in range(8):
        ws, kws = wst[j % 2], ("wst", j % 2)
        P.dma(ws[:], wv[:, :, j * 256:(j + 1) * 256], w=[kws])
        P.pool(lambda e, ws=ws, j=j: e.tensor_copy(wbf[:, :, j * 256:(j + 1) * 256], ws[:]), r=[kws], w=["wout"])
    nev = 0
    for tt in range(T // 128):
        i2 = tt % 2
        r0 = tt * 128
        km, kx, kmT, kpre = ("mrow", i2), ("xrow", i2), ("mT", i2), ("pre", i2)
        P.dma(mrow[i2][:], mixed[r0:r0 + 128, :], w=[km])
        P.dma(xrow[i2][:], x_res[r0:r0 + 128, :], w=[kx])
        for g in range(4):
            pi = nev % 4
            ps, kp = psb[pi], ("ps", pi)
            for j in range(4):
                kc = g * 4 + j
                P.pe(lambda e, ps=ps, kc=kc, j=j, i2=i2: e.transpose(ps[:, j * 128:(j + 1) * 128],
                                                                 mrow[i2][:, kc * 128:(kc + 1) * 128], c["ident"][:]),
                     r=[km, "ident"], w=[kp])
            evac(P, nev, mT[i2][:, g * 4:(g + 1) * 4, :], ps[:].rearrange("p (j t) -> p j t", j=4), r=[kp], w=[kmT])
            nev += 1
        for cb in range(4):
            pi = 4 + nev % 4
            ps, kp = psb[pi], ("ps", pi)
            for kc in range(16):
                P.pe(lambda e, ps=ps, kc=kc, cb=cb, i2=i2: e.matmul(ps[:], mT[i2][:, kc, :], wbf[:, kc, cb * 512:(cb + 1) * 512],
                                                                  start=(kc == 0), stop=(kc == 15)),
                     r=[kmT, "wout"], w=[kp])
            P.dve(lambda e, ps=ps, cb=cb, i2=i2: e.scalar_tensor_tensor(
                pre[i2][:, cb * 512:(cb + 1) * 512], xrow[i2][:, cb * 512:(cb + 1) * 512], ALPHA, ps[:], ALU.mult, ALU.add),
                r=[kp, kx], w=[kpre])
            nev += 1
        layer_norm_tile(cx, pre[i2][:], xrow[i2][:], g_b[:], b_b[:], stats, mv, rstd, kpre, kx, "lnst")
        P.dma(h_out[r0:r0 + 128, :], xrow[i2][:], r=[kx], q="pool")
    P.barrier()
    sb.reset(m)


def build_masks(cx, kind):
    P, sb = cx.P, cx.sb
    if "maskf" not in cx.c:
        cx.c["maskf"] = sb.alloc([128, 512], F32, "maskf")
    mf = cx.c["maskf"]
    out = []
    for j in range(4):
        mb = sb.alloc([128, 512], BF16, "maskb")
        k = ("mask", kind, j)
        P.pool(lambda e: e.memset(mf[:], 0.0), w=["maskf"])
        if kind == "causal":
            P.pool(lambda e, j=j: e.affine_select(out=mf[:], in_=mf[:], pattern=[[1, 512]], compare_op=ALU.is_ge,
                                                 fill=NEG, base=-128 * j, channel_multiplier=-1), r=["maskf"], w=["maskf"])
        else:
            for hf in range(2):
                P.pool(lambda e, j=j, hf=hf: e.affine_select(
                    out=mf[hf * 64:(hf + 1) * 64, :], in_=mf[hf * 64:(hf + 1) * 64, :], pattern=[[1, 512]],
                    compare_op=ALU.is_ge, fill=NEG, base=-128 * j - 64 * hf, channel_multiplier=0),
                    r=["maskf"], w=["maskf"])
        P.pool(lambda e, mb=mb: e.tensor_copy(mb[:], mf[:]), r=["maskf"], w=[k])
        out.append((mb, k))
    return out


def attn_bufs(cx, nh, dv):
    sb, S = cx.sb, cx.S
    nring = 2 * (S // 128)
    return dict(PT=[sb.alloc([128, 512], BF16, "PT") for _ in range(nring)],
                o_t=[sb.alloc([128, 4, nh * dv], F32, "o_t") for _ in range(2)],
                rec=[sb.alloc([128, 4], F32, "rec") for _ in range(2)])


def attn_core(cx, nh, dv, scale, score_ops, bias_ap, masks, v_ap, out_cb, tag, sbanks=(0, 1, 2), bufs=None):
    P, sb, c = cx.P, cx.sb, cx.c
    S = cx.S
    psb = cx.ps
    nring = 2 * (S // 128)
    PT, o_t, rec = bufs["PT"], bufs["o_t"], bufs["rec"]
    tag = "att"
    st = {"npt": 0, "nsc": 0}
    units = [(qb, h) for qb in range(S // 512) for h in range(nh)]

    def scores(qb, h):
        pts = []
        for kt in range(4 * qb + 4):
            j = kt - 4 * qb
            bi_ = sbanks[st["nsc"] % len(sbanks)]
            pss, kpss = psb[bi_], ("ps", bi_)
            st["nsc"] += 1
            ops = list(score_ops(h, kt, qb))
            if j >= 0:
                mb, km = masks[j]
                ops.append((c["identb"][:], mb[:], [km, "identb"]))
            for i, (lh, rh, ks) in enumerate(ops):
                P.pe(lambda e, pss=pss, lh=lh, rh=rh, i=i, n=len(ops): e.matmul(pss[:], lh, rh, start=(i == 0), stop=(i == n - 1)),
                     r=ks, w=[kpss])
            pt, kpt = PT[st["npt"] % nring], ("PT", tag, st["npt"] % nring)
            st["npt"] += 1
            if bias_ap is not None:
                bap, kb = bias_ap(h, kt)
                P.act(lambda e, pt=pt, pss=pss, bap=bap: e.activation(pt[:], pss[:], AF.Exp, bias=bap, scale=scale),
                      r=[kpss] + kb, w=[kpt])
            else:
                P.act(lambda e, pt=pt, pss=pss: e.activation(pt[:], pss[:], AF.Exp, scale=scale), r=[kpss], w=[kpt])
            pts.append((pt, kpt))
        return pts

    def pv(ui, qb, h, pts):
        ot, kot = o_t[qb % 2], ("o_t", tag, qb % 2)
        if dv + 1 <= 128:
            pso, kpso = psb[4 + ui % 2], [("ps", 4 + ui % 2)]
            acc = [pso[:, tb * 128: tb * 128 + dv + 1] for tb in range(4)]
        else:
            p0, p1 = psb[4 + 2 * (ui % 2)], psb[5 + 2 * (ui % 2)]
            kpso = [("ps", 4 + 2 * (ui % 2)), ("ps", 5 + 2 * (ui % 2))]
            acc = [p0[:, 0:dv + 1], p0[:, 256:256 + dv + 1], p1[:, 0:dv + 1], p1[:, 256:256 + dv + 1]]
        for tb in range(4):
            last = 4 * qb + tb
            for kt in range(last + 1):
                pt, kpt = pts[kt]
                va, kv = v_ap(h, kt)
                P.pe(lambda e, a=acc[tb], pt=pt, tb=tb, va=va, kt=kt, last=last: e.matmul(
                    a, pt[:, tb * 128:(tb + 1) * 128], va, start=(kt == 0), stop=(kt == last)),
                    r=[kpt] + kv, w=kpso)
        rc, krc = rec[ui % 2], ("rec", tag, ui % 2)
        for tb in range(4):
            P.dve(lambda e, rc=rc, a=acc[tb], tb=tb: e.reciprocal(rc[:, tb:tb + 1], a[:, dv:dv + 1]), r=kpso, w=[krc])
        for tb in range(4):
            if tb % 2 == 0:
                P.dve(lambda e, rc=rc, a=acc[tb], tb=tb, h=h, ot=ot: e.tensor_scalar_mul(
                    ot[:, tb, h * dv:(h + 1) * dv], a[:, 0:dv], rc[:, tb:tb + 1]), r=kpso + [krc], w=[kot])
            else:
                P.act(lambda e, rc=rc, a=acc[tb], tb=tb, h=h, ot=ot: e.mul(
                    ot[:, tb, h * dv:(h + 1) * dv], a[:, 0:dv], rc[:, tb:tb + 1]), r=kpso + [krc], w=[kot])
        if h == nh - 1:
            out_cb(qb, ot, kot)

    prev = None
    for ui, (qb, h) in enumerate(units):
        pts = scores(qb, h)
        if prev is not None:
            pv(*prev)
        prev = (ui, qb, h, pts)
    pv(*prev)


def rms_out_cb(cx, col0, g_b, kg, mixed, tag):
    P, sb = cx.P, cx.sb
    junk = sb.alloc([128, 512], F32, "junk")
    ssq = sb.alloc([128, 4], F32, "ssq")
    ybuf = [sb.alloc([128, 4, 512], F32, "ybuf") for _ in range(2)]
    S = cx.S

    def cb(qb, ot, kot, b):
        ks = ("ssq", tag)
        yb, ky = ybuf[qb % 2], ("ybuf", tag, qb % 2)
        for tb in range(4):
            P.act(lambda e, tb=tb: e.activation(junk[:], ot[:, tb, :], AF.Square, accum_out=ssq[:, tb:tb + 1]),
                  r=[kot], w=[ks, ("junk", tag)])
        rstd_from_ssq(P, ssq[:], ssq[:], 512, RMS_EPS, [ks], [ks])
        for tb in range(4):
            P.dve(lambda e, tb=tb, yb=yb: e.scalar_tensor_tensor(yb[:, tb, :], ot[:, tb, :], ssq[:, tb:tb + 1], g_b,
                                                               ALU.mult, ALU.mult), r=[kot, ks, kg], w=[ky])
        r0 = b * S + qb * 512
        P.dma(mixed[r0:r0 + 512, col0:col0 + 512].rearrange("(tb p) n -> p tb n", p=128), yb[:], r=[ky], q="pool")
    return lambda b: (lambda qb, ot, kot: cb(qb, ot, kot, b))


def stage_fox(cx, qkT, fT, u_tok, b_f, out_g, mixed):
    nc, P, sb, c = cx.nc, cx.P, cx.sb, cx.c
    S, NB = cx.S, cx.NB
    nt = S // 128
    m = sb.mark()
    masks = c["masks_causal"]
    qT = sb.alloc([128, 4, S], BF16, "qT")
    kT = sb.alloc([128, 4, S], BF16, "kT")
    vaug = sb.alloc([128, nt, 8, 65], BF16, "vaug")
    vst = [sb.alloc([128, 512], F32, "vst") for _ in range(2)]
    fx = sb.alloc([8, S], F32, "fx")
    fa = sb.alloc([8, S], F32, "fa")
    Fc = sb.alloc([8, S], F32, "Fc")
    ones8 = sb.alloc([8, S], F32, "ones8")
    F8 = sb.alloc([8, S], BF16, "F8")
    bfc = sb.alloc([8, 1], F32, "bfc")
    sel = sb.alloc([8, 8, 128], BF16, "sel")
    nF = sb.alloc([128, nt, 8], F32, "nF")
    g_b = sb.alloc([128, 512], F32, "g_b")
    bcast_load(cx, g_b[:], out_g, "fox_g")
    P.dma(bfc[:], b_f.rearrange("(h o) -> h o", o=1), w=["bfc"])
    P.pool(lambda e: e.memset(ones8[:], 1.0), w=["ones8"])
    P.pool(lambda e: e.memset(vaug[:, :, :, 64:65], 1.0), w=["vones"])
    P.dve(lambda e: e.tensor_copy(sel[:], c["ident"][0:8, 0:8].unsqueeze(2).to_broadcast([8, 8, 128])), r=["ident"], w=["sel"])
    ocb = rms_out_cb(cx, 0, g_b[:], "fox_g", mixed, "fox")
    abufs = attn_bufs(cx, 8, 64)
    for b in range(NB):
        t0 = b * S
        tag = "fox%d" % b
        P.dma(qT[:], qkT[0:512, t0:t0 + S].rearrange("(hp p) t -> p hp t", p=128), w=["qT"])
        P.dma(kT[:], qkT[512:1024, t0:t0 + S].rearrange("(hp p) t -> p hp t", p=128), w=["kT"])
        P.dma(fx[:], fT[0:8, t0:t0 + S], w=["fx"])
        for tt in range(nt):
            vs, kvs = vst[tt % 2], ("vst", tt % 2)
            P.dma(vs[:], u_tok[t0 + tt * 128:t0 + (tt + 1) * 128, 1024:1536], w=[kvs])
            P.pool(lambda e, vs=vs, tt=tt: e.tensor_copy(vaug[:, tt, :, 0:64], vs[:].rearrange("p (h d) -> p h d", h=8)),
                   r=[kvs], w=[("v", tt)])
        P.dve(lambda e: e.tensor_scalar_add(fx[:], fx[:], bfc[:, 0:1]), r=["fx", "bfc"], w=["fx"])
        P.act(lambda e: e.activation(fa[:], fx[:], AF.Abs), r=["fx"], w=["fa"])
        P.act(lambda e: e.activation(fa[:], fa[:], AF.Exp, scale=-1.0), r=["fa"], w=["fa"])
        P.act(lambda e: e.activation(fa[:], fa[:], AF.Ln, bias=1.0), r=["fa"], w=["fa"])
        P.dve(lambda e: e.tensor_scalar_min(fx[:], fx[:], 0.0), r=["fx"], w=["fx"])
        P.dve(lambda e: e.tensor_sub(fx[:], fx[:], fa[:]), r=["fx", "fa"], w=["fx"])
        P.dve(lambda e: e.tensor_tensor_scan(Fc[:], ones8[:], fx[:], 0.0, ALU.mult, ALU.add), r=["fx", "ones8"], w=["Fc"])
        P.act(lambda e: e.mul(F8[:], Fc[:], 8.0), r=["Fc"], w=["F8"])
        for tt in range(nt):
            ps, kp = cx.ps[7], ("ps", 7)
            P.pe(lambda e, tt=tt, ps=ps: e.transpose(ps[:, 0:8], Fc[0:8, tt * 128:(tt + 1) * 128], c["ident"][0:8, 0:8]),
                 r=["Fc", "ident"], w=[kp])
            P.act(lambda e, tt=tt, ps=ps: e.mul(nF[:, tt, :], ps[:, 0:8], -1.0), r=[kp], w=["nF"])

        def score_ops(h, kt, qb):
            hp, base = h // 2, (h % 2) * 64
            return [(kT[base:base + 64, hp, kt * 128:(kt + 1) * 128], qT[base:base + 64, hp, qb * 512:(qb + 1) * 512], ["kT", "qT"]),
                    (sel[0:8, h, :], F8[0:8, qb * 512:(qb + 1) * 512], ["sel", "F8"])]

        def bias_ap(h, kt):
            return nF[:, kt, h:h + 1], ["nF"]

        def v_ap(h, kt):
            return vaug[:, kt, h, :], [("v", kt), "vones"]

        attn_core(cx, 8, 64, 0.125, score_ops, bias_ap, masks, v_ap, ocb(b), tag, bufs=abufs)
    P.barrier()
    sb.reset(m)


def rope_tables(cx, pos_dram, b, cosb, sinb, ifr_b, tmpa, rbufs):
    P, sb = cx.P, cx.sb
    S = cx.S
    nt = S // 128
    posi, posf = rbufs["posi"], rbufs["posf"]
    TWO_PI = 2.0 * np.pi
    P.dma(posi[:], pos_dram[b, :].rearrange("(t p) -> p t", p=128), w=["posi"], slow=True)
    P.dve(lambda e: e.tensor_copy(posf[:], posi[:]), r=["posi"], w=["posf"])
    for tt in range(nt):
        P.dve(lambda e, tt=tt: e.tensor_scalar_mul(tmpa[:, tt, :], ifr_b, posf[:, tt:tt + 1]), r=["posf", "ifr"], w=["ang"])
    ti, tf = rbufs["ti"], rbufs["tf"]
    C1, C2 = 6.28125, 2.0 * np.pi - 6.28125

    def sin_of(out, shift, key):
        P.dve(lambda e: e.tensor_scalar(out, tmpa[:], shift, 1.0 / TWO_PI, ALU.add, ALU.mult), r=["ang"], w=[key])
        P.dve(lambda e: e.tensor_copy(ti[:], out), r=[key], w=["ropei"])
        P.dve(lambda e: e.tensor_copy(tf[:], ti[:]), r=["ropei"], w=["ropef"])
        P.dve(lambda e: e.tensor_scalar_add(out, tmpa[:], shift), r=["ang"], w=[key])
        P.dve(lambda e: e.scalar_tensor_tensor(out, tf[:], -C1, out, ALU.mult, ALU.add), r=["ropef", key], w=[key])
        P.dve(lambda e: e.scalar_tensor_tensor(out, tf[:], -C2, out, ALU.mult, ALU.add), r=["ropef", key], w=[key])
        P.dve(lambda e: e.tensor_scalar(tf[:], out, np.pi, -TWO_PI, ALU.is_gt, ALU.mult), r=[key], w=["ropef"])
        P.dve(lambda e: e.tensor_add(out, out, tf[:]), r=["ropef", key], w=[key])
        P.dve(lambda e: e.tensor_scalar(tf[:], out, -np.pi, TWO_PI, ALU.is_lt, ALU.mult), r=[key], w=["ropef"])
        P.dve(lambda e: e.tensor_add(out, out, tf[:]), r=["ropef", key], w=[key])
        P.dve(lambda e: e.tensor_scalar(out, out, 3.1415925, -3.1415925, ALU.min, ALU.max), r=[key], w=[key])
        P.act(lambda e: e.activation(out, out, AF.Sin), r=[key], w=[key])

    sin_of(sinb[:], 0.0, "sinb")
    sin_of(cosb[:], 0.5 * np.pi, "cosb")


def rope_apply(P, out, x, cos, sin, t1, t2, nh, r, w):
    cb = cos.unsqueeze(1).to_broadcast([128, nh, 32])
    sn = sin.unsqueeze(1).to_broadcast([128, nh, 32])
    x1, x2 = x[:, :, 0:32], x[:, :, 32:64]
    kk = ("ropetmp",)
    P.dve(lambda e: e.tensor_mul(t1, x1, cb), r=r, w=[kk])
    P.dve(lambda e: e.tensor_mul(t2, x2, sn), r=r, w=[kk])
    P.dve(lambda e: e.tensor_sub(out[:, :, 0:32], t1, t2), r=[kk], w=w)
    P.dve(lambda e: e.tensor_mul(t1, x2, cb), r=r + [kk], w=[kk])
    P.dve(lambda e: e.tensor_mul(t2, x1, sn), r=r + [kk], w=[kk])
    P.dve(lambda e: e.tensor_add(out[:, :, 32:64], t1, t2), r=[kk], w=w)


def load_cast_w(cx, dst_bf, src_view, stage, key, eng="pool"):
    P = cx.P
    ks = ("wstage", id(stage))
    P.dma(stage, src_view, w=[ks])
    if eng == "pool":
        P.pool(lambda e: e.tensor_copy(dst_bf, stage), r=[ks], w=[key])
    else:
        P.dve(lambda e: e.tensor_copy(dst_bf, stage), r=[ks], w=[key])


def stage_mla(cx, u_tok, pos, ifr, qg, kvg, w_uq, w_ukv, out_g, mixed):
    nc, P, sb, c = cx.nc, cx.P, cx.sb, cx.c
    S, NB = cx.S, cx.NB
    nt = S // 128
    psb = cx.ps
    m = sb.mark()
    masks = c["masks_chunk64"]
    wq_n = sb.alloc([128, 3, 512], BF16, "wq_n")
    wq_p = sb.alloc([128, 3, 256], BF16, "wq_p")
    wk_n = sb.alloc([128, 512], BF16, "wk_n")
    wk_v = sb.alloc([128, 512], BF16, "wk_v")
    wstg = sb.alloc([128, 3, 768], F32, "wstg")
    P.dma(wstg[:], w_uq.rearrange("(kc p) n -> p kc n", p=128), w=["wstg"])
    v4 = wstg[:].rearrange("p k (h d) -> p k h d", h=4)
    for kc in range(3):
        P.pool(lambda e, kc=kc: e.tensor_copy(wq_n[:, kc, :].rearrange("p (h d) -> p h d", h=4), v4[:, kc, :, 0:128]), r=["wstg"], w=["wq"])
        P.pool(lambda e, kc=kc: e.tensor_copy(wq_p[:, kc, :].rearrange("p (h d) -> p h d", h=4), v4[:, kc, :, 128:192]), r=["wstg"], w=["wq"])
    wstg2 = sb.alloc([128, 1024], F32, "wstg2")
    P.dma(wstg2[:], w_ukv, w=["wstg2"])
    v5 = wstg2[:].rearrange("p (h d) -> p h d", h=4)
    P.pool(lambda e: e.tensor_copy(wk_n[:].rearrange("p (h d) -> p h d", h=4), v5[:, :, 0:128]), r=["wstg2"], w=["wk"])
    P.pool(lambda e: e.tensor_copy(wk_v[:].rearrange("p (h d) -> p h d", h=4), v5[:, :, 128:256]), r=["wstg2"], w=["wk"])
    qg_b = sb.alloc([128, 384], F32, "qg_b")
    kvg_b = sb.alloc([128, 128], F32, "kvg_b")
    g_b = sb.alloc([128, 512], F32, "g_b")
    ifr_b = sb.alloc([128, 32], F32, "ifr_b")
    bcast_load(cx, qg_b[:], qg, "qg")
    bcast_load(cx, kvg_b[:], kvg, "kvg")
    bcast_load(cx, g_b[:], out_g, "mla_g")
    bcast_load(cx, ifr_b[:], ifr, "ifr")
    cosb = sb.alloc([128, nt, 32], F32, "cosb")
    sinb = sb.alloc([128, nt, 32], F32, "sinb")
    ang = sb.alloc([128, nt, 32], F32, "ang")
    cqnT = sb.alloc([128, 3, S], BF16, "cqnT")
    ckvnT = sb.alloc([128, S], BF16, "ckvnT")
    qnT = sb.alloc([128, 4, S], BF16, "qnT")
    qpT = sb.alloc([128, 2, S], BF16, "qpT")
    knT = sb.alloc([128, 4, S], BF16, "knT")
    kpT = sb.alloc([128, S], BF16, "kpT")
    vaug = sb.alloc([128, nt, 4, 129], BF16, "vaug")
    urow = [sb.alloc([128, 576], F32, "urow") for _ in range(2)]
    cn = [sb.alloc([128, 640], F32, "cn") for _ in range(2)]
    qpe = [sb.alloc([128, 256], F32, "qpe") for _ in range(2)]
    junk = sb.alloc([128, 384], F32, "junk")
    ssq = sb.alloc([128, 2], F32, "ssq2")
    t1 = sb.alloc([128, 4, 32], F32, "t1")
    t2 = sb.alloc([128, 4, 32], F32, "t2")
    P.pool(lambda e: e.memset(vaug[:, :, :, 128:129], 1.0), w=["vones"])
    ocb = rms_out_cb(cx, 512, g_b[:], "mla_g", mixed, "mla")
    abufs = attn_bufs(cx, 4, 128)
    rbufs = dict(posi=sb.alloc([128, nt], I32, "posi"), posf=sb.alloc([128, nt], F32, "posf"),
                 ti=sb.alloc([128, nt, 32], I32, "ropei"), tf=sb.alloc([128, nt, 32], F32, "ropef"))
    for b in range(NB):
        t0 = b * S
        tag = "mla%d" % b
        rope_tables(cx, pos, b, cosb, sinb, ifr_b[:], ang, rbufs)
        nev = 0
        for tt in range(nt):
            i2 = tt % 2
            ur, kur = urow[i2], ("urow", i2)
            cnt, kcn = cn[i2], ("cn", i2)
            P.dma(ur[:], u_tok[t0 + tt * 128:t0 + (tt + 1) * 128, C_MLA:C_MLA + 576], w=[kur])
            P.act(lambda e, ur=ur: e.activation(junk[:, 0:384], ur[:, 0:384], AF.Square, accum_out=ssq[:, 0:1]), r=[kur], w=["ssq2", "junk"])
            P.act(lambda e, ur=ur: e.activation(junk[:, 0:128], ur[:, 384:512], AF.Square, accum_out=ssq[:, 1:2]), r=[kur], w=["ssq2", "junk"])
            rstd_from_ssq(P, ssq[:, 0:1], ssq[:, 0:1], 384, RMS_EPS, ["ssq2"], ["ssq2"])
            rstd_from_ssq(P, ssq[:, 1:2], ssq[:, 1:2], 128, RMS_EPS, ["ssq2"], ["ssq2"])
            P.dve(lambda e, ur=ur, cnt=cnt: e.scalar_tensor_tensor(cnt[:, 0:384], ur[:, 0:384], ssq[:, 0:1], qg_b[:], ALU.mult, ALU.mult),
                  r=[kur, "ssq2", "qg"], w=[kcn])
            P.dve(lambda e, ur=ur, cnt=cnt: e.scalar_tensor_tensor(cnt[:, 384:512], ur[:, 384:512], ssq[:, 1:2], kvg_b[:], ALU.mult, ALU.mult),
                  r=[kur, "ssq2", "kvg"], w=[kcn])
            rope_apply(P, cnt[:, 512:576].rearrange("p (h d) -> p h d", h=1), ur[:, 512:576].rearrange("p (h d) -> p h d", h=1),
                       cosb[:, tt, :], sinb[:, tt, :], t1[:, 0:1, :], t2[:, 0:1, :], 1, [kur, "cosb", "sinb"], [kcn])
            P.dve(lambda e, cnt=cnt: e.tensor_copy(cnt[:, 576:640], cnt[:, 512:576]), r=[kcn], w=[kcn])
            pi = 2 + nev % 2
            nev += 1
            ps, kp = psb[pi], ("ps", pi)
            for j in range(4):
                P.pe(lambda e, ps=ps, cnt=cnt, j=j: e.transpose(ps[:, j * 128:(j + 1) * 128], cnt[:, j * 128:(j + 1) * 128], c["ident"][:]),
                     r=[kcn, "ident"], w=[kp])
            P.act(lambda e, ps=ps, tt=tt: e.copy(cqnT[:, :, tt * 128:(tt + 1) * 128], ps[:, 0:384].rearrange("p (j t) -> p j t", j=3)),
                  r=[kp], w=[("cqnT", tt)])
            P.dve(lambda e, ps=ps, tt=tt: e.tensor_copy(ckvnT[:, tt * 128:(tt + 1) * 128], ps[:, 384:512]), r=[kp], w=[("ckvnT", tt)])
            pi = 2 + nev % 2
            nev += 1
            ps, kp = psb[pi], ("ps", pi)
            P.pe(lambda e, ps=ps, cnt=cnt: e.transpose(ps[:, 0:128], cnt[:, 512:640], c["ident"][:]), r=[kcn, "ident"], w=[kp])
            P.act(lambda e, ps=ps, tt=tt: e.copy(kpT[:, tt * 128:(tt + 1) * 128], ps[:, 0:128]), r=[kp], w=[("kpT", tt)])
            pi = 2 + nev % 2
            nev += 1
            ps, kp = psb[pi], ("ps", pi)
            for kc in range(3):
                P.pe(lambda e, ps=ps, kc=kc, tt=tt: e.matmul(ps[:, 0:256], cqnT[:, kc, tt * 128:(tt + 1) * 128], wq_p[:, kc, :],
                                                            start=(kc == 0), stop=(kc == 2)), r=[("cqnT", tt), "wq"], w=[kp])
            qp, kqp = qpe[i2], ("qpe", i2)
            rope_apply(P, qp[:].rearrange("p (h d) -> p h d", h=4), ps[:, 0:256].rearrange("p (h d) -> p h d", h=4),
                       cosb[:, tt, :], sinb[:, tt, :], t1[:], t2[:], 4, [kp, "cosb", "sinb"], [kqp])
            pi = 2 + nev % 2
            nev += 1
            ps, kp = psb[pi], ("ps", pi)
            for j in range(2):
                P.pe(lambda e, ps=ps, qp=qp, j=j: e.transpose(ps[:, j * 128:(j + 1) * 128], qp[:, j * 128:(j + 1) * 128], c["ident"][:]),
                     r=[kqp, "ident"], w=[kp])
            P.act(lambda e, ps=ps, tt=tt: e.copy(qpT[:, :, tt * 128:(tt + 1) * 128], ps[:, 0:256].rearrange("p (j t) -> p j t", j=2)),
                  r=[kp], w=[("qpT", tt)])
            pi = 2 + nev % 2
            nev += 1
            ps, kp = psb[pi], ("ps", pi)
            P.pe(lambda e, ps=ps, tt=tt: e.matmul(ps[:], ckvnT[:, tt * 128:(tt + 1) * 128], wk_v[:], start=True, stop=True),
                 r=[("ckvnT", tt), "wk"], w=[kp])
            P.dve(lambda e, ps=ps, tt=tt: e.tensor_copy(vaug[:, tt, :, 0:128], ps[:].rearrange("p (h d) -> p h d", h=4)),
                  r=[kp], w=[("v", tt)])
        for tb in range(S // 512):
            for h in range(4):
                pi = 2 + nev % 2
                nev += 1
                ps, kp = psb[pi], ("ps", pi)
                for kc in range(3):
                    P.pe(lambda e, ps=ps, kc=kc, h=h, tb=tb: e.matmul(ps[:], wq_n[:, kc, h * 128:(h + 1) * 128], cqnT[:, kc, tb * 512:(tb + 1) * 512],
                                                                  start=(kc == 0), stop=(kc == 2)),
                         r=[("cqnT", tb * 4 + i) for i in range(4)] + ["wq"], w=[kp])
                evac(P, nev, qnT[:, h, tb * 512:(tb + 1) * 512], ps[:], r=[kp], w=[("qnT", tb)])
                pi = 2 + nev % 2
                nev += 1
                ps, kp = psb[pi], ("ps", pi)
                P.pe(lambda e, ps=ps, h=h, tb=tb: e.matmul(ps[:], wk_n[:, h * 128:(h + 1) * 128], ckvnT[:, tb * 512:(tb + 1) * 512],
                                                         start=True, stop=True),
                     r=[("ckvnT", tb * 4 + i) for i in range(4)] + ["wk"], w=[kp])
                evac(P, nev, knT[:, h, tb * 512:(tb + 1) * 512], ps[:], r=[kp], w=[("knT", tb)])

        def score_ops(h, kt, qb):
            base = (h % 2) * 64
            return [(knT[:, h, kt * 128:(kt + 1) * 128], qnT[:, h, qb * 512:(qb + 1) * 512], [("knT", kt // 4), ("qnT", qb)]),
                    (kpT[base:base + 64, kt * 128:(kt + 1) * 128], qpT[base:base + 64, h // 2, qb * 512:(qb + 1) * 512],
                     [("kpT", kt)] + [("qpT", qb * 4 + i) for i in range(4)])]

        def v_ap(h, kt):
            return vaug[:, kt, h, :], [("v", kt), "vones"]

        attn_core(cx, 4, 128, 192 ** -0.5, score_ops, None, masks, v_ap, ocb(b), tag, sbanks=(0, 1), bufs=abufs)
    P.barrier()
    sb.reset(m)


def run_interleaved(gens):
    gens = list(gens)
    while gens:
        for g in list(gens):
            try:
                next(g)
            except StopIteration:
                gens.remove(g)


def dplr_consts(cx):
    P, sb = cx.P, cx.sb
    c = cx.c
    if "MS" in c:
        return
    MS = sb.alloc([128, 128], F32, "MS")
    MI = sb.alloc([128, 128], F32, "MI")
    MZ = sb.alloc([128, 128], F32, "MZ")
    ML = sb.alloc([128, 128], F32, "ML")
    BLK = sb.alloc([128, 128], F32, "BLK")
    for (M, base, cm, pat, lo_zero) in ((MS, -1, -1, 1, "ur"), (MI, 0, -1, 1, "ur"), (MZ, -1, 1, -1, "ll"), (ML, 0, 1, -1, "ll")):
        k = "dplrmask"
        P.pool(lambda e, M=M: e.memset(M[:], 1.0), w=[k])
        P.pool(lambda e, M=M, base=base, cm=cm, pat=pat: e.affine_select(
            out=M[:], in_=M[:], pattern=[[pat, 128]], compare_op=ALU.is_ge, fill=0.0, base=base, channel_multiplier=cm), r=[k], w=[k])
        if lo_zero == "ur":
            P.pool(lambda e, M=M: e.memset(M[0:64, 64:128], 0.0), r=[k], w=[k])
        else:
            P.pool(lambda e, M=M: e.memset(M[64:128, 0:64], 0.0), r=[k], w=[k])
    P.pool(lambda e: e.memset(BLK[:], 0.0), w=["dplrmask"])
    P.pool(lambda e: e.memset(BLK[0:64, 0:64], 1.0), r=["dplrmask"], w=["dplrmask"])
    P.pool(lambda e: e.memset(BLK[64:128, 64:128], 1.0), r=["dplrmask"], w=["dplrmask"])
    c.update(MS=MS, MI=MI, MZ=MZ, ML=ML, BLK=BLK)


def dplr_tile(cx, NH, NK, geo, st, tag, banks=(0, 1, 2)):
    P, c = cx.P, cx.c
    B0, B1, B2 = banks
    psb = {0: cx.ps[B0], 1: cx.ps[B1], 2: cx.ps[B2], 3: cx.ps[B0], 4: cx.ps[B1], 5: cx.ps[B2], 6: cx.ps[B0]}
    bk = {0: B0, 1: B1, 2: B2, 3: B0, 4: B1, 5: B2, 6: B0}
    HP = 128 // NK
    NG = NH // HP
    HG = 512 // 128
    ngr = (NH + HG - 1) // HG
    Xs, Zs, Ps = st["X"], st["Z"], st["P"]
    kX, kZ, kP = [("dX", tag, i) for i in range(2)], [("dZ", tag, i) for i in range(2)], [("dP", tag, i) for i in range(2)]
    identb3 = c["identb"][:].unsqueeze(1).to_broadcast([128, NH, 128])
    P.dve(lambda e: e.tensor_tensor(Ps[0][:], Xs[0][:], identb3, ALU.add), r=[kX[0], "identb"], w=[kP[0]])
    cur = 0
    for lvl in range(1, 6):
        nxt = 1 - cur
        for g in range(ngr):
            hs = list(range(g * HG, min(NH, (g + 1) * HG)))
            n = len(hs)
            if lvl < 5:
                for i, h in enumerate(hs):
                    P.pe(lambda e, i=i, h=h, cur=cur: e.matmul(psb[0][:, i * 128:(i + 1) * 128], Zs[cur][:, h, :], Xs[cur][:, h, :],
                                                             start=True, stop=True), r=[kX[cur], kZ[cur]], w=[("ps", bk[0])])
                P.act(lambda e, g=g, n=n, nxt=nxt: e.copy(Xs[nxt][:, g * HG:g * HG + n, :],
                                                         psb[0][:, 0:n * 128].rearrange("p (h t) -> p h t", h=n)),
                      r=[("ps", bk[0])], w=[kX[nxt]])
            for i, h in enumerate(hs):
                P.pe(lambda e, i=i, h=h, cur=cur: e.matmul(psb[1][:, i * 128:(i + 1) * 128], Xs[cur][:, h, :], Zs[cur][:, h, :],
                                                         start=True, stop=True), r=[kX[cur], kZ[cur]], w=[("ps", bk[1])])
            P.dve(lambda e, g=g, n=n, nxt=nxt: e.tensor_copy(Zs[nxt][:, g * HG:g * HG + n, :],
                                                            psb[1][:, 0:n * 128].rearrange("p (h t) -> p h t", h=n)),
                  r=[("ps", bk[1])], w=[kZ[nxt]])
            for i, h in enumerate(hs):
                P.pe(lambda e, i=i, h=h, cur=cur, nxt=nxt: e.matmul(psb[2][:, i * 128:(i + 1) * 128], Zs[nxt][:, h, :], Ps[cur][:, h, :],
                                                                  start=True, stop=True), r=[kZ[nxt], kP[cur]], w=[("ps", bk[2])])
            P.dve(lambda e, g=g, n=n, cur=cur, nxt=nxt: e.tensor_tensor(
                Ps[nxt][:, g * HG:g * HG + n, :], psb[2][:, 0:n * 128].rearrange("p (h t) -> p h t", h=n),
                Ps[cur][:, g * HG:g * HG + n, :], ALU.add), r=[("ps", bk[2]), kP[cur]], w=[kP[nxt]])
            yield
        cur = nxt
    TT, kTT = Ps[cur], kP[cur]
    ST, STb = st["ST"], st["STb"]
    kST = ("dST", tag)
    NV = NK
    W = NH * NV
    Wb, Ub = st["Wb"], st["Ub"]
    for ch in range(2):
        c0 = ch * 64
        M = 64 + c0
        rows = slice(c0, c0 + 64)
        for h in range(NH):
            g_, hb = h // HP, (h % HP) * NK
            at, kat = geo["AT"](h)
            P.pe(lambda e, h=h, at=at, g_=g_, hb=hb, M=M: e.matmul(psb[3][0:M, h * NV:(h + 1) * NV], at[:, 0:M], STb[:, g_, :],
                                                                start=True, stop=False), r=kat + [kST], w=[("ps", bk[3])])
            ak, kak = geo["AAK"](h)
            v, kv = geo["V"](h)
            P.pe(lambda e, h=h, ak=ak, v=v, M=M, rows=rows: e.matmul(psb[3][0:M, h * NV:(h + 1) * NV], ak[rows, 0:M], v[rows, :],
                                                                  start=False, stop=True), r=kak + kv, w=[("ps", bk[3])])
        kW = ("dW", tag)
        if geo.get("WADD") is not None:
            wa, kwa = geo["WADD"]
            P.dve(lambda e, rows=rows, wa=wa: e.tensor_tensor(Wb[rows, :], psb[3][rows, 0:W], wa[rows, :], ALU.add), r=[("ps", bk[3])] + kwa, w=[kW])
        else:
            P.act(lambda e, rows=rows: e.copy(Wb[rows, :], psb[3][rows, 0:W]), r=[("ps", bk[3])], w=[kW])
        yield
        for h in range(NH):
            P.pe(lambda e, h=h, M=M, rows=rows: e.matmul(psb[4][0:M, h * NV:(h + 1) * NV], TT[rows, h, 0:M], Wb[rows, h * NV:(h + 1) * NV],
                                                      start=True, stop=True), r=[kTT, kW], w=[("ps", bk[4])])
        kU = ("dU", tag)
        P.dve(lambda e, rows=rows: e.tensor_copy(Ub[rows, :], psb[4][rows, 0:W]), r=[("ps", bk[4])], w=[kU])
        yield
        for h in range(NH):
            g_, hb = h // HP, (h % HP) * NK
            rt, krt = geo["RT"](h)
            arb, karb = geo["ARB"](h)
            ark, kark = geo["ARK"](h)
            v, kv = geo["V"](h)
            o = psb[5][0:M, h * NV:(h + 1) * NV]
            P.pe(lambda e, o=o, rt=rt, g_=g_, hb=hb, M=M: e.matmul(o, rt[:, 0:M], STb[:, g_, :], start=True, stop=False),
                 r=krt + [kST], w=[("ps", bk[5])])
            P.pe(lambda e, o=o, arb=arb, h=h, M=M, rows=rows: e.matmul(o, arb[rows, 0:M], Ub[rows, h * NV:(h + 1) * NV], start=False, stop=False),
                 r=karb + [kU], w=[("ps", bk[5])])
            P.pe(lambda e, o=o, ark=ark, v=v, M=M, rows=rows: e.matmul(o, ark[rows, 0:M], v[rows, :], start=False, stop=True),
                 r=kark + kv, w=[("ps", bk[5])])
        yo, kyo = geo["Y"]
        P.act(lambda e, rows=rows: e.copy(yo[rows, :], psb[5][rows, 0:W]), r=[("ps", bk[5])], w=[kyo])
        bd, kbd = geo["BD"]
        kd, kkd = geo["KD"]
        for h in range(NH):
            g_ = h // HP
            v, kv = geo["V"](h)
            o = psb[6][:, h * NV:(h + 1) * NV]
            P.pe(lambda e, o=o, g_=g_, h=h, rows=rows: e.matmul(o, bd[rows, g_ * 128:(g_ + 1) * 128], Ub[rows, h * NV:(h + 1) * NV],
                                                             start=True, stop=False), r=kbd + [kU], w=[("ps", bk[6])])
            P.pe(lambda e, o=o, g_=g_, v=v, rows=rows: e.matmul(o, kd[rows, g_ * 128:(g_ + 1) * 128], v[rows, :], start=False, stop=True),
                 r=kkd + kv, w=[("ps", bk[6])])
        gc_, kgc = geo["GC"](ch)
        for hh in range(HP):
            pr = slice(hh * NK, (hh + 1) * NK)
            src = psb[6][pr, 0:NH * NV].rearrange("p (g x v) -> p g x v", g=NG, x=HP)[:, :, hh, :]
            P.dve(lambda e, pr=pr: e.tensor_tensor(ST[pr, :, :], ST[pr, :, :], gc_[pr, :].unsqueeze(2).to_broadcast([NK, NG, NV]), ALU.mult),
                  r=[kST] + kgc, w=[kST])
            P.dve(lambda e, pr=pr, src=src: e.tensor_tensor(ST[pr, :, :], ST[pr, :, :], src, ALU.add), r=[kST, ("ps", bk[6])], w=[kST])
        P.act(lambda e: e.copy(STb[:], ST[:]), r=[kST], w=[kST])
        yield


def stage_rwkv(cx, u_tok, prm, mixed):
    nc, P, sb, c, psb = cx.nc, cx.P, cx.sb, cx.c, cx.ps
    S, NB = cx.S, cx.NB
    nt = S // 128
    m = sb.mark()
    dplr_consts(cx)
    NH, NK = 8, 64
    f32t = lambda name, w=512: sb.alloc([128, w], F32, name)
    mu_b = f32t("mu_b", 1792)
    bc = {}
    for nm in ("w0", "a0", "k_k", "k_a", "r_k", "ln_g", "ln_b"):
        bc[nm] = f32t(nm + "_b")
        bcast_load(cx, bc[nm][:], prm[nm], "rw_" + nm)
    bcast_load(cx, mu_b[:], prm["mu"], "rw_mu")
    w2b = sb.alloc([64, 512], BF16, "w2b")
    a2b = sb.alloc([128, 512], BF16, "a2b")
    g2b = sb.alloc([128, 512], BF16, "g2b")
    wtmp = f32t("wtmp")
    load_cast_w(cx, w2b[:], prm["w2"], wtmp[0:64, :], "rw_w2")
    wtmp2 = f32t("wtmp2")
    load_cast_w(cx, a2b[64:128, :], prm["a2"], wtmp2[64:128, :], "rw_a2")
    wtmp3 = f32t("wtmp3")
    load_cast_w(cx, g2b[:], prm["g2"], wtmp3[:], "rw_g2")
    Ut = [sb.alloc([128, 1792], F32, "Ut") for _ in range(2)]
    Up = [sb.alloc([128, 1792], F32, "Up") for _ in range(2)]
    xs = sb.alloc([128, 1792], F32, "xs")
    la = f32t("la", 256)
    lT = sb.alloc([128, 256], BF16, "lT")
    names = ["lw", "aa", "gt", "Gs", "eG", "enG", "eGm", "eD", "kx", "sq", "kk", "k2", "bv", "tR", "tK", "tB", "tA", "rk", "yt", "y2"]
    T_ = {n: f32t(n) for n in names}
    small = {n: sb.alloc([128, 8], F32, n) for n in ("ssq", "rn", "bs", "s1", "s2", "mean", "var", "rstd")}
    PB = []
    for _b in range(NB):
        d_ = dict(Kd=sb.alloc([128, 512], BF16, "Kd"), Bd=sb.alloc([128, 512], BF16, "Bd"), Vb=sb.alloc([128, 512], BF16, "Vb"),
                  ART=sb.alloc([128, 8, 2, 128], BF16, "ARTz"), KT=sb.alloc([128, 4, 128], BF16, "KT"), BT=sb.alloc([128, 4, 128], BF16, "BT"),
                  AM3=sb.alloc([128, 8, 384], BF16, "AM3"), gC=[sb.alloc([128, 4], F32, "gC") for _ in range(2)],
                  yt=f32t("ytb"), gt=f32t("gtb"), vk=f32t("vkb"), bs=sb.alloc([128, 8], F32, "bsb"),
                  st=dict(X=[sb.alloc([128, 8, 128], BF16, "dX") for _ in range(2)], Z=[sb.alloc([128, 8, 128], BF16, "dZ") for _ in range(2)],
                          P=[sb.alloc([128, 8, 128], BF16, "dP") for _ in range(2)], ST=sb.alloc([128, 4, 64], F32, "ST"),
                          STb=sb.alloc([128, 4, 64], BF16, "STb"), Wb=sb.alloc([128, 512], BF16, "Wb"), Ub=sb.alloc([128, 512], BF16, "Ub")))
        P.pool(lambda e, A_=d_["ART"]: e.memset(A_[:], 0.0), w=[("ARTz0", _b)])
        PB.append(d_)
    mask4 = sb.alloc([128, 4, 128], F32, "mask4")
    for i, Mk in enumerate(("MS", "MI", "MS", "MI")):
        P.pool(lambda e, i=i, Mk=Mk: e.tensor_copy(mask4[:, i, :], c[Mk][:]), r=["dplrmask"], w=["mask4"])
    MZ4 = c["MZ"][:].unsqueeze(1).to_broadcast([128, 4, 128])
    h3 = lambda t: t.rearrange("p (h d) -> p h d", h=8)
    b3 = lambda t: t.unsqueeze(2).to_broadcast([128, 8, 64])
    if True:
        def tile_prep(b, tt):
            tag = "rw%d" % b
            kST = ("dST", tag)
            pb = PB[b]
            Kd, Bd, Vb, ART, KT, BT, AM3, gC, st = (pb[k] for k in ("Kd", "Bd", "Vb", "ART", "KT", "BT", "AM3", "gC", "st"))
            kgt, kbs, kvk, kyt = ("gt", b), ("bs", b), ("vk", b), ("yt", b)
            if tt == 0:
                P.pool(lambda e: e.memset(st["ST"][:], 0.0), w=[kST])
                P.pool(lambda e: e.memset(st["STb"][:], 0.0), r=[kST], w=[kST])
            i2 = (tt * NB + b) % 2
            r0 = b * S + tt * 128
            U_, Up_ = Ut[i2], Up[i2]
            kU_, kUp = ("Ut", i2), ("Up", i2)
            cs = slice(C_RWKV, C_RWKV + 1792)
            P.dma(U_[:], u_tok[r0:r0 + 128, cs], w=[kU_])
            if tt == 0:
                P.pool(lambda e, Up_=Up_: e.memset(Up_[0:1, :], 0.0), w=[kUp])
                P.dma(Up_[1:128, :], u_tok[r0:r0 + 127, cs], w=[kUp])
            else:
                P.dma(Up_[:], u_tok[r0 - 1:r0 + 127, cs], w=[kUp])
            P.pool(lambda e, U_=U_, Up_=Up_: e.tensor_sub(Up_[:], Up_[:], U_[:]), r=[kU_, kUp], w=[kUp])
            P.pool(lambda e, Up_=Up_: e.tensor_mul(Up_[:], Up_[:], mu_b[:]), r=[kUp, "rw_mu"], w=[kUp])
            P.dve(lambda e, U_=U_, Up_=Up_: e.tensor_add(xs[:], U_[:], Up_[:]), r=[kU_, kUp], w=["xs"])
            r_, k_, v_ = xs[:, 0:512], xs[:, 512:1024], xs[:, 1024:1536]
            P.act(lambda e: e.copy(pb["vk"][:], v_), r=["xs"], w=[kvk])
            P.act(lambda e: e.activation(la[:, 0:64], xs[:, 1536:1600], AF.Tanh), r=["xs"], w=["la"])
            P.act(lambda e: e.copy(la[:, 64:128], xs[:, 1600:1664]), r=["xs"], w=["la"])
            P.act(lambda e: e.activation(la[:, 128:256], xs[:, 1664:1792], AF.Sigmoid), r=["xs"], w=["la"])
            for j in range(2):
                P.pe(lambda e, j=j: e.transpose(psb[7][:, j * 128:(j + 1) * 128], la[:, j * 128:(j + 1) * 128], c["ident"][:]),
                     r=["la", "ident"], w=[("ps", 7)])
            P.act(lambda e: e.copy(lT[:], psb[7][:, 0:256]), r=[("ps", 7)], w=["lT"])
            P.pe(lambda e: e.matmul(psb[3][:], lT[0:64, 0:128], w2b[0:64, :], start=True, stop=True), r=["lT", "rw_w2"], w=[("ps", 3)])
            P.pe(lambda e: e.matmul(psb[4][:], lT[64:128, 0:128], a2b[64:128, :], start=True, stop=True), r=["lT", "rw_a2"], w=[("ps", 4)])
            P.pe(lambda e: e.matmul(psb[5][:], lT[:, 128:256], g2b[:], start=True, stop=True), r=["lT", "rw_g2"], w=[("ps", 5)])
            P.dve(lambda e: e.tensor_add(T_["lw"][:], psb[3][:], bc["w0"][:]), r=[("ps", 3), "rw_w0"], w=["lw"])
            P.act(lambda e: e.activation(T_["lw"][:], T_["lw"][:], AF.Sigmoid), r=["lw"], w=["lw"])
            P.act(lambda e: e.mul(T_["lw"][:], T_["lw"][:], -float(np.exp(-0.5))), r=["lw"], w=["lw"])
            P.dve(lambda e: e.tensor_add(T_["aa"][:], psb[4][:], bc["a0"][:]), r=[("ps", 4), "rw_a0"], w=["aa"])
            P.act(lambda e: e.activation(T_["aa"][:], T_["aa"][:], AF.Sigmoid), r=["aa"], w=["aa"])
            P.act(lambda e: e.copy(pb["gt"][:], psb[5][:]), r=[("ps", 5)], w=[kgt])
            P.pe(lambda e: e.matmul(psb[3][:], c["MI"][:], T_["lw"][:], start=True, stop=True), r=["dplrmask", "lw"], w=[("ps", 3)])
            P.pe(lambda e: e.matmul(psb[4][:], c["BLK"][:], T_["lw"][:], start=True, stop=True), r=["dplrmask", "lw"], w=[("ps", 4)])
            P.act(lambda e: e.copy(T_["Gs"][:], psb[3][:]), r=[("ps", 3)], w=["Gs"])
            P.act(lambda e: e.activation(T_["eG"][:], T_["Gs"][:], AF.Exp), r=["Gs"], w=["eG"])
            P.act(lambda e: e.activation(T_["enG"][:], T_["Gs"][:], AF.Exp, scale=-1.0), r=["Gs"], w=["enG"])
            P.dve(lambda e: e.tensor_sub(T_["eGm"][:], T_["Gs"][:], T_["lw"][:]), r=["Gs", "lw"], w=["eGm"])
            P.act(lambda e: e.activation(T_["eGm"][:], T_["eGm"][:], AF.Exp), r=["eGm"], w=["eGm"])
            P.dve(lambda e: e.tensor_sub(T_["eD"][:], psb[4][:], T_["Gs"][:]), r=[("ps", 4), "Gs"], w=["eD"])
            P.act(lambda e: e.activation(T_["eD"][:], T_["eD"][:], AF.Exp), r=["eD"], w=["eD"])
            P.dve(lambda e: e.tensor_mul(T_["kx"][:], k_, bc["k_k"][:]), r=["xs", "rw_k_k"], w=["kx"])
            P.pool(lambda e: e.tensor_mul(T_["sq"][:], T_["kx"][:], T_["kx"][:]), r=["kx"], w=["sq"])
            P.dve(lambda e: e.tensor_reduce(small["ssq"][:], h3(T_["sq"][:]), AX.X, ALU.add), r=["sq"], w=["ssq"])
            P.dve(lambda e: e.tensor_scalar_add(small["rn"][:], small["ssq"][:], 1e-6), r=["ssq"], w=["rn"])
            P.act(lambda e: e.activation(small["rn"][:], small["rn"][:], AF.Sqrt), r=["rn"], w=["rn"])
            P.dve(lambda e: e.reciprocal(small["rn"][:], small["rn"][:]), r=["rn"], w=["rn"])
            P.dve(lambda e: e.tensor_mul(h3(T_["kk"][:]), h3(T_["kx"][:]), b3(small["rn"][:])), r=["kx", "rn"], w=["kk"])
            P.dve(lambda e: e.scalar_tensor_tensor(T_["k2"][:], T_["aa"][:], -1.0, bc["k_a"][:], ALU.add, ALU.mult), r=["aa", "rw_k_a"], w=["k2"])
            P.dve(lambda e: e.scalar_tensor_tensor(T_["k2"][:], T_["k2"][:], 1.0, k_, ALU.add, ALU.mult), r=["k2", "xs"], w=["k2"])
            P.dve(lambda e: e.tensor_mul(T_["bv"][:], T_["kk"][:], T_["aa"][:]), r=["kk", "aa"], w=["bv"])
            P.pool(lambda e: e.tensor_mul(T_["tR"][:], r_, T_["eG"][:]), r=["xs", "eG"], w=["tR"])
            P.pool(lambda e: e.tensor_mul(T_["tK"][:], T_["k2"][:], T_["enG"][:]), r=["k2", "enG"], w=["tK"])
            P.pool(lambda e: e.tensor_mul(T_["tB"][:], T_["bv"][:], T_["enG"][:]), r=["bv", "enG"], w=["tB"])
            P.dve(lambda e: e.scalar_tensor_tensor(T_["tA"][:], T_["kk"][:], -1.0, T_["eGm"][:], ALU.mult, ALU.mult), r=["kk", "eGm"], w=["tA"])
            kgeo = ("geo", tag)
            P.pool(lambda e: e.tensor_mul(Kd[:], T_["k2"][:], T_["eD"][:]), r=["k2", "eD"], w=[kgeo])
            P.pool(lambda e: e.tensor_mul(Bd[:], T_["bv"][:], T_["eD"][:]), r=["bv", "eD"], w=[kgeo])
            P.act(lambda e: e.copy(Vb[:], v_), r=["xs"], w=[kgeo])
            P.pool(lambda e: e.tensor_mul(T_["rk"][:], r_, T_["k2"][:]), r=["xs", "k2"], w=["rk"])
            P.pool(lambda e: e.tensor_mul(T_["rk"][:], T_["rk"][:], bc["r_k"][:]), r=["rk", "rw_r_k"], w=["rk"])
            P.dve(lambda e: e.tensor_reduce(pb["bs"][:], h3(T_["rk"][:]), AX.X, ALU.add), r=["rk"], w=[kbs])
            for (src, ksrc, dst) in ((T_["tA"], "tA", lambda q: ART[:, q, 0, :]), (T_["tR"], "tR", lambda q: ART[:, q, 1, :]),
                                      (T_["tK"], "tK", lambda q: KT[:, q, :]), (T_["tB"], "tB", lambda q: BT[:, q, :])):
                for q in range(4):
                    P.pe(lambda e, src=src, q=q: e.transpose(psb[7][:, q * 128:(q + 1) * 128], src[:, q * 128:(q + 1) * 128], c["ident"][:]),
                         r=[ksrc, "ident"], w=[("ps", 7)])
                if ksrc in ("tA", "tR"):
                    j = 0 if ksrc == "tA" else 1
                    for x in range(2):
                        pr = slice(x * 64, (x + 1) * 64)
                        dst_ = ART[pr, :, :, :].rearrange("p (q x) a t -> p q x a t", x=2)[:, :, x, j, :]
                        if x == 0:
                            P.act(lambda e, dst_=dst_, pr=pr: e.copy(dst_, psb[7][pr, :].rearrange("p (q t) -> p q t", q=4)), r=[("ps", 7), ("ARTz0", b)], w=[kgeo])
                        else:
                            P.dve(lambda e, dst_=dst_, pr=pr: e.tensor_copy(dst_, psb[7][pr, :].rearrange("p (q t) -> p q t", q=4)), r=[("ps", 7), ("ARTz0", b)], w=[kgeo])
                elif ksrc == "tK":
                    P.dve(lambda e: e.tensor_copy(KT[:], psb[7][:].rearrange("p (q t) -> p q t", q=4)), r=[("ps", 7)], w=[kgeo])
                else:
                    P.dve(lambda e: e.tensor_copy(BT[:], psb[7][:].rearrange("p (q t) -> p q t", q=4)), r=[("ps", 7)], w=[kgeo])
            for q in range(4):
                P.pe(lambda e, q=q: e.transpose(psb[7][:, q * 128:(q + 1) * 128], T_["eG"][:, q * 128:(q + 1) * 128], c["ident"][:]),
                     r=["eG", "ident"], w=[("ps", 7)])
            pv7 = psb[7][:].rearrange("p (q t) -> p q t", q=4)
            P.dve(lambda e: e.tensor_copy(gC[0][:], pv7[:, :, 63]), r=[("ps", 7)], w=[kgeo])
            P.dve(lambda e: e.tensor_copy(gC[1][:], pv7[:, :, 127]), r=[("ps", 7)], w=[kgeo])
            kX0, kZ0 = ("dX", tag, 0), ("dZ", tag, 0)
            for h in range(8):
                q, hb = h // 2, (h % 2) * 64
                pa = psb[h % 2]
                kpa = ("ps", h % 2)
                rhs2 = ART[:, h, :, :].rearrange("p a t -> p (a t)")
                P.pe(lambda e, pa=pa, q=q, rhs2=rhs2: e.matmul(pa[:, 0:256], BT[:, q, :], rhs2, start=True, stop=True), r=[kgeo], w=[kpa])
                P.pe(lambda e, pa=pa, q=q, rhs2=rhs2: e.matmul(pa[:, 256:512], KT[:, q, :], rhs2, start=True, stop=True), r=[kgeo], w=[kpa])
                pav = pa[:].rearrange("p (a t) -> p a t", a=4)
                P.dve(lambda e, h=h, pav=pav: e.tensor_tensor(st["X"][0][:, h, :], pav[:, 0, :], mask4[:, 0, :], ALU.mult), r=[kpa, "mask4"], w=[kX0])
                P.dve(lambda e, h=h, pav=pav: e.tensor_tensor(AM3[:, h, :].rearrange("p (a t) -> p a t", a=3), pav[:, 1:4, :], mask4[:, 1:4, :], ALU.mult),
                      r=[kpa, "mask4"], w=[("AM3", tag)])
                pz = psb[2]
                P.pe(lambda e, q=q, hb=hb, h=h: e.matmul(psb[2][:, (h % 4) * 128:(h % 4 + 1) * 128], ART[:, h, 0, :], BT[:, q, :],
                                                        start=True, stop=True), r=[kgeo], w=[("ps", 2)])
                if h % 4 == 3:
                    g4 = h // 4
                    P.dve(lambda e, g4=g4: e.tensor_tensor(st["Z"][0][:, g4 * 4:(g4 + 1) * 4, :], psb[2][:].rearrange("p (h t) -> p h t", h=4), MZ4, ALU.mult),
                          r=[("ps", 2), "dplrmask"], w=[kZ0])
            geo = dict(
                AT=lambda h: (ART[:, h, 0, :], [kgeo]),
                RT=lambda h: (ART[:, h, 1, :], [kgeo]),
                ARB=lambda h: (AM3[:, h, 0:128], [("AM3", tag)]),
                AAK=lambda h: (AM3[:, h, 128:256], [("AM3", tag)]),
                ARK=lambda h: (AM3[:, h, 256:384], [("AM3", tag)]),
                V=lambda h: (Vb[:, h * 64:(h + 1) * 64], [kgeo]),
                BD=(Bd, [kgeo]), KD=(Kd, [kgeo]), GC=lambda ch: (gC[ch], [kgeo]),
                Y=(pb["yt"], kyt), WADD=None)
            return geo

        def tile_post(b, tt):
            pb = PB[b]
            r0 = b * S + tt * 128
            kgt, kbs, kvk, kyt = ("gt", b), ("bs", b), ("vk", b), ("yt", b)
            yt, y2 = pb["yt"], T_["y2"]
            P.pool(lambda e: e.tensor_mul(y2[:], yt[:], yt[:]), r=[kyt], w=["y2"])
            P.dve(lambda e: e.tensor_reduce(small["s1"][:], h3(yt[:]), AX.X, ALU.add), r=[kyt], w=["s1"])
            P.dve(lambda e: e.tensor_reduce(small["s2"][:], h3(y2[:]), AX.X, ALU.add), r=["y2"], w=["s2"])
            P.dve(lambda e: e.tensor_scalar_mul(small["mean"][:], small["s1"][:], 1.0 / 64), r=["s1"], w=["mean"])
            P.dve(lambda e: e.tensor_mul(small["var"][:], small["mean"][:], small["mean"][:]), r=["mean"], w=["var"])
            P.dve(lambda e: e.scalar_tensor_tensor(small["var"][:], small["s2"][:], 1.0 / 64, small["var"][:], ALU.mult, ALU.subtract), r=["s2", "var"], w=["var"])
            P.dve(lambda e: e.tensor_scalar_add(small["rstd"][:], small["var"][:], 64e-5), r=["var"], w=["rstd"])
            P.act(lambda e: e.activation(small["rstd"][:], small["rstd"][:], AF.Sqrt), r=["rstd"], w=["rstd"])
            P.dve(lambda e: e.reciprocal(small["rstd"][:], small["rstd"][:]), r=["rstd"], w=["rstd"])
            P.dve(lambda e: e.tensor_sub(h3(yt[:]), h3(yt[:]), b3(small["mean"][:])), r=[kyt, "mean"], w=[kyt])
            P.dve(lambda e: e.tensor_mul(h3(yt[:]), h3(yt[:]), b3(small["rstd"][:])), r=[kyt, "rstd"], w=[kyt])
            P.pool(lambda e: e.tensor_mul(yt[:], yt[:], bc["ln_g"][:]), r=[kyt, "rw_ln_g"], w=[kyt])
            P.pool(lambda e: e.tensor_add(yt[:], yt[:], bc["ln_b"][:]), r=[kyt, "rw_ln_b"], w=[kyt])
            P.dve(lambda e: e.tensor_mul(h3(y2[:]), h3(pb["vk"][:]), b3(pb["bs"][:])), r=[kvk, kbs, "y2"], w=["y2"])
            P.dve(lambda e: e.tensor_add(yt[:], yt[:], y2[:]), r=[kyt, "y2"], w=[kyt])
            P.dve(lambda e: e.tensor_mul(y2[:], yt[:], pb["gt"][:]), r=[kyt, kgt], w=["y2"])
            P.dma(mixed[r0:r0 + 128, 1024:1536], y2[:], r=["y2"], q="pool")
    for tt in range(nt):
        geos = [tile_prep(b, tt) for b in range(NB)]
        run_interleaved([dplr_tile(cx, 8, 64, geos[b], PB[b]["st"], "rw%d" % b, banks=(3 * (b % 2), 3 * (b % 2) + 1, 3 * (b % 2) + 2))
                         for b in range(NB)])
        for b in range(NB):
            tile_post(b, tt)
    P.barrier()
    sb.reset(m)


def stage_gdn(cx, u_tok, prm, mixed):
    nc, P, sb, c, psb = cx.nc, cx.P, cx.sb, cx.c, cx.ps
    S, NB = cx.S, cx.NB
    nt = S // 128
    m = sb.mark()
    dplr_consts(cx)
    NH, NK = 4, 128
    cw_b = [sb.alloc([128, 1536], F32, "cw_b") for _ in range(4)]
    for j in range(4):
        bcast_load(cx, cw_b[j][:], prm["conv_w"][j, :], "gd_cw")
    A_b = sb.alloc([128, 4], F32, "A_b")
    dtb_b = sb.alloc([128, 4], F32, "dtb_b")
    ng_b = sb.alloc([128, 4, 128], F32, "ng_b")
    bcast_load(cx, A_b[:], prm["a_log"], "gd_A")
    bcast_load(cx, dtb_b[:], prm["dt_bias"], "gd_dtb")
    for h in range(4):
        bcast_load(cx, ng_b[:, h, :], prm["norm_g"], "gd_ng")
    P.act(lambda e: e.activation(A_b[:], A_b[:], AF.Exp), r=["gd_A"], w=["gd_A"])
    ones = sb.alloc([128, 128], F32, "ones")
    P.pool(lambda e: e.memset(ones[:], 1.0), w=["ones"])
    BIGM = sb.alloc([128, 4, 128], F32, "BIGM")
    for h in range(4):
        P.dve(lambda e, h=h: e.tensor_scalar(BIGM[:, h, :], c["ML"][:], -1.0, 30000.0, ALU.add, ALU.mult), r=["dplrmask"], w=["BIGM"])
    P.dve(lambda e: e.tensor_scalar_mul(BIGM[:], BIGM[:], -1.0), r=["BIGM"], w=["BIGM"])
    MZ4 = c["MZ"][:].unsqueeze(1).to_broadcast([128, 4, 128])
    sh = [sb.alloc([128, 1536], F32, "sh") for _ in range(4)]
    acc = sb.alloc([128, 1536], F32, "acc")
    zab = sb.alloc([128, 520], F32, "zab")
    f4 = lambda n: sb.alloc([128, 4], F32, n)
    sm = {n: f4(n) for n in ("ssq", "rnq", "rnk", "beta", "nbeta", "sp", "g", "gcs", "egc", "edec", "ssqo", "tmp", "tmp2")}
    gC = [f4("gC0"), f4("gC1")]
    diag = sb.alloc([128, 4, 128], F32, "diag")
    D4 = sb.alloc([128, 4, 128], F32, "D4")
    DS4 = sb.alloc([128, 4, 128], F32, "DS4")
    bf = lambda n: sb.alloc([128, 512], BF16, n)
    qn, kn, ta, tr, Kd, Vp = bf("qn"), bf("kn"), bf("ta"), bf("tr"), bf("Kd"), bf("Vp")
    kT, qT, AT, RT = [sb.alloc([128, 4, 128], BF16, n) for n in ("kT", "qT", "AT", "RT")]
    Zt, Art = sb.alloc([128, 4, 128], BF16, "Zt"), sb.alloc([128, 4, 128], BF16, "Art")
    XK, ArT = sb.alloc([128, 4, 128], BF16, "XK"), sb.alloc([128, 4, 128], BF16, "ArT")
    yt = sb.alloc([128, 512], F32, "yt")
    y2 = sb.alloc([128, 512], F32, "y2")
    sz = sb.alloc([128, 512], F32, "sz")
    st = dict(X=[sb.alloc([128, 4, 128], BF16, "dX") for _ in range(2)], Z=[sb.alloc([128, 4, 128], BF16, "dZ") for _ in range(2)],
              P=[sb.alloc([128, 4, 128], BF16, "dP") for _ in range(2)], ST=sb.alloc([128, 4, 128], F32, "ST"),
              STb=sb.alloc([128, 4, 128], BF16, "STb"), Wb=sb.alloc([128, 512], BF16, "Wb"), Ub=sb.alloc([128, 512], BF16, "Ub"))
    h3 = lambda t: t.rearrange("p (h d) -> p h d", h=4)
    b3 = lambda t: t.unsqueeze(2).to_broadcast([128, 4, 128])
    pbf = lambda i: psb[i][:].bitcast(BF16)
    PB = []
    for _b in range(NB):
        PB.append(dict(Kd=bf("Kd"), Vp=bf("Vp"), AT=sb.alloc([128, 4, 128], BF16, "AT"), RT=sb.alloc([128, 4, 128], BF16, "RT"),
                       XK=sb.alloc([128, 4, 128], BF16, "XK"), ArT=sb.alloc([128, 4, 128], BF16, "ArT"), gC=[f4("gC0"), f4("gC1")],
                       yt=sb.alloc([128, 512], F32, "ytb"), sz=sb.alloc([128, 512], F32, "szb"),
                       st=dict(X=[sb.alloc([128, 4, 128], BF16, "dX") for _ in range(2)], Z=[sb.alloc([128, 4, 128], BF16, "dZ") for _ in range(2)],
                               P=[sb.alloc([128, 4, 128], BF16, "dP") for _ in range(2)], ST=sb.alloc([128, 4, 128], F32, "ST"),
                               STb=sb.alloc([128, 4, 128], BF16, "STb"), Wb=sb.alloc([128, 512], BF16, "Wb"), Ub=sb.alloc([128, 512], BF16, "Ub"))))
    if True:
        def tile_prep(b, tt):
            tag = "gd%d" % b
            kST = ("dST", tag)
            pb = PB[b]
            Kd, Vp, AT, RT, XK, ArT, gC, st, yt, sz = (pb[k] for k in ("Kd", "Vp", "AT", "RT", "XK", "ArT", "gC", "st", "yt", "sz"))
            kyt, ksz = ("yt", b), ("sz", b)
            if tt == 0:
                P.pool(lambda e: e.memset(st["ST"][:], 0.0), w=[kST])
                P.pool(lambda e: e.memset(st["STb"][:], 0.0), r=[kST], w=[kST])
            r0 = b * S + tt * 128
            for j in range(4):
                d = 3 - j
                ksh = ("sh", j)
                if tt == 0 and d > 0:
                    P.pool(lambda e, j=j, d=d: e.memset(sh[j][0:d, :], 0.0), w=[ksh])
                    P.dma(sh[j][d:128, :], u_tok[r0:r0 + 128 - d, C_GDN:C_GDN + 1536], w=[ksh])
                else:
                    P.dma(sh[j][:], u_tok[r0 - d:r0 - d + 128, C_GDN:C_GDN + 1536], w=[ksh])
            P.dma(zab[:], u_tok[r0:r0 + 128, C_GDN + 1536:C_GDN + 2056], w=["zab"])
            P.dve(lambda e: e.tensor_mul(acc[:], sh[3][:], cw_b[3][:]), r=[("sh", 3), "gd_cw"], w=["acc"])
            for j in range(3):
                P.pool(lambda e, j=j: e.tensor_mul(sh[j][:], sh[j][:], cw_b[j][:]), r=[("sh", j), "gd_cw"], w=[("sh", j)])
                P.dve(lambda e, j=j: e.tensor_add(acc[:], acc[:], sh[j][:]), r=[("sh", j), "acc"], w=["acc"])
            P.act(lambda e: e.activation(acc[:], acc[:], AF.Silu), r=["acc"], w=["acc"])
            q_, k_, v_ = acc[:, 0:512], acc[:, 512:1024], acc[:, 1024:1536]
            P.pool(lambda e: e.tensor_mul(y2[:], q_, q_), r=["acc"], w=["y2"])
            P.dve(lambda e: e.tensor_reduce(sm["ssq"][:], h3(y2[:]), AX.X, ALU.add), r=["y2"], w=["ssq"])
            P.dve(lambda e: e.tensor_scalar_add(sm["rnq"][:], sm["ssq"][:], 1e-6), r=["ssq"], w=["rnq"])
            P.act(lambda e: e.activation(sm["rnq"][:], sm["rnq"][:], AF.Sqrt), r=["rnq"], w=["rnq"])
            P.dve(lambda e: e.reciprocal(sm["rnq"][:], sm["rnq"][:]), r=["rnq"], w=["rnq"])
            P.dve(lambda e: e.tensor_scalar_mul(sm["rnq"][:], sm["rnq"][:], 128 ** -0.5), r=["rnq"], w=["rnq"])
            P.pool(lambda e: e.tensor_mul(y2[:], k_, k_), r=["acc", "ssq"], w=["y2"])
            P.dve(lambda e: e.tensor_reduce(sm["ssq"][:], h3(y2[:]), AX.X, ALU.add), r=["y2", "rnq"], w=["ssq"])
            P.dve(lambda e: e.tensor_scalar_add(sm["rnk"][:], sm["ssq"][:], 1e-6), r=["ssq"], w=["rnk"])
            P.act(lambda e: e.activation(sm["rnk"][:], sm["rnk"][:], AF.Sqrt), r=["rnk"], w=["rnk"])
            P.dve(lambda e: e.reciprocal(sm["rnk"][:], sm["rnk"][:]), r=["rnk"], w=["rnk"])
            kg = ("gdgeo", tag)
            P.dve(lambda e: e.tensor_mul(h3(qn[:]), h3(q_), b3(sm["rnq"][:])), r=["acc", "rnq"], w=[kg])
            P.dve(lambda e: e.tensor_mul(h3(kn[:]), h3(k_), b3(sm["rnk"][:])), r=["acc", "rnk"], w=[kg])
            P.act(lambda e: e.activation(sm["beta"][:], zab[:, 512:516], AF.Sigmoid), r=["zab"], w=["beta"])
            P.dve(lambda e: e.tensor_scalar_mul(sm["nbeta"][:], sm["beta"][:], -1.0), r=["beta"], w=["nbeta"])
            P.dve(lambda e: e.tensor_add(sm["sp"][:], zab[:, 516:520], dtb_b[:]), r=["zab", "gd_dtb"], w=["sp"])
            P.act(lambda e: e.activation(sm["tmp"][:], sm["sp"][:], AF.Abs), r=["sp"], w=["tmp"])
            P.act(lambda e: e.activation(sm["tmp"][:], sm["tmp"][:], AF.Exp, scale=-1.0), r=["tmp"], w=["tmp"])
            P.act(lambda e: e.activation(sm["tmp"][:], sm["tmp"][:], AF.Ln, bias=1.0), r=["tmp"], w=["tmp"])
            P.dve(lambda e: e.tensor_scalar_max(sm["sp"][:], sm["sp"][:], 0.0), r=["sp"], w=["sp"])
            P.dve(lambda e: e.tensor_add(sm["sp"][:], sm["sp"][:], sm["tmp"][:]), r=["sp", "tmp"], w=["sp"])
            P.dve(lambda e: e.scalar_tensor_tensor(sm["g"][:], sm["sp"][:], -1.0, A_b[:], ALU.mult, ALU.mult), r=["sp", "gd_A"], w=["g"])
            P.pe(lambda e: e.matmul(psb[7][:, 0:4], c["MI"][:], sm["g"][:], start=True, stop=True), r=["dplrmask", "g"], w=[("ps", 7)])
            P.pe(lambda e: e.matmul(psb[7][:, 4:8], c["BLK"][:], sm["g"][:], start=True, stop=True), r=["dplrmask", "g"], w=[("ps", 7)])
            P.act(lambda e: e.copy(sm["gcs"][:], psb[7][:, 0:4]), r=[("ps", 7)], w=["gcs"])
            P.dve(lambda e: e.tensor_sub(sm["edec"][:], psb[7][:, 4:8], sm["gcs"][:]), r=[("ps", 7), "gcs"], w=["edec"])
            P.act(lambda e: e.activation(sm["edec"][:], sm["edec"][:], AF.Exp), r=["edec"], w=["edec"])
            P.act(lambda e: e.activation(sm["egc"][:], sm["gcs"][:], AF.Exp), r=["gcs"], w=["egc"])
            P.pe(lambda e: e.matmul(psb[3][:, 0:4], ones[0:64, :], sm["g"][0:64, :], start=True, stop=True), r=["ones", "g"], w=[("ps", 3)])
            P.pe(lambda e: e.matmul(psb[4][:, 0:4], ones[64:128, :], sm["g"][64:128, :], start=True, stop=True), r=["ones", "g"], w=[("ps", 4)])
            P.act(lambda e: e.activation(gC[0][:], psb[3][:, 0:4], AF.Exp), r=[("ps", 3)], w=[kg])
            P.act(lambda e: e.activation(gC[1][:], psb[4][:, 0:4], AF.Exp), r=[("ps", 4)], w=[kg])
            P.dve(lambda e: e.tensor_mul(h3(tr[:]), h3(qn[:]), b3(sm["egc"][:])), r=[kg, "egc"], w=["tr"])
            P.dve(lambda e: e.tensor_mul(sm["tmp2"][:], sm["nbeta"][:], sm["egc"][:]), r=["nbeta", "egc"], w=["tmp2"])
            P.dve(lambda e: e.tensor_mul(h3(ta[:]), h3(kn[:]), b3(sm["tmp2"][:])), r=[kg, "tmp2"], w=["ta"])
            P.dve(lambda e: e.tensor_mul(h3(Kd[:]), h3(kn[:]), b3(sm["edec"][:])), r=[kg, "edec"], w=[kg])
            P.dve(lambda e: e.tensor_mul(h3(Vp[:]), h3(v_), b3(sm["beta"][:])), r=["acc", "beta"], w=[kg])
            for (src, ksrc, dst) in ((kn, kg, kT), (qn, kg, qT), (ta, "ta", AT), (tr, "tr", RT)):
                for h in range(4):
                    P.pe(lambda e, src=src, h=h: e.transpose(pbf(7)[:, h * 128:(h + 1) * 128], src[:, h * 128:(h + 1) * 128], c["identb"][:]),
                         r=[ksrc, "identb"], w=[("ps", 7)])
                P.act(lambda e, dst=dst: e.copy(dst[:], pbf(7)[:, 0:512].rearrange("p (h t) -> p h t", h=4)), r=[("ps", 7)], w=[kg])
            for h in range(4):
                P.dve(lambda e, h=h: e.tensor_scalar_mul(diag[:, h, :], c["ident"][:], sm["gcs"][:, h:h + 1]), r=["gcs", "ident"], w=["diag"])
            P.pe(lambda e: e.matmul(psb[5][:], ones[:], diag[:].rearrange("p h t -> p (h t)"), start=True, stop=False), r=["ones", "diag"], w=[("ps", 5)])
            P.pe(lambda e: e.matmul(psb[5][:], c["ident"][:], BIGM[:].rearrange("p h t -> p (h t)"), start=False, stop=True), r=["ident", "BIGM"], w=[("ps", 5)])
            for h in range(4):
                P.act(lambda e, h=h: e.activation(D4[:, h, :], psb[5][:, h * 128:(h + 1) * 128], AF.Exp, bias=sm["gcs"][:, h:h + 1], scale=-1.0),
                      r=[("ps", 5), "gcs"], w=["D4"])
            P.pool(lambda e: e.tensor_mul(DS4[:], D4[:], MZ4), r=["D4", "dplrmask"], w=["DS4"])
            for h in range(4):
                P.pe(lambda e, h=h: e.matmul(psb[3][:, h * 128:(h + 1) * 128], kT[:, h, :], kT[:, h, :], start=True, stop=True), r=[kg], w=[("ps", 3)])
            for h in range(4):
                P.pe(lambda e, h=h: e.matmul(psb[4][:, h * 128:(h + 1) * 128], qT[:, h, :], kT[:, h, :], start=True, stop=True), r=[kg], w=[("ps", 4)])
            for h in range(4):
                P.dve(lambda e, h=h: e.scalar_tensor_tensor(Zt[:, h, :], psb[3][:, h * 128:(h + 1) * 128], sm["nbeta"][:, h:h + 1], DS4[:, h, :],
                                                          ALU.mult, ALU.mult), r=[("ps", 3), "nbeta", "DS4"], w=["Zt"])
            P.dve(lambda e: e.tensor_tensor(Art[:], psb[4][:].rearrange("p (h t) -> p h t", h=4), D4[:], ALU.mult), r=[("ps", 4), "D4"], w=["Art"])
            kX0, kZ0 = ("dX", tag, 0), ("dZ", tag, 0)
            P.pool(lambda e: e.tensor_copy(st["Z"][0][:], Zt[:]), r=["Zt"], w=[kZ0])
            for h in range(4):
                P.pe(lambda e, h=h: e.transpose(pbf(7)[:, h * 128:(h + 1) * 128], Zt[:, h, :], c["identb"][:]), r=["Zt", "identb"], w=[("ps", 7)])
            P.act(lambda e: e.copy(XK[:], pbf(7)[:, 0:512].rearrange("p (h t) -> p h t", h=4)), r=[("ps", 7)], w=[kg])
            P.dve(lambda e: e.tensor_copy(st["X"][0][:], pbf(7)[:, 0:512].rearrange("p (h t) -> p h t", h=4)), r=[("ps", 7)], w=[kX0])
            for h in range(4):
                P.pe(lambda e, h=h: e.transpose(pbf(7)[:, h * 128:(h + 1) * 128], Art[:, h, :], c["identb"][:]), r=["Art", "identb"], w=[("ps", 7)])
            P.act(lambda e: e.copy(ArT[:], pbf(7)[:, 0:512].rearrange("p (h t) -> p h t", h=4)), r=[("ps", 7)], w=[kg])
            geo = dict(
                AT=lambda h: (AT[:, h, :], [kg]), RT=lambda h: (RT[:, h, :], [kg]),
                ARB=lambda h: (ArT[:, h, :], [kg]), ARK=lambda h: (ArT[:, h, :], [kg]), AAK=lambda h: (XK[:, h, :], [kg]),
                V=lambda h: (Vp[:, h * 128:(h + 1) * 128], [kg]),
                BD=(Kd, [kg]), KD=(Kd, [kg]), GC=lambda ch: (gC[ch], [kg]), Y=(yt, kyt), WADD=None)
            P.act(lambda e: e.activation(sz[:], zab[:, 0:512], AF.Silu), r=["zab"], w=[ksz])
            return geo

        def tile_post(b, tt):
            pb = PB[b]
            yt, sz = pb["yt"], pb["sz"]
            kyt, ksz = ("yt", b), ("sz", b)
            r0 = b * S + tt * 128
            P.pool(lambda e: e.tensor_mul(y2[:], yt[:], yt[:]), r=[kyt], w=["y2"])
            P.dve(lambda e: e.tensor_reduce(sm["ssqo"][:], h3(y2[:]), AX.X, ALU.add), r=["y2"], w=["ssqo"])
            rstd_from_ssq(P, sm["ssqo"][:], sm["ssqo"][:], 128, RMS_EPS, ["ssqo"], ["ssqo"])
            P.dve(lambda e: e.tensor_mul(h3(yt[:]), h3(yt[:]), b3(sm["ssqo"][:])), r=[kyt, "ssqo"], w=[kyt])
            P.pool(lambda e: e.tensor_mul(yt[:], yt[:], ng_b[:].rearrange("p h d -> p (h d)")), r=[kyt, "gd_ng"], w=[kyt])
            P.dve(lambda e: e.tensor_mul(y2[:], yt[:], sz[:]), r=[kyt, ksz, "ssqo"], w=["y2"])
            P.dma(mixed[r0:r0 + 128, 1536:2048], y2[:], r=["y2"], q="pool")
    for tt in range(nt):
        geos = [tile_prep(b, tt) for b in range(NB)]
        run_interleaved([dplr_tile(cx, 4, 128, geos[b], PB[b]["st"], "gd%d" % b, banks=(3 * (b % 2), 3 * (b % 2) + 1, 3 * (b % 2) + 2))
                         for b in range(NB)])
        for b in range(NB):
            tile_post(b, tt)
    P.barrier()
    sb.reset(m)


MOE_CAP = 384


def stage_moe(cx, h1, prm, x_out, xs_d, ys_d):
    nc, P, sb, c, psb = cx.nc, cx.P, cx.sb, cx.c, cx.ps
    T = cx.T
    ntt = T // 128
    CAP = MOE_CAP
    NROW = 32 * CAP
    BIG = 1.0e4
    m = sb.mark()
    route_i = sb.alloc([128, ntt, 2], I32, "route_i")
    route_g = sb.alloc([128, ntt, 2], F32, "route_g")
    m2 = sb.mark()
    Wr = sb.alloc([128, 16, 36], F32, "Wr")
    P.dma(Wr[:, :, 0:4], prm["w_grp"].rearrange("(kc p) n -> p kc n", p=128), w=["Wr"], slow=True)
    P.dma(Wr[:, :, 4:36], prm["w_exp"].rearrange("(kc p) n -> p kc n", p=128), w=["Wr"], slow=True)
    br_b = sb.alloc([128, 36], F32, "br_b")
    bcast_load(cx, br_b[:, 0:4], prm["b_grp"], "br")
    bcast_load(cx, br_b[:, 4:36], prm["b_exp"], "br")
    SUb, eoff, trash = c["SUb"], c["eoff"], c["trash"]
    onesb = sb.alloc([128, 128], BF16, "onesb")
    P.pool(lambda e: e.memset(onesb[:], 1.0), w=["onesb"])
    zf = sb.alloc([128, 2048], F32, "zf")
    P.pool(lambda e: e.memset(zf[:], 0.0), w=["zf"])
    P.dma(ys_d[NROW:NROW + 128, :], zf[:], r=["zf"], w=["ys_d0"])
    carry = sb.alloc([128, 32], F32, "carry")
    P.pool(lambda e: e.memset(carry[:], 0.0), w=["carry"])
    hrow = [sb.alloc([128, 2048], F32, "hrow") for _ in range(2)]
    hbf = [sb.alloc([128, 2048], BF16, "hbf") for _ in range(2)]
    hT = [sb.alloc([128, 16, 128], F32, "hT") for _ in range(2)]
    f = lambda n, w: sb.alloc([128, w], F32, n)
    lg, mg, eg, sg_, pg, gsel, tmp4 = f("lg", 36), f("mg", 1), f("eg", 4), f("sg", 1), f("pg", 1), f("gsel", 4), f("tmp4", 4)
    lem, top8, sel1, sel2, selb_f, pos, tmp32 = f("lem", 32), f("top8", 8), f("sel1", 32), f("sel2", 32), f("selsum", 32), f("pos", 32), f("tmp32", 32)
    selb = sb.alloc([128, 32], BF16, "selb")
    dd, w1, w2, slotf, valid = f("dd", 1), f("w1", 1), f("w2", 1), f("slotf", 2), f("valid", 2)
    zt = sb.alloc([128, 2048], BF16, "zt")
    P.pool(lambda e: e.memset(zt[:], 0.0), w=["zt"])
    rpp = NROW // 128
    for z0 in range(0, rpp, 16):
        P.dma(xs_d.rearrange("(p r) d -> p r d", p=128)[:, z0:z0 + 16, :], zt[:].unsqueeze(1).to_broadcast([128, 16, 2048]), r=["zt"], w=["xs_d"])
    nev = 0
    for tt in range(ntt):
        i2 = tt % 2
        r0 = tt * 128
        kh, khb, khT = ("hrow", i2), ("hbf", i2), ("hT", i2)
        P.dma(hrow[i2][:], h1[r0:r0 + 128, :], w=[kh])
        P.pool(lambda e, i2=i2: e.tensor_copy(hbf[i2][:], hrow[i2][:]), r=[kh], w=[khb])
        for g in range(4):
            pi = nev % 2
            nev += 1
            for j in range(4):
                kc = g * 4 + j
                P.pe(lambda e, pi=pi, kc=kc, j=j, i2=i2: e.transpose(psb[pi][:, j * 128:(j + 1) * 128], hrow[i2][:, kc * 128:(kc + 1) * 128], c["ident"][:]),
                     r=[kh, "ident"], w=[("ps", pi)])
            evac(P, nev, hT[i2][:, g * 4:(g + 1) * 4, :], psb[pi][:].rearrange("p (j t) -> p j t", j=4), r=[("ps", pi)], w=[khT])
        for kc in range(16):
            P.pe(lambda e, kc=kc, i2=i2: e.matmul(psb[2][:, 0:36], hT[i2][:, kc, :], Wr[:, kc, :], start=(kc == 0), stop=(kc == 15)),
                 r=[khT, "Wr"], w=[("ps", 2)])
        K = "rt"
        P.dve(lambda e: e.tensor_add(lg[:], psb[2][:, 0:36], br_b[:]), r=[("ps", 2), "br"], w=[K])
        P.dve(lambda e: e.tensor_reduce(mg[:], lg[:, 0:4], AX.X, ALU.max), r=[K], w=[K])
        P.dve(lambda e: e.tensor_scalar(gsel[:], lg[:, 0:4], mg[:, 0:1], None, ALU.is_equal), r=[K], w=[K])
        P.dve(lambda e: e.tensor_scalar_mul(tmp4[:, 0:1], mg[:], -1.0), r=[K], w=[K])
        P.act(lambda e: e.activation(eg[:], lg[:, 0:4], AF.Exp, bias=tmp4[:, 0:1], accum_out=sg_[:]), r=[K], w=[K])
        P.dve(lambda e: e.reciprocal(pg[:], sg_[:]), r=[K], w=[K])
        P.dve(lambda e: e.tensor_scalar(tmp4[:], gsel[:], BIG, -BIG, ALU.mult, ALU.add), r=[K], w=[K])
        P.dve(lambda e: e.tensor_add(lem[:].rearrange("p (g x) -> p g x", g=4), lg[:, 4:36].rearrange("p (g x) -> p g x", g=4),
                                    tmp4[:].unsqueeze(2).to_broadcast([128, 4, 8])), r=[K], w=[K])
        P.dve(lambda e: e.max(top8[:], lem[:]), r=[K], w=[K])
        P.dve(lambda e: e.tensor_scalar(sel1[:], lem[:], top8[:, 0:1], None, ALU.is_equal), r=[K], w=[K])
        P.dve(lambda e: e.tensor_scalar(sel2[:], lem[:], top8[:, 1:2], None, ALU.is_equal), r=[K], w=[K])
        P.dve(lambda e: e.tensor_sub(dd[:], top8[:, 1:2], top8[:, 0:1]), r=[K], w=[K])
        P.act(lambda e: e.activation(dd[:], dd[:], AF.Exp), r=[K], w=[K])
        P.dve(lambda e: e.tensor_scalar_add(w1[:], dd[:], 1.0), r=[K], w=[K])
        P.dve(lambda e: e.reciprocal(w1[:], w1[:]), r=[K], w=[K])
        P.dve(lambda e: e.tensor_mul(w2[:], w1[:], dd[:]), r=[K], w=[K])
        P.dve(lambda e: e.tensor_add(selb_f[:], sel1[:], sel2[:]), r=[K], w=[K])
        P.dve(lambda e: e.tensor_copy(selb[:], selb_f[:]), r=[K], w=["selb"])
        P.pe(lambda e: e.matmul(psb[3][:, 0:32], SUb[:], selb[:], start=True, stop=True), r=["SUb", "selb"], w=[("ps", 3)])
        P.pe(lambda e: e.matmul(psb[3][:, 32:64], onesb[:], selb[:], start=True, stop=True), r=["onesb", "selb"], w=[("ps", 3)])
        P.dve(lambda e: e.tensor_add(pos[:], psb[3][:, 0:32], carry[:]), r=[("ps", 3), "carry"], w=[K])
        P.dve(lambda e: e.tensor_add(carry[:], carry[:], psb[3][:, 32:64]), r=[("ps", 3), "carry"], w=["carry"])
        for k_, selk in enumerate((sel1, sel2)):
            P.dve(lambda e, selk=selk: e.tensor_mul(tmp32[:], selk[:], pos[:]), r=[K], w=[K])
            P.dve(lambda e, k_=k_: e.tensor_reduce(slotf[:, k_:k_ + 1], tmp32[:], AX.X, ALU.add), r=[K], w=[K])
            P.dve(lambda e, k_=k_: e.tensor_scalar(valid[:, k_:k_ + 1], slotf[:, k_:k_ + 1], float(CAP) - 0.5, None, ALU.is_lt), r=[K], w=[K])
            P.dve(lambda e, selk=selk: e.tensor_mul(tmp32[:], selk[:], eoff[:]), r=[K, "eoff"], w=[K])
            P.dve(lambda e: e.tensor_reduce(dd[:], tmp32[:], AX.X, ALU.add), r=[K], w=[K])
            P.dve(lambda e, k_=k_: e.tensor_add(slotf[:, k_:k_ + 1], slotf[:, k_:k_ + 1], dd[:]), r=[K], w=[K])
            P.dve(lambda e, k_=k_: e.tensor_sub(slotf[:, k_:k_ + 1], slotf[:, k_:k_ + 1], trash[:]), r=[K, "trash"], w=[K])
            P.dve(lambda e, k_=k_: e.tensor_mul(slotf[:, k_:k_ + 1], slotf[:, k_:k_ + 1], valid[:, k_:k_ + 1]), r=[K], w=[K])
            P.dve(lambda e, k_=k_: e.tensor_add(slotf[:, k_:k_ + 1], slotf[:, k_:k_ + 1], trash[:]), r=[K, "trash"], w=[K])
            wk = w1 if k_ == 0 else w2
            P.dve(lambda e, k_=k_, wk=wk, tt=tt: e.scalar_tensor_tensor(route_g[:, tt, k_:k_ + 1], wk[:], pg[:, 0:1], valid[:, k_:k_ + 1], ALU.mult, ALU.mult),
                  r=[K], w=[("route", tt)])
        P.dve(lambda e, tt=tt: e.tensor_copy(route_i[:, tt, :], slotf[:]), r=[K], w=[("route", tt)])
        for k_ in range(2):
            P.add("pool", lambda e, tt=tt, k_=k_, i2=i2: e.indirect_dma_start(
                out=xs_d[:, :], out_offset=bass.IndirectOffsetOnAxis(ap=route_i[:, tt, k_:k_ + 1], axis=0),
                in_=hbf[i2][:, :], in_offset=None), r=[khb, ("route", tt)], w=["xs_d"], sw=True)
    P.barrier()
    sb.reset(m2)
    stg = [sb.alloc([128, 8, 512], F32, "stg") for _ in range(3)]
    wg = [sb.alloc([128, 16, 512], BF16, "wg") for _ in range(2)]
    wu = [sb.alloc([128, 16, 512], BF16, "wu") for _ in range(2)]
    wd = [sb.alloc([128, 4, 2048], BF16, "wd") for _ in range(2)]
    xrow = [sb.alloc([128, 2048], BF16, "xrowb") for _ in range(2)]
    xsT = sb.alloc([128, 16, CAP], BF16, "xsT")
    HT = sb.alloc([128, 4, CAP], BF16, "HT")
    sgt = [sb.alloc([128, CAP], F32, "sgt") for _ in range(2)]
    yst = [sb.alloc([128, 2048], F32, "yst") for _ in range(2)]
    nst = 0
    ncast = 0
    nps = 0
    ntl = CAP // 128
    pbf = lambda i: psb[i][:].bitcast(BF16)

    def cast(dst, src, r, w):
        nonlocal ncast
        i = ncast % 3
        ncast += 1
        if i == 0:
            P.pool(lambda e: e.tensor_copy(dst, src), r=r, w=w)
        elif i == 1:
            P.act(lambda e: e.copy(dst, src), r=r, w=w)
        else:
            P.dve(lambda e: e.tensor_copy(dst, src), r=r, w=w)

    for ex in range(32):
        e2 = ex % 2
        kw = ("wexp", e2)
        for (dst, src4) in ((wg[e2], prm["w_gate"][ex].rearrange("(kc p) n -> p kc n", p=128)),
                            (wu[e2], prm["w_up"][ex].rearrange("(kc p) n -> p kc n", p=128))):
            for hf in range(2):
                s_, ks = stg[nst % 3], ("stg", nst % 3)
                nst += 1
                P.dma(s_[:], src4[:, hf * 8:(hf + 1) * 8, :], w=[ks])
                cast(dst[:, hf * 8:(hf + 1) * 8, :], s_[:], [ks], [kw])
        wdv = prm["w_down"][ex].rearrange("(fc p) n -> p fc n", p=128)
        for hf in range(2):
            s_, ks = stg[nst % 3], ("stg", nst % 3)
            nst += 1
            sv = s_[:].rearrange("p a b -> p (a b)").rearrange("p (f n) -> p f n", f=2)
            P.dma(sv, wdv[:, hf * 2:(hf + 1) * 2, :], w=[ks])
            cast(wd[e2][:, hf * 2:(hf + 1) * 2, :], sv, [ks], [kw])
        for tl in range(ntl):
            xr, kxr = xrow[tl % 2], ("xrowb", tl % 2)
            P.dma(xr[:], xs_d[ex * CAP + tl * 128: ex * CAP + (tl + 1) * 128, :], r=["xs_d"], w=[kxr])
            for g in range(2):
                pi = nps % 2
                nps += 1
                for j in range(8):
                    kc = g * 8 + j
                    P.pe(lambda e, pi=pi, xr=xr, kc=kc, j=j: e.transpose(pbf(pi)[:, j * 128:(j + 1) * 128], xr[:, kc * 128:(kc + 1) * 128], c["identb"][:]),
                         r=[kxr, "identb"], w=[("ps", pi)])
                evac(P, nps, xsT[:, g * 8:(g + 1) * 8, tl * 128:(tl + 1) * 128], pbf(pi).rearrange("p (j t) -> p j t", j=8), r=[("ps", pi)], w=["xsT"])
        for fc in range(4):
            pg_, pu_ = 2 + (fc % 2) * 2, 3 + (fc % 2) * 2
            for kc in range(16):
                P.pe(lambda e, kc=kc, fc=fc, pg_=pg_, e2=e2: e.matmul(psb[pg_][:, 0:CAP], wg[e2][:, kc, fc * 128:(fc + 1) * 128], xsT[:, kc, :],
                                                                   start=(kc == 0), stop=(kc == 15)), r=[kw, "xsT"], w=[("ps", pg_)])
            for kc in range(16):
                P.pe(lambda e, kc=kc, fc=fc, pu_=pu_, e2=e2: e.matmul(psb[pu_][:, 0:CAP], wu[e2][:, kc, fc * 128:(fc + 1) * 128], xsT[:, kc, :],
                                                                   start=(kc == 0), stop=(kc == 15)), r=[kw, "xsT"], w=[("ps", pu_)])
            sg2, ksg = sgt[fc % 2], ("sgt", fc % 2)
            P.act(lambda e, sg2=sg2, pg_=pg_: e.activation(sg2[:], psb[pg_][:, 0:CAP], AF.Silu), r=[("ps", pg_)], w=[ksg])
            P.dve(lambda e, sg2=sg2, pu_=pu_, fc=fc: e.tensor_tensor(HT[:, fc, :], sg2[:], psb[pu_][:, 0:CAP], ALU.mult), r=[ksg, ("ps", pu_)], w=["HT"])
        for tl in range(ntl):
            ys, kys = yst[tl % 2], ("yst", tl % 2)
            for db in range(4):
                pi = 6 + nps % 2
                nps += 1
                for fc in range(4):
                    P.pe(lambda e, pi=pi, fc=fc, tl=tl, db=db, e2=e2: e.matmul(psb[pi][:], HT[:, fc, tl * 128:(tl + 1) * 128], wd[e2][:, fc, db * 512:(db + 1) * 512],
                                                                            start=(fc == 0), stop=(fc == 3)), r=["HT", kw], w=[("ps", pi)])
                evac(P, nps, ys[:, db * 512:(db + 1) * 512], psb[pi][:], r=[("ps", pi)], w=[kys])
            P.dma(ys_d[ex * CAP + tl * 128: ex * CAP + (tl + 1) * 128, :], ys[:], r=[kys], w=["ys_d"], q="pool")
    P.barrier()
    sb.reset(m2)
    y1 = [sb.alloc([128, 2048], F32, "y1") for _ in range(2)]
    y2 = [sb.alloc([128, 2048], F32, "y2") for _ in range(2)]
    hr = [sb.alloc([128, 2048], F32, "hr") for _ in range(2)]
    pre = sb.alloc([128, 2048], F32, "pre")
    g_b = sb.alloc([128, 2048], F32, "g_b")
    b_b = sb.alloc([128, 2048], F32, "b_b")
    stats = sb.alloc([128, 4, 6], F32, "stats")
    mv = sb.alloc([128, 2], F32, "mv")
    rstd = sb.alloc([128, 1], F32, "rstd")
    bcast_load(cx, g_b[:], prm["ln_g"], "lnp")
    bcast_load(cx, b_b[:], prm["ln_b"], "lnp")
    for i2 in range(2):
        P.pool(lambda e, i2=i2: e.memset(y1[i2][:], 0.0), w=[("y1", i2)])
        P.pool(lambda e, i2=i2: e.memset(y2[i2][:], 0.0), w=[("y2", i2)])
    for tt in range(ntt):
        i2 = tt % 2
        r0 = tt * 128
        for k_, yy in enumerate((y1, y2)):
            ky = ("y1" if k_ == 0 else "y2", i2)
            P.add("pool", lambda e, tt=tt, k_=k_, yy=yy, i2=i2: e.indirect_dma_start(
                out=yy[i2][:, :], out_offset=None, in_=ys_d[:, :],
                in_offset=bass.IndirectOffsetOnAxis(ap=route_i[:, tt, k_:k_ + 1], axis=0)),
                r=["ys_d", ("route", tt)], w=[ky], sw=True)
        khr = ("hr", i2)
        P.dma(hr[i2][:], h1[r0:r0 + 128, :], w=[khr])
        P.dve(lambda e, i2=i2, tt=tt: e.tensor_scalar_mul(y1[i2][:], y1[i2][:], route_g[:, tt, 0:1]), r=[("y1", i2), ("route", tt)], w=[("y1", i2)])
        P.dve(lambda e, i2=i2, tt=tt: e.scalar_tensor_tensor(y1[i2][:], y2[i2][:], route_g[:, tt, 1:2], y1[i2][:], ALU.mult, ALU.add),
              r=[("y1", i2), ("y2", i2), ("route", tt)], w=[("y1", i2)])
        P.dve(lambda e, i2=i2: e.scalar_tensor_tensor(pre[:], hr[i2][:], ALPHA, y1[i2][:], ALU.mult, ALU.add), r=[khr, ("y1", i2)], w=["pre"])
        layer_norm_tile(cx, pre[:], hr[i2][:], g_b[:], b_b[:], stats, mv, rstd, "pre", khr, "lnst")
        P.dma(x_out[r0:r0 + 128, :], hr[i2][:], r=[khr], q="pool")
    P.barrier()
    sb.reset(m)


PARAM_SHAPES = {
    "w_in": [2048, N_IN], "fox_b_f": [8], "fox_out_g": [512], "mla_q_norm_g": [384], "mla_kv_norm_g": [128],
    "mla_w_uq": [384, 768], "mla_w_ukv": [128, 1024], "mla_out_g": [512], "rwkv_mu": [1792], "rwkv_w0": [512],
    "rwkv_w2": [64, 512], "rwkv_a0": [512], "rwkv_a2": [64, 512], "rwkv_g2": [128, 512], "rwkv_k_k": [512],
    "rwkv_k_a": [512], "rwkv_r_k": [512], "rwkv_ln_g": [512], "rwkv_ln_b": [512], "gdn_conv_w": [4, 1536],
    "gdn_a_log": [4], "gdn_dt_bias": [4], "gdn_norm_g": [128], "w_out": [2048, 2048], "ln1_g": [2048], "ln1_b": [2048],
    "moe_w_grp": [2048, 4], "moe_b_grp": [4], "moe_w_exp": [2048, 32], "moe_b_exp": [32], "moe_w_gate": [32, 2048, 512],
    "moe_w_up": [32, 2048, 512], "moe_w_down": [32, 512, 2048], "ln2_g": [2048], "ln2_b": [2048],
}


def build_program(S, NB, depth):
    nc = bass.Bass("TRN2", target_bir_lowering=False)
    cx = Ctx(nc, S, NB)
    T = cx.T
    x_in = cx.dt("x", [T, 2048], F32, "ExternalInput")
    pos = cx.dt("positions", [NB, S], I32, "ExternalInput")
    ifr = cx.dt("inv_freq", [32], F32, "ExternalInput")
    W = {k: cx.dt(k, [depth] + v, F32, "ExternalInput") for k, v in PARAM_SHAPES.items()}
    y = cx.dt("y", [T, 2048], F32, "ExternalOutput")
    u_tok = cx.dt("u_tok", [T, N_IN], F32)
    qkT = cx.dt("qkT", [1024, T], BF16)
    fT = cx.dt("fT", [8, T], F32)
    mixed = cx.dt("mixed", [T, 2048], F32)
    h1 = cx.dt("h1", [T, 2048], F32)
    xs_d = cx.dt("xs_d", [32 * MOE_CAP + 128, 2048], BF16)
    ys_d = cx.dt("ys_d", [32 * MOE_CAP + 128, 2048], F32)
    xbuf = [cx.dt("xres%d" % i, [T, 2048], F32) for i in range(2)]
    make_consts(cx)
    cur = x_in
    for l in range(depth):
        nxt = y if l == depth - 1 else xbuf[l % 2]
        stage_inproj(cx, cur, W["w_in"][l], u_tok, qkT, fT)
        stage_fox(cx, qkT, fT, u_tok, W["fox_b_f"][l], W["fox_out_g"][l], mixed)
        stage_mla(cx, u_tok, pos, ifr, W["mla_q_norm_g"][l], W["mla_kv_norm_g"][l], W["mla_w_uq"][l], W["mla_w_ukv"][l],
                  W["mla_out_g"][l], mixed)
        stage_rwkv(cx, u_tok, {k: W["rwkv_" + k][l] for k in ("mu", "w0", "w2", "a0", "a2", "g2", "k_k", "k_a", "r_k", "ln_g", "ln_b")}, mixed)
        stage_gdn(cx, u_tok, {k: W["gdn_" + k][l] for k in ("conv_w", "a_log", "dt_bias", "norm_g")}, mixed)
        stage_outproj_ln(cx, mixed, W["w_out"][l], cur, W["ln1_g"][l], W["ln1_b"][l], h1)
        prm = {"w_grp": W["moe_w_grp"][l], "b_grp": W["moe_b_grp"][l], "w_exp": W["moe_w_exp"][l], "b_exp": W["moe_b_exp"][l],
               "w_gate": W["moe_w_gate"][l], "w_up": W["moe_w_up"][l], "w_down": W["moe_w_down"][l], "ln_g": W["ln2_g"][l], "ln_b": W["ln2_b"][l]}
        stage_moe(cx, h1, prm, nxt, xs_d, ys_d)
        cur = nxt
    cx.P.emit()
    return nc, cx


def kernel(**inputs):
    n_cores = 8
    x = np.ascontiguousarray(np.asarray(inputs["x"], dtype=np.float32))
    B, S, Dm = x.shape
    NB = B // n_cores
    depth = int(np.asarray(inputs["w_in"]).shape[0])
    nc, _ = build_program(S, NB, depth)
    positions = np.ascontiguousarray(np.asarray(inputs["positions"], dtype=np.int32))
    inv_freq = (np.float32(10000.0) ** (-np.arange(32, dtype=np.float32) / np.float32(32))).astype(np.float32)
    shared = {k: np.ascontiguousarray(np.asarray(inputs[k], dtype=np.float32)) for k in PARAM_SHAPES}
    in_maps = []
    for c in range(n_cores):
        d = dict(shared)
        d["x"] = x[c * NB:(c + 1) * NB].reshape(NB * S, Dm)
        d["positions"] = positions[c * NB:(c + 1) * NB]
        d["inv_freq"] = inv_freq
        in_maps.append(d)
    res = run_bass_kernel_spmd(nc, in_maps, core_ids=list(range(n_cores)))
    out = np.concatenate([np.asarray(r["y"], dtype=np.float32).reshape(NB, S, Dm) for r in res.results], axis=0)
    return out
```

```python
import numpy as np
import concourse.bass as bass
import concourse.mybir as mybir
from concourse.bass_utils import run_bass_kernel_spmd

F32 = mybir.dt.float32
BF16 = mybir.dt.bfloat16
I32 = mybir.dt.int32
U32 = mybir.dt.uint32
AF = mybir.ActivationFunctionType
ALU = mybir.AluOpType
AX = mybir.AxisListType

D = 2048
GW = 512
N_IN = 5968
C_FOX, C_MLA, C_RWKV, C_GDN = 0, 1544, 2120, 3912
ALPHA = (2 * 4) ** 0.25
LN_EPS = 1e-5
RMS_EPS = 1e-6
NEG = -30000.0


class _Op:
    __slots__ = ("stream", "fn", "deps", "sig", "ticket", "dma", "dma_idx", "idx", "sw")


class Prog:
    STREAMS = ("pe", "act", "dve", "pool", "sp")
    NDMA = 48

    def __init__(self, nc):
        self.nc = nc
        self.ops = []
        self.by_stream = {s: [] for s in self.STREAMS}
        self.last_w = {}
        self.readers = {}
        self.n_dma = 0
        self.dma_ops = []
        self.barrier_deps = {s: [] for s in self.STREAMS}

    POOLQ = "pool"
    NSW = 40

    def add(self, stream, fn, r=(), w=(), dma=False, sw=False):
        op = _Op()
        op.stream, op.fn, op.dma, op.sig, op.ticket = stream, fn, dma, False, 0
        op.sw = sw
        op.idx = len(self.ops)
        pk = [k for k in r if isinstance(k, tuple) and k and k[0] == "ps"]
        if pk:
            r = [k for k in r if not (isinstance(k, tuple) and k and k[0] == "ps")]
            w = list(w) + pk
        deps = set()
        for k in r:
            d = self.last_w.get(k)
            if d is not None:
                deps.add(d)
        for k in w:
            d = self.last_w.get(k)
            if d is not None:
                deps.add(d)
            for rd in self.readers.get(k, ()):
                deps.add(rd)
        for k in r:
            self.readers.setdefault(k, []).append(op.idx)
        for k in w:
            self.last_w[k] = op.idx
            self.readers[k] = []
        bd = self.barrier_deps[stream]
        if bd:
            deps.update(bd)
            self.barrier_deps[stream] = []
        if dma:
            op.dma_idx = self.n_dma
            self.n_dma += 1
            if op.dma_idx >= self.NDMA:
                deps.add(self.dma_ops[op.dma_idx - self.NDMA])
            self.dma_ops.append(op.idx)
        deps.discard(op.idx)
        fin = []
        for d in deps:
            o = self.ops[d]
            if o.stream == stream and not o.dma and stream == "pe":
                continue
            fin.append(d)
            if not o.dma:
                o.sig = True
        op.deps = fin
        self.ops.append(op)
        self.by_stream[stream].append(op)
        return op

    def barrier(self):
        deps = []
        for s in self.STREAMS:
            for o in reversed(self.by_stream[s]):
                if not o.dma:
                    deps.append(o.idx)
                    break
        deps.extend(self.dma_ops[-self.NDMA:])
        for s in self.STREAMS:
            self.barrier_deps[s] = list(deps)
        self.last_w = {}
        self.readers = {}

    def pe(self, fn, r=(), w=()):
        return self.add("pe", fn, r, w)

    def act(self, fn, r=(), w=()):
        return self.add("act", fn, r, w)

    def dve(self, fn, r=(), w=()):
        return self.add("dve", fn, r, w)

    def pool(self, fn, r=(), w=()):
        return self.add("pool", fn, r, w)

    def dma(self, out, in_, r=(), w=(), q="sp", slow=False):
        if q == "pool":
            q = self.POOLQ
        if slow:
            return self.add(q, lambda e: e.dma_start(out=out, in_=in_, allow_slow_non_contiguous=True), r, w, dma=True)
        return self.add(q, lambda e: e.dma_start(out=out, in_=in_), r, w, dma=True)

    def emit(self, final_wait=True):
        nc = self.nc
        for s in self.STREAMS:
            t = 0
            for o in self.by_stream[s]:
                if not o.dma and o.sig:
                    t += 1
                    o.ticket = t
        import contextlib
        with contextlib.ExitStack() as es:
            esem = {s: es.enter_context(nc.semaphore("e_" + s)) for s in self.STREAMS}
            dsem = [es.enter_context(nc.semaphore("d%d" % i)) for i in range(self.NDMA)]
            nsw = sum(1 for o in self.ops if o.sw)
            SWBASE = 215
            swsems = [es.enter_context(nc.semaphore("sw%d" % i, num=SWBASE + i)) for i in range(min(nsw, self.NSW))]
            swctr = [0, 1]
            block = es.enter_context(nc.Block())
            ops = self.ops
            NDMA = self.NDMA

            def completion(o):
                if o.dma:
                    return ("d", o.dma_idx % NDMA), dsem[o.dma_idx % NDMA], 16 * (o.dma_idx // NDMA + 1)
                return ("e", o.stream), esem[o.stream], o.ticket

            def run_stream(s, eng):
                waited = {}
                pending = []

                def flush():
                    for (sem_, val_, o_) in pending:
                        eng.wait_ge(sem_, val_)
                        if o_.sig:
                            eng.nop().then_inc(esem[s], 1)
                    del pending[:]

                for o in self.by_stream[s]:
                    need = {}
                    for d in o.deps:
                        key, sem, val = completion(ops[d])
                        if waited.get(key, 0) >= val:
                            continue
                        if key not in need or need[key][1] < val:
                            need[key] = (sem, val)
                    if pending and ((not o.sw) or need or len(pending) >= 2):
                        flush()
                    for key, (sem, val) in need.items():
                        eng.wait_ge(sem, val)
                        waited[key] = val
                    if o.sw and swctr[0] == len(swsems):
                        flush()
                        eng.dma_reset(range(SWBASE, SWBASE + len(swsems)))
                        swctr[0] = 0
                        swctr[1] += 1
                    inst = o.fn(eng)
                    if o.sw:
                        sw_ = swsems[swctr[0]]
                        swctr[0] += 1
                        inst.then_inc(sw_, 16)
                        pending.append((sw_, 16 * swctr[1], o))
                    elif o.dma:
                        inst.then_inc(dsem[o.dma_idx % NDMA], 16)
                    elif o.sig:
                        inst.then_inc(esem[s], 1)
                flush()
                if s == "sp" and final_wait:
                    for i in range(min(NDMA, self.n_dma)):
                        last = i + ((self.n_dma - 1 - i) // NDMA) * NDMA
                        val = 16 * (last // NDMA + 1)
                        if waited.get(("d", i), 0) < val:
                            eng.wait_ge(dsem[i], val)

            @block.tensor
            def _(e):
                run_stream("pe", e)

            @block.scalar
            def _(e):
                run_stream("act", e)

            @block.vector
            def _(e):
                run_stream("dve", e)

            @block.gpsimd
            def _(e):
                run_stream("pool", e)

            @block.sync
            def _(e):
                run_stream("sp", e)


class SB:
    def __init__(self, nc, base=16512, cap=229344):
        self.nc, self.base, self.cap, self.top = nc, base, cap, base
        self.n = 0

    def mark(self):
        return self.top

    def reset(self, m):
        self.top = m

    def alloc(self, shape, dtype, name="t"):
        esz = {F32: 4, BF16: 2, I32: 4, U32: 4}[dtype]
        per = esz
        for s in shape[1:]:
            per *= s
        per = (per + 31) // 32 * 32
        off = self.top
        assert off + per <= self.cap, "SBUF overflow %s %d+%d" % (name, off, per)
        self.top += per
        self.n += 1
        return self.nc.alloc_sbuf_tensor_at("%s_%d" % (name, self.n), list(shape), dtype, offset=off)


class Ctx:
    def __init__(self, nc, S, NB, io=None):
        self.nc = nc
        self.S = S
        self.NB = NB
        self.T = S * NB
        self.P = Prog(nc)
        self.sb = SB(nc)
        self.io = io or {}
        self.dram = {}
        self.uid = 0

    def dt(self, name, shape, dtype, kind=None):
        k = self.io.get(name, kind or "Internal")
        t = self.nc.dram_tensor(name, list(shape), dtype, kind=k).ap()
        self.dram[name] = t
        return t

    def key(self, base):
        self.uid += 1
        return (base, self.uid)


def make_consts(cx):
    nc, P, sb = cx.nc, cx.P, cx.sb
    c = {}
    ident = sb.alloc([128, 128], F32, "ident")
    P.pool(lambda e: e.memset(ident[:], 0.0), w=["ident"])
    P.pool(lambda e: e.affine_select(out=ident[:], in_=ident[:], pattern=[[-1, 128]],
                                     compare_op=ALU.not_equal, fill=1.0, base=0, channel_multiplier=1),
           r=["ident"], w=["ident"])
    identb = sb.alloc([128, 128], BF16, "identb")
    P.dve(lambda e: e.tensor_copy(identb[:], ident[:]), r=["ident"], w=["identb"])
    c["ident"], c["identb"] = ident, identb
    cx.ps = [nc.alloc_psum_tensor("psb%d" % i, [128, 512], F32) for i in range(8)]
    cx.c = c
    dplr_consts(cx)
    SU = sb.alloc([128, 128], F32, "SU")
    SUb = sb.alloc([128, 128], BF16, "SUb")
    P.pool(lambda e: e.memset(SU[:], 1.0), w=["SU"])
    P.pool(lambda e: e.affine_select(out=SU[:], in_=SU[:], pattern=[[1, 128]], compare_op=ALU.is_ge, fill=0.0, base=-1, channel_multiplier=-1),
           r=["SU"], w=["SU"])
    P.pool(lambda e: e.tensor_copy(SUb[:], SU[:]), r=["SU"], w=["SUb"])
    eoff = sb.alloc([128, 32], F32, "eoff")
    P.pool(lambda e: e.iota(eoff[:], pattern=[[MOE_CAP, 32]], base=0, channel_multiplier=0, allow_small_or_imprecise_dtypes=True), w=["eoff"])
    trash = sb.alloc([128, 1], F32, "trash")
    P.pool(lambda e: e.iota(trash[:], pattern=[[0, 1]], base=32 * MOE_CAP, channel_multiplier=1, allow_small_or_imprecise_dtypes=True), w=["trash"])
    c.update(SUb=SUb, eoff=eoff, trash=trash)
    c["masks_causal"] = build_masks(cx, "causal")
    c["masks_chunk64"] = build_masks(cx, "chunk64")
    P.barrier()
    return c


def evac(P, i, out, in_, r, w):
    if i % 2 == 0:
        P.act(lambda e: e.copy(out, in_), r, w)
    else:
        P.dve(lambda e: e.tensor_copy(out, in_), r, w)


def build_xT(cx, src_dram, b, xT, ps_banks, xrow_bufs, tag):
    P, c = cx.P, cx.c
    S = cx.S
    nt = S // 128
    for tt in range(nt):
        xr = xrow_bufs[tt % len(xrow_bufs)]
        kx = ("xrow", tag, tt % len(xrow_bufs))
        r0 = b * S + tt * 128
        P.dma(xr[:], src_dram[r0:r0 + 128, :], w=[kx])
        for g in range(4):
            ps = ps_banks[(tt * 4 + g) % len(ps_banks)]
            kp = ("psT", tag, (tt * 4 + g) % len(ps_banks))
            for j in range(4):
                kc = g * 4 + j
                P.pe(lambda e, ps=ps, xr=xr, kc=kc, j=j: e.transpose(ps[:, j * 128:(j + 1) * 128],
                                                                   xr[:, kc * 128:(kc + 1) * 128], c["ident"][:]),
                     r=[kx, "ident"], w=[kp])
            o = xT[:, g * 4:(g + 1) * 4, tt * 128:(tt + 1) * 128]
            i_ = ps[:].rearrange("p (j t) -> p j t", j=4)
            evac(P, tt * 4 + g, o, i_, r=[kp], w=[("xT", tag, tt)])


def stage_inproj(cx, x_dram, w_in, u_tok, qkT, fT):
    nc, P, sb, c = cx.nc, cx.P, cx.sb, cx.c
    S, NB = cx.S, cx.NB
    nt = S // 128
    m = sb.mark()
    xT = sb.alloc([128, 16, S], BF16, "xT")
    xrows = [sb.alloc([128, 2048], F32, "xrow") for _ in range(2)]
    wst = [sb.alloc([128, 16, 512], F32, "wst") for _ in range(2)]
    wbf = [sb.alloc([128, 16, 512], BF16, "wbf") for _ in range(2)]
    ost = [sb.alloc([128, 512], F32, "ost") for _ in range(4)]
    obf = [sb.alloc([128, 512], BF16, "obf") for _ in range(2)]
    psb = cx.ps
    wv = w_in.rearrange("(kc p) n -> p kc n", p=128)
    blocks = [(c0, min(512, N_IN - c0)) for c0 in range(0, N_IN, 512)]
    nev = 0
    for b in range(NB):
        build_xT(cx, x_dram, b, xT, psb[0:4], xrows, "A%d" % b)
        xkeys = [("xT", "A%d" % b, tt) for tt in range(nt)]
        for bi, (c0, cw) in enumerate(blocks):
            it = b * len(blocks) + bi
            ws, wb = wst[it % 2], wbf[it % 2]
            kws, kwb = ("wst", it % 2), ("wbf", it % 2)
            P.dma(ws[:, :, 0:cw], wv[:, :, c0:c0 + cw], w=[kws])
            if it % 2 == 0:
                P.dve(lambda e, wb=wb, ws=ws, cw=cw: e.tensor_copy(wb[:, :, 0:cw], ws[:, :, 0:cw]), r=[kws], w=[kwb])
            else:
                P.act(lambda e, wb=wb, ws=ws, cw=cw: e.copy(wb[:, :, 0:cw], ws[:, :, 0:cw]), r=[kws], w=[kwb])
            for tt in range(nt):
                if c0 in (0, 512):
                    break
                pi = 4 + (nev % 4)
                ps, kp = psb[pi], ("psA", pi)
                for kc in range(16):
                    P.pe(lambda e, ps=ps, kc=kc, tt=tt, wb=wb, cw=cw: e.matmul(
                        ps[:, 0:cw], xT[:, kc, tt * 128:(tt + 1) * 128], wb[:, kc, 0:cw],
                        start=(kc == 0), stop=(kc == 15)), r=[xkeys[tt], kwb], w=[kp])
                o, ko = ost[nev % 4], ("ost", nev % 4)
                evac(P, nev, o[:, 0:cw], ps[:, 0:cw], r=[kp], w=[ko])
                r0 = b * S + tt * 128
                P.dma(u_tok[r0:r0 + 128, c0:c0 + cw], o[:, 0:cw], r=[ko], q="pool")
                nev += 1
            fm = []
            if c0 in (0, 512):
                fm = [(c0 + j * 128, 128) for j in range(4)]
            elif c0 == 1536:
                fm = [(1536, 8)]
            for (f0, fw) in fm:
                for tb in range(S // 512):
                    pi = 4 + (nev % 4)
                    ps, kp = psb[pi], ("psA", pi)
                    for kc in range(16):
                        P.pe(lambda e, ps=ps, kc=kc, tb=tb, wb=wb, f0=f0, fw=fw, c0=c0: e.matmul(
                            ps[0:fw, :], wb[:, kc, f0 - c0:f0 - c0 + fw], xT[:, kc, tb * 512:(tb + 1) * 512],
                            start=(kc == 0), stop=(kc == 15)), r=xkeys[tb * 4:tb * 4 + 4] + [kwb], w=[kp])
                    t0 = b * S + tb * 512
                    if fw == 128:
                        o, ko = obf[nev % 2], ("obf", nev % 2)
                        evac(P, nev, o[:], ps[:], r=[kp], w=[ko])
                        P.dma(qkT[f0:f0 + 128, t0:t0 + 512], o[:], r=[ko], q="pool")
                    else:
                        o, ko = ost[nev % 4], ("ost", nev % 4)
                        evac(P, nev, o[0:8, :], ps[0:8, :], r=[kp], w=[ko])
                        P.dma(fT[0:8, t0:t0 + 512], o[0:8, :], r=[ko], q="pool")
                    nev += 1
    P.barrier()
    sb.reset(m)


def bcast_load(cx, dst, src_row, key, q="sp"):
    cx.P.dma(dst, src_row.partition_broadcast(128), w=[key], q=q)


def rstd_from_ssq(P, out, ssq, n, eps, r, w):
    P.dve(lambda e: e.tensor_scalar(out, ssq, 1.0 / n, eps, ALU.mult, ALU.add), r=r, w=w)
    P.act(lambda e: e.activation(out, out, AF.Sqrt), r=w, w=w)
    P.dve(lambda e: e.reciprocal(out, out), r=w, w=w)


def layer_norm_tile(cx, pre, y, g_b, b_b, stats, mv, rstd, kpre, ky, kst, aff="pool"):
    P = cx.P
    for j in range(4):
        P.dve(lambda e, j=j: e.bn_stats(stats[:, j, :], pre[:, j * 512:(j + 1) * 512]), r=[kpre], w=[kst])
    P.dve(lambda e: e.bn_aggr(mv[:], stats[:].rearrange("p a b -> p (a b)")), r=[kst], w=[kst])
    P.dve(lambda e: e.tensor_scalar_add(rstd[:], mv[:, 1:2], LN_EPS), r=[kst], w=[kst])
    P.act(lambda e: e.activation(rstd[:], rstd[:], AF.Sqrt), r=[kst], w=[kst])
    P.dve(lambda e: e.reciprocal(rstd[:], rstd[:]), r=[kst], w=[kst])
    P.dve(lambda e: e.tensor_scalar(y, pre, mv[:, 0:1], rstd[:], ALU.subtract, ALU.mult), r=[kst, kpre], w=[ky])
    eng_ = P.pool if aff == "pool" else P.dve
    eng_(lambda e: e.tensor_mul(y, y, g_b), r=[ky, "lnp"], w=[ky])
    eng_(lambda e: e.tensor_add(y, y, b_b), r=[ky, "lnp"], w=[ky])


def stage_outproj_ln(cx, mixed, w_out, x_res, ln_g, ln_b, h_out):
    nc, P, sb, c = cx.nc, cx.P, cx.sb, cx.c
    T = cx.T
    m = sb.mark()
    wbf = sb.alloc([128, 16, 2048], BF16, "woutbf")
    wst = [sb.alloc([128, 16, 256], F32, "wst") for _ in range(2)]
    mrow = [sb.alloc([128, 2048], F32, "mrow") for _ in range(2)]
    xrow = [sb.alloc([128, 2048], F32, "xrow") for _ in range(2)]
    mT = [sb.alloc([128, 16, 128], BF16, "mT") for _ in range(2)]
    pre = [sb.alloc([128, 2048], F32, "pre") for _ in range(2)]
    g_b = sb.alloc([128, 2048], F32, "g_b")
    b_b = sb.alloc([128, 2048], F32, "b_b")
    stats = sb.alloc([128, 4, 6], F32, "stats")
    mv = sb.alloc([128, 2], F32, "mv")
    rstd = sb.alloc([128, 1], F32, "rstd")
    psb = cx.ps
    bcast_load(cx, g_b[:], ln_g, "lnp")
    bcast_load(cx, b_b[:], ln_b, "lnp")
    wv = w_out.rearrange("(kc p) n -> p kc n", p=128)
    for j in range(8):
        ws, kws = wst[j % 2], ("wst", j % 2)
        P.dma(ws[:], wv[:, :, j * 256:(j + 1) * 256], w=[kws])
        if j % 2 == 0:
            P.dve(lambda e, ws=ws, j=j: e.tensor_copy(wbf[:, :, j * 256:(j + 1) * 256], ws[:]), r=[kws], w=["wout"])
        else:
            P.act(lambda e, ws=ws, j=j: e.copy(wbf[:, :, j * 256:(j + 1) * 256], ws[:]), r=[kws], w=["wout"])
    nev = 0
    for tt in range(T // 128):
        i2 = tt % 2
        r0 = tt * 128
        km, kx, kmT, kpre = ("mrow", i2), ("xrow", i2), ("mT", i2), ("pre", i2)
        P.dma(mrow[i2][:], mixed[r0:r0 + 128, :], w=[km])
        P.dma(xrow[i2][:], x_res[r0:r0 + 128, :], w=[kx])
        for g in range(4):
            pi = nev % 4
            ps, kp = psb[pi], ("ps", pi)
            for j in range(4):
                kc = g * 4 + j
                P.pe(lambda e, ps=ps, kc=kc, j=j, i2=i2: e.transpose(ps[:, j * 128:(j + 1) * 128],
                                                                 mrow[i2][:, kc * 128:(kc + 1) * 128], c["ident"][:]),
                     r=[km, "ident"], w=[kp])
            evac(P, nev, mT[i2][:, g * 4:(g + 1) * 4, :], ps[:].rearrange("p (j t) -> p j t", j=4), r=[kp], w=[kmT])
            nev += 1
        for cb in range(4):
            pi = 4 + nev % 4
            ps, kp = psb[pi], ("ps", pi)
            for kc in range(16):
                P.pe(lambda e, ps=ps, kc=kc, cb=cb, i2=i2: e.matmul(ps[:], mT[i2][:, kc, :], wbf[:, kc, cb * 512:(cb + 1) * 512],
                                                                  start=(kc == 0), stop=(kc == 15)),
                     r=[kmT, "wout"], w=[kp])
            P.dve(lambda e, ps=ps, cb=cb, i2=i2: e.scalar_tensor_tensor(
                pre[i2][:, cb * 512:(cb + 1) * 512], xrow[i2][:, cb * 512:(cb + 1) * 512], ALPHA, ps[:], ALU.mult, ALU.add),
                r=[kp, kx], w=[kpre])
            nev += 1
        layer_norm_tile(cx, pre[i2][:], xrow[i2][:], g_b[:], b_b[:], stats, mv, rstd, kpre, kx, "lnst")
        P.dma(h_out[r0:r0 + 128, :], xrow[i2][:], r=[kx], q="pool")
    P.barrier()
    sb.reset(m)


def build_masks(cx, kind):
    P, sb = cx.P, cx.sb
    if "maskf" not in cx.c:
        cx.c["maskf"] = sb.alloc([128, 512], F32, "maskf")
    mf = cx.c["maskf"]
    out = []
    for j in range(4):
        mb = sb.alloc([128, 512], BF16, "maskb")
        k = ("mask", kind, j)
        P.pool(lambda e: e.memset(mf[:], 0.0), w=["maskf"])
        if kind == "causal":
            P.pool(lambda e, j=j: e.affine_select(out=mf[:], in_=mf[:], pattern=[[1, 512]], compare_op=ALU.is_ge,
                                                 fill=NEG, base=-128 * j, channel_multiplier=-1), r=["maskf"], w=["maskf"])
        else:
            for hf in range(2):
                P.pool(lambda e, j=j, hf=hf: e.affine_select(
                    out=mf[hf * 64:(hf + 1) * 64, :], in_=mf[hf * 64:(hf + 1) * 64, :], pattern=[[1, 512]],
                    compare_op=ALU.is_ge, fill=NEG, base=-128 * j - 64 * hf, channel_multiplier=0),
                    r=["maskf"], w=["maskf"])
        P.pool(lambda e, mb=mb: e.tensor_copy(mb[:], mf[:]), r=["maskf"], w=[k])
        out.append((mb, k))
    return out


def attn_bufs(cx, nh, dv):
    sb, S = cx.sb, cx.S
    nring = 2 * (S // 128)
    return dict(PT=[sb.alloc([128, 512], BF16, "PT") for _ in range(nring)],
                o_t=[sb.alloc([128, 4, nh * dv], F32, "o_t") for _ in range(2)],
                rec=[sb.alloc([128, 4], F32, "rec") for _ in range(2)])


def attn_core(cx, nh, dv, scale, score_ops, bias_ap, masks, v_ap, out_cb, tag, sbanks=(0, 1, 2), bufs=None):
    P, sb, c = cx.P, cx.sb, cx.c
    S = cx.S
    psb = cx.ps
    nring = 2 * (S // 128)
    PT, o_t, rec = bufs["PT"], bufs["o_t"], bufs["rec"]
    tag = "att"
    st = {"npt": 0, "nsc": 0}
    units = [(qb, h) for qb in range(S // 512) for h in range(nh)]

    def scores(qb, h):
        pts = []
        for kt in range(4 * qb + 4):
            j = kt - 4 * qb
            bi_ = sbanks[st["nsc"] % len(sbanks)]
            pss, kpss = psb[bi_], ("ps", bi_)
            st["nsc"] += 1
            ops = list(score_ops(h, kt, qb))
            if j >= 0:
                mb, km = masks[j]
                ops.append((c["identb"][:], mb[:], [km, "identb"]))
            for i, (lh, rh, ks) in enumerate(ops):
                P.pe(lambda e, pss=pss, lh=lh, rh=rh, i=i, n=len(ops): e.matmul(pss[:], lh, rh, start=(i == 0), stop=(i == n - 1)),
                     r=ks, w=[kpss])
            pt, kpt = PT[st["npt"] % nring], ("PT", tag, st["npt"] % nring)
            st["npt"] += 1
            if bias_ap is not None:
                bap, kb = bias_ap(h, kt)
                P.act(lambda e, pt=pt, pss=pss, bap=bap: e.activation(pt[:], pss[:], AF.Exp, bias=bap, scale=scale),
                      r=[kpss] + kb, w=[kpt])
            else:
                P.act(lambda e, pt=pt, pss=pss: e.activation(pt[:], pss[:], AF.Exp, scale=scale), r=[kpss], w=[kpt])
            pts.append((pt, kpt))
        return pts

    def pv(ui, qb, h, pts):
        ot, kot = o_t[qb % 2], ("o_t", tag, qb % 2)
        if dv + 1 <= 128:
            pso, kpso = psb[4 + ui % 2], [("ps", 4 + ui % 2)]
            acc = [pso[:, tb * 128: tb * 128 + dv + 1] for tb in range(4)]
        else:
            p0, p1 = psb[4 + 2 * (ui % 2)], psb[5 + 2 * (ui % 2)]
            kpso = [("ps", 4 + 2 * (ui % 2)), ("ps", 5 + 2 * (ui % 2))]
            acc = [p0[:, 0:dv + 1], p0[:, 256:256 + dv + 1], p1[:, 0:dv + 1], p1[:, 256:256 + dv + 1]]
        for tb in range(4):
            last = 4 * qb + tb
            for kt in range(last + 1):
                pt, kpt = pts[kt]
                va, kv = v_ap(h, kt)
                P.pe(lambda e, a=acc[tb], pt=pt, tb=tb, va=va, kt=kt, last=last: e.matmul(
                    a, pt[:, tb * 128:(tb + 1) * 128], va, start=(kt == 0), stop=(kt == last)),
                    r=[kpt] + kv, w=kpso)
        rc, krc = rec[ui % 2], ("rec", tag, ui % 2)
        for tb in range(4):
            P.dve(lambda e, rc=rc, a=acc[tb], tb=tb: e.reciprocal(rc[:, tb:tb + 1], a[:, dv:dv + 1]), r=kpso, w=[krc])
        for tb in range(4):
            if tb % 2 == 0:
                P.dve(lambda e, rc=rc, a=acc[tb], tb=tb, h=h, ot=ot: e.tensor_scalar_mul(
                    ot[:, tb, h * dv:(h + 1) * dv], a[:, 0:dv], rc[:, tb:tb + 1]), r=kpso + [krc], w=[kot])
            else:
                P.act(lambda e, rc=rc, a=acc[tb], tb=tb, h=h, ot=ot: e.mul(
                    ot[:, tb, h * dv:(h + 1) * dv], a[:, 0:dv], rc[:, tb:tb + 1]), r=kpso + [krc], w=[kot])
        if h == nh - 1:
            out_cb(qb, ot, kot)

    prev = None
    for ui, (qb, h) in enumerate(units):
        pts = scores(qb, h)
        if prev is not None:
            pv(*prev)
        prev = (ui, qb, h, pts)
    pv(*prev)


def rms_out_cb(cx, col0, g_b, kg, mixed, tag):
    P, sb = cx.P, cx.sb
    junk = sb.alloc([128, 512], F32, "junk")
    ssq = sb.alloc([128, 4], F32, "ssq")
    ybuf = [sb.alloc([128, 4, 512], F32, "ybuf") for _ in range(2)]
    S = cx.S

    def cb(qb, ot, kot, b):
        ks = ("ssq", tag)
        yb, ky = ybuf[qb % 2], ("ybuf", tag, qb % 2)
        for tb in range(4):
            P.act(lambda e, tb=tb: e.activation(junk[:], ot[:, tb, :], AF.Square, accum_out=ssq[:, tb:tb + 1]),
                  r=[kot], w=[ks, ("junk", tag)])
        rstd_from_ssq(P, ssq[:], ssq[:], 512, RMS_EPS, [ks], [ks])
        for tb in range(4):
            P.dve(lambda e, tb=tb, yb=yb: e.scalar_tensor_tensor(yb[:, tb, :], ot[:, tb, :], ssq[:, tb:tb + 1], g_b,
                                                               ALU.mult, ALU.mult), r=[kot, ks, kg], w=[ky])
        r0 = b * S + qb * 512
        P.dma(mixed[r0:r0 + 512, col0:col0 + 512].rearrange("(tb p) n -> p tb n", p=128), yb[:], r=[ky], q="pool")
    return lambda b: (lambda qb, ot, kot: cb(qb, ot, kot, b))


def stage_fox(cx, qkT, fT, u_tok, b_f, out_g, mixed):
    nc, P, sb, c = cx.nc, cx.P, cx.sb, cx.c
    S, NB = cx.S, cx.NB
    nt = S // 128
    m = sb.mark()
    masks = c["masks_causal"]
    qT = sb.alloc([128, 4, S], BF16, "qT")
    kT = sb.alloc([128, 4, S], BF16, "kT")
    vaug = sb.alloc([128, nt, 8, 65], BF16, "vaug")
    vst = [sb.alloc([128, 512], F32, "vst") for _ in range(2)]
    fx = sb.alloc([8, S], F32, "fx")
    fa = sb.alloc([8, S], F32, "fa")
    Fc = sb.alloc([8, S], F32, "Fc")
    ones8 = sb.alloc([8, S], F32, "ones8")
    F8 = sb.alloc([8, S], BF16, "F8")
    bfc = sb.alloc([8, 1], F32, "bfc")
    sel = sb.alloc([8, 8, 128], BF16, "sel")
    nF = sb.alloc([128, nt, 8], F32, "nF")
    g_b = sb.alloc([128, 512], F32, "g_b")
    bcast_load(cx, g_b[:], out_g, "fox_g")
    P.dma(bfc[:], b_f.rearrange("(h o) -> h o", o=1), w=["bfc"])
    P.pool(lambda e: e.memset(ones8[:], 1.0), w=["ones8"])
    P.pool(lambda e: e.memset(vaug[:, :, :, 64:65], 1.0), w=["vones"])
    P.dve(lambda e: e.tensor_copy(sel[:], c["ident"][0:8, 0:8].unsqueeze(2).to_broadcast([8, 8, 128])), r=["ident"], w=["sel"])
    ocb = rms_out_cb(cx, 0, g_b[:], "fox_g", mixed, "fox")
    abufs = attn_bufs(cx, 8, 64)
    for b in range(NB):
        t0 = b * S
        tag = "fox%d" % b
        P.dma(qT[:], qkT[0:512, t0:t0 + S].rearrange("(hp p) t -> p hp t", p=128), w=["qT"])
        P.dma(kT[:], qkT[512:1024, t0:t0 + S].rearrange("(hp p) t -> p hp t", p=128), w=["kT"])
        P.dma(fx[:], fT[0:8, t0:t0 + S], w=["fx"])
        for tt in range(nt):
            vs, kvs = vst[tt % 2], ("vst", tt % 2)
            P.dma(vs[:], u_tok[t0 + tt * 128:t0 + (tt + 1) * 128, 1024:1536], w=[kvs])
            P.pool(lambda e, vs=vs, tt=tt: e.tensor_copy(vaug[:, tt, :, 0:64], vs[:].rearrange("p (h d) -> p h d", h=8)),
                   r=[kvs], w=[("v", tt)])
        P.dve(lambda e: e.tensor_scalar_add(fx[:], fx[:], bfc[:, 0:1]), r=["fx", "bfc"], w=["fx"])
        P.act(lambda e: e.activation(fa[:], fx[:], AF.Abs), r=["fx"], w=["fa"])
        P.act(lambda e: e.activation(fa[:], fa[:], AF.Exp, scale=-1.0), r=["fa"], w=["fa"])
        P.act(lambda e: e.activation(fa[:], fa[:], AF.Ln, bias=1.0), r=["fa"], w=["fa"])
        P.dve(lambda e: e.tensor_scalar_min(fx[:], fx[:], 0.0), r=["fx"], w=["fx"])
        P.dve(lambda e: e.tensor_sub(fx[:], fx[:], fa[:]), r=["fx", "fa"], w=["fx"])
        P.dve(lambda e: e.tensor_tensor_scan(Fc[:], ones8[:], fx[:], 0.0, ALU.mult, ALU.add), r=["fx", "ones8"], w=["Fc"])
        P.act(lambda e: e.mul(F8[:], Fc[:], 8.0), r=["Fc"], w=["F8"])
        for tt in range(nt):
            ps, kp = cx.ps[7], ("ps", 7)
            P.pe(lambda e, tt=tt, ps=ps: e.transpose(ps[:, 0:8], Fc[0:8, tt * 128:(tt + 1) * 128], c["ident"][0:8, 0:8]),
                 r=["Fc", "ident"], w=[kp])
            P.act(lambda e, tt=tt, ps=ps: e.mul(nF[:, tt, :], ps[:, 0:8], -1.0), r=[kp], w=["nF"])

        def score_ops(h, kt, qb):
            hp, base = h // 2, (h % 2) * 64
            return [(kT[base:base + 64, hp, kt * 128:(kt + 1) * 128], qT[base:base + 64, hp, qb * 512:(qb + 1) * 512], ["kT", "qT"]),
                    (sel[0:8, h, :], F8[0:8, qb * 512:(qb + 1) * 512], ["sel", "F8"])]

        def bias_ap(h, kt):
            return nF[:, kt, h:h + 1], ["nF"]

        def v_ap(h, kt):
            return vaug[:, kt, h, :], [("v", kt), "vones"]

        attn_core(cx, 8, 64, 0.125, score_ops, bias_ap, masks, v_ap, ocb(b), tag, bufs=abufs)
    P.barrier()
    sb.reset(m)


def rope_tables(cx, pos_dram, b, cosb, sinb, ifr_b, tmpa, rbufs):
    P, sb = cx.P, cx.sb
    S = cx.S
    nt = S // 128
    posi, posf = rbufs["posi"], rbufs["posf"]
    TWO_PI = 2.0 * np.pi
    P.dma(posi[:], pos_dram[b, :].rearrange("(t p) -> p t", p=128), w=["posi"], slow=True)
    P.dve(lambda e: e.tensor_copy(posf[:], posi[:]), r=["posi"], w=["posf"])
    for tt in range(nt):
        P.dve(lambda e, tt=tt: e.tensor_scalar_mul(tmpa[:, tt, :], ifr_b, posf[:, tt:tt + 1]), r=["posf", "ifr"], w=["ang"])
    ti, tf = rbufs["ti"], rbufs["tf"]
    C1, C2 = 6.28125, 2.0 * np.pi - 6.28125

    def sin_of(out, shift, key):
        P.dve(lambda e: e.tensor_scalar(out, tmpa[:], shift, 1.0 / TWO_PI, ALU.add, ALU.mult), r=["ang"], w=[key])
        P.dve(lambda e: e.tensor_copy(ti[:], out), r=[key], w=["ropei"])
        P.dve(lambda e: e.tensor_copy(tf[:], ti[:]), r=["ropei"], w=["ropef"])
        P.dve(lambda e: e.tensor_scalar_add(out, tmpa[:], shift), r=["ang"], w=[key])
        P.dve(lambda e: e.scalar_tensor_tensor(out, tf[:], -C1, out, ALU.mult, ALU.add), r=["ropef", key], w=[key])
        P.dve(lambda e: e.scalar_tensor_tensor(out, tf[:], -C2, out, ALU.mult, ALU.add), r=["ropef", key], w=[key])
        P.dve(lambda e: e.tensor_scalar(tf[:], out, np.pi, -TWO_PI, ALU.is_gt, ALU.mult), r=[key], w=["ropef"])
        P.dve(lambda e: e.tensor_add(out, out, tf[:]), r=["ropef", key], w=[key])
        P.dve(lambda e: e.tensor_scalar(tf[:], out, -np.pi, TWO_PI, ALU.is_lt, ALU.mult), r=[key], w=["ropef"])
        P.dve(lambda e: e.tensor_add(out, out, tf[:]), r=["ropef", key], w=[key])
        P.dve(lambda e: e.tensor_scalar(out, out, 3.1415925, -3.1415925, ALU.min, ALU.max), r=[key], w=[key])
        P.act(lambda e: e.activation(out, out, AF.Sin), r=[key], w=[key])

    sin_of(sinb[:], 0.0, "sinb")
    sin_of(cosb[:], 0.5 * np.pi, "cosb")


def rope_apply(P, out, x, cos, sin, t1, t2, nh, r, w):
    cb = cos.unsqueeze(1).to_broadcast([128, nh, 32])
    sn = sin.unsqueeze(1).to_broadcast([128, nh, 32])
    x1, x2 = x[:, :, 0:32], x[:, :, 32:64]
    kk = ("ropetmp",)
    P.dve(lambda e: e.tensor_mul(t1, x1, cb), r=r, w=[kk])
    P.dve(lambda e: e.tensor_mul(t2, x2, sn), r=r, w=[kk])
    P.dve(lambda e: e.tensor_sub(out[:, :, 0:32], t1, t2), r=[kk], w=w)
    P.dve(lambda e: e.tensor_mul(t1, x2, cb), r=r + [kk], w=[kk])
    P.dve(lambda e: e.tensor_mul(t2, x1, sn), r=r + [kk], w=[kk])
    P.dve(lambda e: e.tensor_add(out[:, :, 32:64], t1, t2), r=[kk], w=w)


def load_cast_w(cx, dst_bf, src_view, stage, key, eng="dve"):
    P = cx.P
    ks = ("wstage", id(stage))
    P.dma(stage, src_view, w=[ks])
    if eng == "pool":
        P.pool(lambda e: e.tensor_copy(dst_bf, stage), r=[ks], w=[key])
    else:
        P.dve(lambda e: e.tensor_copy(dst_bf, stage), r=[ks], w=[key])


def stage_mla(cx, u_tok, pos, ifr, qg, kvg, w_uq, w_ukv, out_g, mixed):
    nc, P, sb, c = cx.nc, cx.P, cx.sb, cx.c
    S, NB = cx.S, cx.NB
    nt = S // 128
    psb = cx.ps
    m = sb.mark()
    masks = c["masks_chunk64"]
    wq_n = sb.alloc([128, 3, 512], BF16, "wq_n")
    wq_p = sb.alloc([128, 3, 256], BF16, "wq_p")
    wk_n = sb.alloc([128, 512], BF16, "wk_n")
    wk_v = sb.alloc([128, 512], BF16, "wk_v")
    wstg = sb.alloc([128, 3, 768], F32, "wstg")
    P.dma(wstg[:], w_uq.rearrange("(kc p) n -> p kc n", p=128), w=["wstg"])
    v4 = wstg[:].rearrange("p k (h d) -> p k h d", h=4)
    for kc in range(3):
        P.pool(lambda e, kc=kc: e.tensor_copy(wq_n[:, kc, :].rearrange("p (h d) -> p h d", h=4), v4[:, kc, :, 0:128]), r=["wstg"], w=["wq"])
        P.pool(lambda e, kc=kc: e.tensor_copy(wq_p[:, kc, :].rearrange("p (h d) -> p h d", h=4), v4[:, kc, :, 128:192]), r=["wstg"], w=["wq"])
    wstg2 = sb.alloc([128, 1024], F32, "wstg2")
    P.dma(wstg2[:], w_ukv, w=["wstg2"])
    v5 = wstg2[:].rearrange("p (h d) -> p h d", h=4)
    P.pool(lambda e: e.tensor_copy(wk_n[:].rearrange("p (h d) -> p h d", h=4), v5[:, :, 0:128]), r=["wstg2"], w=["wk"])
    P.pool(lambda e: e.tensor_copy(wk_v[:].rearrange("p (h d) -> p h d", h=4), v5[:, :, 128:256]), r=["wstg2"], w=["wk"])
    qg_b = sb.alloc([128, 384], F32, "qg_b")
    kvg_b = sb.alloc([128, 128], F32, "kvg_b")
    g_b = sb.alloc([128, 512], F32, "g_b")
    ifr_b = sb.alloc([128, 32], F32, "ifr_b")
    bcast_load(cx, qg_b[:], qg, "qg")
    bcast_load(cx, kvg_b[:], kvg, "kvg")
    bcast_load(cx, g_b[:], out_g, "mla_g")
    bcast_load(cx, ifr_b[:], ifr, "ifr")
    cosb = sb.alloc([128, nt, 32], F32, "cosb")
    sinb = sb.alloc([128, nt, 32], F32, "sinb")
    ang = sb.alloc([128, nt, 32], F32, "ang")
    cqnT = sb.alloc([128, 3, S], BF16, "cqnT")
    ckvnT = sb.alloc([128, S], BF16, "ckvnT")
    qnT = sb.alloc([128, 4, S], BF16, "qnT")
    qpT = sb.alloc([128, 2, S], BF16, "qpT")
    knT = sb.alloc([128, 4, S], BF16, "knT")
    kpT = sb.alloc([128, S], BF16, "kpT")
    vaug = sb.alloc([128, nt, 4, 129], BF16, "vaug")
    urow = [sb.alloc([128, 576], F32, "urow") for _ in range(2)]
    cn = [sb.alloc([128, 640], F32, "cn") for _ in range(2)]
    qpe = [sb.alloc([128, 256], F32, "qpe") for _ in range(2)]
    junk = sb.alloc([128, 384], F32, "junk")
    ssq = sb.alloc([128, 2], F32, "ssq2")
    t1 = sb.alloc([128, 4, 32], F32, "t1")
    t2 = sb.alloc([128, 4, 32], F32, "t2")
    P.pool(lambda e: e.memset(vaug[:, :, :, 128:129], 1.0), w=["vones"])
    ocb = rms_out_cb(cx, 512, g_b[:], "mla_g", mixed, "mla")
    abufs = attn_bufs(cx, 4, 128)
    rbufs = dict(posi=sb.alloc([128, nt], I32, "posi"), posf=sb.alloc([128, nt], F32, "posf"),
                 ti=sb.alloc([128, nt, 32], I32, "ropei"), tf=sb.alloc([128, nt, 32], F32, "ropef"))
    for b in range(NB):
        t0 = b * S
        tag = "mla%d" % b
        rope_tables(cx, pos, b, cosb, sinb, ifr_b[:], ang, rbufs)
        nev = 0
        for tt in range(nt):
            i2 = tt % 2
            ur, kur = urow[i2], ("urow", i2)
            cnt, kcn = cn[i2], ("cn", i2)
            P.dma(ur[:], u_tok[t0 + tt * 128:t0 + (tt + 1) * 128, C_MLA:C_MLA + 576], w=[kur])
            P.act(lambda e, ur=ur: e.activation(junk[:, 0:384], ur[:, 0:384], AF.Square, accum_out=ssq[:, 0:1]), r=[kur], w=["ssq2", "junk"])
            P.act(lambda e, ur=ur: e.activation(junk[:, 0:128], ur[:, 384:512], AF.Square, accum_out=ssq[:, 1:2]), r=[kur], w=["ssq2", "junk"])
            rstd_from_ssq(P, ssq[:, 0:1], ssq[:, 0:1], 384, RMS_EPS, ["ssq2"], ["ssq2"])
            rstd_from_ssq(P, ssq[:, 1:2], ssq[:, 1:2], 128, RMS_EPS, ["ssq2"], ["ssq2"])
            P.dve(lambda e, ur=ur, cnt=cnt: e.scalar_tensor_tensor(cnt[:, 0:384], ur[:, 0:384], ssq[:, 0:1], qg_b[:], ALU.mult, ALU.mult),
                  r=[kur, "ssq2", "qg"], w=[kcn])
            P.dve(lambda e, ur=ur, cnt=cnt: e.scalar_tensor_tensor(cnt[:, 384:512], ur[:, 384:512], ssq[:, 1:2], kvg_b[:], ALU.mult, ALU.mult),
                  r=[kur, "ssq2", "kvg"], w=[kcn])
            rope_apply(P, cnt[:, 512:576].rearrange("p (h d) -> p h d", h=1), ur[:, 512:576].rearrange("p (h d) -> p h d", h=1),
                       cosb[:, tt, :], sinb[:, tt, :], t1[:, 0:1, :], t2[:, 0:1, :], 1, [kur, "cosb", "sinb"], [kcn])
            P.dve(lambda e, cnt=cnt: e.tensor_copy(cnt[:, 576:640], cnt[:, 512:576]), r=[kcn], w=[kcn])
            pi = 2 + nev % 2
            nev += 1
            ps, kp = psb[pi], ("ps", pi)
            for j in range(4):
                P.pe(lambda e, ps=ps, cnt=cnt, j=j: e.transpose(ps[:, j * 128:(j + 1) * 128], cnt[:, j * 128:(j + 1) * 128], c["ident"][:]),
                     r=[kcn, "ident"], w=[kp])
            P.act(lambda e, ps=ps, tt=tt: e.copy(cqnT[:, :, tt * 128:(tt + 1) * 128], ps[:, 0:384].rearrange("p (j t) -> p j t", j=3)),
                  r=[kp], w=[("cqnT", tt)])
            P.dve(lambda e, ps=ps, tt=tt: e.tensor_copy(ckvnT[:, tt * 128:(tt + 1) * 128], ps[:, 384:512]), r=[kp], w=[("ckvnT", tt)])
            pi = 2 + nev % 2
            nev += 1
            ps, kp = psb[pi], ("ps", pi)
            P.pe(lambda e, ps=ps, cnt=cnt: e.transpose(ps[:, 0:128], cnt[:, 512:640], c["ident"][:]), r=[kcn, "ident"], w=[kp])
            P.act(lambda e, ps=ps, tt=tt: e.copy(kpT[:, tt * 128:(tt + 1) * 128], ps[:, 0:128]), r=[kp], w=[("kpT", tt)])
            pi = 2 + nev % 2
            nev += 1
            ps, kp = psb[pi], ("ps", pi)
            for kc in range(3):
                P.pe(lambda e, ps=ps, kc=kc, tt=tt: e.matmul(ps[:, 0:256], cqnT[:, kc, tt * 128:(tt + 1) * 128], wq_p[:, kc, :],
                                                            start=(kc == 0), stop=(kc == 2)), r=[("cqnT", tt), "wq"], w=[kp])
            qp, kqp = qpe[i2], ("qpe", i2)
            rope_apply(P, qp[:].rearrange("p (h d) -> p h d", h=4), ps[:, 0:256].rearrange("p (h d) -> p h d", h=4),
                       cosb[:, tt, :], sinb[:, tt, :], t1[:], t2[:], 4, [kp, "cosb", "sinb"], [kqp])
            pi = 2 + nev % 2
            nev += 1
            ps, kp = psb[pi], ("ps", pi)
            for j in range(2):
                P.pe(lambda e, ps=ps, qp=qp, j=j: e.transpose(ps[:, j * 128:(j + 1) * 128], qp[:, j * 128:(j + 1) * 128], c["ident"][:]),
                     r=[kqp, "ident"], w=[kp])
            P.act(lambda e, ps=ps, tt=tt: e.copy(qpT[:, :, tt * 128:(tt + 1) * 128], ps[:, 0:256].rearrange("p (j t) -> p j t", j=2)),
                  r=[kp], w=[("qpT", tt)])
            pi = 2 + nev % 2
            nev += 1
            ps, kp = psb[pi], ("ps", pi)
            P.pe(lambda e, ps=ps, tt=tt: e.matmul(ps[:], ckvnT[:, tt * 128:(tt + 1) * 128], wk_v[:], start=True, stop=True),
                 r=[("ckvnT", tt), "wk"], w=[kp])
            P.dve(lambda e, ps=ps, tt=tt: e.tensor_copy(vaug[:, tt, :, 0:128], ps[:].rearrange("p (h d) -> p h d", h=4)),
                  r=[kp], w=[("v", tt)])
        for tb in range(S // 512):
            for h in range(4):
                pi = 2 + nev % 2
                nev += 1
                ps, kp = psb[pi], ("ps", pi)
                for kc in range(3):
                    P.pe(lambda e, ps=ps, kc=kc, h=h, tb=tb: e.matmul(ps[:], wq_n[:, kc, h * 128:(h + 1) * 128], cqnT[:, kc, tb * 512:(tb + 1) * 512],
                                                                  start=(kc == 0), stop=(kc == 2)),
                         r=[("cqnT", tb * 4 + i) for i in range(4)] + ["wq"], w=[kp])
                evac(P, nev, qnT[:, h, tb * 512:(tb + 1) * 512], ps[:], r=[kp], w=[("qnT", tb)])
                pi = 2 + nev % 2
                nev += 1
                ps, kp = psb[pi], ("ps", pi)
                P.pe(lambda e, ps=ps, h=h, tb=tb: e.matmul(ps[:], wk_n[:, h * 128:(h + 1) * 128], ckvnT[:, tb * 512:(tb + 1) * 512],
                                                         start=True, stop=True),
                     r=[("ckvnT", tb * 4 + i) for i in range(4)] + ["wk"], w=[kp])
                evac(P, nev, knT[:, h, tb * 512:(tb + 1) * 512], ps[:], r=[kp], w=[("knT", tb)])

        def score_ops(h, kt, qb):
            base = (h % 2) * 64
            return [(knT[:, h, kt * 128:(kt + 1) * 128], qnT[:, h, qb * 512:(qb + 1) * 512], [("knT", kt // 4), ("qnT", qb)]),
                    (kpT[base:base + 64, kt * 128:(kt + 1) * 128], qpT[base:base + 64, h // 2, qb * 512:(qb + 1) * 512],
                     [("kpT", kt)] + [("qpT", qb * 4 + i) for i in range(4)])]

        def v_ap(h, kt):
            return vaug[:, kt, h, :], [("v", kt), "vones"]

        attn_core(cx, 4, 128, 192 ** -0.5, score_ops, None, masks, v_ap, ocb(b), tag, sbanks=(0, 1), bufs=abufs)
    P.barrier()
    sb.reset(m)


def run_interleaved(gens):
    gens = list(gens)
    while gens:
        for g in list(gens):
            try:
                next(g)
            except StopIteration:
                gens.remove(g)


def dplr_consts(cx):
    P, sb = cx.P, cx.sb
    c = cx.c
    if "MS" in c:
        return
    MS = sb.alloc([128, 128], F32, "MS")
    MI = sb.alloc([128, 128], F32, "MI")
    MZ = sb.alloc([128, 128], F32, "MZ")
    ML = sb.alloc([128, 128], F32, "ML")
    BLK = sb.alloc([128, 128], F32, "BLK")
    for (M, base, cm, pat, lo_zero) in ((MS, -1, -1, 1, "ur"), (MI, 0, -1, 1, "ur"), (MZ, -1, 1, -1, "ll"), (ML, 0, 1, -1, "ll")):
        k = "dplrmask"
        P.pool(lambda e, M=M: e.memset(M[:], 1.0), w=[k])
        P.pool(lambda e, M=M, base=base, cm=cm, pat=pat: e.affine_select(
            out=M[:], in_=M[:], pattern=[[pat, 128]], compare_op=ALU.is_ge, fill=0.0, base=base, channel_multiplier=cm), r=[k], w=[k])
        if lo_zero == "ur":
            P.pool(lambda e, M=M: e.memset(M[0:64, 64:128], 0.0), r=[k], w=[k])
        else:
            P.pool(lambda e, M=M: e.memset(M[64:128, 0:64], 0.0), r=[k], w=[k])
    P.pool(lambda e: e.memset(BLK[:], 0.0), w=["dplrmask"])
    P.pool(lambda e: e.memset(BLK[0:64, 0:64], 1.0), r=["dplrmask"], w=["dplrmask"])
    P.pool(lambda e: e.memset(BLK[64:128, 64:128], 1.0), r=["dplrmask"], w=["dplrmask"])
    c.update(MS=MS, MI=MI, MZ=MZ, ML=ML, BLK=BLK)


def dplr_tile(cx, NH, NK, geo, st, tag, banks=(0, 1, 2)):
    P, c = cx.P, cx.c
    B0, B1, B2 = banks
    psb = {0: cx.ps[B0], 1: cx.ps[B1], 2: cx.ps[B2], 3: cx.ps[B0], 4: cx.ps[B1], 5: cx.ps[B2], 6: cx.ps[B0]}
    bk = {0: B0, 1: B1, 2: B2, 3: B0, 4: B1, 5: B2, 6: B0}
    HP = 128 // NK
    NG = NH // HP
    HG = 512 // 128
    ngr = (NH + HG - 1) // HG
    Xs, Zs, Ps = st["X"], st["Z"], st["P"]
    kX, kZ, kP = [("dX", tag, i) for i in range(2)], [("dZ", tag, i) for i in range(2)], [("dP", tag, i) for i in range(2)]
    identb3 = c["identb"][:].unsqueeze(1).to_broadcast([128, NH, 128])
    P.dve(lambda e: e.tensor_tensor(Ps[0][:], Xs[0][:], identb3, ALU.add), r=[kX[0], "identb"], w=[kP[0]])
    cur = 0
    for lvl in range(1, 6):
        nxt = 1 - cur
        for g in range(ngr):
            hs = list(range(g * HG, min(NH, (g + 1) * HG)))
            n = len(hs)
            if lvl < 5:
                for i, h in enumerate(hs):
                    P.pe(lambda e, i=i, h=h, cur=cur: e.matmul(psb[0][:, i * 128:(i + 1) * 128], Zs[cur][:, h, :], Xs[cur][:, h, :],
                                                             start=True, stop=True), r=[kX[cur], kZ[cur]], w=[("ps", bk[0])])
                P.act(lambda e, g=g, n=n, nxt=nxt: e.copy(Xs[nxt][:, g * HG:g * HG + n, :],
                                                         psb[0][:, 0:n * 128].rearrange("p (h t) -> p h t", h=n)),
                      r=[("ps", bk[0])], w=[kX[nxt]])
            for i, h in enumerate(hs):
                P.pe(lambda e, i=i, h=h, cur=cur: e.matmul(psb[1][:, i * 128:(i + 1) * 128], Xs[cur][:, h, :], Zs[cur][:, h, :],
                                                         start=True, stop=True), r=[kX[cur], kZ[cur]], w=[("ps", bk[1])])
            P.dve(lambda e, g=g, n=n, nxt=nxt: e.tensor_copy(Zs[nxt][:, g * HG:g * HG + n, :],
                                                            psb[1][:, 0:n * 128].rearrange("p (h t) -> p h t", h=n)),
                  r=[("ps", bk[1])], w=[kZ[nxt]])
            for i, h in enumerate(hs):
                P.pe(lambda e, i=i, h=h, cur=cur, nxt=nxt: e.matmul(psb[2][:, i * 128:(i + 1) * 128], Zs[nxt][:, h, :], Ps[cur][:, h, :],
                                                                  start=True, stop=True), r=[kZ[nxt], kP[cur]], w=[("ps", bk[2])])
            P.dve(lambda e, g=g, n=n, cur=cur, nxt=nxt: e.tensor_tensor(
                Ps[nxt][:, g * HG:g * HG + n, :], psb[2][:, 0:n * 128].rearrange("p (h t) -> p h t", h=n),
                Ps[cur][:, g * HG:g * HG + n, :], ALU.add), r=[("ps", bk[2]), kP[cur]], w=[kP[nxt]])
            yield
        cur = nxt
    TT, kTT = Ps[cur], kP[cur]
    ST, STb = st["ST"], st["STb"]
    kST = ("dST", tag)
    NV = NK
    W = NH * NV
    Wb, Ub = st["Wb"], st["Ub"]
    for ch in range(2):
        c0 = ch * 64
        M = 64 + c0
        rows = slice(c0, c0 + 64)
        for h in range(NH):
            g_, hb = h // HP, (h % HP) * NK
            at, kat = geo["AT"](h)
            P.pe(lambda e, h=h, at=at, g_=g_, hb=hb, M=M: e.matmul(psb[3][0:M, h * NV:(h + 1) * NV], at[:, 0:M], STb[:, g_, :],
                                                                start=True, stop=False), r=kat + [kST], w=[("ps", bk[3])])
            ak, kak = geo["AAK"](h)
            v, kv = geo["V"](h)
            P.pe(lambda e, h=h, ak=ak, v=v, M=M, rows=rows: e.matmul(psb[3][0:M, h * NV:(h + 1) * NV], ak[rows, 0:M], v[rows, :],
                                                                  start=False, stop=True), r=kak + kv, w=[("ps", bk[3])])
        kW = ("dW", tag)
        if geo.get("WADD") is not None:
            wa, kwa = geo["WADD"]
            P.dve(lambda e, rows=rows, wa=wa: e.tensor_tensor(Wb[rows, :], psb[3][rows, 0:W], wa[rows, :], ALU.add), r=[("ps", bk[3])] + kwa, w=[kW])
        else:
            P.act(lambda e, rows=rows: e.copy(Wb[rows, :], psb[3][rows, 0:W]), r=[("ps", bk[3])], w=[kW])
        yield
        for h in range(NH):
            P.pe(lambda e, h=h, M=M, rows=rows: e.matmul(psb[4][0:M, h * NV:(h + 1) * NV], TT[rows, h, 0:M], Wb[rows, h * NV:(h + 1) * NV],
                                                      start=True, stop=True), r=[kTT, kW], w=[("ps", bk[4])])
        kU = ("dU", tag)
        P.dve(lambda e, rows=rows: e.tensor_copy(Ub[rows, :], psb[4][rows, 0:W]), r=[("ps", bk[4])], w=[kU])
        yield
        for h in range(NH):
            g_, hb = h // HP, (h % HP) * NK
            rt, krt = geo["RT"](h)
            arb, karb = geo["ARB"](h)
            ark, kark = geo["ARK"](h)
            v, kv = geo["V"](h)
            o = psb[5][0:M, h * NV:(h + 1) * NV]
            P.pe(lambda e, o=o, rt=rt, g_=g_, hb=hb, M=M: e.matmul(o, rt[:, 0:M], STb[:, g_, :], start=True, stop=False),
                 r=krt + [kST], w=[("ps", bk[5])])
            P.pe(lambda e, o=o, arb=arb, h=h, M=M, rows=rows: e.matmul(o, arb[rows, 0:M], Ub[rows, h * NV:(h + 1) * NV], start=False, stop=False),
                 r=karb + [kU], w=[("ps", bk[5])])
            P.pe(lambda e, o=o, ark=ark, v=v, M=M, rows=rows: e.matmul(o, ark[rows, 0:M], v[rows, :], start=False, stop=True),
                 r=kark + kv, w=[("ps", bk[5])])
        yo, kyo = geo["Y"]
        P.act(lambda e, rows=rows: e.copy(yo[rows, :], psb[5][rows, 0:W]), r=[("ps", bk[5])], w=[kyo])
        bd, kbd = geo["BD"]
        kd, kkd = geo["KD"]
        for h in range(NH):
            g_ = h // HP
            v, kv = geo["V"](h)
            o = psb[6][:, h * NV:(h + 1) * NV]
            P.pe(lambda e, o=o, g_=g_, h=h, rows=rows: e.matmul(o, bd[rows, g_ * 128:(g_ + 1) * 128], Ub[rows, h * NV:(h + 1) * NV],
                                                             start=True, stop=False), r=kbd + [kU], w=[("ps", bk[6])])
            P.pe(lambda e, o=o, g_=g_, v=v, rows=rows: e.matmul(o, kd[rows, g_ * 128:(g_ + 1) * 128], v[rows, :], start=False, stop=True),
                 r=kkd + kv, w=[("ps", bk[6])])
        gc_, kgc = geo["GC"](ch)
        for hh in range(HP):
            pr = slice(hh * NK, (hh + 1) * NK)
            src = psb[6][pr, 0:NH * NV].rearrange("p (g x v) -> p g x v", g=NG, x=HP)[:, :, hh, :]
            P.dve(lambda e, pr=pr: e.tensor_tensor(ST[pr, :, :], ST[pr, :, :], gc_[pr, :].unsqueeze(2).to_broadcast([NK, NG, NV]), ALU.mult),
                  r=[kST] + kgc, w=[kST])
            P.dve(lambda e, pr=pr, src=src: e.tensor_tensor(ST[pr, :, :], ST[pr, :, :], src, ALU.add), r=[kST, ("ps", bk[6])], w=[kST])
        P.act(lambda e: e.copy(STb[:], ST[:]), r=[kST], w=[kST])
        yield


def stage_rwkv(cx, u_tok, prm, mixed):
    nc, P, sb, c, psb = cx.nc, cx.P, cx.sb, cx.c, cx.ps
    S, NB = cx.S, cx.NB
    nt = S // 128
    m = sb.mark()
    dplr_consts(cx)
    NH, NK = 8, 64
    f32t = lambda name, w=512: sb.alloc([128, w], F32, name)
    mu_b = f32t("mu_b", 1792)
    bc = {}
    for nm in ("w0", "a0", "k_k", "k_a", "r_k", "ln_g", "ln_b"):
        bc[nm] = f32t(nm + "_b")
        bcast_load(cx, bc[nm][:], prm[nm], "rw_" + nm)
    bcast_load(cx, mu_b[:], prm["mu"], "rw_mu")
    w2b = sb.alloc([64, 512], BF16, "w2b")
    a2b = sb.alloc([128, 512], BF16, "a2b")
    g2b = sb.alloc([128, 512], BF16, "g2b")
    wtmp = f32t("wtmp")
    load_cast_w(cx, w2b[:], prm["w2"], wtmp[0:64, :], "rw_w2")
    wtmp2 = f32t("wtmp2")
    load_cast_w(cx, a2b[64:128, :], prm["a2"], wtmp2[64:128, :], "rw_a2")
    wtmp3 = f32t("wtmp3")
    load_cast_w(cx, g2b[:], prm["g2"], wtmp3[:], "rw_g2")
    Ut = [sb.alloc([128, 1792], F32, "Ut") for _ in range(2)]
    Up = [sb.alloc([128, 1792], F32, "Up") for _ in range(2)]
    xs = sb.alloc([128, 1792], F32, "xs")
    la = f32t("la", 256)
    lT = sb.alloc([128, 256], BF16, "lT")
    names = ["lw", "aa", "gt", "Gs", "eG", "enG", "eGm", "eD", "kx", "sq", "kk", "k2", "bv", "tR", "tK", "tB", "tA", "rk", "yt", "y2"]
    T_ = {n: f32t(n) for n in names}
    small = {n: sb.alloc([128, 8], F32, n) for n in ("ssq", "rn", "bs", "s1", "s2", "mean", "var", "rstd")}
    PB = []
    for _b in range(NB):
        d_ = dict(Kd=sb.alloc([128, 512], BF16, "Kd"), Bd=sb.alloc([128, 512], BF16, "Bd"), Vb=sb.alloc([128, 512], BF16, "Vb"),
                  ART=sb.alloc([128, 8, 2, 128], BF16, "ARTz"), KT=sb.alloc([128, 4, 128], BF16, "KT"), BT=sb.alloc([128, 4, 128], BF16, "BT"),
                  AM3=sb.alloc([128, 8, 384], BF16, "AM3"), gC=[sb.alloc([128, 4], F32, "gC") for _ in range(2)],
                  yt=f32t("ytb"), gt=f32t("gtb"), vk=f32t("vkb"), bs=sb.alloc([128, 8], F32, "bsb"),
                  st=dict(X=[sb.alloc([128, 8, 128], BF16, "dX") for _ in range(2)], Z=[sb.alloc([128, 8, 128], BF16, "dZ") for _ in range(2)],
                          P=[sb.alloc([128, 8, 128], BF16, "dP") for _ in range(2)], ST=sb.alloc([128, 4, 64], F32, "ST"),
                          STb=sb.alloc([128, 4, 64], BF16, "STb"), Wb=sb.alloc([128, 512], BF16, "Wb"), Ub=sb.alloc([128, 512], BF16, "Ub")))
        P.pool(lambda e, A_=d_["ART"]: e.memset(A_[:], 0.0), w=[("ARTz0", _b)])
        PB.append(d_)
    mask4 = sb.alloc([128, 4, 128], F32, "mask4")
    for i, Mk in enumerate(("MS", "MI", "MS", "MI")):
        P.pool(lambda e, i=i, Mk=Mk: e.tensor_copy(mask4[:, i, :], c[Mk][:]), r=["dplrmask"], w=["mask4"])
    MZ4 = c["MZ"][:].unsqueeze(1).to_broadcast([128, 4, 128])
    h3 = lambda t: t.rearrange("p (h d) -> p h d", h=8)
    b3 = lambda t: t.unsqueeze(2).to_broadcast([128, 8, 64])
    if True:
        def tile_prep(b, tt):
            tag = "rw%d" % b
            kST = ("dST", tag)
            pb = PB[b]
            Kd, Bd, Vb, ART, KT, BT, AM3, gC, st = (pb[k] for k in ("Kd", "Bd", "Vb", "ART", "KT", "BT", "AM3", "gC", "st"))
            kgt, kbs, kvk, kyt = ("gt", b), ("bs", b), ("vk", b), ("yt", b)
            if tt == 0:
                P.pool(lambda e: e.memset(st["ST"][:], 0.0), w=[kST])
                P.pool(lambda e: e.memset(st["STb"][:], 0.0), r=[kST], w=[kST])
            i2 = (tt * NB + b) % 2
            r0 = b * S + tt * 128
            U_, Up_ = Ut[i2], Up[i2]
            kU_, kUp = ("Ut", i2), ("Up", i2)
            cs = slice(C_RWKV, C_RWKV + 1792)
            P.dma(U_[:], u_tok[r0:r0 + 128, cs], w=[kU_])
            if tt == 0:
                P.pool(lambda e, Up_=Up_: e.memset(Up_[0:1, :], 0.0), w=[kUp])
                P.dma(Up_[1:128, :], u_tok[r0:r0 + 127, cs], w=[kUp])
            else:
                P.dma(Up_[:], u_tok[r0 - 1:r0 + 127, cs], w=[kUp])
            P.pool(lambda e, U_=U_, Up_=Up_: e.tensor_sub(Up_[:], Up_[:], U_[:]), r=[kU_, kUp], w=[kUp])
            P.pool(lambda e, Up_=Up_: e.tensor_mul(Up_[:], Up_[:], mu_b[:]), r=[kUp, "rw_mu"], w=[kUp])
            P.dve(lambda e, U_=U_, Up_=Up_: e.tensor_add(xs[:], U_[:], Up_[:]), r=[kU_, kUp], w=["xs"])
            r_, k_, v_ = xs[:, 0:512], xs[:, 512:1024], xs[:, 1024:1536]
            P.act(lambda e: e.copy(pb["vk"][:], v_), r=["xs"], w=[kvk])
            P.act(lambda e: e.activation(la[:, 0:64], xs[:, 1536:1600], AF.Tanh), r=["xs"], w=["la"])
            P.act(lambda e: e.copy(la[:, 64:128], xs[:, 1600:1664]), r=["xs"], w=["la"])
            P.act(lambda e: e.activation(la[:, 128:256], xs[:, 1664:1792], AF.Sigmoid), r=["xs"], w=["la"])
            for j in range(2):
                P.pe(lambda e, j=j: e.transpose(psb[7][:, j * 128:(j + 1) * 128], la[:, j * 128:(j + 1) * 128], c["ident"][:]),
                     r=["la", "ident"], w=[("ps", 7)])
            P.act(lambda e: e.copy(lT[:], psb[7][:, 0:256]), r=[("ps", 7)], w=["lT"])
            P.pe(lambda e: e.matmul(psb[3][:], lT[0:64, 0:128], w2b[0:64, :], start=True, stop=True), r=["lT", "rw_w2"], w=[("ps", 3)])
            P.pe(lambda e: e.matmul(psb[4][:], lT[64:128, 0:128], a2b[64:128, :], start=True, stop=True), r=["lT", "rw_a2"], w=[("ps", 4)])
            P.pe(lambda e: e.matmul(psb[5][:], lT[:, 128:256], g2b[:], start=True, stop=True), r=["lT", "rw_g2"], w=[("ps", 5)])
            P.dve(lambda e: e.tensor_add(T_["lw"][:], psb[3][:], bc["w0"][:]), r=[("ps", 3), "rw_w0"], w=["lw"])
            P.act(lambda e: e.activation(T_["lw"][:], T_["lw"][:], AF.Sigmoid), r=["lw"], w=["lw"])
            P.act(lambda e: e.mul(T_["lw"][:], T_["lw"][:], -float(np.exp(-0.5))), r=["lw"], w=["lw"])
            P.dve(lambda e: e.tensor_add(T_["aa"][:], psb[4][:], bc["a0"][:]), r=[("ps", 4), "rw_a0"], w=["aa"])
            P.act(lambda e: e.activation(T_["aa"][:], T_["aa"][:], AF.Sigmoid), r=["aa"], w=["aa"])
            P.act(lambda e: e.copy(pb["gt"][:], psb[5][:]), r=[("ps", 5)], w=[kgt])
            P.pe(lambda e: e.matmul(psb[3][:], c["MI"][:], T_["lw"][:], start=True, stop=True), r=["dplrmask", "lw"], w=[("ps", 3)])
            P.pe(lambda e: e.matmul(psb[4][:], c["BLK"][:], T_["lw"][:], start=True, stop=True), r=["dplrmask", "lw"], w=[("ps", 4)])
            P.act(lambda e: e.copy(T_["Gs"][:], psb[3][:]), r=[("ps", 3)], w=["Gs"])
            P.act(lambda e: e.activation(T_["eG"][:], T_["Gs"][:], AF.Exp), r=["Gs"], w=["eG"])
            P.act(lambda e: e.activation(T_["enG"][:], T_["Gs"][:], AF.Exp, scale=-1.0), r=["Gs"], w=["enG"])
            P.dve(lambda e: e.tensor_sub(T_["eGm"][:], T_["Gs"][:], T_["lw"][:]), r=["Gs", "lw"], w=["eGm"])
            P.act(lambda e: e.activation(T_["eGm"][:], T_["eGm"][:], AF.Exp), r=["eGm"], w=["eGm"])
            P.dve(lambda e: e.tensor_sub(T_["eD"][:], psb[4][:], T_["Gs"][:]), r=[("ps", 4), "Gs"], w=["eD"])
            P.act(lambda e: e.activation(T_["eD"][:], T_["eD"][:], AF.Exp), r=["eD"], w=["eD"])
            P.dve(lambda e: e.tensor_mul(T_["kx"][:], k_, bc["k_k"][:]), r=["xs", "rw_k_k"], w=["kx"])
            P.pool(lambda e: e.tensor_mul(T_["sq"][:], T_["kx"][:], T_["kx"][:]), r=["kx"], w=["sq"])
            P.dve(lambda e: e.tensor_reduce(small["ssq"][:], h3(T_["sq"][:]), AX.X, ALU.add), r=["sq"], w=["ssq"])
            P.dve(lambda e: e.tensor_scalar_add(small["rn"][:], small["ssq"][:], 1e-6), r=["ssq"], w=["rn"])
            P.act(lambda e: e.activation(small["rn"][:], small["rn"][:], AF.Sqrt), r=["rn"], w=["rn"])
            P.dve(lambda e: e.reciprocal(small["rn"][:], small["rn"][:]), r=["rn"], w=["rn"])
            P.dve(lambda e: e.tensor_mul(h3(T_["kk"][:]), h3(T_["kx"][:]), b3(small["rn"][:])), r=["kx", "rn"], w=["kk"])
            P.dve(lambda e: e.scalar_tensor_tensor(T_["k2"][:], T_["aa"][:], -1.0, bc["k_a"][:], ALU.add, ALU.mult), r=["aa", "rw_k_a"], w=["k2"])
            P.dve(lambda e: e.scalar_tensor_tensor(T_["k2"][:], T_["k2"][:], 1.0, k_, ALU.add, ALU.mult), r=["k2", "xs"], w=["k2"])
            P.dve(lambda e: e.tensor_mul(T_["bv"][:], T_["kk"][:], T_["aa"][:]), r=["kk", "aa"], w=["bv"])
            P.pool(lambda e: e.tensor_mul(T_["tR"][:], r_, T_["eG"][:]), r=["xs", "eG"], w=["tR"])
            P.pool(lambda e: e.tensor_mul(T_["tK"][:], T_["k2"][:], T_["enG"][:]), r=["k2", "enG"], w=["tK"])
            P.pool(lambda e: e.tensor_mul(T_["tB"][:], T_["bv"][:], T_["enG"][:]), r=["bv", "enG"], w=["tB"])
            P.dve(lambda e: e.scalar_tensor_tensor(T_["tA"][:], T_["kk"][:], -1.0, T_["eGm"][:], ALU.mult, ALU.mult), r=["kk", "eGm"], w=["tA"])
            kgeo = ("geo", tag)
            P.pool(lambda e: e.tensor_mul(Kd[:], T_["k2"][:], T_["eD"][:]), r=["k2", "eD"], w=[kgeo])
            P.pool(lambda e: e.tensor_mul(Bd[:], T_["bv"][:], T_["eD"][:]), r=["bv", "eD"], w=[kgeo])
            P.act(lambda e: e.copy(Vb[:], v_), r=["xs"], w=[kgeo])
            P.pool(lambda e: e.tensor_mul(T_["rk"][:], r_, T_["k2"][:]), r=["xs", "k2"], w=["rk"])
            P.pool(lambda e: e.tensor_mul(T_["rk"][:], T_["rk"][:], bc["r_k"][:]), r=["rk", "rw_r_k"], w=["rk"])
            P.dve(lambda e: e.tensor_reduce(pb["bs"][:], h3(T_["rk"][:]), AX.X, ALU.add), r=["rk"], w=[kbs])
            for (src, ksrc, dst) in ((T_["tA"], "tA", lambda q: ART[:, q, 0, :]), (T_["tR"], "tR", lambda q: ART[:, q, 1, :]),
                                      (T_["tK"], "tK", lambda q: KT[:, q, :]), (T_["tB"], "tB", lambda q: BT[:, q, :])):
                for q in range(4):
                    P.pe(lambda e, src=src, q=q: e.transpose(psb[7][:, q * 128:(q + 1) * 128], src[:, q * 128:(q + 1) * 128], c["ident"][:]),
                         r=[ksrc, "ident"], w=[("ps", 7)])
                if ksrc in ("tA", "tR"):
                    j = 0 if ksrc == "tA" else 1
                    for x in range(2):
                        pr = slice(x * 64, (x + 1) * 64)
                        dst_ = ART[pr, :, :, :].rearrange("p (q x) a t -> p q x a t", x=2)[:, :, x, j, :]
                        if x == 0:
                            P.act(lambda e, dst_=dst_, pr=pr: e.copy(dst_, psb[7][pr, :].rearrange("p (q t) -> p q t", q=4)), r=[("ps", 7), ("ARTz0", b)], w=[kgeo])
                        else:
                            P.dve(lambda e, dst_=dst_, pr=pr: e.tensor_copy(dst_, psb[7][pr, :].rearrange("p (q t) -> p q t", q=4)), r=[("ps", 7), ("ARTz0", b)], w=[kgeo])
                elif ksrc == "tK":
                    P.dve(lambda e: e.tensor_copy(KT[:], psb[7][:].rearrange("p (q t) -> p q t", q=4)), r=[("ps", 7)], w=[kgeo])
                else:
                    P.dve(lambda e: e.tensor_copy(BT[:], psb[7][:].rearrange("p (q t) -> p q t", q=4)), r=[("ps", 7)], w=[kgeo])
            for q in range(4):
                P.pe(lambda e, q=q: e.transpose(psb[7][:, q * 128:(q + 1) * 128], T_["eG"][:, q * 128:(q + 1) * 128], c["ident"][:]),
                     r=["eG", "ident"], w=[("ps", 7)])
            pv7 = psb[7][:].rearrange("p (q t) -> p q t", q=4)
            P.dve(lambda e: e.tensor_copy(gC[0][:], pv7[:, :, 63]), r=[("ps", 7)], w=[kgeo])
            P.dve(lambda e: e.tensor_copy(gC[1][:], pv7[:, :, 127]), r=[("ps", 7)], w=[kgeo])
            kX0, kZ0 = ("dX", tag, 0), ("dZ", tag, 0)
            for h in range(8):
                q, hb = h // 2, (h % 2) * 64
                pa = psb[h % 2]
                kpa = ("ps", h % 2)
                rhs2 = ART[:, h, :, :].rearrange("p a t -> p (a t)")
                P.pe(lambda e, pa=pa, q=q, rhs2=rhs2: e.matmul(pa[:, 0:256], BT[:, q, :], rhs2, start=True, stop=True), r=[kgeo], w=[kpa])
                P.pe(lambda e, pa=pa, q=q, rhs2=rhs2: e.matmul(pa[:, 256:512], KT[:, q, :], rhs2, start=True, stop=True), r=[kgeo], w=[kpa])
                pav = pa[:].rearrange("p (a t) -> p a t", a=4)
                P.dve(lambda e, h=h, pav=pav: e.tensor_tensor(st["X"][0][:, h, :], pav[:, 0, :], mask4[:, 0, :], ALU.mult), r=[kpa, "mask4"], w=[kX0])
                P.dve(lambda e, h=h, pav=pav: e.tensor_tensor(AM3[:, h, :].rearrange("p (a t) -> p a t", a=3), pav[:, 1:4, :], mask4[:, 1:4, :], ALU.mult),
                      r=[kpa, "mask4"], w=[("AM3", tag)])
                pz = psb[2]
                P.pe(lambda e, q=q, hb=hb, h=h: e.matmul(psb[2][:, (h % 4) * 128:(h % 4 + 1) * 128], ART[:, h, 0, :], BT[:, q, :],
                                                        start=True, stop=True), r=[kgeo], w=[("ps", 2)])
                if h % 4 == 3:
                    g4 = h // 4
                    P.dve(lambda e, g4=g4: e.tensor_tensor(st["Z"][0][:, g4 * 4:(g4 + 1) * 4, :], psb[2][:].rearrange("p (h t) -> p h t", h=4), MZ4, ALU.mult),
                          r=[("ps", 2), "dplrmask"], w=[kZ0])
            geo = dict(
                AT=lambda h: (ART[:, h, 0, :], [kgeo]),
                RT=lambda h: (ART[:, h, 1, :], [kgeo]),
                ARB=lambda h: (AM3[:, h, 0:128], [("AM3", tag)]),
                AAK=lambda h: (AM3[:, h, 128:256], [("AM3", tag)]),
                ARK=lambda h: (AM3[:, h, 256:384], [("AM3", tag)]),
                V=lambda h: (Vb[:, h * 64:(h + 1) * 64], [kgeo]),
                BD=(Bd, [kgeo]), KD=(Kd, [kgeo]), GC=lambda ch: (gC[ch], [kgeo]),
                Y=(pb["yt"], kyt), WADD=None)
            return geo

        def tile_post(b, tt):
            pb = PB[b]
            r0 = b * S + tt * 128
            kgt, kbs, kvk, kyt = ("gt", b), ("bs", b), ("vk", b), ("yt", b)
            yt, y2 = pb["yt"], T_["y2"]
            P.pool(lambda e: e.tensor_mul(y2[:], yt[:], yt[:]), r=[kyt], w=["y2"])
            P.dve(lambda e: e.tensor_reduce(small["s1"][:], h3(yt[:]), AX.X, ALU.add), r=[kyt], w=["s1"])
            P.dve(lambda e: e.tensor_reduce(small["s2"][:], h3(y2[:]), AX.X, ALU.add), r=["y2"], w=["s2"])
            P.dve(lambda e: e.tensor_scalar_mul(small["mean"][:], small["s1"][:], 1.0 / 64), r=["s1"], w=["mean"])
            P.dve(lambda e: e.tensor_mul(small["var"][:], small["mean"][:], small["mean"][:]), r=["mean"], w=["var"])
            P.dve(lambda e: e.scalar_tensor_tensor(small["var"][:], small["s2"][:], 1.0 / 64, small["var"][:], ALU.mult, ALU.subtract), r=["s2", "var"], w=["var"])
            P.dve(lambda e: e.tensor_scalar_add(small["rstd"][:], small["var"][:], 64e-5), r=["var"], w=["rstd"])
            P.act(lambda e: e.activation(small["rstd"][:], small["rstd"][:], AF.Sqrt), r=["rstd"], w=["rstd"])
            P.dve(lambda e: e.reciprocal(small["rstd"][:], small["rstd"][:]), r=["rstd"], w=["rstd"])
            P.dve(lambda e: e.tensor_sub(h3(yt[:]), h3(yt[:]), b3(small["mean"][:])), r=[kyt, "mean"], w=[kyt])
            P.dve(lambda e: e.tensor_mul(h3(yt[:]), h3(yt[:]), b3(small["rstd"][:])), r=[kyt, "rstd"], w=[kyt])
            P.pool(lambda e: e.tensor_mul(yt[:], yt[:], bc["ln_g"][:]), r=[kyt, "rw_ln_g"], w=[kyt])
            P.pool(lambda e: e.tensor_add(yt[:], yt[:], bc["ln_b"][:]), r=[kyt, "rw_ln_b"], w=[kyt])
            P.dve(lambda e: e.tensor_mul(h3(y2[:]), h3(pb["vk"][:]), b3(pb["bs"][:])), r=[kvk, kbs, "y2"], w=["y2"])
            P.dve(lambda e: e.tensor_add(yt[:], yt[:], y2[:]), r=[kyt, "y2"], w=[kyt])
            P.dve(lambda e: e.tensor_mul(y2[:], yt[:], pb["gt"][:]), r=[kyt, kgt], w=["y2"])
            P.dma(mixed[r0:r0 + 128, 1024:1536], y2[:], r=["y2"], q="pool")
    for tt in range(nt):
        geos = [tile_prep(b, tt) for b in range(NB)]
        run_interleaved([dplr_tile(cx, 8, 64, geos[b], PB[b]["st"], "rw%d" % b, banks=(3 * (b % 2), 3 * (b % 2) + 1, 3 * (b % 2) + 2))
                         for b in range(NB)])
        for b in range(NB):
            tile_post(b, tt)
    P.barrier()
    sb.reset(m)


def stage_gdn(cx, u_tok, prm, mixed):
    nc, P, sb, c, psb = cx.nc, cx.P, cx.sb, cx.c, cx.ps
    S, NB = cx.S, cx.NB
    nt = S // 128
    m = sb.mark()
    dplr_consts(cx)
    NH, NK = 4, 128
    cw_b = [sb.alloc([128, 1536], F32, "cw_b") for _ in range(4)]
    for j in range(4):
        bcast_load(cx, cw_b[j][:], prm["conv_w"][j, :], "gd_cw")
    A_b = sb.alloc([128, 4], F32, "A_b")
    dtb_b = sb.alloc([128, 4], F32, "dtb_b")
    ng_b = sb.alloc([128, 4, 128], F32, "ng_b")
    bcast_load(cx, A_b[:], prm["a_log"], "gd_A")
    bcast_load(cx, dtb_b[:], prm["dt_bias"], "gd_dtb")
    for h in range(4):
        bcast_load(cx, ng_b[:, h, :], prm["norm_g"], "gd_ng")
    P.act(lambda e: e.activation(A_b[:], A_b[:], AF.Exp), r=["gd_A"], w=["gd_A"])
    ones = sb.alloc([128, 128], F32, "ones")
    P.pool(lambda e: e.memset(ones[:], 1.0), w=["ones"])
    BIGM = sb.alloc([128, 4, 128], F32, "BIGM")
    for h in range(4):
        P.dve(lambda e, h=h: e.tensor_scalar(BIGM[:, h, :], c["ML"][:], -1.0, 30000.0, ALU.add, ALU.mult), r=["dplrmask"], w=["BIGM"])
    P.dve(lambda e: e.tensor_scalar_mul(BIGM[:], BIGM[:], -1.0), r=["BIGM"], w=["BIGM"])
    MZ4 = c["MZ"][:].unsqueeze(1).to_broadcast([128, 4, 128])
    sh = [sb.alloc([128, 1536], F32, "sh") for _ in range(4)]
    acc = sb.alloc([128, 1536], F32, "acc")
    zab = sb.alloc([128, 520], F32, "zab")
    f4 = lambda n: sb.alloc([128, 4], F32, n)
    sm = {n: f4(n) for n in ("ssq", "rnq", "rnk", "beta", "nbeta", "sp", "g", "gcs", "egc", "edec", "ssqo", "tmp", "tmp2")}
    gC = [f4("gC0"), f4("gC1")]
    diag = sb.alloc([128, 4, 128], F32, "diag")
    D4 = sb.alloc([128, 4, 128], F32, "D4")
    DS4 = sb.alloc([128, 4, 128], F32, "DS4")
    bf = lambda n: sb.alloc([128, 512], BF16, n)
    qn, kn, ta, tr, Kd, Vp = bf("qn"), bf("kn"), bf("ta"), bf("tr"), bf("Kd"), bf("Vp")
    kT, qT, AT, RT = [sb.alloc([128, 4, 128], BF16, n) for n in ("kT", "qT", "AT", "RT")]
    Zt, Art = sb.alloc([128, 4, 128], BF16, "Zt"), sb.alloc([128, 4, 128], BF16, "Art")
    XK, ArT = sb.alloc([128, 4, 128], BF16, "XK"), sb.alloc([128, 4, 128], BF16, "ArT")
    yt = sb.alloc([128, 512], F32, "yt")
    y2 = sb.alloc([128, 512], F32, "y2")
    sz = sb.alloc([128, 512], F32, "sz")
    st = dict(X=[sb.alloc([128, 4, 128], BF16, "dX") for _ in range(2)], Z=[sb.alloc([128, 4, 128], BF16, "dZ") for _ in range(2)],
              P=[sb.alloc([128, 4, 128], BF16, "dP") for _ in range(2)], ST=sb.alloc([128, 4, 128], F32, "ST"),
              STb=sb.alloc([128, 4, 128], BF16, "STb"), Wb=sb.alloc([128, 512], BF16, "Wb"), Ub=sb.alloc([128, 512], BF16, "Ub"))
    h3 = lambda t: t.rearrange("p (h d) -> p h d", h=4)
    b3 = lambda t: t.unsqueeze(2).to_broadcast([128, 4, 128])
    pbf = lambda i: psb[i][:].bitcast(BF16)
    PB = []
    for _b in range(NB):
        PB.append(dict(Kd=bf("Kd"), Vp=bf("Vp"), AT=sb.alloc([128, 4, 128], BF16, "AT"), RT=sb.alloc([128, 4, 128], BF16, "RT"),
                       XK=sb.alloc([128, 4, 128], BF16, "XK"), ArT=sb.alloc([128, 4, 128], BF16, "ArT"), gC=[f4("gC0"), f4("gC1")],
                       yt=sb.alloc([128, 512], F32, "ytb"), sz=sb.alloc([128, 512], F32, "szb"),
                       st=dict(X=[sb.alloc([128, 4, 128], BF16, "dX") for _ in range(2)], Z=[sb.alloc([128, 4, 128], BF16, "dZ") for _ in range(2)],
                               P=[sb.alloc([128, 4, 128], BF16, "dP") for _ in range(2)], ST=sb.alloc([128, 4, 128], F32, "ST"),
                               STb=sb.alloc([128, 4, 128], BF16, "STb"), Wb=sb.alloc([128, 512], BF16, "Wb"), Ub=sb.alloc([128, 512], BF16, "Ub"))))
    if True:
        def tile_prep(b, tt):
            tag = "gd%d" % b
            kST = ("dST", tag)
            pb = PB[b]
            Kd, Vp, AT, RT, XK, ArT, gC, st, yt, sz = (pb[k] for k in ("Kd", "Vp", "AT", "RT", "XK", "ArT", "gC", "st", "yt", "sz"))
            kyt, ksz = ("yt", b), ("sz", b)
            if tt == 0:
                P.pool(lambda e: e.memset(st["ST"][:], 0.0), w=[kST])
                P.pool(lambda e: e.memset(st["STb"][:], 0.0), r=[kST], w=[kST])
            r0 = b * S + tt * 128
            for j in range(4):
                d = 3 - j
                ksh = ("sh", j)
                if tt == 0 and d > 0:
                    P.pool(lambda e, j=j, d=d: e.memset(sh[j][0:d, :], 0.0), w=[ksh])
                    P.dma(sh[j][d:128, :], u_tok[r0:r0 + 128 - d, C_GDN:C_GDN + 1536], w=[ksh])
                else:
                    P.dma(sh[j][:], u_tok[r0 - d:r0 - d + 128, C_GDN:C_GDN + 1536], w=[ksh])
            P.dma(zab[:], u_tok[r0:r0 + 128, C_GDN + 1536:C_GDN + 2056], w=["zab"])
            P.dve(lambda e: e.tensor_mul(acc[:], sh[3][:], cw_b[3][:]), r=[("sh", 3), "gd_cw"], w=["acc"])
            for j in range(3):
                P.pool(lambda e, j=j: e.tensor_mul(sh[j][:], sh[j][:], cw_b[j][:]), r=[("sh", j), "gd_cw"], w=[("sh", j)])
                P.dve(lambda e, j=j: e.tensor_add(acc[:], acc[:], sh[j][:]), r=[("sh", j), "acc"], w=["acc"])
            P.act(lambda e: e.activation(acc[:], acc[:], AF.Silu), r=["acc"], w=["acc"])
            q_, k_, v_ = acc[:, 0:512], acc[:, 512:1024], acc[:, 1024:1536]
            P.pool(lambda e: e.tensor_mul(y2[:], q_, q_), r=["acc"], w=["y2"])
            P.dve(lambda e: e.tensor_reduce(sm["ssq"][:], h3(y2[:]), AX.X, ALU.add), r=["y2"], w=["ssq"])
            P.dve(lambda e: e.tensor_scalar_add(sm["rnq"][:], sm["ssq"][:], 1e-6), r=["ssq"], w=["rnq"])
            P.act(lambda e: e.activation(sm["rnq"][:], sm["rnq"][:], AF.Sqrt), r=["rnq"], w=["rnq"])
            P.dve(lambda e: e.reciprocal(sm["rnq"][:], sm["rnq"][:]), r=["rnq"], w=["rnq"])
            P.dve(lambda e: e.tensor_scalar_mul(sm["rnq"][:], sm["rnq"][:], 128 ** -0.5), r=["rnq"], w=["rnq"])
            P.pool(lambda e: e.tensor_mul(y2[:], k_, k_), r=["acc", "ssq"], w=["y2"])
            P.dve(lambda e: e.tensor_reduce(sm["ssq"][:], h3(y2[:]), AX.X, ALU.add), r=["y2", "rnq"], w=["ssq"])
            P.dve(lambda e: e.tensor_scalar_add(sm["rnk"][:], sm["ssq"][:], 1e-6), r=["ssq"], w=["rnk"])
            P.act(lambda e: e.activation(sm["rnk"][:], sm["rnk"][:], AF.Sqrt), r=["rnk"], w=["rnk"])
            P.dve(lambda e: e.reciprocal(sm["rnk"][:], sm["rnk"][:]), r=["rnk"], w=["rnk"])
            kg = ("gdgeo", tag)
            P.dve(lambda e: e.tensor_mul(h3(qn[:]), h3(q_), b3(sm["rnq"][:])), r=["acc", "rnq"], w=[kg])
            P.dve(lambda e: e.tensor_mul(h3(kn[:]), h3(k_), b3(sm["rnk"][:])), r=["acc", "rnk"], w=[kg])
            P.act(lambda e: e.activation(sm["beta"][:], zab[:, 512:516], AF.Sigmoid), r=["zab"], w=["beta"])
            P.dve(lambda e: e.tensor_scalar_mul(sm["nbeta"][:], sm["beta"][:], -1.0), r=["beta"], w=["nbeta"])
            P.dve(lambda e: e.tensor_add(sm["sp"][:], zab[:, 516:520], dtb_b[:]), r=["zab", "gd_dtb"], w=["sp"])
            P.act(lambda e: e.activation(sm["tmp"][:], sm["sp"][:], AF.Abs), r=["sp"], w=["tmp"])
            P.act(lambda e: e.activation(sm["tmp"][:], sm["tmp"][:], AF.Exp, scale=-1.0), r=["tmp"], w=["tmp"])
            P.act(lambda e: e.activation(sm["tmp"][:], sm["tmp"][:], AF.Ln, bias=1.0), r=["tmp"], w=["tmp"])
            P.dve(lambda e: e.tensor_scalar_max(sm["sp"][:], sm["sp"][:], 0.0), r=["sp"], w=["sp"])
            P.dve(lambda e: e.tensor_add(sm["sp"][:], sm["sp"][:], sm["tmp"][:]), r=["sp", "tmp"], w=["sp"])
            P.dve(lambda e: e.scalar_tensor_tensor(sm["g"][:], sm["sp"][:], -1.0, A_b[:], ALU.mult, ALU.mult), r=["sp", "gd_A"], w=["g"])
            P.pe(lambda e: e.matmul(psb[7][:, 0:4], c["MI"][:], sm["g"][:], start=True, stop=True), r=["dplrmask", "g"], w=[("ps", 7)])
            P.pe(lambda e: e.matmul(psb[7][:, 4:8], c["BLK"][:], sm["g"][:], start=True, stop=True), r=["dplrmask", "g"], w=[("ps", 7)])
            P.act(lambda e: e.copy(sm["gcs"][:], psb[7][:, 0:4]), r=[("ps", 7)], w=["gcs"])
            P.dve(lambda e: e.tensor_sub(sm["edec"][:], psb[7][:, 4:8], sm["gcs"][:]), r=[("ps", 7), "gcs"], w=["edec"])
            P.act(lambda e: e.activation(sm["edec"][:], sm["edec"][:], AF.Exp), r=["edec"], w=["edec"])
            P.act(lambda e: e.activation(sm["egc"][:], sm["gcs"][:], AF.Exp), r=["gcs"], w=["egc"])
            P.pe(lambda e: e.matmul(psb[3][:, 0:4], ones[0:64, :], sm["g"][0:64, :], start=True, stop=True), r=["ones", "g"], w=[("ps", 3)])
            P.pe(lambda e: e.matmul(psb[4][:, 0:4], ones[64:128, :], sm["g"][64:128, :], start=True, stop=True), r=["ones", "g"], w=[("ps", 4)])
            P.act(lambda e: e.activation(gC[0][:], psb[3][:, 0:4], AF.Exp), r=[("ps", 3)], w=[kg])
            P.act(lambda e: e.activation(gC[1][:], psb[4][:, 0:4], AF.Exp), r=[("ps", 4)], w=[kg])
            P.dve(lambda e: e.tensor_mul(h3(tr[:]), h3(qn[:]), b3(sm["egc"][:])), r=[kg, "egc"], w=["tr"])
            P.dve(lambda e: e.tensor_mul(sm["tmp2"][:], sm["nbeta"][:], sm["egc"][:]), r=["nbeta", "egc"], w=["tmp2"])
            P.dve(lambda e: e.tensor_mul(h3(ta[:]), h3(kn[:]), b3(sm["tmp2"][:])), r=[kg, "tmp2"], w=["ta"])
            P.dve(lambda e: e.tensor_mul(h3(Kd[:]), h3(kn[:]), b3(sm["edec"][:])), r=[kg, "edec"], w=[kg])
            P.dve(lambda e: e.tensor_mul(h3(Vp[:]), h3(v_), b3(sm["beta"][:])), r=["acc", "beta"], w=[kg])
            for (src, ksrc, dst) in ((kn, kg, kT), (qn, kg, qT), (ta, "ta", AT), (tr, "tr", RT)):
                for h in range(4):
                    P.pe(lambda e, src=src, h=h: e.transpose(pbf(7)[:, h * 128:(h + 1) * 128], src[:, h * 128:(h + 1) * 128], c["identb"][:]),
                         r=[ksrc, "identb"], w=[("ps", 7)])
                P.act(lambda e, dst=dst: e.copy(dst[:], pbf(7)[:, 0:512].rearrange("p (h t) -> p h t", h=4)), r=[("ps", 7)], w=[kg])
            for h in range(4):
                P.dve(lambda e, h=h: e.tensor_scalar_mul(diag[:, h, :], c["ident"][:], sm["gcs"][:, h:h + 1]), r=["gcs", "ident"], w=["diag"])
            P.pe(lambda e: e.matmul(psb[5][:], ones[:], diag[:].rearrange("p h t -> p (h t)"), start=True, stop=False), r=["ones", "diag"], w=[("ps", 5)])
            P.pe(lambda e: e.matmul(psb[5][:], c["ident"][:], BIGM[:].rearrange("p h t -> p (h t)"), start=False, stop=True), r=["ident", "BIGM"], w=[("ps", 5)])
            for h in range(4):
                P.act(lambda e, h=h: e.activation(D4[:, h, :], psb[5][:, h * 128:(h + 1) * 128], AF.Exp, bias=sm["gcs"][:, h:h + 1], scale=-1.0),
                      r=[("ps", 5), "gcs"], w=["D4"])
            P.pool(lambda e: e.tensor_mul(DS4[:], D4[:], MZ4), r=["D4", "dplrmask"], w=["DS4"])
            for h in range(4):
                P.pe(lambda e, h=h: e.matmul(psb[3][:, h * 128:(h + 1) * 128], kT[:, h, :], kT[:, h, :], start=True, stop=True), r=[kg], w=[("ps", 3)])
            for h in range(4):
                P.pe(lambda e, h=h: e.matmul(psb[4][:, h * 128:(h + 1) * 128], qT[:, h, :], kT[:, h, :], start=True, stop=True), r=[kg], w=[("ps", 4)])
            for h in range(4):
                P.dve(lambda e, h=h: e.scalar_tensor_tensor(Zt[:, h, :], psb[3][:, h * 128:(h + 1) * 128], sm["nbeta"][:, h:h + 1], DS4[:, h, :],
                                                          ALU.mult, ALU.mult), r=[("ps", 3), "nbeta", "DS4"], w=["Zt"])
            P.dve(lambda e: e.tensor_tensor(Art[:], psb[4][:].rearrange("p (h t) -> p h t", h=4), D4[:], ALU.mult), r=[("ps", 4), "D4"], w=["Art"])
            kX0, kZ0 = ("dX", tag, 0), ("dZ", tag, 0)
            P.pool(lambda e: e.tensor_copy(st["Z"][0][:], Zt[:]), r=["Zt"], w=[kZ0])
            for h in range(4):
                P.pe(lambda e, h=h: e.transpose(pbf(7)[:, h * 128:(h + 1) * 128], Zt[:, h, :], c["identb"][:]), r=["Zt", "identb"], w=[("ps", 7)])
            P.act(lambda e: e.copy(XK[:], pbf(7)[:, 0:512].rearrange("p (h t) -> p h t", h=4)), r=[("ps", 7)], w=[kg])
            P.dve(lambda e: e.tensor_copy(st["X"][0][:], pbf(7)[:, 0:512].rearrange("p (h t) -> p h t", h=4)), r=[("ps", 7)], w=[kX0])
            for h in range(4):
                P.pe(lambda e, h=h: e.transpose(pbf(7)[:, h * 128:(h + 1) * 128], Art[:, h, :], c["identb"][:]), r=["Art", "identb"], w=[("ps", 7)])
            P.act(lambda e: e.copy(ArT[:], pbf(7)[:, 0:512].rearrange("p (h t) -> p h t", h=4)), r=[("ps", 7)], w=[kg])
            geo = dict(
                AT=lambda h: (AT[:, h, :], [kg]), RT=lambda h: (RT[:, h, :], [kg]),
                ARB=lambda h: (ArT[:, h, :], [kg]), ARK=lambda h: (ArT[:, h, :], [kg]), AAK=lambda h: (XK[:, h, :], [kg]),
                V=lambda h: (Vp[:, h * 128:(h + 1) * 128], [kg]),
                BD=(Kd, [kg]), KD=(Kd, [kg]), GC=lambda ch: (gC[ch], [kg]), Y=(yt, kyt), WADD=None)
            P.act(lambda e: e.activation(sz[:], zab[:, 0:512], AF.Silu), r=["zab"], w=[ksz])
            return geo

        def tile_post(b, tt):
            pb = PB[b]
            yt, sz = pb["yt"], pb["sz"]
            kyt, ksz = ("yt", b), ("sz", b)
            r0 = b * S + tt * 128
            P.pool(lambda e: e.tensor_mul(y2[:], yt[:], yt[:]), r=[kyt], w=["y2"])
            P.dve(lambda e: e.tensor_reduce(sm["ssqo"][:], h3(y2[:]), AX.X, ALU.add), r=["y2"], w=["ssqo"])
            rstd_from_ssq(P, sm["ssqo"][:], sm["ssqo"][:], 128, RMS_EPS, ["ssqo"], ["ssqo"])
            P.dve(lambda e: e.tensor_mul(h3(yt[:]), h3(yt[:]), b3(sm["ssqo"][:])), r=[kyt, "ssqo"], w=[kyt])
            P.pool(lambda e: e.tensor_mul(yt[:], yt[:], ng_b[:].rearrange("p h d -> p (h d)")), r=[kyt, "gd_ng"], w=[kyt])
            P.dve(lambda e: e.tensor_mul(y2[:], yt[:], sz[:]), r=[kyt, ksz, "ssqo"], w=["y2"])
            P.dma(mixed[r0:r0 + 128, 1536:2048], y2[:], r=["y2"], q="pool")
    for tt in range(nt):
        geos = [tile_prep(b, tt) for b in range(NB)]
        run_interleaved([dplr_tile(cx, 4, 128, geos[b], PB[b]["st"], "gd%d" % b, banks=(3 * (b % 2), 3 * (b % 2) + 1, 3 * (b % 2) + 2))
                         for b in range(NB)])
        for b in range(NB):
            tile_post(b, tt)
    P.barrier()
    sb.reset(m)


MOE_PHASES = "123"
MOE_CAST = "ad"
MOE_CAP = 384


def stage_moe(cx, h1, prm, x_out, xs_d, ys_d):
    nc, P, sb, c, psb = cx.nc, cx.P, cx.sb, cx.c, cx.ps
    T = cx.T
    ntt = T // 128
    CAP = MOE_CAP
    NROW = 32 * CAP
    BIG = 1.0e4
    m = sb.mark()
    route_i = sb.alloc([128, ntt, 2], I32, "route_i")
    route_g = sb.alloc([128, ntt, 2], F32, "route_g")
    m2 = sb.mark()
    Wr = sb.alloc([128, 16, 36], F32, "Wr")
    P.dma(Wr[:, :, 0:4], prm["w_grp"].rearrange("(kc p) n -> p kc n", p=128), w=["Wr"], slow=True)
    P.dma(Wr[:, :, 4:36], prm["w_exp"].rearrange("(kc p) n -> p kc n", p=128), w=["Wr"], slow=True)
    br_b = sb.alloc([128, 36], F32, "br_b")
    bcast_load(cx, br_b[:, 0:4], prm["b_grp"], "br")
    bcast_load(cx, br_b[:, 4:36], prm["b_exp"], "br")
    SUb, eoff, trash = c["SUb"], c["eoff"], c["trash"]
    onesb = sb.alloc([128, 128], BF16, "onesb")
    P.pool(lambda e: e.memset(onesb[:], 1.0), w=["onesb"])
    zf = sb.alloc([128, 2048], F32, "zf")
    P.pool(lambda e: e.memset(zf[:], 0.0), w=["zf"])
    P.dma(ys_d[NROW:NROW + 128, :], zf[:], r=["zf"], w=["ys_d0"])
    carry = sb.alloc([128, 32], F32, "carry")
    P.pool(lambda e: e.memset(carry[:], 0.0), w=["carry"])
    hrow = [sb.alloc([128, 2048], F32, "hrow") for _ in range(2)]
    hbf = [sb.alloc([128, 2048], BF16, "hbf") for _ in range(2)]
    hT = [sb.alloc([128, 16, 128], F32, "hT") for _ in range(2)]
    f = lambda n, w: sb.alloc([128, w], F32, n)
    lg, mg, eg, sg_, pg, gsel, tmp4 = f("lg", 36), f("mg", 1), f("eg", 4), f("sg", 1), f("pg", 1), f("gsel", 4), f("tmp4", 4)
    lem, top8, sel1, sel2, selb_f, pos, tmp32 = f("lem", 32), f("top8", 8), f("sel1", 32), f("sel2", 32), f("selsum", 32), f("pos", 32), f("tmp32", 32)
    selb = sb.alloc([128, 32], BF16, "selb")
    dd, w1, w2, slotf, valid = f("dd", 1), f("w1", 1), f("w2", 1), f("slotf", 2), f("valid", 2)
    zt = sb.alloc([128, 2048], BF16, "zt")
    P.pool(lambda e: e.memset(zt[:], 0.0), w=["zt"])
    rpp = NROW // 128
    for z0 in range(0, rpp, 16):
        P.dma(xs_d.rearrange("(p r) d -> p r d", p=128)[:, z0:z0 + 16, :], zt[:].unsqueeze(1).to_broadcast([128, 16, 2048]), r=["zt"], w=["xs_d"])
    nev = 0
    for tt in range(ntt):
        i2 = tt % 2
        r0 = tt * 128
        kh, khb, khT = ("hrow", i2), ("hbf", i2), ("hT", i2)
        P.dma(hrow[i2][:], h1[r0:r0 + 128, :], w=[kh])
        P.act(lambda e, i2=i2: e.copy(hbf[i2][:], hrow[i2][:]), r=[kh], w=[khb])
        for g in range(4):
            pi = nev % 2
            nev += 1
            for j in range(4):
                kc = g * 4 + j
                P.pe(lambda e, pi=pi, kc=kc, j=j, i2=i2: e.transpose(psb[pi][:, j * 128:(j + 1) * 128], hrow[i2][:, kc * 128:(kc + 1) * 128], c["ident"][:]),
                     r=[kh, "ident"], w=[("ps", pi)])
            evac(P, nev, hT[i2][:, g * 4:(g + 1) * 4, :], psb[pi][:].rearrange("p (j t) -> p j t", j=4), r=[("ps", pi)], w=[khT])
        for kc in range(16):
            P.pe(lambda e, kc=kc, i2=i2: e.matmul(psb[2][:, 0:36], hT[i2][:, kc, :], Wr[:, kc, :], start=(kc == 0), stop=(kc == 15)),
                 r=[khT, "Wr"], w=[("ps", 2)])
        K = "rt"
        P.dve(lambda e: e.tensor_add(lg[:], psb[2][:, 0:36], br_b[:]), r=[("ps", 2), "br"], w=[K])
        P.dve(lambda e: e.tensor_reduce(mg[:], lg[:, 0:4], AX.X, ALU.max), r=[K], w=[K])
        P.dve(lambda e: e.tensor_scalar(gsel[:], lg[:, 0:4], mg[:, 0:1], None, ALU.is_equal), r=[K], w=[K])
        P.dve(lambda e: e.tensor_scalar_mul(tmp4[:, 0:1], mg[:], -1.0), r=[K], w=[K])
        P.act(lambda e: e.activation(eg[:], lg[:, 0:4], AF.Exp, bias=tmp4[:, 0:1], accum_out=sg_[:]), r=[K], w=[K])
        P.dve(lambda e: e.reciprocal(pg[:], sg_[:]), r=[K], w=[K])
        P.dve(lambda e: e.tensor_scalar(tmp4[:], gsel[:], BIG, -BIG, ALU.mult, ALU.add), r=[K], w=[K])
        P.dve(lambda e: e.tensor_add(lem[:].rearrange("p (g x) -> p g x", g=4), lg[:, 4:36].rearrange("p (g x) -> p g x", g=4),
                                    tmp4[:].unsqueeze(2).to_broadcast([128, 4, 8])), r=[K], w=[K])
        P.dve(lambda e: e.max(top8[:], lem[:]), r=[K], w=[K])
        P.dve(lambda e: e.tensor_scalar(sel1[:], lem[:], top8[:, 0:1], None, ALU.is_equal), r=[K], w=[K])
        P.dve(lambda e: e.tensor_scalar(sel2[:], lem[:], top8[:, 1:2], None, ALU.is_equal), r=[K], w=[K])
        P.dve(lambda e: e.tensor_sub(dd[:], top8[:, 1:2], top8[:, 0:1]), r=[K], w=[K])
        P.act(lambda e: e.activation(dd[:], dd[:], AF.Exp), r=[K], w=[K])
        P.dve(lambda e: e.tensor_scalar_add(w1[:], dd[:], 1.0), r=[K], w=[K])
        P.dve(lambda e: e.reciprocal(w1[:], w1[:]), r=[K], w=[K])
        P.dve(lambda e: e.tensor_mul(w2[:], w1[:], dd[:]), r=[K], w=[K])
        P.dve(lambda e: e.tensor_add(selb_f[:], sel1[:], sel2[:]), r=[K], w=[K])
        P.dve(lambda e: e.tensor_copy(selb[:], selb_f[:]), r=[K], w=["selb"])
        P.pe(lambda e: e.matmul(psb[3][:, 0:32], SUb[:], selb[:], start=True, stop=True), r=["SUb", "selb"], w=[("ps", 3)])
        P.pe(lambda e: e.matmul(psb[3][:, 32:64], onesb[:], selb[:], start=True, stop=True), r=["onesb", "selb"], w=[("ps", 3)])
        P.dve(lambda e: e.tensor_add(pos[:], psb[3][:, 0:32], carry[:]), r=[("ps", 3), "carry"], w=[K])
        P.dve(lambda e: e.tensor_add(carry[:], carry[:], psb[3][:, 32:64]), r=[("ps", 3), "carry"], w=["carry"])
        for k_, selk in enumerate((sel1, sel2)):
            P.dve(lambda e, selk=selk: e.tensor_mul(tmp32[:], selk[:], pos[:]), r=[K], w=[K])
            P.dve(lambda e, k_=k_: e.tensor_reduce(slotf[:, k_:k_ + 1], tmp32[:], AX.X, ALU.add), r=[K], w=[K])
            P.dve(lambda e, k_=k_: e.tensor_scalar(valid[:, k_:k_ + 1], slotf[:, k_:k_ + 1], float(CAP) - 0.5, None, ALU.is_lt), r=[K], w=[K])
            P.dve(lambda e, selk=selk: e.tensor_mul(tmp32[:], selk[:], eoff[:]), r=[K, "eoff"], w=[K])
            P.dve(lambda e: e.tensor_reduce(dd[:], tmp32[:], AX.X, ALU.add), r=[K], w=[K])
            P.dve(lambda e, k_=k_: e.tensor_add(slotf[:, k_:k_ + 1], slotf[:, k_:k_ + 1], dd[:]), r=[K], w=[K])
            P.dve(lambda e, k_=k_: e.tensor_sub(slotf[:, k_:k_ + 1], slotf[:, k_:k_ + 1], trash[:]), r=[K, "trash"], w=[K])
            P.dve(lambda e, k_=k_: e.tensor_mul(slotf[:, k_:k_ + 1], slotf[:, k_:k_ + 1], valid[:, k_:k_ + 1]), r=[K], w=[K])
            P.dve(lambda e, k_=k_: e.tensor_add(slotf[:, k_:k_ + 1], slotf[:, k_:k_ + 1], trash[:]), r=[K, "trash"], w=[K])
            wk = w1 if k_ == 0 else w2
            P.dve(lambda e, k_=k_, wk=wk, tt=tt: e.scalar_tensor_tensor(route_g[:, tt, k_:k_ + 1], wk[:], pg[:, 0:1], valid[:, k_:k_ + 1], ALU.mult, ALU.mult),
                  r=[K], w=[("route", tt)])
        P.dve(lambda e, tt=tt: e.tensor_copy(route_i[:, tt, :], slotf[:]), r=[K], w=[("route", tt)])
        for k_ in range(2):
            P.add("pool", lambda e, tt=tt, k_=k_, i2=i2: e.indirect_dma_start(
                out=xs_d[:, :], out_offset=bass.IndirectOffsetOnAxis(ap=route_i[:, tt, k_:k_ + 1], axis=0),
                in_=hbf[i2][:, :], in_offset=None), r=[khb, ("route", tt)], w=["xs_d"], sw=True)
    P.barrier()
    sb.reset(m2)
    stg = [sb.alloc([128, 8, 512], F32, "stg") for _ in range(3)]
    wg = [sb.alloc([128, 16, 512], BF16, "wg") for _ in range(2)]
    wu = [sb.alloc([128, 16, 512], BF16, "wu") for _ in range(2)]
    wd = [sb.alloc([128, 4, 2048], BF16, "wd") for _ in range(2)]
    xrow = [sb.alloc([128, 2048], BF16, "xrowb") for _ in range(2)]
    xsT = sb.alloc([128, 16, CAP], BF16, "xsT")
    HT = sb.alloc([128, 4, CAP], BF16, "HT")
    sgt = [sb.alloc([128, CAP], F32, "sgt") for _ in range(2)]
    yst = [sb.alloc([128, 2048], F32, "yst") for _ in range(2)]
    nst = 0
    ncast = 0
    nps = 0
    ntl = CAP // 128
    pbf = lambda i: psb[i][:].bitcast(BF16)

    def cast(dst, src, r, w):
        nonlocal ncast
        i = {"ad": (ncast % 2) + 1, "d": 2, "dda": (2, 2, 1)[ncast % 3], "pad": ncast % 3}[MOE_CAST]
        ncast += 1
        if i == 0:
            P.pool(lambda e: e.tensor_copy(dst, src), r=r, w=w)
        elif i == 1:
            P.act(lambda e: e.copy(dst, src), r=r, w=w)
        else:
            P.dve(lambda e: e.tensor_copy(dst, src), r=r, w=w)

    for ex in range(32 if "2" in MOE_PHASES else 0):
        e2 = ex % 2
        kw = ("wexp", e2)
        for (dst, src4) in ((wg[e2], prm["w_gate"][ex].rearrange("(kc p) n -> p kc n", p=128)),
                            (wu[e2], prm["w_up"][ex].rearrange("(kc p) n -> p kc n", p=128))):
            for hf in range(2):
                s_, ks = stg[nst % 3], ("stg", nst % 3)
                nst += 1
                P.dma(s_[:], src4[:, hf * 8:(hf + 1) * 8, :], w=[ks])
                cast(dst[:, hf * 8:(hf + 1) * 8, :], s_[:], [ks], [kw])
        wdv = prm["w_down"][ex].rearrange("(fc p) n -> p fc n", p=128)
        for hf in range(2):
            s_, ks = stg[nst % 3], ("stg", nst % 3)
            nst += 1
            sv = s_[:].rearrange("p a b -> p (a b)").rearrange("p (f n) -> p f n", f=2)
            P.dma(sv, wdv[:, hf * 2:(hf + 1) * 2, :], w=[ks])
            cast(wd[e2][:, hf * 2:(hf + 1) * 2, :], sv, [ks], [kw])
        for tl in range(ntl):
            xr, kxr = xrow[tl % 2], ("xrowb", tl % 2)
            P.dma(xr[:], xs_d[ex * CAP + tl * 128: ex * CAP + (tl + 1) * 128, :], r=["xs_d"], w=[kxr])
            for g in range(2):
                pi = nps % 2
                nps += 1
                for j in range(8):
                    kc = g * 8 + j
                    P.pe(lambda e, pi=pi, xr=xr, kc=kc, j=j: e.transpose(pbf(pi)[:, j * 128:(j + 1) * 128], xr[:, kc * 128:(kc + 1) * 128], c["identb"][:]),
                         r=[kxr, "identb"], w=[("ps", pi)])
                evac(P, nps, xsT[:, g * 8:(g + 1) * 8, tl * 128:(tl + 1) * 128], pbf(pi).rearrange("p (j t) -> p j t", j=8), r=[("ps", pi)], w=["xsT"])
        for fc in range(4):
            pg_, pu_ = 2 + (fc % 2) * 2, 3 + (fc % 2) * 2
            for kc in range(16):
                P.pe(lambda e, kc=kc, fc=fc, pg_=pg_, e2=e2: e.matmul(psb[pg_][:, 0:CAP], wg[e2][:, kc, fc * 128:(fc + 1) * 128], xsT[:, kc, :],
                                                                   start=(kc == 0), stop=(kc == 15)), r=[kw, "xsT"], w=[("ps", pg_)])
            for kc in range(16):
                P.pe(lambda e, kc=kc, fc=fc, pu_=pu_, e2=e2: e.matmul(psb[pu_][:, 0:CAP], wu[e2][:, kc, fc * 128:(fc + 1) * 128], xsT[:, kc, :],
                                                                   start=(kc == 0), stop=(kc == 15)), r=[kw, "xsT"], w=[("ps", pu_)])
            sg2, ksg = sgt[fc % 2], ("sgt", fc % 2)
            P.act(lambda e, sg2=sg2, pg_=pg_: e.activation(sg2[:], psb[pg_][:, 0:CAP], AF.Silu), r=[("ps", pg_)], w=[ksg])
            P.dve(lambda e, sg2=sg2, pu_=pu_, fc=fc: e.tensor_tensor(HT[:, fc, :], sg2[:], psb[pu_][:, 0:CAP], ALU.mult), r=[ksg, ("ps", pu_)], w=["HT"])
        for tl in range(ntl):
            ys, kys = yst[tl % 2], ("yst", tl % 2)
            for db in range(4):
                pi = 6 + nps % 2
                nps += 1
                for fc in range(4):
                    P.pe(lambda e, pi=pi, fc=fc, tl=tl, db=db, e2=e2: e.matmul(psb[pi][:], HT[:, fc, tl * 128:(tl + 1) * 128], wd[e2][:, fc, db * 512:(db + 1) * 512],
                                                                            start=(fc == 0), stop=(fc == 3)), r=["HT", kw], w=[("ps", pi)])
                evac(P, nps, ys[:, db * 512:(db + 1) * 512], psb[pi][:], r=[("ps", pi)], w=[kys])
            P.dma(ys_d[ex * CAP + tl * 128: ex * CAP + (tl + 1) * 128, :], ys[:], r=[kys], w=["ys_d"], q="pool")
    P.barrier()
    sb.reset(m2)
    y1 = [sb.alloc([128, 2048], F32, "y1") for _ in range(2)]
    y2 = [sb.alloc([128, 2048], F32, "y2") for _ in range(2)]
    hr = [sb.alloc([128, 2048], F32, "hr") for _ in range(2)]
    pre = sb.alloc([128, 2048], F32, "pre")
    g_b = sb.alloc([128, 2048], F32, "g_b")
    b_b = sb.alloc([128, 2048], F32, "b_b")
    stats = sb.alloc([128, 4, 6], F32, "stats")
    mv = sb.alloc([128, 2], F32, "mv")
    rstd = sb.alloc([128, 1], F32, "rstd")
    bcast_load(cx, g_b[:], prm["ln_g"], "lnp")
    bcast_load(cx, b_b[:], prm["ln_b"], "lnp")
    for i2 in range(2):
        P.pool(lambda e, i2=i2: e.memset(y1[i2][:], 0.0), w=[("y1", i2)])
        P.pool(lambda e, i2=i2: e.memset(y2[i2][:], 0.0), w=[("y2", i2)])
    for tt in range(ntt if "3" in MOE_PHASES else 0):
        i2 = tt % 2
        r0 = tt * 128
        for k_, yy in enumerate((y1, y2)):
            ky = ("y1" if k_ == 0 else "y2", i2)
            P.add("pool", lambda e, tt=tt, k_=k_, yy=yy, i2=i2: e.indirect_dma_start(
                out=yy[i2][:, :], out_offset=None, in_=ys_d[:, :],
                in_offset=bass.IndirectOffsetOnAxis(ap=route_i[:, tt, k_:k_ + 1], axis=0)),
                r=["ys_d", ("route", tt)], w=[ky], sw=True)
        khr = ("hr", i2)
        P.dma(hr[i2][:], h1[r0:r0 + 128, :], w=[khr])
        P.dve(lambda e, i2=i2, tt=tt: e.tensor_scalar_mul(y1[i2][:], y1[i2][:], route_g[:, tt, 0:1]), r=[("y1", i2), ("route", tt)], w=[("y1", i2)])
        P.dve(lambda e, i2=i2, tt=tt: e.scalar_tensor_tensor(y1[i2][:], y2[i2][:], route_g[:, tt, 1:2], y1[i2][:], ALU.mult, ALU.add),
              r=[("y1", i2), ("y2", i2), ("route", tt)], w=[("y1", i2)])
        P.dve(lambda e, i2=i2: e.scalar_tensor_tensor(pre[:], hr[i2][:], ALPHA, y1[i2][:], ALU.mult, ALU.add), r=[khr, ("y1", i2)], w=["pre"])
        layer_norm_tile(cx, pre[:], hr[i2][:], g_b[:], b_b[:], stats, mv, rstd, "pre", khr, "lnst", aff="dve")
        P.dma(x_out[r0:r0 + 128, :], hr[i2][:], r=[khr], q="pool")
    P.barrier()
    sb.reset(m)


PARAM_SHAPES = {
    "w_in": [2048, N_IN], "fox_b_f": [8], "fox_out_g": [512], "mla_q_norm_g": [384], "mla_kv_norm_g": [128],
    "mla_w_uq": [384, 768], "mla_w_ukv": [128, 1024], "mla_out_g": [512], "rwkv_mu": [1792], "rwkv_w0": [512],
    "rwkv_w2": [64, 512], "rwkv_a0": [512], "rwkv_a2": [64, 512], "rwkv_g2": [128, 512], "rwkv_k_k": [512],
    "rwkv_k_a": [512], "rwkv_r_k": [512], "rwkv_ln_g": [512], "rwkv_ln_b": [512], "gdn_conv_w": [4, 1536],
    "gdn_a_log": [4], "gdn_dt_bias": [4], "gdn_norm_g": [128], "w_out": [2048, 2048], "ln1_g": [2048], "ln1_b": [2048],
    "moe_w_grp": [2048, 4], "moe_b_grp": [4], "moe_w_exp": [2048, 32], "moe_b_exp": [32], "moe_w_gate": [32, 2048, 512],
    "moe_w_up": [32, 2048, 512], "moe_w_down": [32, 512, 2048], "ln2_g": [2048], "ln2_b": [2048],
}


def build_program(S, NB, depth):
    nc = bass.Bass("TRN2", target_bir_lowering=False)
    cx = Ctx(nc, S, NB)
    T = cx.T
    x_in = cx.dt("x", [T, 2048], F32, "ExternalInput")
    pos = cx.dt("positions", [NB, S], I32, "ExternalInput")
    ifr = cx.dt("inv_freq", [32], F32, "ExternalInput")
    W = {k: cx.dt(k, [depth] + v, F32, "ExternalInput") for k, v in PARAM_SHAPES.items()}
    y = cx.dt("y", [T, 2048], F32, "ExternalOutput")
    u_tok = cx.dt("u_tok", [T, N_IN], F32)
    qkT = cx.dt("qkT", [1024, T], BF16)
    fT = cx.dt("fT", [8, T], F32)
    mixed = cx.dt("mixed", [T, 2048], F32)
    h1 = cx.dt("h1", [T, 2048], F32)
    xs_d = cx.dt("xs_d", [32 * MOE_CAP + 128, 2048], BF16)
    ys_d = cx.dt("ys_d", [32 * MOE_CAP + 128, 2048], F32)
    xbuf = [cx.dt("xres%d" % i, [T, 2048], F32) for i in range(2)]
    make_consts(cx)
    cur = x_in
    for l in range(depth):
        nxt = y if l == depth - 1 else xbuf[l % 2]
        stage_inproj(cx, cur, W["w_in"][l], u_tok, qkT, fT)
        stage_fox(cx, qkT, fT, u_tok, W["fox_b_f"][l], W["fox_out_g"][l], mixed)
        stage_mla(cx, u_tok, pos, ifr, W["mla_q_norm_g"][l], W["mla_kv_norm_g"][l], W["mla_w_uq"][l], W["mla_w_ukv"][l],
                  W["mla_out_g"][l], mixed)
        stage_rwkv(cx, u_tok, {k: W["rwkv_" + k][l] for k in ("mu", "w0", "w2", "a0", "a2", "g2", "k_k", "k_a", "r_k", "ln_g", "ln_b")}, mixed)
        stage_gdn(cx, u_tok, {k: W["gdn_" + k][l] for k in ("conv_w", "a_log", "dt_bias", "norm_g")}, mixed)
        stage_outproj_ln(cx, mixed, W["w_out"][l], cur, W["ln1_g"][l], W["ln1_b"][l], h1)
        prm = {"w_grp": W["moe_w_grp"][l], "b_grp": W["moe_b_grp"][l], "w_exp": W["moe_w_exp"][l], "b_exp": W["moe_b_exp"][l],
               "w_gate": W["moe_w_gate"][l], "w_up": W["moe_w_up"][l], "w_down": W["moe_w_down"][l], "ln_g": W["ln2_g"][l], "ln_b": W["ln2_b"][l]}
        stage_moe(cx, h1, prm, nxt, xs_d, ys_d)
        cur = nxt
    cx.P.emit()
    return nc, cx


def kernel(**inputs):
    n_cores = 8
    x = np.ascontiguousarray(np.asarray(inputs["x"], dtype=np.float32))
    B, S, Dm = x.shape
    NB = B // n_cores
    depth = int(np.asarray(inputs["w_in"]).shape[0])
    nc, _ = build_program(S, NB, depth)
    positions = np.ascontiguousarray(np.asarray(inputs["positions"], dtype=np.int32))
    inv_freq = (np.float32(10000.0) ** (-np.arange(32, dtype=np.float32) / np.float32(32))).astype(np.float32)
    shared = {k: np.ascontiguousarray(np.asarray(inputs[k], dtype=np.float32)) for k in PARAM_SHAPES}
    in_maps = []
    for c in range(n_cores):
        d = dict(shared)
        d["x"] = x[c * NB:(c + 1) * NB].reshape(NB * S, Dm)
        d["positions"] = positions[c * NB:(c + 1) * NB]
        d["inv_freq"] = inv_freq
        in_maps.append(d)
    res = run_bass_kernel_spmd(nc, in_maps, core_ids=list(range(n_cores)))
    out = np.concatenate([np.asarray(r["y"], dtype=np.float32).reshape(NB, S, Dm) for r in res.results], axis=0)
    return out
```

```python
import numpy as np
import concourse.bass as bass
import concourse.mybir as mybir
from concourse.bass_utils import run_bass_kernel_spmd

F32 = mybir.dt.float32
BF16 = mybir.dt.bfloat16
I32 = mybir.dt.int32
U32 = mybir.dt.uint32
AF = mybir.ActivationFunctionType
ALU = mybir.AluOpType
AX = mybir.AxisListType

D = 2048
GW = 512
N_IN = 5968
C_FOX, C_MLA, C_RWKV, C_GDN = 0, 1544, 2120, 3912
ALPHA = (2 * 4) ** 0.25
LN_EPS = 1e-5
RMS_EPS = 1e-6
NEG = -30000.0


class _Op:
    __slots__ = ("stream", "fn", "deps", "sig", "ticket", "dma", "dma_idx", "idx", "sw")


class Prog:
    STREAMS = ("pe", "act", "dve", "pool", "sp")
    NDMA = 48

    def __init__(self, nc):
        self.nc = nc
        self.ops = []
        self.by_stream = {s: [] for s in self.STREAMS}
        self.last_w = {}
        self.readers = {}
        self.n_dma = 0
        self.dma_ops = []
        self.barrier_deps = {s: [] for s in self.STREAMS}

    POOLQ = "pool"
    NSW = 40

    def add(self, stream, fn, r=(), w=(), dma=False, sw=False):
        op = _Op()
        op.stream, op.fn, op.dma, op.sig, op.ticket = stream, fn, dma, False, 0
        op.sw = sw
        op.idx = len(self.ops)
        pk = [k for k in r if isinstance(k, tuple) and k and k[0] == "ps"]
        if pk:
            r = [k for k in r if not (isinstance(k, tuple) and k and k[0] == "ps")]
            w = list(w) + pk
        deps = set()
        for k in r:
            d = self.last_w.get(k)
            if d is not None:
                deps.add(d)
        for k in w:
            d = self.last_w.get(k)
            if d is not None:
                deps.add(d)
            for rd in self.readers.get(k, ()):
                deps.add(rd)
        for k in r:
            self.readers.setdefault(k, []).append(op.idx)
        for k in w:
            self.last_w[k] = op.idx
            self.readers[k] = []
        bd = self.barrier_deps[stream]
        if bd:
            deps.update(bd)
            self.barrier_deps[stream] = []
        if dma:
            op.dma_idx = self.n_dma
            self.n_dma += 1
            if op.dma_idx >= self.NDMA:
                deps.add(self.dma_ops[op.dma_idx - self.NDMA])
            self.dma_ops.append(op.idx)
        deps.discard(op.idx)
        fin = []
        for d in deps:
            o = self.ops[d]
            if o.stream == stream and not o.dma and stream == "pe":
                continue
            fin.append(d)
            if not o.dma:
                o.sig = True
        op.deps = fin
        self.ops.append(op)
        self.by_stream[stream].append(op)
        return op

    def barrier(self):
        deps = []
        for s in self.STREAMS:
            for o in reversed(self.by_stream[s]):
                if not o.dma:
                    deps.append(o.idx)
                    break
        deps.extend(self.dma_ops[-self.NDMA:])
        for s in self.STREAMS:
            self.barrier_deps[s] = list(deps)
        self.last_w = {}
        self.readers = {}

    def pe(self, fn, r=(), w=()):
        return self.add("pe", fn, r, w)

    def act(self, fn, r=(), w=()):
        return self.add("act", fn, r, w)

    def dve(self, fn, r=(), w=()):
        return self.add("dve", fn, r, w)

    def pool(self, fn, r=(), w=()):
        return self.add("pool", fn, r, w)

    def dma(self, out, in_, r=(), w=(), q="sp", slow=False):
        if q == "pool":
            q = self.POOLQ
        if slow:
            return self.add(q, lambda e: e.dma_start(out=out, in_=in_, allow_slow_non_contiguous=True), r, w, dma=True)
        return self.add(q, lambda e: e.dma_start(out=out, in_=in_), r, w, dma=True)

    def emit(self, final_wait=True):
        nc = self.nc
        for s in self.STREAMS:
            t = 0
            for o in self.by_stream[s]:
                if not o.dma and o.sig:
                    t += 1
                    o.ticket = t
        import contextlib
        with contextlib.ExitStack() as es:
            esem = {s: es.enter_context(nc.semaphore("e_" + s)) for s in self.STREAMS}
            dsem = [es.enter_context(nc.semaphore("d%d" % i)) for i in range(self.NDMA)]
            nsw = sum(1 for o in self.ops if o.sw)
            SWBASE = 215
            swsems = [es.enter_context(nc.semaphore("sw%d" % i, num=SWBASE + i)) for i in range(min(nsw, self.NSW))]
            swctr = [0, 1]
            block = es.enter_context(nc.Block())
            ops = self.ops
            NDMA = self.NDMA

            def completion(o):
                if o.dma:
                    return ("d", o.dma_idx % NDMA), dsem[o.dma_idx % NDMA], 16 * (o.dma_idx // NDMA + 1)
                return ("e", o.stream), esem[o.stream], o.ticket

            def run_stream(s, eng):
                waited = {}
                pending = []

                def flush():
                    for (sem_, val_, o_) in pending:
                        eng.wait_ge(sem_, val_)
                        if o_.sig:
                            eng.nop().then_inc(esem[s], 1)
                    del pending[:]

                for o in self.by_stream[s]:
                    need = {}
                    for d in o.deps:
                        key, sem, val = completion(ops[d])
                        if waited.get(key, 0) >= val:
                            continue
                        if key not in need or need[key][1] < val:
                            need[key] = (sem, val)
                    if pending and ((not o.sw) or need or len(pending) >= 2):
                        flush()
                    for key, (sem, val) in need.items():
                        eng.wait_ge(sem, val)
                        waited[key] = val
                    if o.sw and swctr[0] == len(swsems):
                        flush()
                        eng.dma_reset(range(SWBASE, SWBASE + len(swsems)))
                        swctr[0] = 0
                        swctr[1] += 1
                    inst = o.fn(eng)
                    if o.sw:
                        sw_ = swsems[swctr[0]]
                        swctr[0] += 1
                        inst.then_inc(sw_, 16)
                        pending.append((sw_, 16 * swctr[1], o))
                    elif o.dma:
                        inst.then_inc(dsem[o.dma_idx % NDMA], 16)
                    elif o.sig:
                        inst.then_inc(esem[s], 1)
                flush()
                if s == "sp" and final_wait:
                    for i in range(min(NDMA, self.n_dma)):
                        last = i + ((self.n_dma - 1 - i) // NDMA) * NDMA
                        val = 16 * (last // NDMA + 1)
                        if waited.get(("d", i), 0) < val:
                            eng.wait_ge(dsem[i], val)

            @block.tensor
            def _(e):
                run_stream("pe", e)

            @block.scalar
            def _(e):
                run_stream("act", e)

            @block.vector
            def _(e):
                run_stream("dve", e)

            @block.gpsimd
            def _(e):
                run_stream("pool", e)

            @block.sync
            def _(e):
                run_stream("sp", e)


class SB:
    def __init__(self, nc, base=16512, cap=229344):
        self.nc, self.base, self.cap, self.top = nc, base, cap, base
        self.n = 0

    def mark(self):
        return self.top

    def reset(self, m):
        self.top = m

    def alloc(self, shape, dtype, name="t"):
        esz = {F32: 4, BF16: 2, I32: 4, U32: 4}[dtype]
        per = esz
        for s in shape[1:]:
            per *= s
        per = (per + 31) // 32 * 32
        off = self.top
        assert off + per <= self.cap, "SBUF overflow %s %d+%d" % (name, off, per)
        self.top += per
        self.n += 1
        return self.nc.alloc_sbuf_tensor_at("%s_%d" % (name, self.n), list(shape), dtype, offset=off)


class Ctx:
    def __init__(self, nc, S, NB, io=None):
        self.nc = nc
        self.S = S
        self.NB = NB
        self.T = S * NB
        self.P = Prog(nc)
        self.sb = SB(nc)
        self.io = io or {}
        self.dram = {}
        self.uid = 0

    def dt(self, name, shape, dtype, kind=None):
        k = self.io.get(name, kind or "Internal")
        t = self.nc.dram_tensor(name, list(shape), dtype, kind=k).ap()
        self.dram[name] = t
        return t

    def key(self, base):
        self.uid += 1
        return (base, self.uid)


def make_consts(cx):
    nc, P, sb = cx.nc, cx.P, cx.sb
    c = {}
    ident = sb.alloc([128, 128], F32, "ident")
    P.pool(lambda e: e.memset(ident[:], 0.0), w=["ident"])
    P.pool(lambda e: e.affine_select(out=ident[:], in_=ident[:], pattern=[[-1, 128]],
                                     compare_op=ALU.not_equal, fill=1.0, base=0, channel_multiplier=1),
           r=["ident"], w=["ident"])
    identb = sb.alloc([128, 128], BF16, "identb")
    P.dve(lambda e: e.tensor_copy(identb[:], ident[:]), r=["ident"], w=["identb"])
    c["ident"], c["identb"] = ident, identb
    cx.ps = [nc.alloc_psum_tensor("psb%d" % i, [128, 512], F32) for i in range(8)]
    cx.c = c
    dplr_consts(cx)
    SU = sb.alloc([128, 128], F32, "SU")
    SUb = sb.alloc([128, 128], BF16, "SUb")
    P.pool(lambda e: e.memset(SU[:], 1.0), w=["SU"])
    P.pool(lambda e: e.affine_select(out=SU[:], in_=SU[:], pattern=[[1, 128]], compare_op=ALU.is_ge, fill=0.0, base=-1, channel_multiplier=-1),
           r=["SU"], w=["SU"])
    P.pool(lambda e: e.tensor_copy(SUb[:], SU[:]), r=["SU"], w=["SUb"])
    eoff = sb.alloc([128, 32], F32, "eoff")
    P.pool(lambda e: e.iota(eoff[:], pattern=[[MOE_CAP, 32]], base=0, channel_multiplier=0, allow_small_or_imprecise_dtypes=True), w=["eoff"])
    trash = sb.alloc([128, 1], F32, "trash")
    P.pool(lambda e: e.iota(trash[:], pattern=[[0, 1]], base=32 * MOE_CAP, channel_multiplier=1, allow_small_or_imprecise_dtypes=True), w=["trash"])
    c.update(SUb=SUb, eoff=eoff, trash=trash)
    c["masks_causal"] = build_masks(cx, "causal")
    c["masks_chunk64"] = build_masks(cx, "chunk64")
    P.barrier()
    return c


def evac(P, i, out, in_, r, w):
    if i % 2 == 0:
        P.act(lambda e: e.copy(out, in_), r, w)
    else:
        P.dve(lambda e: e.tensor_copy(out, in_), r, w)


def build_xT(cx, src_dram, b, xT, ps_banks, xrow_bufs, tag):
    P, c = cx.P, cx.c
    S = cx.S
    nt = S // 128
    for tt in range(nt):
        xr = xrow_bufs[tt % len(xrow_bufs)]
        kx = ("xrow", tag, tt % len(xrow_bufs))
        r0 = b * S + tt * 128
        P.dma(xr[:], src_dram[r0:r0 + 128, :], w=[kx])
        for g in range(4):
            ps = ps_banks[(tt * 4 + g) % len(ps_banks)]
            kp = ("psT", tag, (tt * 4 + g) % len(ps_banks))
            for j in range(4):
                kc = g * 4 + j
                P.pe(lambda e, ps=ps, xr=xr, kc=kc, j=j: e.transpose(ps[:, j * 128:(j + 1) * 128],
                                                                   xr[:, kc * 128:(kc + 1) * 128], c["ident"][:]),
                     r=[kx, "ident"], w=[kp])
            o = xT[:, g * 4:(g + 1) * 4, tt * 128:(tt + 1) * 128]
            i_ = ps[:].rearrange("p (j t) -> p j t", j=4)
            evac(P, tt * 4 + g, o, i_, r=[kp], w=[("xT", tag, tt)])


def stage_inproj(cx, x_dram, w_in, u_tok, qkT, fT):
    nc, P, sb, c = cx.nc, cx.P, cx.sb, cx.c
    S, NB = cx.S, cx.NB
    nt = S // 128
    m = sb.mark()
    xT = sb.alloc([128, 16, S], BF16, "xT")
    xrows = [sb.alloc([128, 2048], F32, "xrow") for _ in range(2)]
    wbf = [sb.alloc([128, 16, 512], BF16, "wbf") for _ in range(2)]
    ost = [sb.alloc([128, 512], F32, "ost") for _ in range(4)]
    obf = [sb.alloc([128, 512], BF16, "obf") for _ in range(2)]
    psb = cx.ps
    wv = w_in.rearrange("(kc p) n -> p kc n", p=128)
    blocks = [(c0, min(512, N_IN - c0)) for c0 in range(0, N_IN, 512)]
    nev = 0
    allb = [(b_, bi_) for b_ in range(NB) for bi_ in range(len(blocks))]

    def load_w(it):
        c0_, cw_ = blocks[it % len(blocks)]
        P.dma(wbf[it % 2][:, :, 0:cw_], wv[:, :, c0_:c0_ + cw_], w=[("wbf", it % 2)], q="pool")

    load_w(0)
    for b in range(NB):
        build_xT(cx, x_dram, b, xT, psb[0:4], xrows, "A%d" % b)
        xkeys = [("xT", "A%d" % b, tt) for tt in range(nt)]
        for bi, (c0, cw) in enumerate(blocks):
            it = b * len(blocks) + bi
            wb = wbf[it % 2]
            kwb = ("wbf", it % 2)
            if it + 1 < len(allb):
                load_w(it + 1)
            for tt in range(nt):
                if c0 in (0, 512):
                    break
                pi = 4 + (nev % 4)
                ps, kp = psb[pi], ("psA", pi)
                for kc in range(16):
                    P.pe(lambda e, ps=ps, kc=kc, tt=tt, wb=wb, cw=cw: e.matmul(
                        ps[:, 0:cw], xT[:, kc, tt * 128:(tt + 1) * 128], wb[:, kc, 0:cw],
                        start=(kc == 0), stop=(kc == 15)), r=[xkeys[tt], kwb], w=[kp])
                o, ko = ost[nev % 4], ("ost", nev % 4)
                evac(P, nev, o[:, 0:cw], ps[:, 0:cw], r=[kp], w=[ko])
                r0 = b * S + tt * 128
                P.dma(u_tok[r0:r0 + 128, c0:c0 + cw], o[:, 0:cw], r=[ko], q="pool")
                nev += 1
            fm = []
            if c0 in (0, 512):
                fm = [(c0 + j * 128, 128) for j in range(4)]
            elif c0 == 1536:
                fm = [(1536, 8)]
            for (f0, fw) in fm:
                for tb in range(S // 512):
                    pi = 4 + (nev % 4)
                    ps, kp = psb[pi], ("psA", pi)
                    for kc in range(16):
                        P.pe(lambda e, ps=ps, kc=kc, tb=tb, wb=wb, f0=f0, fw=fw, c0=c0: e.matmul(
                            ps[0:fw, :], wb[:, kc, f0 - c0:f0 - c0 + fw], xT[:, kc, tb * 512:(tb + 1) * 512],
                            start=(kc == 0), stop=(kc == 15)), r=xkeys[tb * 4:tb * 4 + 4] + [kwb], w=[kp])
                    t0 = b * S + tb * 512
                    if fw == 128:
                        o, ko = obf[nev % 2], ("obf", nev % 2)
                        evac(P, nev, o[:], ps[:], r=[kp], w=[ko])
                        P.dma(qkT[f0:f0 + 128, t0:t0 + 512], o[:], r=[ko], q="pool")
                    else:
                        o, ko = ost[nev % 4], ("ost", nev % 4)
                        evac(P, nev, o[0:8, :], ps[0:8, :], r=[kp], w=[ko])
                        P.dma(fT[0:8, t0:t0 + 512], o[0:8, :], r=[ko], q="pool")
                    nev += 1
    P.barrier()
    sb.reset(m)


def bcast_load(cx, dst, src_row, key, q="sp"):
    cx.P.dma(dst, src_row.partition_broadcast(128), w=[key], q=q)


def rstd_from_ssq(P, out, ssq, n, eps, r, w):
    P.dve(lambda e: e.tensor_scalar(out, ssq, 1.0 / n, eps, ALU.mult, ALU.add), r=r, w=w)
    P.act(lambda e: e.activation(out, out, AF.Sqrt), r=w, w=w)
    P.dve(lambda e: e.reciprocal(out, out), r=w, w=w)


def layer_norm_tile(cx, pre, y, g_b, b_b, stats, mv, rstd, kpre, ky, kst, aff="pool"):
    P = cx.P
    for j in range(4):
        P.dve(lambda e, j=j: e.bn_stats(stats[:, j, :], pre[:, j * 512:(j + 1) * 512]), r=[kpre], w=[kst])
    P.dve(lambda e: e.bn_aggr(mv[:], stats[:].rearrange("p a b -> p (a b)")), r=[kst], w=[kst])
    P.dve(lambda e: e.tensor_scalar_add(rstd[:], mv[:, 1:2], LN_EPS), r=[kst], w=[kst])
    P.act(lambda e: e.activation(rstd[:], rstd[:], AF.Sqrt), r=[kst], w=[kst])
    P.dve(lambda e: e.reciprocal(rstd[:], rstd[:]), r=[kst], w=[kst])
    P.dve(lambda e: e.tensor_scalar(y, pre, mv[:, 0:1], rstd[:], ALU.subtract, ALU.mult), r=[kst, kpre], w=[ky])
    eng_ = P.pool if aff == "pool" else P.dve
    eng_(lambda e: e.tensor_mul(y, y, g_b), r=[ky, "lnp"], w=[ky])
    eng_(lambda e: e.tensor_add(y, y, b_b), r=[ky, "lnp"], w=[ky])


def stage_outproj_ln(cx, mixed, w_out, x_res, ln_g, ln_b, h_out):
    nc, P, sb, c = cx.nc, cx.P, cx.sb, cx.c
    T = cx.T
    m = sb.mark()
    wbf = sb.alloc([128, 16, 2048], BF16, "woutbf")
    wst = [sb.alloc([128, 16, 256], F32, "wst") for _ in range(2)]
    mrow = [sb.alloc([128, 2048], F32, "mrow") for _ in range(2)]
    xrow = [sb.alloc([128, 2048], F32, "xrow") for _ in range(2)]
    mT = [sb.alloc([128, 16, 128], BF16, "mT") for _ in range(2)]
    pre = [sb.alloc([128, 2048], F32, "pre") for _ in range(2)]
    g_b = sb.alloc([128, 2048], F32, "g_b")
    b_b = sb.alloc([128, 2048], F32, "b_b")
    stats = sb.alloc([128, 4, 6], F32, "stats")
    mv = sb.alloc([128, 2], F32, "mv")
    rstd = sb.alloc([128, 1], F32, "rstd")
    psb = cx.ps
    bcast_load(cx, g_b[:], ln_g, "lnp")
    bcast_load(cx, b_b[:], ln_b, "lnp")
    wv = w_out.rearrange("(kc p) n -> p kc n", p=128)
    for j in range(8):
        ws, kws = wst[j % 2], ("wst", j % 2)
        P.dma(ws[:], wv[:, :, j * 256:(j + 1) * 256], w=[kws])
        if j % 2 == 0:
            P.dve(lambda e, ws=ws, j=j: e.tensor_copy(wbf[:, :, j * 256:(j + 1) * 256], ws[:]), r=[kws], w=["wout"])
        else:
            P.act(lambda e, ws=ws, j=j: e.copy(wbf[:, :, j * 256:(j + 1) * 256], ws[:]), r=[kws], w=["wout"])
    nev = 0
    for tt in range(T // 128):
        i2 = tt % 2
        r0 = tt * 128
        km, kx, kmT, kpre = ("mrow", i2), ("xrow", i2), ("mT", i2), ("pre", i2)
        P.dma(mrow[i2][:], mixed[r0:r0 + 128, :], w=[km])
        P.dma(xrow[i2][:], x_res[r0:r0 + 128, :], w=[kx])
        for g in range(4):
            pi = nev % 4
            ps, kp = psb[pi], ("ps", pi)
            for j in range(4):
                kc = g * 4 + j
                P.pe(lambda e, ps=ps, kc=kc, j=j, i2=i2: e.transpose(ps[:, j * 128:(j + 1) * 128],
                                                                 mrow[i2][:, kc * 128:(kc + 1) * 128], c["ident"][:]),
                     r=[km, "ident"], w=[kp])
            evac(P, nev, mT[i2][:, g * 4:(g + 1) * 4, :], ps[:].rearrange("p (j t) -> p j t", j=4), r=[kp], w=[kmT])
            nev += 1
        for cb in range(4):
            pi = 4 + nev % 4
            ps, kp = psb[pi], ("ps", pi)
            for kc in range(16):
                P.pe(lambda e, ps=ps, kc=kc, cb=cb, i2=i2: e.matmul(ps[:], mT[i2][:, kc, :], wbf[:, kc, cb * 512:(cb + 1) * 512],
                                                                  start=(kc == 0), stop=(kc == 15)),
                     r=[kmT, "wout"], w=[kp])
            P.dve(lambda e, ps=ps, cb=cb, i2=i2: e.scalar_tensor_tensor(
                pre[i2][:, cb * 512:(cb + 1) * 512], xrow[i2][:, cb * 512:(cb + 1) * 512], ALPHA, ps[:], ALU.mult, ALU.add),
                r=[kp, kx], w=[kpre])
            nev += 1
        layer_norm_tile(cx, pre[i2][:], xrow[i2][:], g_b[:], b_b[:], stats, mv, rstd, kpre, kx, "lnst")
        P.dma(h_out[r0:r0 + 128, :], xrow[i2][:], r=[kx], q="pool")
    P.barrier()
    sb.reset(m)


def build_masks(cx, kind):
    P, sb = cx.P, cx.sb
    if "maskf" not in cx.c:
        cx.c["maskf"] = sb.alloc([128, 512], F32, "maskf")
    mf = cx.c["maskf"]
    out = []
    for j in range(4):
        mb = sb.alloc([128, 512], BF16, "maskb")
        k = ("mask", kind, j)
        P.pool(lambda e: e.memset(mf[:], 0.0), w=["maskf"])
        if kind == "causal":
            P.pool(lambda e, j=j: e.affine_select(out=mf[:], in_=mf[:], pattern=[[1, 512]], compare_op=ALU.is_ge,
                                                 fill=NEG, base=-128 * j, channel_multiplier=-1), r=["maskf"], w=["maskf"])
        else:
            for hf in range(2):
                P.pool(lambda e, j=j, hf=hf: e.affine_select(
                    out=mf[hf * 64:(hf + 1) * 64, :], in_=mf[hf * 64:(hf + 1) * 64, :], pattern=[[1, 512]],
                    compare_op=ALU.is_ge, fill=NEG, base=-128 * j - 64 * hf, channel_multiplier=0),
                    r=["maskf"], w=["maskf"])
        P.pool(lambda e, mb=mb: e.tensor_copy(mb[:], mf[:]), r=["maskf"], w=[k])
        out.append((mb, k))
    return out


def attn_bufs(cx, nh, dv):
    sb, S = cx.sb, cx.S
    nring = 2 * (S // 128)
    return dict(PT=[sb.alloc([128, 512], BF16, "PT") for _ in range(nring)],
                o_t=[sb.alloc([128, 4, nh * dv], F32, "o_t") for _ in range(2)],
                rec=[sb.alloc([128, 4], F32, "rec") for _ in range(2)])


def attn_core(cx, nh, dv, scale, score_ops, bias_ap, masks, v_ap, out_cb, tag, sbanks=(0, 1, 2), bufs=None):
    P, sb, c = cx.P, cx.sb, cx.c
    S = cx.S
    psb = cx.ps
    nring = 2 * (S // 128)
    PT, o_t, rec = bufs["PT"], bufs["o_t"], bufs["rec"]
    tag = "att"
    st = {"npt": 0, "nsc": 0}
    units = [(qb, h) for qb in range(S // 512) for h in range(nh)]

    def scores(qb, h):
        pts = []
        for kt in range(4 * qb + 4):
            j = kt - 4 * qb
            bi_ = sbanks[st["nsc"] % len(sbanks)]
            pss, kpss = psb[bi_], ("ps", bi_)
            st["nsc"] += 1
            ops = list(score_ops(h, kt, qb))
            if j >= 0:
                mb, km = masks[j]
                ops.append((c["identb"][:], mb[:], [km, "identb"]))
            for i, (lh, rh, ks) in enumerate(ops):
                P.pe(lambda e, pss=pss, lh=lh, rh=rh, i=i, n=len(ops): e.matmul(pss[:], lh, rh, start=(i == 0), stop=(i == n - 1)),
                     r=ks, w=[kpss])
            pt, kpt = PT[st["npt"] % nring], ("PT", tag, st["npt"] % nring)
            st["npt"] += 1
            if bias_ap is not None:
                bap, kb = bias_ap(h, kt)
                P.act(lambda e, pt=pt, pss=pss, bap=bap: e.activation(pt[:], pss[:], AF.Exp, bias=bap, scale=scale),
                      r=[kpss] + kb, w=[kpt])
            else:
                P.act(lambda e, pt=pt, pss=pss: e.activation(pt[:], pss[:], AF.Exp, scale=scale), r=[kpss], w=[kpt])
            pts.append((pt, kpt))
        return pts

    def pv(ui, qb, h, pts):
        ot, kot = o_t[qb % 2], ("o_t", tag, qb % 2)
        if dv + 1 <= 128:
            pso, kpso = psb[4 + ui % 2], [("ps", 4 + ui % 2)]
            acc = [pso[:, tb * 128: tb * 128 + dv + 1] for tb in range(4)]
        else:
            p0, p1 = psb[4 + 2 * (ui % 2)], psb[5 + 2 * (ui % 2)]
            kpso = [("ps", 4 + 2 * (ui % 2)), ("ps", 5 + 2 * (ui % 2))]
            acc = [p0[:, 0:dv + 1], p0[:, 256:256 + dv + 1], p1[:, 0:dv + 1], p1[:, 256:256 + dv + 1]]
        for tb in range(4):
            last = 4 * qb + tb
            for kt in range(last + 1):
                pt, kpt = pts[kt]
                va, kv = v_ap(h, kt)
                P.pe(lambda e, a=acc[tb], pt=pt, tb=tb, va=va, kt=kt, last=last: e.matmul(
                    a, pt[:, tb * 128:(tb + 1) * 128], va, start=(kt == 0), stop=(kt == last)),
                    r=[kpt] + kv, w=kpso)
        rc, krc = rec[ui % 2], ("rec", tag, ui % 2)
        for tb in range(4):
            P.dve(lambda e, rc=rc, a=acc[tb], tb=tb: e.reciprocal(rc[:, tb:tb + 1], a[:, dv:dv + 1]), r=kpso, w=[krc])
        for tb in range(4):
            if tb % 2 == 0:
                P.dve(lambda e, rc=rc, a=acc[tb], tb=tb, h=h, ot=ot: e.tensor_scalar_mul(
                    ot[:, tb, h * dv:(h + 1) * dv], a[:, 0:dv], rc[:, tb:tb + 1]), r=kpso + [krc], w=[kot])
            else:
                P.act(lambda e, rc=rc, a=acc[tb], tb=tb, h=h, ot=ot: e.mul(
                    ot[:, tb, h * dv:(h + 1) * dv], a[:, 0:dv], rc[:, tb:tb + 1]), r=kpso + [krc], w=[kot])
        if h == nh - 1:
            out_cb(qb, ot, kot)

    prev = None
    for ui, (qb, h) in enumerate(units):
        pts = scores(qb, h)
        if prev is not None:
            pv(*prev)
        prev = (ui, qb, h, pts)
    pv(*prev)


def rms_out_cb(cx, col0, g_b, kg, mixed, tag):
    P, sb = cx.P, cx.sb
    junk = sb.alloc([128, 512], F32, "junk")
    ssq = sb.alloc([128, 4], F32, "ssq")
    ybuf = [sb.alloc([128, 4, 512], F32, "ybuf") for _ in range(2)]
    S = cx.S

    def cb(qb, ot, kot, b):
        ks = ("ssq", tag)
        yb, ky = ybuf[qb % 2], ("ybuf", tag, qb % 2)
        for tb in range(4):
            P.act(lambda e, tb=tb: e.activation(junk[:], ot[:, tb, :], AF.Square, accum_out=ssq[:, tb:tb + 1]),
                  r=[kot], w=[ks, ("junk", tag)])
        rstd_from_ssq(P, ssq[:], ssq[:], 512, RMS_EPS, [ks], [ks])
        for tb in range(4):
            P.dve(lambda e, tb=tb, yb=yb: e.scalar_tensor_tensor(yb[:, tb, :], ot[:, tb, :], ssq[:, tb:tb + 1], g_b,
                                                               ALU.mult, ALU.mult), r=[kot, ks, kg], w=[ky])
        r0 = b * S + qb * 512
        P.dma(mixed[r0:r0 + 512, col0:col0 + 512].rearrange("(tb p) n -> p tb n", p=128), yb[:], r=[ky], q="pool")
    return lambda b: (lambda qb, ot, kot: cb(qb, ot, kot, b))


def stage_fox(cx, qkT, fT, u_tok, b_f, out_g, mixed):
    nc, P, sb, c = cx.nc, cx.P, cx.sb, cx.c
    S, NB = cx.S, cx.NB
    nt = S // 128
    m = sb.mark()
    masks = c["masks_causal"]
    qT = sb.alloc([128, 4, S], BF16, "qT")
    kT = sb.alloc([128, 4, S], BF16, "kT")
    vaug = sb.alloc([128, nt, 8, 65], BF16, "vaug")
    vst = [sb.alloc([128, 512], F32, "vst") for _ in range(2)]
    fx = sb.alloc([8, S], F32, "fx")
    fa = sb.alloc([8, S], F32, "fa")
    Fc = sb.alloc([8, S], F32, "Fc")
    ones8 = sb.alloc([8, S], F32, "ones8")
    F8 = sb.alloc([8, S], BF16, "F8")
    bfc = sb.alloc([8, 1], F32, "bfc")
    sel = sb.alloc([8, 8, 128], BF16, "sel")
    nF = sb.alloc([128, nt, 8], F32, "nF")
    g_b = sb.alloc([128, 512], F32, "g_b")
    bcast_load(cx, g_b[:], out_g, "fox_g")
    P.dma(bfc[:], b_f.rearrange("(h o) -> h o", o=1), w=["bfc"])
    P.pool(lambda e: e.memset(ones8[:], 1.0), w=["ones8"])
    P.pool(lambda e: e.memset(vaug[:, :, :, 64:65], 1.0), w=["vones"])
    P.dve(lambda e: e.tensor_copy(sel[:], c["ident"][0:8, 0:8].unsqueeze(2).to_broadcast([8, 8, 128])), r=["ident"], w=["sel"])
    ocb = rms_out_cb(cx, 0, g_b[:], "fox_g", mixed, "fox")
    abufs = attn_bufs(cx, 8, 64)
    for b in range(NB):
        t0 = b * S
        tag = "fox%d" % b
        P.dma(qT[:], qkT[0:512, t0:t0 + S].rearrange("(hp p) t -> p hp t", p=128), w=["qT"])
        P.dma(kT[:], qkT[512:1024, t0:t0 + S].rearrange("(hp p) t -> p hp t", p=128), w=["kT"])
        P.dma(fx[:], fT[0:8, t0:t0 + S], w=["fx"])
        for tt in range(nt):
            vs, kvs = vst[tt % 2], ("vst", tt % 2)
            P.dma(vs[:], u_tok[t0 + tt * 128:t0 + (tt + 1) * 128, 1024:1536], w=[kvs])
            P.pool(lambda e, vs=vs, tt=tt: e.tensor_copy(vaug[:, tt, :, 0:64], vs[:].rearrange("p (h d) -> p h d", h=8)),
                   r=[kvs], w=[("v", tt)])
        P.dve(lambda e: e.tensor_scalar_add(fx[:], fx[:], bfc[:, 0:1]), r=["fx", "bfc"], w=["fx"])
        P.act(lambda e: e.activation(fa[:], fx[:], AF.Abs), r=["fx"], w=["fa"])
        P.act(lambda e: e.activation(fa[:], fa[:], AF.Exp, scale=-1.0), r=["fa"], w=["fa"])
        P.act(lambda e: e.activation(fa[:], fa[:], AF.Ln, bias=1.0), r=["fa"], w=["fa"])
        P.dve(lambda e: e.tensor_scalar_min(fx[:], fx[:], 0.0), r=["fx"], w=["fx"])
        P.dve(lambda e: e.tensor_sub(fx[:], fx[:], fa[:]), r=["fx", "fa"], w=["fx"])
        P.dve(lambda e: e.tensor_tensor_scan(Fc[:], ones8[:], fx[:], 0.0, ALU.mult, ALU.add), r=["fx", "ones8"], w=["Fc"])
        P.act(lambda e: e.mul(F8[:], Fc[:], 8.0), r=["Fc"], w=["F8"])
        for tt in range(nt):
            ps, kp = cx.ps[7], ("ps", 7)
            P.pe(lambda e, tt=tt, ps=ps: e.transpose(ps[:, 0:8], Fc[0:8, tt * 128:(tt + 1) * 128], c["ident"][0:8, 0:8]),
                 r=["Fc", "ident"], w=[kp])
            P.act(lambda e, tt=tt, ps=ps: e.mul(nF[:, tt, :], ps[:, 0:8], -1.0), r=[kp], w=["nF"])

        def score_ops(h, kt, qb):
            hp, base = h // 2, (h % 2) * 64
            return [(kT[base:base + 64, hp, kt * 128:(kt + 1) * 128], qT[base:base + 64, hp, qb * 512:(qb + 1) * 512], ["kT", "qT"]),
                    (sel[0:8, h, :], F8[0:8, qb * 512:(qb + 1) * 512], ["sel", "F8"])]

        def bias_ap(h, kt):
            return nF[:, kt, h:h + 1], ["nF"]

        def v_ap(h, kt):
            return vaug[:, kt, h, :], [("v", kt), "vones"]

        attn_core(cx, 8, 64, 0.125, score_ops, bias_ap, masks, v_ap, ocb(b), tag, bufs=abufs)
    P.barrier()
    sb.reset(m)


def rope_tables(cx, pos_dram, b, cosb, sinb, ifr_b, tmpa, rbufs):
    P, sb = cx.P, cx.sb
    S = cx.S
    nt = S // 128
    posi, posf = rbufs["posi"], rbufs["posf"]
    TWO_PI = 2.0 * np.pi
    P.dma(posi[:], pos_dram[b, :].rearrange("(t p) -> p t", p=128), w=["posi"], slow=True)
    P.dve(lambda e: e.tensor_copy(posf[:], posi[:]), r=["posi"], w=["posf"])
    for tt in range(nt):
        P.dve(lambda e, tt=tt: e.tensor_scalar_mul(tmpa[:, tt, :], ifr_b, posf[:, tt:tt + 1]), r=["posf", "ifr"], w=["ang"])
    ti, tf = rbufs["ti"], rbufs["tf"]
    C1, C2 = 6.28125, 2.0 * np.pi - 6.28125

    def sin_of(out, shift, key):
        P.dve(lambda e: e.tensor_scalar(out, tmpa[:], shift, 1.0 / TWO_PI, ALU.add, ALU.mult), r=["ang"], w=[key])
        P.dve(lambda e: e.tensor_copy(ti[:], out), r=[key], w=["ropei"])
        P.dve(lambda e: e.tensor_copy(tf[:], ti[:]), r=["ropei"], w=["ropef"])
        P.dve(lambda e: e.tensor_scalar_add(out, tmpa[:], shift), r=["ang"], w=[key])
        P.dve(lambda e: e.scalar_tensor_tensor(out, tf[:], -C1, out, ALU.mult, ALU.add), r=["ropef", key], w=[key])
        P.dve(lambda e: e.scalar_tensor_tensor(out, tf[:], -C2, out, ALU.mult, ALU.add), r=["ropef", key], w=[key])
        P.dve(lambda e: e.tensor_scalar(tf[:], out, np.pi, -TWO_PI, ALU.is_gt, ALU.mult), r=[key], w=["ropef"])
        P.dve(lambda e: e.tensor_add(out, out, tf[:]), r=["ropef", key], w=[key])
        P.dve(lambda e: e.tensor_scalar(tf[:], out, -np.pi, TWO_PI, ALU.is_lt, ALU.mult), r=[key], w=["ropef"])
        P.dve(lambda e: e.tensor_add(out, out, tf[:]), r=["ropef", key], w=[key])
        P.dve(lambda e: e.tensor_scalar(out, out, 3.1415925, -3.1415925, ALU.min, ALU.max), r=[key], w=[key])
        P.act(lambda e: e.activation(out, out, AF.Sin), r=[key], w=[key])

    sin_of(sinb[:], 0.0, "sinb")
    sin_of(cosb[:], 0.5 * np.pi, "cosb")


def rope_apply(P, out, x, cos, sin, t1, t2, nh, r, w):
    cb = cos.unsqueeze(1).to_broadcast([128, nh, 32])
    sn = sin.unsqueeze(1).to_broadcast([128, nh, 32])
    x1, x2 = x[:, :, 0:32], x[:, :, 32:64]
    kk = ("ropetmp",)
    P.dve(lambda e: e.tensor_mul(t1, x1, cb), r=r, w=[kk])
    P.dve(lambda e: e.tensor_mul(t2, x2, sn), r=r, w=[kk])
    P.dve(lambda e: e.tensor_sub(out[:, :, 0:32], t1, t2), r=[kk], w=w)
    P.dve(lambda e: e.tensor_mul(t1, x2, cb), r=r + [kk], w=[kk])
    P.dve(lambda e: e.tensor_mul(t2, x1, sn), r=r + [kk], w=[kk])
    P.dve(lambda e: e.tensor_add(out[:, :, 32:64], t1, t2), r=[kk], w=w)


def load_cast_w(cx, dst_bf, src_view, stage, key, eng="dve"):
    P = cx.P
    ks = ("wstage", id(stage))
    P.dma(stage, src_view, w=[ks])
    if eng == "pool":
        P.pool(lambda e: e.tensor_copy(dst_bf, stage), r=[ks], w=[key])
    else:
        P.dve(lambda e: e.tensor_copy(dst_bf, stage), r=[ks], w=[key])


def stage_mla(cx, u_tok, pos, ifr, qg, kvg, w_uq, w_ukv, out_g, mixed):
    nc, P, sb, c = cx.nc, cx.P, cx.sb, cx.c
    S, NB = cx.S, cx.NB
    nt = S // 128
    psb = cx.ps
    m = sb.mark()
    masks = c["masks_chunk64"]
    wq_n = sb.alloc([128, 3, 512], BF16, "wq_n")
    wq_p = sb.alloc([128, 3, 256], BF16, "wq_p")
    wk_n = sb.alloc([128, 512], BF16, "wk_n")
    wk_v = sb.alloc([128, 512], BF16, "wk_v")
    wstg = sb.alloc([128, 3, 768], F32, "wstg")
    P.dma(wstg[:], w_uq.rearrange("(kc p) n -> p kc n", p=128), w=["wstg"])
    v4 = wstg[:].rearrange("p k (h d) -> p k h d", h=4)
    for kc in range(3):
        P.pool(lambda e, kc=kc: e.tensor_copy(wq_n[:, kc, :].rearrange("p (h d) -> p h d", h=4), v4[:, kc, :, 0:128]), r=["wstg"], w=["wq"])
        P.pool(lambda e, kc=kc: e.tensor_copy(wq_p[:, kc, :].rearrange("p (h d) -> p h d", h=4), v4[:, kc, :, 128:192]), r=["wstg"], w=["wq"])
    wstg2 = sb.alloc([128, 1024], F32, "wstg2")
    P.dma(wstg2[:], w_ukv, w=["wstg2"])
    v5 = wstg2[:].rearrange("p (h d) -> p h d", h=4)
    P.pool(lambda e: e.tensor_copy(wk_n[:].rearrange("p (h d) -> p h d", h=4), v5[:, :, 0:128]), r=["wstg2"], w=["wk"])
    P.pool(lambda e: e.tensor_copy(wk_v[:].rearrange("p (h d) -> p h d", h=4), v5[:, :, 128:256]), r=["wstg2"], w=["wk"])
    qg_b = sb.alloc([128, 384], F32, "qg_b")
    kvg_b = sb.alloc([128, 128], F32, "kvg_b")
    g_b = sb.alloc([128, 512], F32, "g_b")
    ifr_b = sb.alloc([128, 32], F32, "ifr_b")
    bcast_load(cx, qg_b[:], qg, "qg")
    bcast_load(cx, kvg_b[:], kvg, "kvg")
    bcast_load(cx, g_b[:], out_g, "mla_g")
    bcast_load(cx, ifr_b[:], ifr, "ifr")
    cosb = sb.alloc([128, nt, 32], F32, "cosb")
    sinb = sb.alloc([128, nt, 32], F32, "sinb")
    ang = sb.alloc([128, nt, 32], F32, "ang")
    cqnT = sb.alloc([128, 3, S], BF16, "cqnT")
    ckvnT = sb.alloc([128, S], BF16, "ckvnT")
    qnT = sb.alloc([128, 4, S], BF16, "qnT")
    qpT = sb.alloc([128, 2, S], BF16, "qpT")
    knT = sb.alloc([128, 4, S], BF16, "knT")
    kpT = sb.alloc([128, S], BF16, "kpT")
    vaug = sb.alloc([128, nt, 4, 129], BF16, "vaug")
    urow = [sb.alloc([128, 576], F32, "urow") for _ in range(2)]
    cn = [sb.alloc([128, 640], F32, "cn") for _ in range(2)]
    qpe = [sb.alloc([128, 256], F32, "qpe") for _ in range(2)]
    junk = sb.alloc([128, 384], F32, "junk")
    ssq = sb.alloc([128, 2], F32, "ssq2")
    t1 = sb.alloc([128, 4, 32], F32, "t1")
    t2 = sb.alloc([128, 4, 32], F32, "t2")
    P.pool(lambda e: e.memset(vaug[:, :, :, 128:129], 1.0), w=["vones"])
    ocb = rms_out_cb(cx, 512, g_b[:], "mla_g", mixed, "mla")
    abufs = attn_bufs(cx, 4, 128)
    rbufs = dict(posi=sb.alloc([128, nt], I32, "posi"), posf=sb.alloc([128, nt], F32, "posf"),
                 ti=sb.alloc([128, nt, 32], I32, "ropei"), tf=sb.alloc([128, nt, 32], F32, "ropef"))
    for b in range(NB):
        t0 = b * S
        tag = "mla%d" % b
        rope_tables(cx, pos, b, cosb, sinb, ifr_b[:], ang, rbufs)
        nev = 0
        for tt in range(nt):
            i2 = tt % 2
            ur, kur = urow[i2], ("urow", i2)
            cnt, kcn = cn[i2], ("cn", i2)
            P.dma(ur[:], u_tok[t0 + tt * 128:t0 + (tt + 1) * 128, C_MLA:C_MLA + 576], w=[kur])
            P.act(lambda e, ur=ur: e.activation(junk[:, 0:384], ur[:, 0:384], AF.Square, accum_out=ssq[:, 0:1]), r=[kur], w=["ssq2", "junk"])
            P.act(lambda e, ur=ur: e.activation(junk[:, 0:128], ur[:, 384:512], AF.Square, accum_out=ssq[:, 1:2]), r=[kur], w=["ssq2", "junk"])
            rstd_from_ssq(P, ssq[:, 0:1], ssq[:, 0:1], 384, RMS_EPS, ["ssq2"], ["ssq2"])
            rstd_from_ssq(P, ssq[:, 1:2], ssq[:, 1:2], 128, RMS_EPS, ["ssq2"], ["ssq2"])
            P.dve(lambda e, ur=ur, cnt=cnt: e.scalar_tensor_tensor(cnt[:, 0:384], ur[:, 0:384], ssq[:, 0:1], qg_b[:], ALU.mult, ALU.mult),
                  r=[kur, "ssq2", "qg"], w=[kcn])
            P.dve(lambda e, ur=ur, cnt=cnt: e.scalar_tensor_tensor(cnt[:, 384:512], ur[:, 384:512], ssq[:, 1:2], kvg_b[:], ALU.mult, ALU.mult),
                  r=[kur, "ssq2", "kvg"], w=[kcn])
            rope_apply(P, cnt[:, 512:576].rearrange("p (h d) -> p h d", h=1), ur[:, 512:576].rearrange("p (h d) -> p h d", h=1),
                       cosb[:, tt, :], sinb[:, tt, :], t1[:, 0:1, :], t2[:, 0:1, :], 1, [kur, "cosb", "sinb"], [kcn])
            P.dve(lambda e, cnt=cnt: e.tensor_copy(cnt[:, 576:640], cnt[:, 512:576]), r=[kcn], w=[kcn])
            pi = 2 + nev % 2
            nev += 1
            ps, kp = psb[pi], ("ps", pi)
            for j in range(4):
                P.pe(lambda e, ps=ps, cnt=cnt, j=j: e.transpose(ps[:, j * 128:(j + 1) * 128], cnt[:, j * 128:(j + 1) * 128], c["ident"][:]),
                     r=[kcn, "ident"], w=[kp])
            P.act(lambda e, ps=ps, tt=tt: e.copy(cqnT[:, :, tt * 128:(tt + 1) * 128], ps[:, 0:384].rearrange("p (j t) -> p j t", j=3)),
                  r=[kp], w=[("cqnT", tt)])
            P.dve(lambda e, ps=ps, tt=tt: e.tensor_copy(ckvnT[:, tt * 128:(tt + 1) * 128], ps[:, 384:512]), r=[kp], w=[("ckvnT", tt)])
            pi = 2 + nev % 2
            nev += 1
            ps, kp = psb[pi], ("ps", pi)
            P.pe(lambda e, ps=ps, cnt=cnt: e.transpose(ps[:, 0:128], cnt[:, 512:640], c["ident"][:]), r=[kcn, "ident"], w=[kp])
            P.act(lambda e, ps=ps, tt=tt: e.copy(kpT[:, tt * 128:(tt + 1) * 128], ps[:, 0:128]), r=[kp], w=[("kpT", tt)])
            pi = 2 + nev % 2
            nev += 1
            ps, kp = psb[pi], ("ps", pi)
            for kc in range(3):
                P.pe(lambda e, ps=ps, kc=kc, tt=tt: e.matmul(ps[:, 0:256], cqnT[:, kc, tt * 128:(tt + 1) * 128], wq_p[:, kc, :],
                                                            start=(kc == 0), stop=(kc == 2)), r=[("cqnT", tt), "wq"], w=[kp])
            qp, kqp = qpe[i2], ("qpe", i2)
            rope_apply(P, qp[:].rearrange("p (h d) -> p h d", h=4), ps[:, 0:256].rearrange("p (h d) -> p h d", h=4),
                       cosb[:, tt, :], sinb[:, tt, :], t1[:], t2[:], 4, [kp, "cosb", "sinb"], [kqp])
            pi = 2 + nev % 2
            nev += 1
            ps, kp = psb[pi], ("ps", pi)
            for j in range(2):
                P.pe(lambda e, ps=ps, qp=qp, j=j: e.transpose(ps[:, j * 128:(j + 1) * 128], qp[:, j * 128:(j + 1) * 128], c["ident"][:]),
                     r=[kqp, "ident"], w=[kp])
            P.act(lambda e, ps=ps, tt=tt: e.copy(qpT[:, :, tt * 128:(tt + 1) * 128], ps[:, 0:256].rearrange("p (j t) -> p j t", j=2)),
                  r=[kp], w=[("qpT", tt)])
            pi = 2 + nev % 2
            nev += 1
            ps, kp = psb[pi], ("ps", pi)
            P.pe(lambda e, ps=ps, tt=tt: e.matmul(ps[:], ckvnT[:, tt * 128:(tt + 1) * 128], wk_v[:], start=True, stop=True),
                 r=[("ckvnT", tt), "wk"], w=[kp])
            P.dve(lambda e, ps=ps, tt=tt: e.tensor_copy(vaug[:, tt, :, 0:128], ps[:].rearrange("p (h d) -> p h d", h=4)),
                  r=[kp], w=[("v", tt)])
        for tb in range(S // 512):
            for h in range(4):
                pi = 2 + nev % 2
                nev += 1
                ps, kp = psb[pi], ("ps", pi)
                for kc in range(3):
                    P.pe(lambda e, ps=ps, kc=kc, h=h, tb=tb: e.matmul(ps[:], wq_n[:, kc, h * 128:(h + 1) * 128], cqnT[:, kc, tb * 512:(tb + 1) * 512],
                                                                  start=(kc == 0), stop=(kc == 2)),
                         r=[("cqnT", tb * 4 + i) for i in range(4)] + ["wq"], w=[kp])
                evac(P, nev, qnT[:, h, tb * 512:(tb + 1) * 512], ps[:], r=[kp], w=[("qnT", tb)])
                pi = 2 + nev % 2
                nev += 1
                ps, kp = psb[pi], ("ps", pi)
                P.pe(lambda e, ps=ps, h=h, tb=tb: e.matmul(ps[:], wk_n[:, h * 128:(h + 1) * 128], ckvnT[:, tb * 512:(tb + 1) * 512],
                                                         start=True, stop=True),
                     r=[("ckvnT", tb * 4 + i) for i in range(4)] + ["wk"], w=[kp])
                evac(P, nev, knT[:, h, tb * 512:(tb + 1) * 512], ps[:], r=[kp], w=[("knT", tb)])

        def score_ops(h, kt, qb):
            base = (h % 2) * 64
            return [(knT[:, h, kt * 128:(kt + 1) * 128], qnT[:, h, qb * 512:(qb + 1) * 512], [("knT", kt // 4), ("qnT", qb)]),
                    (kpT[base:base + 64, kt * 128:(kt + 1) * 128], qpT[base:base + 64, h // 2, qb * 512:(qb + 1) * 512],
                     [("kpT", kt)] + [("qpT", qb * 4 + i) for i in range(4)])]

        def v_ap(h, kt):
            return vaug[:, kt, h, :], [("v", kt), "vones"]

        attn_core(cx, 4, 128, 192 ** -0.5, score_ops, None, masks, v_ap, ocb(b), tag, sbanks=(0, 1), bufs=abufs)
    P.barrier()
    sb.reset(m)


def run_interleaved(gens):
    gens = list(gens)
    while gens:
        for g in list(gens):
            try:
                next(g)
            except StopIteration:
                gens.remove(g)


def dplr_consts(cx):
    P, sb = cx.P, cx.sb
    c = cx.c
    if "MS" in c:
        return
    MS = sb.alloc([128, 128], F32, "MS")
    MI = sb.alloc([128, 128], F32, "MI")
    MZ = sb.alloc([128, 128], F32, "MZ")
    ML = sb.alloc([128, 128], F32, "ML")
    BLK = sb.alloc([128, 128], F32, "BLK")
    for (M, base, cm, pat, lo_zero) in ((MS, -1, -1, 1, "ur"), (MI, 0, -1, 1, "ur"), (MZ, -1, 1, -1, "ll"), (ML, 0, 1, -1, "ll")):
        k = "dplrmask"
        P.pool(lambda e, M=M: e.memset(M[:], 1.0), w=[k])
        P.pool(lambda e, M=M, base=base, cm=cm, pat=pat: e.affine_select(
            out=M[:], in_=M[:], pattern=[[pat, 128]], compare_op=ALU.is_ge, fill=0.0, base=base, channel_multiplier=cm), r=[k], w=[k])
        if lo_zero == "ur":
            P.pool(lambda e, M=M: e.memset(M[0:64, 64:128], 0.0), r=[k], w=[k])
        else:
            P.pool(lambda e, M=M: e.memset(M[64:128, 0:64], 0.0), r=[k], w=[k])
    P.pool(lambda e: e.memset(BLK[:], 0.0), w=["dplrmask"])
    P.pool(lambda e: e.memset(BLK[0:64, 0:64], 1.0), r=["dplrmask"], w=["dplrmask"])
    P.pool(lambda e: e.memset(BLK[64:128, 64:128], 1.0), r=["dplrmask"], w=["dplrmask"])
    c.update(MS=MS, MI=MI, MZ=MZ, ML=ML, BLK=BLK)


def dplr_tile(cx, NH, NK, geo, st, tag, banks=(0, 1, 2)):
    P, c = cx.P, cx.c
    B0, B1, B2 = banks
    psb = {0: cx.ps[B0], 1: cx.ps[B1], 2: cx.ps[B2], 3: cx.ps[B0], 4: cx.ps[B1], 5: cx.ps[B2], 6: cx.ps[B0]}
    bk = {0: B0, 1: B1, 2: B2, 3: B0, 4: B1, 5: B2, 6: B0}
    HP = 128 // NK
    NG = NH // HP
    HG = 512 // 128
    ngr = (NH + HG - 1) // HG
    Xs, Zs, Ps = st["X"], st["Z"], st["P"]
    kX, kZ, kP = [("dX", tag, i) for i in range(2)], [("dZ", tag, i) for i in range(2)], [("dP", tag, i) for i in range(2)]
    identb3 = c["identb"][:].unsqueeze(1).to_broadcast([128, NH, 128])
    P.dve(lambda e: e.tensor_tensor(Ps[0][:], Xs[0][:], identb3, ALU.add), r=[kX[0], "identb"], w=[kP[0]])
    cur = 0
    for lvl in range(1, 6):
        nxt = 1 - cur
        for g in range(ngr):
            hs = list(range(g * HG, min(NH, (g + 1) * HG)))
            n = len(hs)
            if lvl < 5:
                for i, h in enumerate(hs):
                    P.pe(lambda e, i=i, h=h, cur=cur: e.matmul(psb[0][:, i * 128:(i + 1) * 128], Zs[cur][:, h, :], Xs[cur][:, h, :],
                                                             start=True, stop=True), r=[kX[cur], kZ[cur]], w=[("ps", bk[0])])
                P.act(lambda e, g=g, n=n, nxt=nxt: e.copy(Xs[nxt][:, g * HG:g * HG + n, :],
                                                         psb[0][:, 0:n * 128].rearrange("p (h t) -> p h t", h=n)),
                      r=[("ps", bk[0])], w=[kX[nxt]])
            for i, h in enumerate(hs):
                P.pe(lambda e, i=i, h=h, cur=cur: e.matmul(psb[1][:, i * 128:(i + 1) * 128], Xs[cur][:, h, :], Zs[cur][:, h, :],
                                                         start=True, stop=True), r=[kX[cur], kZ[cur]], w=[("ps", bk[1])])
            P.dve(lambda e, g=g, n=n, nxt=nxt: e.tensor_copy(Zs[nxt][:, g * HG:g * HG + n, :],
                                                            psb[1][:, 0:n * 128].rearrange("p (h t) -> p h t", h=n)),
                  r=[("ps", bk[1])], w=[kZ[nxt]])
            for i, h in enumerate(hs):
                P.pe(lambda e, i=i, h=h, cur=cur, nxt=nxt: e.matmul(psb[2][:, i * 128:(i + 1) * 128], Zs[nxt][:, h, :], Ps[cur][:, h, :],
                                                                  start=True, stop=True), r=[kZ[nxt], kP[cur]], w=[("ps", bk[2])])
            P.dve(lambda e, g=g, n=n, cur=cur, nxt=nxt: e.tensor_tensor(
                Ps[nxt][:, g * HG:g * HG + n, :], psb[2][:, 0:n * 128].rearrange("p (h t) -> p h t", h=n),
                Ps[cur][:, g * HG:g * HG + n, :], ALU.add), r=[("ps", bk[2]), kP[cur]], w=[kP[nxt]])
            yield
        cur = nxt
    TT, kTT = Ps[cur], kP[cur]
    ST, STb = st["ST"], st["STb"]
    kST = ("dST", tag)
    NV = NK
    W = NH * NV
    Wb, Ub = st["Wb"], st["Ub"]
    for ch in range(2):
        c0 = ch * 64
        M = 64 + c0
        rows = slice(c0, c0 + 64)
        for h in range(NH):
            g_, hb = h // HP, (h % HP) * NK
            at, kat = geo["AT"](h)
            P.pe(lambda e, h=h, at=at, g_=g_, hb=hb, M=M: e.matmul(psb[3][0:M, h * NV:(h + 1) * NV], at[:, 0:M], STb[:, g_, :],
                                                                start=True, stop=False), r=kat + [kST], w=[("ps", bk[3])])
            ak, kak = geo["AAK"](h)
            v, kv = geo["V"](h)
            P.pe(lambda e, h=h, ak=ak, v=v, M=M, rows=rows: e.matmul(psb[3][0:M, h * NV:(h + 1) * NV], ak[rows, 0:M], v[rows, :],
                                                                  start=False, stop=True), r=kak + kv, w=[("ps", bk[3])])
        kW = ("dW", tag)
        if geo.get("WADD") is not None:
            wa, kwa = geo["WADD"]
            P.dve(lambda e, rows=rows, wa=wa: e.tensor_tensor(Wb[rows, :], psb[3][rows, 0:W], wa[rows, :], ALU.add), r=[("ps", bk[3])] + kwa, w=[kW])
        else:
            P.act(lambda e, rows=rows: e.copy(Wb[rows, :], psb[3][rows, 0:W]), r=[("ps", bk[3])], w=[kW])
        yield
        for h in range(NH):
            P.pe(lambda e, h=h, M=M, rows=rows: e.matmul(psb[4][0:M, h * NV:(h + 1) * NV], TT[rows, h, 0:M], Wb[rows, h * NV:(h + 1) * NV],
                                                      start=True, stop=True), r=[kTT, kW], w=[("ps", bk[4])])
        kU = ("dU", tag)
        P.dve(lambda e, rows=rows: e.tensor_copy(Ub[rows, :], psb[4][rows, 0:W]), r=[("ps", bk[4])], w=[kU])
        yield
        for h in range(NH):
            g_, hb = h // HP, (h % HP) * NK
            rt, krt = geo["RT"](h)
            arb, karb = geo["ARB"](h)
            ark, kark = geo["ARK"](h)
            v, kv = geo["V"](h)
            o = psb[5][0:M, h * NV:(h + 1) * NV]
            P.pe(lambda e, o=o, rt=rt, g_=g_, hb=hb, M=M: e.matmul(o, rt[:, 0:M], STb[:, g_, :], start=True, stop=False),
                 r=krt + [kST], w=[("ps", bk[5])])
            P.pe(lambda e, o=o, arb=arb, h=h, M=M, rows=rows: e.matmul(o, arb[rows, 0:M], Ub[rows, h * NV:(h + 1) * NV], start=False, stop=False),
                 r=karb + [kU], w=[("ps", bk[5])])
            P.pe(lambda e, o=o, ark=ark, v=v, M=M, rows=rows: e.matmul(o, ark[rows, 0:M], v[rows, :], start=False, stop=True),
                 r=kark + kv, w=[("ps", bk[5])])
        yo, kyo = geo["Y"]
        P.act(lambda e, rows=rows: e.copy(yo[rows, :], psb[5][rows, 0:W]), r=[("ps", bk[5])], w=[kyo])
        bd, kbd = geo["BD"]
        kd, kkd = geo["KD"]
        for h in range(NH):
            g_ = h // HP
            v, kv = geo["V"](h)
            o = psb[6][:, h * NV:(h + 1) * NV]
            P.pe(lambda e, o=o, g_=g_, h=h, rows=rows: e.matmul(o, bd[rows, g_ * 128:(g_ + 1) * 128], Ub[rows, h * NV:(h + 1) * NV],
                                                             start=True, stop=False), r=kbd + [kU], w=[("ps", bk[6])])
            P.pe(lambda e, o=o, g_=g_, v=v, rows=rows: e.matmul(o, kd[rows, g_ * 128:(g_ + 1) * 128], v[rows, :], start=False, stop=True),
                 r=kkd + kv, w=[("ps", bk[6])])
        gc_, kgc = geo["GC"](ch)
        for hh in range(HP):
            pr = slice(hh * NK, (hh + 1) * NK)
            src = psb[6][pr, 0:NH * NV].rearrange("p (g x v) -> p g x v", g=NG, x=HP)[:, :, hh, :]
            P.dve(lambda e, pr=pr: e.tensor_tensor(ST[pr, :, :], ST[pr, :, :], gc_[pr, :].unsqueeze(2).to_broadcast([NK, NG, NV]), ALU.mult),
                  r=[kST] + kgc, w=[kST])
            P.dve(lambda e, pr=pr, src=src: e.tensor_tensor(ST[pr, :, :], ST[pr, :, :], src, ALU.add), r=[kST, ("ps", bk[6])], w=[kST])
        P.act(lambda e: e.copy(STb[:], ST[:]), r=[kST], w=[kST])
        yield


def stage_rwkv(cx, u_tok, prm, mixed):
    nc, P, sb, c, psb = cx.nc, cx.P, cx.sb, cx.c, cx.ps
    S, NB = cx.S, cx.NB
    nt = S // 128
    m = sb.mark()
    dplr_consts(cx)
    NH, NK = 8, 64
    f32t = lambda name, w=512: sb.alloc([128, w], F32, name)
    mu_b = f32t("mu_b", 1792)
    bc = {}
    for nm in ("w0", "a0", "k_k", "k_a", "r_k", "ln_g", "ln_b"):
        bc[nm] = f32t(nm + "_b")
        bcast_load(cx, bc[nm][:], prm[nm], "rw_" + nm)
    bcast_load(cx, mu_b[:], prm["mu"], "rw_mu")
    w2b = sb.alloc([64, 512], BF16, "w2b")
    a2b = sb.alloc([128, 512], BF16, "a2b")
    g2b = sb.alloc([128, 512], BF16, "g2b")
    wtmp = f32t("wtmp")
    load_cast_w(cx, w2b[:], prm["w2"], wtmp[0:64, :], "rw_w2")
    wtmp2 = f32t("wtmp2")
    load_cast_w(cx, a2b[64:128, :], prm["a2"], wtmp2[64:128, :], "rw_a2")
    wtmp3 = f32t("wtmp3")
    load_cast_w(cx, g2b[:], prm["g2"], wtmp3[:], "rw_g2")
    Ut = [sb.alloc([128, 1792], F32, "Ut") for _ in range(2)]
    Up = [sb.alloc([128, 1792], F32, "Up") for _ in range(2)]
    xs = sb.alloc([128, 1792], F32, "xs")
    la = f32t("la", 256)
    lT = sb.alloc([128, 256], BF16, "lT")
    names = ["lw", "aa", "gt", "Gs", "eG", "enG", "eGm", "eD", "kx", "sq", "kk", "k2", "bv", "tR", "tK", "tB", "tA", "rk", "yt", "y2"]
    T_ = {n: f32t(n) for n in names}
    small = {n: sb.alloc([128, 8], F32, n) for n in ("ssq", "rn", "bs", "s1", "s2", "mean", "var", "rstd")}
    PB = []
    for _b in range(NB):
        d_ = dict(Kd=sb.alloc([128, 512], BF16, "Kd"), Bd=sb.alloc([128, 512], BF16, "Bd"), Vb=sb.alloc([128, 512], BF16, "Vb"),
                  ART=sb.alloc([128, 8, 2, 128], BF16, "ARTz"), KT=sb.alloc([128, 4, 128], BF16, "KT"), BT=sb.alloc([128, 4, 128], BF16, "BT"),
                  AM3=sb.alloc([128, 8, 384], BF16, "AM3"), gC=[sb.alloc([128, 4], F32, "gC") for _ in range(2)],
                  yt=f32t("ytb"), gt=f32t("gtb"), vk=f32t("vkb"), bs=sb.alloc([128, 8], F32, "bsb"),
                  st=dict(X=[sb.alloc([128, 8, 128], BF16, "dX") for _ in range(2)], Z=[sb.alloc([128, 8, 128], BF16, "dZ") for _ in range(2)],
                          P=[sb.alloc([128, 8, 128], BF16, "dP") for _ in range(2)], ST=sb.alloc([128, 4, 64], F32, "ST"),
                          STb=sb.alloc([128, 4, 64], BF16, "STb"), Wb=sb.alloc([128, 512], BF16, "Wb"), Ub=sb.alloc([128, 512], BF16, "Ub")))
        P.pool(lambda e, A_=d_["ART"]: e.memset(A_[:], 0.0), w=[("ARTz0", _b)])
        PB.append(d_)
    mask4 = sb.alloc([128, 4, 128], F32, "mask4")
    for i, Mk in enumerate(("MS", "MI", "MS", "MI")):
        P.pool(lambda e, i=i, Mk=Mk: e.tensor_copy(mask4[:, i, :], c[Mk][:]), r=["dplrmask"], w=["mask4"])
    MZ4 = c["MZ"][:].unsqueeze(1).to_broadcast([128, 4, 128])
    h3 = lambda t: t.rearrange("p (h d) -> p h d", h=8)
    b3 = lambda t: t.unsqueeze(2).to_broadcast([128, 8, 64])
    if True:
        def tile_prep(b, tt):
            tag = "rw%d" % b
            kST = ("dST", tag)
            pb = PB[b]
            Kd, Bd, Vb, ART, KT, BT, AM3, gC, st = (pb[k] for k in ("Kd", "Bd", "Vb", "ART", "KT", "BT", "AM3", "gC", "st"))
            kgt, kbs, kvk, kyt = ("gt", b), ("bs", b), ("vk", b), ("yt", b)
            if tt == 0:
                P.pool(lambda e: e.memset(st["ST"][:], 0.0), w=[kST])
                P.pool(lambda e: e.memset(st["STb"][:], 0.0), r=[kST], w=[kST])
            i2 = (tt * NB + b) % 2
            r0 = b * S + tt * 128
            U_, Up_ = Ut[i2], Up[i2]
            kU_, kUp = ("Ut", i2), ("Up", i2)
            cs = slice(C_RWKV, C_RWKV + 1792)
            P.dma(U_[:], u_tok[r0:r0 + 128, cs], w=[kU_])
            if tt == 0:
                P.pool(lambda e, Up_=Up_: e.memset(Up_[0:1, :], 0.0), w=[kUp])
                P.dma(Up_[1:128, :], u_tok[r0:r0 + 127, cs], w=[kUp])
            else:
                P.dma(Up_[:], u_tok[r0 - 1:r0 + 127, cs], w=[kUp])
            P.pool(lambda e, U_=U_, Up_=Up_: e.tensor_sub(Up_[:], Up_[:], U_[:]), r=[kU_, kUp], w=[kUp])
            P.pool(lambda e, Up_=Up_: e.tensor_mul(Up_[:], Up_[:], mu_b[:]), r=[kUp, "rw_mu"], w=[kUp])
            P.dve(lambda e, U_=U_, Up_=Up_: e.tensor_add(xs[:], U_[:], Up_[:]), r=[kU_, kUp], w=["xs"])
            r_, k_, v_ = xs[:, 0:512], xs[:, 512:1024], xs[:, 1024:1536]
            P.act(lambda e: e.copy(pb["vk"][:], v_), r=["xs"], w=[kvk])
            P.act(lambda e: e.activation(la[:, 0:64], xs[:, 1536:1600], AF.Tanh), r=["xs"], w=["la"])
            P.act(lambda e: e.copy(la[:, 64:128], xs[:, 1600:1664]), r=["xs"], w=["la"])
            P.act(lambda e: e.activation(la[:, 128:256], xs[:, 1664:1792], AF.Sigmoid), r=["xs"], w=["la"])
            for j in range(2):
                P.pe(lambda e, j=j: e.transpose(psb[7][:, j * 128:(j + 1) * 128], la[:, j * 128:(j + 1) * 128], c["ident"][:]),
                     r=["la", "ident"], w=[("ps", 7)])
            P.act(lambda e: e.copy(lT[:], psb[7][:, 0:256]), r=[("ps", 7)], w=["lT"])
            P.pe(lambda e: e.matmul(psb[3][:], lT[0:64, 0:128], w2b[0:64, :], start=True, stop=True), r=["lT", "rw_w2"], w=[("ps", 3)])
            P.pe(lambda e: e.matmul(psb[4][:], lT[64:128, 0:128], a2b[64:128, :], start=True, stop=True), r=["lT", "rw_a2"], w=[("ps", 4)])
            P.pe(lambda e: e.matmul(psb[5][:], lT[:, 128:256], g2b[:], start=True, stop=True), r=["lT", "rw_g2"], w=[("ps", 5)])
            P.dve(lambda e: e.tensor_add(T_["lw"][:], psb[3][:], bc["w0"][:]), r=[("ps", 3), "rw_w0"], w=["lw"])
            P.act(lambda e: e.activation(T_["lw"][:], T_["lw"][:], AF.Sigmoid), r=["lw"], w=["lw"])
            P.act(lambda e: e.mul(T_["lw"][:], T_["lw"][:], -float(np.exp(-0.5))), r=["lw"], w=["lw"])
            P.dve(lambda e: e.tensor_add(T_["aa"][:], psb[4][:], bc["a0"][:]), r=[("ps", 4), "rw_a0"], w=["aa"])
            P.act(lambda e: e.activation(T_["aa"][:], T_["aa"][:], AF.Sigmoid), r=["aa"], w=["aa"])
            P.act(lambda e: e.copy(pb["gt"][:], psb[5][:]), r=[("ps", 5)], w=[kgt])
            P.pe(lambda e: e.matmul(psb[3][:], c["MI"][:], T_["lw"][:], start=True, stop=True), r=["dplrmask", "lw"], w=[("ps", 3)])
            P.pe(lambda e: e.matmul(psb[4][:], c["BLK"][:], T_["lw"][:], start=True, stop=True), r=["dplrmask", "lw"], w=[("ps", 4)])
            P.act(lambda e: e.copy(T_["Gs"][:], psb[3][:]), r=[("ps", 3)], w=["Gs"])
            P.act(lambda e: e.activation(T_["eG"][:], T_["Gs"][:], AF.Exp), r=["Gs"], w=["eG"])
            P.act(lambda e: e.activation(T_["enG"][:], T_["Gs"][:], AF.Exp, scale=-1.0), r=["Gs"], w=["enG"])
            P.dve(lambda e: e.tensor_sub(T_["eGm"][:], T_["Gs"][:], T_["lw"][:]), r=["Gs", "lw"], w=["eGm"])
            P.act(lambda e: e.activation(T_["eGm"][:], T_["eGm"][:], AF.Exp), r=["eGm"], w=["eGm"])
            P.dve(lambda e: e.tensor_sub(T_["eD"][:], psb[4][:], T_["Gs"][:]), r=[("ps", 4), "Gs"], w=["eD"])
            P.act(lambda e: e.activation(T_["eD"][:], T_["eD"][:], AF.Exp), r=["eD"], w=["eD"])
            P.dve(lambda e: e.tensor_mul(T_["kx"][:], k_, bc["k_k"][:]), r=["xs", "rw_k_k"], w=["kx"])
            P.pool(lambda e: e.tensor_mul(T_["sq"][:], T_["kx"][:], T_["kx"][:]), r=["kx"], w=["sq"])
            P.dve(lambda e: e.tensor_reduce(small["ssq"][:], h3(T_["sq"][:]), AX.X, ALU.add), r=["sq"], w=["ssq"])
            P.dve(lambda e: e.tensor_scalar_add(small["rn"][:], small["ssq"][:], 1e-6), r=["ssq"], w=["rn"])
            P.act(lambda e: e.activation(small["rn"][:], small["rn"][:], AF.Sqrt), r=["rn"], w=["rn"])
            P.dve(lambda e: e.reciprocal(small["rn"][:], small["rn"][:]), r=["rn"], w=["rn"])
            P.dve(lambda e: e.tensor_mul(h3(T_["kk"][:]), h3(T_["kx"][:]), b3(small["rn"][:])), r=["kx", "rn"], w=["kk"])
            P.dve(lambda e: e.scalar_tensor_tensor(T_["k2"][:], T_["aa"][:], -1.0, bc["k_a"][:], ALU.add, ALU.mult), r=["aa", "rw_k_a"], w=["k2"])
            P.dve(lambda e: e.scalar_tensor_tensor(T_["k2"][:], T_["k2"][:], 1.0, k_, ALU.add, ALU.mult), r=["k2", "xs"], w=["k2"])
            P.dve(lambda e: e.tensor_mul(T_["bv"][:], T_["kk"][:], T_["aa"][:]), r=["kk", "aa"], w=["bv"])
            P.pool(lambda e: e.tensor_mul(T_["tR"][:], r_, T_["eG"][:]), r=["xs", "eG"], w=["tR"])
            P.pool(lambda e: e.tensor_mul(T_["tK"][:], T_["k2"][:], T_["enG"][:]), r=["k2", "enG"], w=["tK"])
            P.pool(lambda e: e.tensor_mul(T_["tB"][:], T_["bv"][:], T_["enG"][:]), r=["bv", "enG"], w=["tB"])
            P.dve(lambda e: e.scalar_tensor_tensor(T_["tA"][:], T_["kk"][:], -1.0, T_["eGm"][:], ALU.mult, ALU.mult), r=["kk", "eGm"], w=["tA"])
            kgeo = ("geo", tag)
            P.pool(lambda e: e.tensor_mul(Kd[:], T_["k2"][:], T_["eD"][:]), r=["k2", "eD"], w=[kgeo])
            P.pool(lambda e: e.tensor_mul(Bd[:], T_["bv"][:], T_["eD"][:]), r=["bv", "eD"], w=[kgeo])
            P.act(lambda e: e.copy(Vb[:], v_), r=["xs"], w=[kgeo])
            P.pool(lambda e: e.tensor_mul(T_["rk"][:], r_, T_["k2"][:]), r=["xs", "k2"], w=["rk"])
            P.pool(lambda e: e.tensor_mul(T_["rk"][:], T_["rk"][:], bc["r_k"][:]), r=["rk", "rw_r_k"], w=["rk"])
            P.dve(lambda e: e.tensor_reduce(pb["bs"][:], h3(T_["rk"][:]), AX.X, ALU.add), r=["rk"], w=[kbs])
            for (src, ksrc, dst) in ((T_["tA"], "tA", lambda q: ART[:, q, 0, :]), (T_["tR"], "tR", lambda q: ART[:, q, 1, :]),
                                      (T_["tK"], "tK", lambda q: KT[:, q, :]), (T_["tB"], "tB", lambda q: BT[:, q, :])):
                for q in range(4):
                    P.pe(lambda e, src=src, q=q: e.transpose(psb[7][:, q * 128:(q + 1) * 128], src[:, q * 128:(q + 1) * 128], c["ident"][:]),
                         r=[ksrc, "ident"], w=[("ps", 7)])
                if ksrc in ("tA", "tR"):
                    j = 0 if ksrc == "tA" else 1
                    for x in range(2):
                        pr = slice(x * 64, (x + 1) * 64)
                        dst_ = ART[pr, :, :, :].rearrange("p (q x) a t -> p q x a t", x=2)[:, :, x, j, :]
                        if x == 0:
                            P.act(lambda e, dst_=dst_, pr=pr: e.copy(dst_, psb[7][pr, :].rearrange("p (q t) -> p q t", q=4)), r=[("ps", 7), ("ARTz0", b)], w=[kgeo])
                        else:
                            P.dve(lambda e, dst_=dst_, pr=pr: e.tensor_copy(dst_, psb[7][pr, :].rearrange("p (q t) -> p q t", q=4)), r=[("ps", 7), ("ARTz0", b)], w=[kgeo])
                elif ksrc == "tK":
                    P.dve(lambda e: e.tensor_copy(KT[:], psb[7][:].rearrange("p (q t) -> p q t", q=4)), r=[("ps", 7)], w=[kgeo])
                else:
                    P.dve(lambda e: e.tensor_copy(BT[:], psb[7][:].rearrange("p (q t) -> p q t", q=4)), r=[("ps", 7)], w=[kgeo])
            for q in range(4):
                P.pe(lambda e, q=q: e.transpose(psb[7][:, q * 128:(q + 1) * 128], T_["eG"][:, q * 128:(q + 1) * 128], c["ident"][:]),
                     r=["eG", "ident"], w=[("ps", 7)])
            pv7 = psb[7][:].rearrange("p (q t) -> p q t", q=4)
            P.dve(lambda e: e.tensor_copy(gC[0][:], pv7[:, :, 63]), r=[("ps", 7)], w=[kgeo])
            P.dve(lambda e: e.tensor_copy(gC[1][:], pv7[:, :, 127]), r=[("ps", 7)], w=[kgeo])
            kX0, kZ0 = ("dX", tag, 0), ("dZ", tag, 0)
            for h in range(8):
                q, hb = h // 2, (h % 2) * 64
                pa = psb[h % 2]
                kpa = ("ps", h % 2)
                rhs2 = ART[:, h, :, :].rearrange("p a t -> p (a t)")
                P.pe(lambda e, pa=pa, q=q, rhs2=rhs2: e.matmul(pa[:, 0:256], BT[:, q, :], rhs2, start=True, stop=True), r=[kgeo], w=[kpa])
                P.pe(lambda e, pa=pa, q=q, rhs2=rhs2: e.matmul(pa[:, 256:512], KT[:, q, :], rhs2, start=True, stop=True), r=[kgeo], w=[kpa])
                pav = pa[:].rearrange("p (a t) -> p a t", a=4)
                P.dve(lambda e, h=h, pav=pav: e.tensor_tensor(st["X"][0][:, h, :], pav[:, 0, :], mask4[:, 0, :], ALU.mult), r=[kpa, "mask4"], w=[kX0])
                P.dve(lambda e, h=h, pav=pav: e.tensor_tensor(AM3[:, h, :].rearrange("p (a t) -> p a t", a=3), pav[:, 1:4, :], mask4[:, 1:4, :], ALU.mult),
                      r=[kpa, "mask4"], w=[("AM3", tag)])
                pz = psb[2]
                P.pe(lambda e, q=q, hb=hb, h=h: e.matmul(psb[2][:, (h % 4) * 128:(h % 4 + 1) * 128], ART[:, h, 0, :], BT[:, q, :],
                                                        start=True, stop=True), r=[kgeo], w=[("ps", 2)])
                if h % 4 == 3:
                    g4 = h // 4
                    P.dve(lambda e, g4=g4: e.tensor_tensor(st["Z"][0][:, g4 * 4:(g4 + 1) * 4, :], psb[2][:].rearrange("p (h t) -> p h t", h=4), MZ4, ALU.mult),
                          r=[("ps", 2), "dplrmask"], w=[kZ0])
            geo = dict(
                AT=lambda h: (ART[:, h, 0, :], [kgeo]),
                RT=lambda h: (ART[:, h, 1, :], [kgeo]),
                ARB=lambda h: (AM3[:, h, 0:128], [("AM3", tag)]),
                AAK=lambda h: (AM3[:, h, 128:256], [("AM3", tag)]),
                ARK=lambda h: (AM3[:, h, 256:384], [("AM3", tag)]),
                V=lambda h: (Vb[:, h * 64:(h + 1) * 64], [kgeo]),
                BD=(Bd, [kgeo]), KD=(Kd, [kgeo]), GC=lambda ch: (gC[ch], [kgeo]),
                Y=(pb["yt"], kyt), WADD=None)
            return geo

        def tile_post(b, tt):
            pb = PB[b]
            r0 = b * S + tt * 128
            kgt, kbs, kvk, kyt = ("gt", b), ("bs", b), ("vk", b), ("yt", b)
            yt, y2 = pb["yt"], T_["y2"]
            P.pool(lambda e: e.tensor_mul(y2[:], yt[:], yt[:]), r=[kyt], w=["y2"])
            P.dve(lambda e: e.tensor_reduce(small["s1"][:], h3(yt[:]), AX.X, ALU.add), r=[kyt], w=["s1"])
            P.dve(lambda e: e.tensor_reduce(small["s2"][:], h3(y2[:]), AX.X, ALU.add), r=["y2"], w=["s2"])
            P.dve(lambda e: e.tensor_scalar_mul(small["mean"][:], small["s1"][:], 1.0 / 64), r=["s1"], w=["mean"])
            P.dve(lambda e: e.tensor_mul(small["var"][:], small["mean"][:], small["mean"][:]), r=["mean"], w=["var"])
            P.dve(lambda e: e.scalar_tensor_tensor(small["var"][:], small["s2"][:], 1.0 / 64, small["var"][:], ALU.mult, ALU.subtract), r=["s2", "var"], w=["var"])
            P.dve(lambda e: e.tensor_scalar_add(small["rstd"][:], small["var"][:], 64e-5), r=["var"], w=["rstd"])
            P.act(lambda e: e.activation(small["rstd"][:], small["rstd"][:], AF.Sqrt), r=["rstd"], w=["rstd"])
            P.dve(lambda e: e.reciprocal(small["rstd"][:], small["rstd"][:]), r=["rstd"], w=["rstd"])
            P.dve(lambda e: e.tensor_sub(h3(yt[:]), h3(yt[:]), b3(small["mean"][:])), r=[kyt, "mean"], w=[kyt])
            P.dve(lambda e: e.tensor_mul(h3(yt[:]), h3(yt[:]), b3(small["rstd"][:])), r=[kyt, "rstd"], w=[kyt])
            P.pool(lambda e: e.tensor_mul(yt[:], yt[:], bc["ln_g"][:]), r=[kyt, "rw_ln_g"], w=[kyt])
            P.pool(lambda e: e.tensor_add(yt[:], yt[:], bc["ln_b"][:]), r=[kyt, "rw_ln_b"], w=[kyt])
            P.dve(lambda e: e.tensor_mul(h3(y2[:]), h3(pb["vk"][:]), b3(pb["bs"][:])), r=[kvk, kbs, "y2"], w=["y2"])
            P.dve(lambda e: e.tensor_add(yt[:], yt[:], y2[:]), r=[kyt, "y2"], w=[kyt])
            P.dve(lambda e: e.tensor_mul(y2[:], yt[:], pb["gt"][:]), r=[kyt, kgt], w=["y2"])
            P.dma(mixed[r0:r0 + 128, 1024:1536], y2[:], r=["y2"], q="pool")
    for tt in range(nt):
        geos = [tile_prep(b, tt) for b in range(NB)]
        run_interleaved([dplr_tile(cx, 8, 64, geos[b], PB[b]["st"], "rw%d" % b, banks=(3 * (b % 2), 3 * (b % 2) + 1, 3 * (b % 2) + 2))
                         for b in range(NB)])
        for b in range(NB):
            tile_post(b, tt)
    P.barrier()
    sb.reset(m)


def stage_gdn(cx, u_tok, prm, mixed):
    nc, P, sb, c, psb = cx.nc, cx.P, cx.sb, cx.c, cx.ps
    S, NB = cx.S, cx.NB
    nt = S // 128
    m = sb.mark()
    dplr_consts(cx)
    NH, NK = 4, 128
    cw_b = [sb.alloc([128, 1536], F32, "cw_b") for _ in range(4)]
    for j in range(4):
        bcast_load(cx, cw_b[j][:], prm["conv_w"][j, :], "gd_cw")
    A_b = sb.alloc([128, 4], F32, "A_b")
    dtb_b = sb.alloc([128, 4], F32, "dtb_b")
    ng_b = sb.alloc([128, 4, 128], F32, "ng_b")
    bcast_load(cx, A_b[:], prm["a_log"], "gd_A")
    bcast_load(cx, dtb_b[:], prm["dt_bias"], "gd_dtb")
    for h in range(4):
        bcast_load(cx, ng_b[:, h, :], prm["norm_g"], "gd_ng")
    P.act(lambda e: e.activation(A_b[:], A_b[:], AF.Exp), r=["gd_A"], w=["gd_A"])
    ones = sb.alloc([128, 128], F32, "ones")
    P.pool(lambda e: e.memset(ones[:], 1.0), w=["ones"])
    BIGM = sb.alloc([128, 4, 128], F32, "BIGM")
    for h in range(4):
        P.dve(lambda e, h=h: e.tensor_scalar(BIGM[:, h, :], c["ML"][:], -1.0, 30000.0, ALU.add, ALU.mult), r=["dplrmask"], w=["BIGM"])
    P.dve(lambda e: e.tensor_scalar_mul(BIGM[:], BIGM[:], -1.0), r=["BIGM"], w=["BIGM"])
    MZ4 = c["MZ"][:].unsqueeze(1).to_broadcast([128, 4, 128])
    sh = [sb.alloc([128, 1536], F32, "sh") for _ in range(4)]
    acc = sb.alloc([128, 1536], F32, "acc")
    zab = sb.alloc([128, 520], F32, "zab")
    f4 = lambda n: sb.alloc([128, 4], F32, n)
    sm = {n: f4(n) for n in ("ssq", "rnq", "rnk", "beta", "nbeta", "sp", "g", "gcs", "egc", "edec", "ssqo", "tmp", "tmp2")}
    gC = [f4("gC0"), f4("gC1")]
    diag = sb.alloc([128, 4, 128], F32, "diag")
    D4 = sb.alloc([128, 4, 128], F32, "D4")
    DS4 = sb.alloc([128, 4, 128], F32, "DS4")
    bf = lambda n: sb.alloc([128, 512], BF16, n)
    qn, kn, ta, tr, Kd, Vp = bf("qn"), bf("kn"), bf("ta"), bf("tr"), bf("Kd"), bf("Vp")
    kT, qT, AT, RT = [sb.alloc([128, 4, 128], BF16, n) for n in ("kT", "qT", "AT", "RT")]
    Zt, Art = sb.alloc([128, 4, 128], BF16, "Zt"), sb.alloc([128, 4, 128], BF16, "Art")
    XK, ArT = sb.alloc([128, 4, 128], BF16, "XK"), sb.alloc([128, 4, 128], BF16, "ArT")
    yt = sb.alloc([128, 512], F32, "yt")
    y2 = sb.alloc([128, 512], F32, "y2")
    sz = sb.alloc([128, 512], F32, "sz")
    st = dict(X=[sb.alloc([128, 4, 128], BF16, "dX") for _ in range(2)], Z=[sb.alloc([128, 4, 128], BF16, "dZ") for _ in range(2)],
              P=[sb.alloc([128, 4, 128], BF16, "dP") for _ in range(2)], ST=sb.alloc([128, 4, 128], F32, "ST"),
              STb=sb.alloc([128, 4, 128], BF16, "STb"), Wb=sb.alloc([128, 512], BF16, "Wb"), Ub=sb.alloc([128, 512], BF16, "Ub"))
    h3 = lambda t: t.rearrange("p (h d) -> p h d", h=4)
    b3 = lambda t: t.unsqueeze(2).to_broadcast([128, 4, 128])
    pbf = lambda i: psb[i][:].bitcast(BF16)
    PB = []
    for _b in range(NB):
        PB.append(dict(Kd=bf("Kd"), Vp=bf("Vp"), AT=sb.alloc([128, 4, 128], BF16, "AT"), RT=sb.alloc([128, 4, 128], BF16, "RT"),
                       XK=sb.alloc([128, 4, 128], BF16, "XK"), ArT=sb.alloc([128, 4, 128], BF16, "ArT"), gC=[f4("gC0"), f4("gC1")],
                       yt=sb.alloc([128, 512], F32, "ytb"), sz=sb.alloc([128, 512], F32, "szb"),
                       st=dict(X=[sb.alloc([128, 4, 128], BF16, "dX") for _ in range(2)], Z=[sb.alloc([128, 4, 128], BF16, "dZ") for _ in range(2)],
                               P=[sb.alloc([128, 4, 128], BF16, "dP") for _ in range(2)], ST=sb.alloc([128, 4, 128], F32, "ST"),
                               STb=sb.alloc([128, 4, 128], BF16, "STb"), Wb=sb.alloc([128, 512], BF16, "Wb"), Ub=sb.alloc([128, 512], BF16, "Ub"))))
    if True:
        def tile_prep(b, tt):
            tag = "gd%d" % b
            kST = ("dST", tag)
            pb = PB[b]
            Kd, Vp, AT, RT, XK, ArT, gC, st, yt, sz = (pb[k] for k in ("Kd", "Vp", "AT", "RT", "XK", "ArT", "gC", "st", "yt", "sz"))
            kyt, ksz = ("yt", b), ("sz", b)
            if tt == 0:
                P.pool(lambda e: e.memset(st["ST"][:], 0.0), w=[kST])
                P.pool(lambda e: e.memset(st["STb"][:], 0.0), r=[kST], w=[kST])
            r0 = b * S + tt * 128
            for j in range(4):
                d = 3 - j
                ksh = ("sh", j)
                if tt == 0 and d > 0:
                    P.pool(lambda e, j=j, d=d: e.memset(sh[j][0:d, :], 0.0), w=[ksh])
                    P.dma(sh[j][d:128, :], u_tok[r0:r0 + 128 - d, C_GDN:C_GDN + 1536], w=[ksh])
                else:
                    P.dma(sh[j][:], u_tok[r0 - d:r0 - d + 128, C_GDN:C_GDN + 1536], w=[ksh])
            P.dma(zab[:], u_tok[r0:r0 + 128, C_GDN + 1536:C_GDN + 2056], w=["zab"])
            P.dve(lambda e: e.tensor_mul(acc[:], sh[3][:], cw_b[3][:]), r=[("sh", 3), "gd_cw"], w=["acc"])
            for j in range(3):
                P.pool(lambda e, j=j: e.tensor_mul(sh[j][:], sh[j][:], cw_b[j][:]), r=[("sh", j), "gd_cw"], w=[("sh", j)])
                P.dve(lambda e, j=j: e.tensor_add(acc[:], acc[:], sh[j][:]), r=[("sh", j), "acc"], w=["acc"])
            P.act(lambda e: e.activation(acc[:], acc[:], AF.Silu), r=["acc"], w=["acc"])
            q_, k_, v_ = acc[:, 0:512], acc[:, 512:1024], acc[:, 1024:1536]
            P.pool(lambda e: e.tensor_mul(y2[:], q_, q_), r=["acc"], w=["y2"])
            P.dve(lambda e: e.tensor_reduce(sm["ssq"][:], h3(y2[:]), AX.X, ALU.add), r=["y2"], w=["ssq"])
            P.dve(lambda e: e.tensor_scalar_add(sm["rnq"][:], sm["ssq"][:], 1e-6), r=["ssq"], w=["rnq"])
            P.act(lambda e: e.activation(sm["rnq"][:], sm["rnq"][:], AF.Sqrt), r=["rnq"], w=["rnq"])
            P.dve(lambda e: e.reciprocal(sm["rnq"][:], sm["rnq"][:]), r=["rnq"], w=["rnq"])
            P.dve(lambda e: e.tensor_scalar_mul(sm["rnq"][:], sm["rnq"][:], 128 ** -0.5), r=["rnq"], w=["rnq"])
            P.pool(lambda e: e.tensor_mul(y2[:], k_, k_), r=["acc", "ssq"], w=["y2"])
            P.dve(lambda e: e.tensor_reduce(sm["ssq"][:], h3(y2[:]), AX.X, ALU.add), r=["y2", "rnq"], w=["ssq"])
            P.dve(lambda e: e.tensor_scalar_add(sm["rnk"][:], sm["ssq"][:], 1e-6), r=["ssq"], w=["rnk"])
            P.act(lambda e: e.activation(sm["rnk"][:], sm["rnk"][:], AF.Sqrt), r=["rnk"], w=["rnk"])
            P.dve(lambda e: e.reciprocal(sm["rnk"][:], sm["rnk"][:]), r=["rnk"], w=["rnk"])
            kg = ("gdgeo", tag)
            P.dve(lambda e: e.tensor_mul(h3(qn[:]), h3(q_), b3(sm["rnq"][:])), r=["acc", "rnq"], w=[kg])
            P.dve(lambda e: e.tensor_mul(h3(kn[:]), h3(k_), b3(sm["rnk"][:])), r=["acc", "rnk"], w=[kg])
            P.act(lambda e: e.activation(sm["beta"][:], zab[:, 512:516], AF.Sigmoid), r=["zab"], w=["beta"])
            P.dve(lambda e: e.tensor_scalar_mul(sm["nbeta"][:], sm["beta"][:], -1.0), r=["beta"], w=["nbeta"])
            P.dve(lambda e: e.tensor_add(sm["sp"][:], zab[:, 516:520], dtb_b[:]), r=["zab", "gd_dtb"], w=["sp"])
            P.act(lambda e: e.activation(sm["tmp"][:], sm["sp"][:], AF.Abs), r=["sp"], w=["tmp"])
            P.act(lambda e: e.activation(sm["tmp"][:], sm["tmp"][:], AF.Exp, scale=-1.0), r=["tmp"], w=["tmp"])
            P.act(lambda e: e.activation(sm["tmp"][:], sm["tmp"][:], AF.Ln, bias=1.0), r=["tmp"], w=["tmp"])
            P.dve(lambda e: e.tensor_scalar_max(sm["sp"][:], sm["sp"][:], 0.0), r=["sp"], w=["sp"])
            P.dve(lambda e: e.tensor_add(sm["sp"][:], sm["sp"][:], sm["tmp"][:]), r=["sp", "tmp"], w=["sp"])
            P.dve(lambda e: e.scalar_tensor_tensor(sm["g"][:], sm["sp"][:], -1.0, A_b[:], ALU.mult, ALU.mult), r=["sp", "gd_A"], w=["g"])
            P.pe(lambda e: e.matmul(psb[7][:, 0:4], c["MI"][:], sm["g"][:], start=True, stop=True), r=["dplrmask", "g"], w=[("ps", 7)])
            P.pe(lambda e: e.matmul(psb[7][:, 4:8], c["BLK"][:], sm["g"][:], start=True, stop=True), r=["dplrmask", "g"], w=[("ps", 7)])
            P.act(lambda e: e.copy(sm["gcs"][:], psb[7][:, 0:4]), r=[("ps", 7)], w=["gcs"])
            P.dve(lambda e: e.tensor_sub(sm["edec"][:], psb[7][:, 4:8], sm["gcs"][:]), r=[("ps", 7), "gcs"], w=["edec"])
            P.act(lambda e: e.activation(sm["edec"][:], sm["edec"][:], AF.Exp), r=["edec"], w=["edec"])
            P.act(lambda e: e.activation(sm["egc"][:], sm["gcs"][:], AF.Exp), r=["gcs"], w=["egc"])
            P.pe(lambda e: e.matmul(psb[3][:, 0:4], ones[0:64, :], sm["g"][0:64, :], start=True, stop=True), r=["ones", "g"], w=[("ps", 3)])
            P.pe(lambda e: e.matmul(psb[4][:, 0:4], ones[64:128, :], sm["g"][64:128, :], start=True, stop=True), r=["ones", "g"], w=[("ps", 4)])
            P.act(lambda e: e.activation(gC[0][:], psb[3][:, 0:4], AF.Exp), r=[("ps", 3)], w=[kg])
            P.act(lambda e: e.activation(gC[1][:], psb[4][:, 0:4], AF.Exp), r=[("ps", 4)], w=[kg])
            P.dve(lambda e: e.tensor_mul(h3(tr[:]), h3(qn[:]), b3(sm["egc"][:])), r=[kg, "egc"], w=["tr"])
            P.dve(lambda e: e.tensor_mul(sm["tmp2"][:], sm["nbeta"][:], sm["egc"][:]), r=["nbeta", "egc"], w=["tmp2"])
            P.dve(lambda e: e.tensor_mul(h3(ta[:]), h3(kn[:]), b3(sm["tmp2"][:])), r=[kg, "tmp2"], w=["ta"])
            P.dve(lambda e: e.tensor_mul(h3(Kd[:]), h3(kn[:]), b3(sm["edec"][:])), r=[kg, "edec"], w=[kg])
            P.dve(lambda e: e.tensor_mul(h3(Vp[:]), h3(v_), b3(sm["beta"][:])), r=["acc", "beta"], w=[kg])
            for (src, ksrc, dst) in ((kn, kg, kT), (qn, kg, qT), (ta, "ta", AT), (tr, "tr", RT)):
                for h in range(4):
                    P.pe(lambda e, src=src, h=h: e.transpose(pbf(7)[:, h * 128:(h + 1) * 128], src[:, h * 128:(h + 1) * 128], c["identb"][:]),
                         r=[ksrc, "identb"], w=[("ps", 7)])
                P.act(lambda e, dst=dst: e.copy(dst[:], pbf(7)[:, 0:512].rearrange("p (h t) -> p h t", h=4)), r=[("ps", 7)], w=[kg])
            for h in range(4):
                P.dve(lambda e, h=h: e.tensor_scalar_mul(diag[:, h, :], c["ident"][:], sm["gcs"][:, h:h + 1]), r=["gcs", "ident"], w=["diag"])
            P.pe(lambda e: e.matmul(psb[5][:], ones[:], diag[:].rearrange("p h t -> p (h t)"), start=True, stop=False), r=["ones", "diag"], w=[("ps", 5)])
            P.pe(lambda e: e.matmul(psb[5][:], c["ident"][:], BIGM[:].rearrange("p h t -> p (h t)"), start=False, stop=True), r=["ident", "BIGM"], w=[("ps", 5)])
            for h in range(4):
                P.act(lambda e, h=h: e.activation(D4[:, h, :], psb[5][:, h * 128:(h + 1) * 128], AF.Exp, bias=sm["gcs"][:, h:h + 1], scale=-1.0),
                      r=[("ps", 5), "gcs"], w=["D4"])
            P.pool(lambda e: e.tensor_mul(DS4[:], D4[:], MZ4), r=["D4", "dplrmask"], w=["DS4"])
            for h in range(4):
                P.pe(lambda e, h=h: e.matmul(psb[3][:, h * 128:(h + 1) * 128], kT[:, h, :], kT[:, h, :], start=True, stop=True), r=[kg], w=[("ps", 3)])
            for h in range(4):
                P.pe(lambda e, h=h: e.matmul(psb[4][:, h * 128:(h + 1) * 128], qT[:, h, :], kT[:, h, :], start=True, stop=True), r=[kg], w=[("ps", 4)])
            for h in range(4):
                P.dve(lambda e, h=h: e.scalar_tensor_tensor(Zt[:, h, :], psb[3][:, h * 128:(h + 1) * 128], sm["nbeta"][:, h:h + 1], DS4[:, h, :],
                                                          ALU.mult, ALU.mult), r=[("ps", 3), "nbeta", "DS4"], w=["Zt"])
            P.dve(lambda e: e.tensor_tensor(Art[:], psb[4][:].rearrange("p (h t) -> p h t", h=4), D4[:], ALU.mult), r=[("ps", 4), "D4"], w=["Art"])
            kX0, kZ0 = ("dX", tag, 0), ("dZ", tag, 0)
            P.pool(lambda e: e.tensor_copy(st["Z"][0][:], Zt[:]), r=["Zt"], w=[kZ0])
            for h in range(4):
                P.pe(lambda e, h=h: e.transpose(pbf(7)[:, h * 128:(h + 1) * 128], Zt[:, h, :], c["identb"][:]), r=["Zt", "identb"], w=[("ps", 7)])
            P.act(lambda e: e.copy(XK[:], pbf(7)[:, 0:512].rearrange("p (h t) -> p h t", h=4)), r=[("ps", 7)], w=[kg])
            P.dve(lambda e: e.tensor_copy(st["X"][0][:], pbf(7)[:, 0:512].rearrange("p (h t) -> p h t", h=4)), r=[("ps", 7)], w=[kX0])
            for h in range(4):
                P.pe(lambda e, h=h: e.transpose(pbf(7)[:, h * 128:(h + 1) * 128], Art[:, h, :], c["identb"][:]), r=["Art", "identb"], w=[("ps", 7)])
            P.act(lambda e: e.copy(ArT[:], pbf(7)[:, 0:512].rearrange("p (h t) -> p h t", h=4)), r=[("ps", 7)], w=[kg])
            geo = dict(
                AT=lambda h: (AT[:, h, :], [kg]), RT=lambda h: (RT[:, h, :], [kg]),
                ARB=lambda h: (ArT[:, h, :], [kg]), ARK=lambda h: (ArT[:, h, :], [kg]), AAK=lambda h: (XK[:, h, :], [kg]),
                V=lambda h: (Vp[:, h * 128:(h + 1) * 128], [kg]),
                BD=(Kd, [kg]), KD=(Kd, [kg]), GC=lambda ch: (gC[ch], [kg]), Y=(yt, kyt), WADD=None)
            P.act(lambda e: e.activation(sz[:], zab[:, 0:512], AF.Silu), r=["zab"], w=[ksz])
            return geo

        def tile_post(b, tt):
            pb = PB[b]
            yt, sz = pb["yt"], pb["sz"]
            kyt, ksz = ("yt", b), ("sz", b)
            r0 = b * S + tt * 128
            P.pool(lambda e: e.tensor_mul(y2[:], yt[:], yt[:]), r=[kyt], w=["y2"])
            P.dve(lambda e: e.tensor_reduce(sm["ssqo"][:], h3(y2[:]), AX.X, ALU.add), r=["y2"], w=["ssqo"])
            rstd_from_ssq(P, sm["ssqo"][:], sm["ssqo"][:], 128, RMS_EPS, ["ssqo"], ["ssqo"])
            P.dve(lambda e: e.tensor_mul(h3(yt[:]), h3(yt[:]), b3(sm["ssqo"][:])), r=[kyt, "ssqo"], w=[kyt])
            P.pool(lambda e: e.tensor_mul(yt[:], yt[:], ng_b[:].rearrange("p h d -> p (h d)")), r=[kyt, "gd_ng"], w=[kyt])
            P.dve(lambda e: e.tensor_mul(y2[:], yt[:], sz[:]), r=[kyt, ksz, "ssqo"], w=["y2"])
            P.dma(mixed[r0:r0 + 128, 1536:2048], y2[:], r=["y2"], q="pool")
    for tt in range(nt):
        geos = [tile_prep(b, tt) for b in range(NB)]
        run_interleaved([dplr_tile(cx, 4, 128, geos[b], PB[b]["st"], "gd%d" % b, banks=(3 * (b % 2), 3 * (b % 2) + 1, 3 * (b % 2) + 2))
                         for b in range(NB)])
        for b in range(NB):
            tile_post(b, tt)
    P.barrier()
    sb.reset(m)


MOE_PHASES = "123"
MOE_CAST = "ad"
MOE_CAP = 384


def stage_moe(cx, h1, prm, x_out, xs_d, ys_d):
    nc, P, sb, c, psb = cx.nc, cx.P, cx.sb, cx.c, cx.ps
    T = cx.T
    ntt = T // 128
    CAP = MOE_CAP
    NROW = 32 * CAP
    BIG = 1.0e4
    m = sb.mark()
    route_i = sb.alloc([128, ntt, 2], I32, "route_i")
    route_g = sb.alloc([128, ntt, 2], F32, "route_g")
    m2 = sb.mark()
    Wr = sb.alloc([128, 16, 36], F32, "Wr")
    P.dma(Wr[:, :, 0:4], prm["w_grp"].rearrange("(kc p) n -> p kc n", p=128), w=["Wr"], slow=True)
    P.dma(Wr[:, :, 4:36], prm["w_exp"].rearrange("(kc p) n -> p kc n", p=128), w=["Wr"], slow=True)
    br_b = sb.alloc([128, 36], F32, "br_b")
    bcast_load(cx, br_b[:, 0:4], prm["b_grp"], "br")
    bcast_load(cx, br_b[:, 4:36], prm["b_exp"], "br")
    SUb, eoff, trash = c["SUb"], c["eoff"], c["trash"]
    onesb = sb.alloc([128, 128], BF16, "onesb")
    P.pool(lambda e: e.memset(onesb[:], 1.0), w=["onesb"])
    zf = sb.alloc([128, 2048], F32, "zf")
    P.pool(lambda e: e.memset(zf[:], 0.0), w=["zf"])
    P.dma(ys_d[NROW:NROW + 128, :], zf[:], r=["zf"], w=["ys_d0"])
    carry = sb.alloc([128, 32], F32, "carry")
    P.pool(lambda e: e.memset(carry[:], 0.0), w=["carry"])
    hrow = [sb.alloc([128, 2048], F32, "hrow") for _ in range(2)]
    hbf = [sb.alloc([128, 2048], BF16, "hbf") for _ in range(2)]
    hT = [sb.alloc([128, 16, 128], F32, "hT") for _ in range(2)]
    f = lambda n, w: sb.alloc([128, w], F32, n)
    lg, mg, eg, sg_, pg, gsel, tmp4 = f("lg", 36), f("mg", 1), f("eg", 4), f("sg", 1), f("pg", 1), f("gsel", 4), f("tmp4", 4)
    lem, top8, sel1, sel2, selb_f, pos, tmp32 = f("lem", 32), f("top8", 8), f("sel1", 32), f("sel2", 32), f("selsum", 32), f("pos", 32), f("tmp32", 32)
    selb = sb.alloc([128, 32], BF16, "selb")
    dd, w1, w2, slotf, valid = f("dd", 1), f("w1", 1), f("w2", 1), f("slotf", 2), f("valid", 2)
    zt = sb.alloc([128, 2048], BF16, "zt")
    P.pool(lambda e: e.memset(zt[:], 0.0), w=["zt"])
    rpp = NROW // 128
    for z0 in range(0, rpp, 16):
        P.dma(xs_d.rearrange("(p r) d -> p r d", p=128)[:, z0:z0 + 16, :], zt[:].unsqueeze(1).to_broadcast([128, 16, 2048]), r=["zt"], w=["xs_d"])
    nev = 0
    for tt in range(ntt):
        i2 = tt % 2
        r0 = tt * 128
        kh, khb, khT = ("hrow", i2), ("hbf", i2), ("hT", i2)
        P.dma(hrow[i2][:], h1[r0:r0 + 128, :], w=[kh])
        P.act(lambda e, i2=i2: e.copy(hbf[i2][:], hrow[i2][:]), r=[kh], w=[khb])
        for g in range(4):
            pi = nev % 2
            nev += 1
            for j in range(4):
                kc = g * 4 + j
                P.pe(lambda e, pi=pi, kc=kc, j=j, i2=i2: e.transpose(psb[pi][:, j * 128:(j + 1) * 128], hrow[i2][:, kc * 128:(kc + 1) * 128], c["ident"][:]),
                     r=[kh, "ident"], w=[("ps", pi)])
            evac(P, nev, hT[i2][:, g * 4:(g + 1) * 4, :], psb[pi][:].rearrange("p (j t) -> p j t", j=4), r=[("ps", pi)], w=[khT])
        for kc in range(16):
            P.pe(lambda e, kc=kc, i2=i2: e.matmul(psb[2][:, 0:36], hT[i2][:, kc, :], Wr[:, kc, :], start=(kc == 0), stop=(kc == 15)),
                 r=[khT, "Wr"], w=[("ps", 2)])
        K = "rt"
        P.dve(lambda e: e.tensor_add(lg[:], psb[2][:, 0:36], br_b[:]), r=[("ps", 2), "br"], w=[K])
        P.dve(lambda e: e.tensor_reduce(mg[:], lg[:, 0:4], AX.X, ALU.max), r=[K], w=[K])
        P.dve(lambda e: e.tensor_scalar(gsel[:], lg[:, 0:4], mg[:, 0:1], None, ALU.is_equal), r=[K], w=[K])
        P.dve(lambda e: e.tensor_scalar_mul(tmp4[:, 0:1], mg[:], -1.0), r=[K], w=[K])
        P.act(lambda e: e.activation(eg[:], lg[:, 0:4], AF.Exp, bias=tmp4[:, 0:1], accum_out=sg_[:]), r=[K], w=[K])
        P.dve(lambda e: e.reciprocal(pg[:], sg_[:]), r=[K], w=[K])
        P.dve(lambda e: e.tensor_scalar(tmp4[:], gsel[:], BIG, -BIG, ALU.mult, ALU.add), r=[K], w=[K])
        P.dve(lambda e: e.tensor_add(lem[:].rearrange("p (g x) -> p g x", g=4), lg[:, 4:36].rearrange("p (g x) -> p g x", g=4),
                                    tmp4[:].unsqueeze(2).to_broadcast([128, 4, 8])), r=[K], w=[K])
        P.dve(lambda e: e.max(top8[:], lem[:]), r=[K], w=[K])
        P.dve(lambda e: e.tensor_scalar(sel1[:], lem[:], top8[:, 0:1], None, ALU.is_equal), r=[K], w=[K])
        P.dve(lambda e: e.tensor_scalar(sel2[:], lem[:], top8[:, 1:2], None, ALU.is_equal), r=[K], w=[K])
        P.dve(lambda e: e.tensor_sub(dd[:], top8[:, 1:2], top8[:, 0:1]), r=[K], w=[K])
        P.act(lambda e: e.activation(dd[:], dd[:], AF.Exp), r=[K], w=[K])
        P.dve(lambda e: e.tensor_scalar_add(w1[:], dd[:], 1.0), r=[K], w=[K])
        P.dve(lambda e: e.reciprocal(w1[:], w1[:]), r=[K], w=[K])
        P.dve(lambda e: e.tensor_mul(w2[:], w1[:], dd[:]), r=[K], w=[K])
        P.dve(lambda e: e.tensor_add(selb_f[:], sel1[:], sel2[:]), r=[K], w=[K])
        P.dve(lambda e: e.tensor_copy(selb[:], selb_f[:]), r=[K], w=["selb"])
        P.pe(lambda e: e.matmul(psb[3][:, 0:32], SUb[:], selb[:], start=True, stop=True), r=["SUb", "selb"], w=[("ps", 3)])
        P.pe(lambda e: e.matmul(psb[3][:, 32:64], onesb[:], selb[:], start=True, stop=True), r=["onesb", "selb"], w=[("ps", 3)])
        P.dve(lambda e: e.tensor_add(pos[:], psb[3][:, 0:32], carry[:]), r=[("ps", 3), "carry"], w=[K])
        P.dve(lambda e: e.tensor_add(carry[:], carry[:], psb[3][:, 32:64]), r=[("ps", 3), "carry"], w=["carry"])
        for k_, selk in enumerate((sel1, sel2)):
            P.dve(lambda e, selk=selk: e.tensor_mul(tmp32[:], selk[:], pos[:]), r=[K], w=[K])
            P.dve(lambda e, k_=k_: e.tensor_reduce(slotf[:, k_:k_ + 1], tmp32[:], AX.X, ALU.add), r=[K], w=[K])
            P.dve(lambda e, k_=k_: e.tensor_scalar(valid[:, k_:k_ + 1], slotf[:, k_:k_ + 1], float(CAP) - 0.5, None, ALU.is_lt), r=[K], w=[K])
            P.dve(lambda e, selk=selk: e.tensor_mul(tmp32[:], selk[:], eoff[:]), r=[K, "eoff"], w=[K])
            P.dve(lambda e: e.tensor_reduce(dd[:], tmp32[:], AX.X, ALU.add), r=[K], w=[K])
            P.dve(lambda e, k_=k_: e.tensor_add(slotf[:, k_:k_ + 1], slotf[:, k_:k_ + 1], dd[:]), r=[K], w=[K])
            P.dve(lambda e, k_=k_: e.tensor_sub(slotf[:, k_:k_ + 1], slotf[:, k_:k_ + 1], trash[:]), r=[K, "trash"], w=[K])
            P.dve(lambda e, k_=k_: e.tensor_mul(slotf[:, k_:k_ + 1], slotf[:, k_:k_ + 1], valid[:, k_:k_ + 1]), r=[K], w=[K])
            P.dve(lambda e, k_=k_: e.tensor_add(slotf[:, k_:k_ + 1], slotf[:, k_:k_ + 1], trash[:]), r=[K, "trash"], w=[K])
            wk = w1 if k_ == 0 else w2
            P.dve(lambda e, k_=k_, wk=wk, tt=tt: e.scalar_tensor_tensor(route_g[:, tt, k_:k_ + 1], wk[:], pg[:, 0:1], valid[:, k_:k_ + 1], ALU.mult, ALU.mult),
                  r=[K], w=[("route", tt)])
        P.dve(lambda e, tt=tt: e.tensor_copy(route_i[:, tt, :], slotf[:]), r=[K], w=[("route", tt)])
        for k_ in range(2):
            P.add("pool", lambda e, tt=tt, k_=k_, i2=i2: e.indirect_dma_start(
                out=xs_d[:, :], out_offset=bass.IndirectOffsetOnAxis(ap=route_i[:, tt, k_:k_ + 1], axis=0),
                in_=hbf[i2][:, :], in_offset=None), r=[khb, ("route", tt)], w=["xs_d"], sw=True)
    P.barrier()
    sb.reset(m2)
    wg = [sb.alloc([128, 16, 512], BF16, "wg") for _ in range(2)]
    wu = [sb.alloc([128, 16, 512], BF16, "wu") for _ in range(2)]
    wd = [sb.alloc([128, 4, 2048], BF16, "wd") for _ in range(2)]
    xrow = [sb.alloc([128, 2048], BF16, "xrowb") for _ in range(2)]
    xsT = sb.alloc([128, 16, CAP], BF16, "xsT")
    HT = sb.alloc([128, 4, CAP], BF16, "HT")
    sgt = [sb.alloc([128, CAP], F32, "sgt") for _ in range(2)]
    yst = [sb.alloc([128, 2048], F32, "yst") for _ in range(2)]
    nst = 0
    ncast = 0
    nps = 0
    ntl = CAP // 128
    pbf = lambda i: psb[i][:].bitcast(BF16)

    def cast(dst, src, r, w):
        nonlocal ncast
        i = {"ad": (ncast % 2) + 1, "d": 2, "dda": (2, 2, 1)[ncast % 3], "pad": ncast % 3}[MOE_CAST]
        ncast += 1
        if i == 0:
            P.pool(lambda e: e.tensor_copy(dst, src), r=r, w=w)
        elif i == 1:
            P.act(lambda e: e.copy(dst, src), r=r, w=w)
        else:
            P.dve(lambda e: e.tensor_copy(dst, src), r=r, w=w)

    def load_expert_w(ex):
        e2 = ex % 2
        kw = ("wexp", e2)
        gv = prm["w_gate"][ex].rearrange("(kc p) n -> p kc n", p=128)
        uv = prm["w_up"][ex].rearrange("(kc p) n -> p kc n", p=128)
        dv = prm["w_down"][ex].rearrange("(fc p) n -> p fc n", p=128)
        for hf in range(2):
            P.dma(wg[e2][:, hf * 8:(hf + 1) * 8, :], gv[:, hf * 8:(hf + 1) * 8, :], w=[kw], q="pool")
        for hf in range(2):
            P.dma(wu[e2][:, hf * 8:(hf + 1) * 8, :], uv[:, hf * 8:(hf + 1) * 8, :], w=[kw], q="pool")
        for hf in range(2):
            P.dma(wd[e2][:, hf * 2:(hf + 1) * 2, :], dv[:, hf * 2:(hf + 1) * 2, :], w=[kw], q="pool")

    nexp = 32 if "2" in MOE_PHASES else 0
    if nexp:
        load_expert_w(0)
    for ex in range(nexp):
        e2 = ex % 2
        kw = ("wexp", e2)
        if ex + 1 < nexp:
            load_expert_w(ex + 1)
        for tl in range(ntl):
            xr, kxr = xrow[tl % 2], ("xrowb", tl % 2)
            P.dma(xr[:], xs_d[ex * CAP + tl * 128: ex * CAP + (tl + 1) * 128, :], r=["xs_d"], w=[kxr])
            for g in range(2):
                pi = nps % 2
                nps += 1
                for j in range(8):
                    kc = g * 8 + j
                    P.pe(lambda e, pi=pi, xr=xr, kc=kc, j=j: e.transpose(pbf(pi)[:, j * 128:(j + 1) * 128], xr[:, kc * 128:(kc + 1) * 128], c["identb"][:]),
                         r=[kxr, "identb"], w=[("ps", pi)])
                evac(P, nps, xsT[:, g * 8:(g + 1) * 8, tl * 128:(tl + 1) * 128], pbf(pi).rearrange("p (j t) -> p j t", j=8), r=[("ps", pi)], w=["xsT"])
        for fc in range(4):
            pg_, pu_ = 2 + (fc % 2) * 2, 3 + (fc % 2) * 2
            for kc in range(16):
                P.pe(lambda e, kc=kc, fc=fc, pg_=pg_, e2=e2: e.matmul(psb[pg_][:, 0:CAP], wg[e2][:, kc, fc * 128:(fc + 1) * 128], xsT[:, kc, :],
                                                                   start=(kc == 0), stop=(kc == 15)), r=[kw, "xsT"], w=[("ps", pg_)])
            for kc in range(16):
                P.pe(lambda e, kc=kc, fc=fc, pu_=pu_, e2=e2: e.matmul(psb[pu_][:, 0:CAP], wu[e2][:, kc, fc * 128:(fc + 1) * 128], xsT[:, kc, :],
                                                                   start=(kc == 0), stop=(kc == 15)), r=[kw, "xsT"], w=[("ps", pu_)])
            sg2, ksg = sgt[fc % 2], ("sgt", fc % 2)
            P.act(lambda e, sg2=sg2, pg_=pg_: e.activation(sg2[:], psb[pg_][:, 0:CAP], AF.Silu), r=[("ps", pg_)], w=[ksg])
            P.dve(lambda e, sg2=sg2, pu_=pu_, fc=fc: e.tensor_tensor(HT[:, fc, :], sg2[:], psb[pu_][:, 0:CAP], ALU.mult), r=[ksg, ("ps", pu_)], w=["HT"])
        for tl in range(ntl):
            ys, kys = yst[tl % 2], ("yst", tl % 2)
            for db in range(4):
                pi = 6 + nps % 2
                nps += 1
                for fc in range(4):
                    P.pe(lambda e, pi=pi, fc=fc, tl=tl, db=db, e2=e2: e.matmul(psb[pi][:], HT[:, fc, tl * 128:(tl + 1) * 128], wd[e2][:, fc, db * 512:(db + 1) * 512],
                                                                            start=(fc == 0), stop=(fc == 3)), r=["HT", kw], w=[("ps", pi)])
                evac(P, nps, ys[:, db * 512:(db + 1) * 512], psb[pi][:], r=[("ps", pi)], w=[kys])
            P.dma(ys_d[ex * CAP + tl * 128: ex * CAP + (tl + 1) * 128, :], ys[:], r=[kys], w=["ys_d"], q="pool")
    P.barrier()
    sb.reset(m2)
    y1 = [sb.alloc([128, 2048], F32, "y1") for _ in range(2)]
    y2 = [sb.alloc([128, 2048], F32, "y2") for _ in range(2)]
    hr = [sb.alloc([128, 2048], F32, "hr") for _ in range(2)]
    pre = sb.alloc([128, 2048], F32, "pre")
    g_b = sb.alloc([128, 2048], F32, "g_b")
    b_b = sb.alloc([128, 2048], F32, "b_b")
    stats = sb.alloc([128, 4, 6], F32, "stats")
    mv = sb.alloc([128, 2], F32, "mv")
    rstd = sb.alloc([128, 1], F32, "rstd")
    bcast_load(cx, g_b[:], prm["ln_g"], "lnp")
    bcast_load(cx, b_b[:], prm["ln_b"], "lnp")
    for i2 in range(2):
        P.pool(lambda e, i2=i2: e.memset(y1[i2][:], 0.0), w=[("y1", i2)])
        P.pool(lambda e, i2=i2: e.memset(y2[i2][:], 0.0), w=[("y2", i2)])
    for tt in range(ntt if "3" in MOE_PHASES else 0):
        i2 = tt % 2
        r0 = tt * 128
        for k_, yy in enumerate((y1, y2)):
            ky = ("y1" if k_ == 0 else "y2", i2)
            P.add("pool", lambda e, tt=tt, k_=k_, yy=yy, i2=i2: e.indirect_dma_start(
                out=yy[i2][:, :], out_offset=None, in_=ys_d[:, :],
                in_offset=bass.IndirectOffsetOnAxis(ap=route_i[:, tt, k_:k_ + 1], axis=0)),
                r=["ys_d", ("route", tt)], w=[ky], sw=True)
        khr = ("hr", i2)
        P.dma(hr[i2][:], h1[r0:r0 + 128, :], w=[khr])
        P.dve(lambda e, i2=i2, tt=tt: e.tensor_scalar_mul(y1[i2][:], y1[i2][:], route_g[:, tt, 0:1]), r=[("y1", i2), ("route", tt)], w=[("y1", i2)])
        P.dve(lambda e, i2=i2, tt=tt: e.scalar_tensor_tensor(y1[i2][:], y2[i2][:], route_g[:, tt, 1:2], y1[i2][:], ALU.mult, ALU.add),
              r=[("y1", i2), ("y2", i2), ("route", tt)], w=[("y1", i2)])
        P.dve(lambda e, i2=i2: e.scalar_tensor_tensor(pre[:], hr[i2][:], ALPHA, y1[i2][:], ALU.mult, ALU.add), r=[khr, ("y1", i2)], w=["pre"])
        layer_norm_tile(cx, pre[:], hr[i2][:], g_b[:], b_b[:], stats, mv, rstd, "pre", khr, "lnst", aff="dve")
        P.dma(x_out[r0:r0 + 128, :], hr[i2][:], r=[khr], q="pool")
    P.barrier()
    sb.reset(m)


PARAM_SHAPES = {
    "w_in": [2048, N_IN], "fox_b_f": [8], "fox_out_g": [512], "mla_q_norm_g": [384], "mla_kv_norm_g": [128],
    "mla_w_uq": [384, 768], "mla_w_ukv": [128, 1024], "mla_out_g": [512], "rwkv_mu": [1792], "rwkv_w0": [512],
    "rwkv_w2": [64, 512], "rwkv_a0": [512], "rwkv_a2": [64, 512], "rwkv_g2": [128, 512], "rwkv_k_k": [512],
    "rwkv_k_a": [512], "rwkv_r_k": [512], "rwkv_ln_g": [512], "rwkv_ln_b": [512], "gdn_conv_w": [4, 1536],
    "gdn_a_log": [4], "gdn_dt_bias": [4], "gdn_norm_g": [128], "w_out": [2048, 2048], "ln1_g": [2048], "ln1_b": [2048],
    "moe_w_grp": [2048, 4], "moe_b_grp": [4], "moe_w_exp": [2048, 32], "moe_b_exp": [32], "moe_w_gate": [32, 2048, 512],
    "moe_w_up": [32, 2048, 512], "moe_w_down": [32, 512, 2048], "ln2_g": [2048], "ln2_b": [2048],
}


def build_program(S, NB, depth):
    nc = bass.Bass("TRN2", target_bir_lowering=False)
    cx = Ctx(nc, S, NB)
    T = cx.T
    x_in = cx.dt("x", [T, 2048], F32, "ExternalInput")
    pos = cx.dt("positions", [NB, S], I32, "ExternalInput")
    ifr = cx.dt("inv_freq", [32], F32, "ExternalInput")
    W = {k: cx.dt(k, [depth] + v, F32, "ExternalInput") for k, v in PARAM_SHAPES.items()}
    y = cx.dt("y", [T, 2048], F32, "ExternalOutput")
    u_tok = cx.dt("u_tok", [T, N_IN], F32)
    qkT = cx.dt("qkT", [1024, T], BF16)
    fT = cx.dt("fT", [8, T], F32)
    mixed = cx.dt("mixed", [T, 2048], F32)
    h1 = cx.dt("h1", [T, 2048], F32)
    xs_d = cx.dt("xs_d", [32 * MOE_CAP + 128, 2048], BF16)
    ys_d = cx.dt("ys_d", [32 * MOE_CAP + 128, 2048], F32)
    xbuf = [cx.dt("xres%d" % i, [T, 2048], F32) for i in range(2)]
    make_consts(cx)
    cur = x_in
    for l in range(depth):
        nxt = y if l == depth - 1 else xbuf[l % 2]
        stage_inproj(cx, cur, W["w_in"][l], u_tok, qkT, fT)
        stage_fox(cx, qkT, fT, u_tok, W["fox_b_f"][l], W["fox_out_g"][l], mixed)
        stage_mla(cx, u_tok, pos, ifr, W["mla_q_norm_g"][l], W["mla_kv_norm_g"][l], W["mla_w_uq"][l], W["mla_w_ukv"][l],
                  W["mla_out_g"][l], mixed)
        stage_rwkv(cx, u_tok, {k: W["rwkv_" + k][l] for k in ("mu", "w0", "w2", "a0", "a2", "g2", "k_k", "k_a", "r_k", "ln_g", "ln_b")}, mixed)
        stage_gdn(cx, u_tok, {k: W["gdn_" + k][l] for k in ("conv_w", "a_log", "dt_bias", "norm_g")}, mixed)
        stage_outproj_ln(cx, mixed, W["w_out"][l], cur, W["ln1_g"][l], W["ln1_b"][l], h1)
        prm = {"w_grp": W["moe_w_grp"][l], "b_grp": W["moe_b_grp"][l], "w_exp": W["moe_w_exp"][l], "b_exp": W["moe_b_exp"][l],
               "w_gate": W["moe_w_gate"][l], "w_up": W["moe_w_up"][l], "w_down": W["moe_w_down"][l], "ln_g": W["ln2_g"][l], "ln_b": W["ln2_b"][l]}
        stage_moe(cx, h1, prm, nxt, xs_d, ys_d)
        cur = nxt
    cx.P.emit()
    return nc, cx


def kernel(**inputs):
    n_cores = 8
    x = np.ascontiguousarray(np.asarray(inputs["x"], dtype=np.float32))
    B, S, Dm = x.shape
    NB = B // n_cores
    depth = int(np.asarray(inputs["w_in"]).shape[0])
    nc, _ = build_program(S, NB, depth)
    positions = np.ascontiguousarray(np.asarray(inputs["positions"], dtype=np.int32))
    inv_freq = (np.float32(10000.0) ** (-np.arange(32, dtype=np.float32) / np.float32(32))).astype(np.float32)
    shared = {k: np.ascontiguousarray(np.asarray(inputs[k], dtype=np.float32)) for k in PARAM_SHAPES}
    in_maps = []
    for c in range(n_cores):
        d = dict(shared)
        d["x"] = x[c * NB:(c + 1) * NB].reshape(NB * S, Dm)
        d["positions"] = positions[c * NB:(c + 1) * NB]
        d["inv_freq"] = inv_freq
        in_maps.append(d)
    res = run_bass_kernel_spmd(nc, in_maps, core_ids=list(range(n_cores)))
    out = np.concatenate([np.asarray(r["y"], dtype=np.float32).reshape(NB, S, Dm) for r in res.results], axis=0)
    return out
```

```python
import numpy as np
import concourse.bass as bass
import concourse.mybir as mybir
from concourse.bass_utils import run_bass_kernel_spmd

F32 = mybir.dt.float32
BF16 = mybir.dt.bfloat16
I32 = mybir.dt.int32
U32 = mybir.dt.uint32
AF = mybir.ActivationFunctionType
ALU = mybir.AluOpType
AX = mybir.AxisListType

D = 2048
GW = 512
N_IN = 5968
C_FOX, C_MLA, C_RWKV, C_GDN = 0, 1544, 2120, 3912
ALPHA = (2 * 4) ** 0.25
LN_EPS = 1e-5
RMS_EPS = 1e-6
NEG = -30000.0


class _Op:
    __slots__ = ("stream", "fn", "deps", "sig", "ticket", "dma", "dma_idx", "idx", "sw")


class Prog:
    STREAMS = ("pe", "act", "dve", "pool", "sp")
    NDMA = 48

    def __init__(self, nc):
        self.nc = nc
        self.ops = []
        self.by_stream = {s: [] for s in self.STREAMS}
        self.last_w = {}
        self.readers = {}
        self.n_dma = 0
        self.dma_ops = []
        self.barrier_deps = {s: [] for s in self.STREAMS}

    POOLQ = "pool"
    NSW = 40

    def add(self, stream, fn, r=(), w=(), dma=False, sw=False):
        op = _Op()
        op.stream, op.fn, op.dma, op.sig, op.ticket = stream, fn, dma, False, 0
        op.sw = sw
        op.idx = len(self.ops)
        pk = [k for k in r if isinstance(k, tuple) and k and k[0] == "ps"]
        if pk:
            r = [k for k in r if not (isinstance(k, tuple) and k and k[0] == "ps")]
            w = list(w) + pk
        deps = set()
        for k in r:
            d = self.last_w.get(k)
            if d is not None:
                deps.add(d)
        for k in w:
            d = self.last_w.get(k)
            if d is not None:
                deps.add(d)
            for rd in self.readers.get(k, ()):
                deps.add(rd)
        for k in r:
            self.readers.setdefault(k, []).append(op.idx)
        for k in w:
            self.last_w[k] = op.idx
            self.readers[k] = []
        bd = self.barrier_deps[stream]
        if bd:
            deps.update(bd)
            self.barrier_deps[stream] = []
        if dma:
            op.dma_idx = self.n_dma
            self.n_dma += 1
            if op.dma_idx >= self.NDMA:
                deps.add(self.dma_ops[op.dma_idx - self.NDMA])
            self.dma_ops.append(op.idx)
        deps.discard(op.idx)
        fin = []
        for d in deps:
            o = self.ops[d]
            if o.stream == stream and not o.dma and stream == "pe":
                continue
            fin.append(d)
            if not o.dma:
                o.sig = True
        op.deps = fin
        self.ops.append(op)
        self.by_stream[stream].append(op)
        return op

    def barrier(self):
        deps = []
        for s in self.STREAMS:
            for o in reversed(self.by_stream[s]):
                if not o.dma:
                    deps.append(o.idx)
                    break
        deps.extend(self.dma_ops[-self.NDMA:])
        for s in self.STREAMS:
            self.barrier_deps[s] = list(deps)
        self.last_w = {}
        self.readers = {}

    def pe(self, fn, r=(), w=()):
        return self.add("pe", fn, r, w)

    def act(self, fn, r=(), w=()):
        return self.add("act", fn, r, w)

    def dve(self, fn, r=(), w=()):
        return self.add("dve", fn, r, w)

    def pool(self, fn, r=(), w=()):
        return self.add("pool", fn, r, w)

    def dma(self, out, in_, r=(), w=(), q="sp", slow=False):
        if q == "pool":
            q = self.POOLQ
        if slow:
            return self.add(q, lambda e: e.dma_start(out=out, in_=in_, allow_slow_non_contiguous=True), r, w, dma=True)
        return self.add(q, lambda e: e.dma_start(out=out, in_=in_), r, w, dma=True)

    def emit(self, final_wait=True):
        nc = self.nc
        for s in self.STREAMS:
            t = 0
            for o in self.by_stream[s]:
                if not o.dma and o.sig:
                    t += 1
                    o.ticket = t
        import contextlib
        with contextlib.ExitStack() as es:
            esem = {s: es.enter_context(nc.semaphore("e_" + s)) for s in self.STREAMS}
            dsem = [es.enter_context(nc.semaphore("d%d" % i)) for i in range(self.NDMA)]
            nsw = sum(1 for o in self.ops if o.sw)
            SWBASE = 215
            swsems = [es.enter_context(nc.semaphore("sw%d" % i, num=SWBASE + i)) for i in range(min(nsw, self.NSW))]
            swctr = [0, 1]
            block = es.enter_context(nc.Block())
            ops = self.ops
            NDMA = self.NDMA

            def completion(o):
                if o.dma:
                    return ("d", o.dma_idx % NDMA), dsem[o.dma_idx % NDMA], 16 * (o.dma_idx // NDMA + 1)
                return ("e", o.stream), esem[o.stream], o.ticket

            def run_stream(s, eng):
                waited = {}
                pending = []

                def flush():
                    for (sem_, val_, o_) in pending:
                        eng.wait_ge(sem_, val_)
                        if o_.sig:
                            eng.nop().then_inc(esem[s], 1)
                    del pending[:]

                for o in self.by_stream[s]:
                    need = {}
                    for d in o.deps:
                        key, sem, val = completion(ops[d])
                        if waited.get(key, 0) >= val:
                            continue
                        if key not in need or need[key][1] < val:
                            need[key] = (sem, val)
                    if pending and ((not o.sw) or need or len(pending) >= 2):
                        flush()
                    for key, (sem, val) in need.items():
                        eng.wait_ge(sem, val)
                        waited[key] = val
                    if o.sw and swctr[0] == len(swsems):
                        flush()
                        eng.dma_reset(range(SWBASE, SWBASE + len(swsems)))
                        swctr[0] = 0
                        swctr[1] += 1
                    inst = o.fn(eng)
                    if o.sw:
                        sw_ = swsems[swctr[0]]
                        swctr[0] += 1
                        inst.then_inc(sw_, 16)
                        pending.append((sw_, 16 * swctr[1], o))
                    elif o.dma:
                        inst.then_inc(dsem[o.dma_idx % NDMA], 16)
                    elif o.sig:
                        inst.then_inc(esem[s], 1)
                flush()
                if s == "sp" and final_wait:
                    for i in range(min(NDMA, self.n_dma)):
                        last = i + ((self.n_dma - 1 - i) // NDMA) * NDMA
                        val = 16 * (last // NDMA + 1)
                        if waited.get(("d", i), 0) < val:
                            eng.wait_ge(dsem[i], val)

            @block.tensor
            def _(e):
                run_stream("pe", e)

            @block.scalar
            def _(e):
                run_stream("act", e)

            @block.vector
            def _(e):
                run_stream("dve", e)

            @block.gpsimd
            def _(e):
                run_stream("pool", e)

            @block.sync
            def _(e):
                run_stream("sp", e)


class SB:
    def __init__(self, nc, base=16512, cap=229344):
        self.nc, self.base, self.cap, self.top = nc, base, cap, base
        self.n = 0

    def mark(self):
        return self.top

    def reset(self, m):
        self.top = m

    def alloc(self, shape, dtype, name="t"):
        esz = {F32: 4, BF16: 2, I32: 4, U32: 4}[dtype]
        per = esz
        for s in shape[1:]:
            per *= s
        per = (per + 31) // 32 * 32
        off = self.top
        assert off + per <= self.cap, "SBUF overflow %s %d+%d" % (name, off, per)
        self.top += per
        self.n += 1
        return self.nc.alloc_sbuf_tensor_at("%s_%d" % (name, self.n), list(shape), dtype, offset=off)


class Ctx:
    def __init__(self, nc, S, NB, io=None):
        self.nc = nc
        self.S = S
        self.NB = NB
        self.T = S * NB
        self.P = Prog(nc)
        self.sb = SB(nc)
        self.io = io or {}
        self.dram = {}
        self.uid = 0

    def dt(self, name, shape, dtype, kind=None):
        k = self.io.get(name, kind or "Internal")
        t = self.nc.dram_tensor(name, list(shape), dtype, kind=k).ap()
        self.dram[name] = t
        return t

    def key(self, base):
        self.uid += 1
        return (base, self.uid)


def make_consts(cx):
    nc, P, sb = cx.nc, cx.P, cx.sb
    c = {}
    ident = sb.alloc([128, 128], F32, "ident")
    P.pool(lambda e: e.memset(ident[:], 0.0), w=["ident"])
    P.pool(lambda e: e.affine_select(out=ident[:], in_=ident[:], pattern=[[-1, 128]],
                                     compare_op=ALU.not_equal, fill=1.0, base=0, channel_multiplier=1),
           r=["ident"], w=["ident"])
    identb = sb.alloc([128, 128], BF16, "identb")
    P.dve(lambda e: e.tensor_copy(identb[:], ident[:]), r=["ident"], w=["identb"])
    c["ident"], c["identb"] = ident, identb
    cx.ps = [nc.alloc_psum_tensor("psb%d" % i, [128, 512], F32) for i in range(8)]
    cx.c = c
    dplr_consts(cx)
    SU = sb.alloc([128, 128], F32, "SU")
    SUb = sb.alloc([128, 128], BF16, "SUb")
    P.pool(lambda e: e.memset(SU[:], 1.0), w=["SU"])
    P.pool(lambda e: e.affine_select(out=SU[:], in_=SU[:], pattern=[[1, 128]], compare_op=ALU.is_ge, fill=0.0, base=-1, channel_multiplier=-1),
           r=["SU"], w=["SU"])
    P.pool(lambda e: e.tensor_copy(SUb[:], SU[:]), r=["SU"], w=["SUb"])
    eoff = sb.alloc([128, 32], F32, "eoff")
    P.pool(lambda e: e.iota(eoff[:], pattern=[[MOE_CAP, 32]], base=0, channel_multiplier=0, allow_small_or_imprecise_dtypes=True), w=["eoff"])
    trash = sb.alloc([128, 1], F32, "trash")
    P.pool(lambda e: e.iota(trash[:], pattern=[[0, 1]], base=32 * MOE_CAP, channel_multiplier=1, allow_small_or_imprecise_dtypes=True), w=["trash"])
    c.update(SUb=SUb, eoff=eoff, trash=trash)
    c["masks_causal"] = build_masks(cx, "causal")
    c["masks_chunk64"] = build_masks(cx, "chunk64")
    P.barrier()
    return c


def evac(P, i, out, in_, r, w):
    if i % 2 == 0:
        P.act(lambda e: e.copy(out, in_), r, w)
    else:
        P.dve(lambda e: e.tensor_copy(out, in_), r, w)


def build_xT(cx, src_dram, b, xT, ps_banks, xrow_bufs, tag):
    P, c = cx.P, cx.c
    S = cx.S
    nt = S // 128
    for tt in range(nt):
        xr = xrow_bufs[tt % len(xrow_bufs)]
        kx = ("xrow", tag, tt % len(xrow_bufs))
        r0 = b * S + tt * 128
        P.dma(xr[:], src_dram[r0:r0 + 128, :], w=[kx])
        for g in range(4):
            ps = ps_banks[(tt * 4 + g) % len(ps_banks)]
            kp = ("psT", tag, (tt * 4 + g) % len(ps_banks))
            for j in range(4):
                kc = g * 4 + j
                P.pe(lambda e, ps=ps, xr=xr, kc=kc, j=j: e.transpose(ps[:, j * 128:(j + 1) * 128],
                                                                   xr[:, kc * 128:(kc + 1) * 128], c["ident"][:]),
                     r=[kx, "ident"], w=[kp])
            o = xT[:, g * 4:(g + 1) * 4, tt * 128:(tt + 1) * 128]
            i_ = ps[:].rearrange("p (j t) -> p j t", j=4)
            evac(P, tt * 4 + g, o, i_, r=[kp], w=[("xT", tag, tt)])


def stage_inproj(cx, x_dram, w_in, u_tok, qkT, fT):
    nc, P, sb, c = cx.nc, cx.P, cx.sb, cx.c
    S, NB = cx.S, cx.NB
    nt = S // 128
    m = sb.mark()
    xT = sb.alloc([128, 16, S], BF16, "xT")
    xrows = [sb.alloc([128, 2048], F32, "xrow") for _ in range(2)]
    wst = [sb.alloc([128, 16, 512], F32, "wst") for _ in range(2)]
    wbf = [sb.alloc([128, 16, 512], BF16, "wbf") for _ in range(2)]
    ost = [sb.alloc([128, 512], F32, "ost") for _ in range(4)]
    obf = [sb.alloc([128, 512], BF16, "obf") for _ in range(2)]
    psb = cx.ps
    wv = w_in.rearrange("(kc p) n -> p kc n", p=128)
    blocks = [(c0, min(512, N_IN - c0)) for c0 in range(0, N_IN, 512)]
    nev = 0
    for b in range(NB):
        build_xT(cx, x_dram, b, xT, psb[0:4], xrows, "A%d" % b)
        xkeys = [("xT", "A%d" % b, tt) for tt in range(nt)]
        for bi, (c0, cw) in enumerate(blocks):
            it = b * len(blocks) + bi
            ws, wb = wst[it % 2], wbf[it % 2]
            kws, kwb = ("wst", it % 2), ("wbf", it % 2)
            P.dma(ws[:, :, 0:cw], wv[:, :, c0:c0 + cw], w=[kws])
            if it % 2 == 0:
                P.dve(lambda e, wb=wb, ws=ws, cw=cw: e.tensor_copy(wb[:, :, 0:cw], ws[:, :, 0:cw]), r=[kws], w=[kwb])
            else:
                P.act(lambda e, wb=wb, ws=ws, cw=cw: e.copy(wb[:, :, 0:cw], ws[:, :, 0:cw]), r=[kws], w=[kwb])
            for tt in range(nt):
                if c0 in (0, 512):
                    break
                pi = 4 + (nev % 4)
                ps, kp = psb[pi], ("psA", pi)
                for kc in range(16):
                    P.pe(lambda e, ps=ps, kc=kc, tt=tt, wb=wb, cw=cw: e.matmul(
                        ps[:, 0:cw], xT[:, kc, tt * 128:(tt + 1) * 128], wb[:, kc, 0:cw],
                        start=(kc == 0), stop=(kc == 15)), r=[xkeys[tt], kwb], w=[kp])
                o, ko = ost[nev % 4], ("ost", nev % 4)
                evac(P, nev, o[:, 0:cw], ps[:, 0:cw], r=[kp], w=[ko])
                r0 = b * S + tt * 128
                P.dma(u_tok[r0:r0 + 128, c0:c0 + cw], o[:, 0:cw], r=[ko], q="pool")
                nev += 1
            fm = []
            if c0 in (0, 512):
                fm = [(c0 + j * 128, 128) for j in range(4)]
            elif c0 == 1536:
                fm = [(1536, 8)]
            for (f0, fw) in fm:
                for tb in range(S // 512):
                    pi = 4 + (nev % 4)
                    ps, kp = psb[pi], ("psA", pi)
                    for kc in range(16):
                        P.pe(lambda e, ps=ps, kc=kc, tb=tb, wb=wb, f0=f0, fw=fw, c0=c0: e.matmul(
                            ps[0:fw, :], wb[:, kc, f0 - c0:f0 - c0 + fw], xT[:, kc, tb * 512:(tb + 1) * 512],
                            start=(kc == 0), stop=(kc == 15)), r=xkeys[tb * 4:tb * 4 + 4] + [kwb], w=[kp])
                    t0 = b * S + tb * 512
                    if fw == 128:
                        o, ko = obf[nev % 2], ("obf", nev % 2)
                        evac(P, nev, o[:], ps[:], r=[kp], w=[ko])
                        P.dma(qkT[f0:f0 + 128, t0:t0 + 512], o[:], r=[ko], q="pool")
                    else:
                        o, ko = ost[nev % 4], ("ost", nev % 4)
                        evac(P, nev, o[0:8, :], ps[0:8, :], r=[kp], w=[ko])
                        P.dma(fT[0:8, t0:t0 + 512], o[0:8, :], r=[ko], q="pool")
                    nev += 1
    P.barrier()
    sb.reset(m)


def bcast_load(cx, dst, src_row, key, q="sp"):
    cx.P.dma(dst, src_row.partition_broadcast(128), w=[key], q=q)


def rstd_from_ssq(P, out, ssq, n, eps, r, w):
    P.dve(lambda e: e.tensor_scalar(out, ssq, 1.0 / n, eps, ALU.mult, ALU.add), r=r, w=w)
    P.act(lambda e: e.activation(out, out, AF.Sqrt), r=w, w=w)
    P.dve(lambda e: e.reciprocal(out, out), r=w, w=w)


def layer_norm_tile(cx, pre, y, g_b, b_b, stats, mv, rstd, kpre, ky, kst, aff="pool"):
    P = cx.P
    for j in range(4):
        P.dve(lambda e, j=j: e.bn_stats(stats[:, j, :], pre[:, j * 512:(j + 1) * 512]), r=[kpre], w=[kst])
    P.dve(lambda e: e.bn_aggr(mv[:], stats[:].rearrange("p a b -> p (a b)")), r=[kst], w=[kst])
    P.dve(lambda e: e.tensor_scalar_add(rstd[:], mv[:, 1:2], LN_EPS), r=[kst], w=[kst])
    P.act(lambda e: e.activation(rstd[:], rstd[:], AF.Sqrt), r=[kst], w=[kst])
    P.dve(lambda e: e.reciprocal(rstd[:], rstd[:]), r=[kst], w=[kst])
    P.dve(lambda e: e.tensor_scalar(y, pre, mv[:, 0:1], rstd[:], ALU.subtract, ALU.mult), r=[kst, kpre], w=[ky])
    eng_ = P.pool if aff == "pool" else P.dve
    eng_(lambda e: e.tensor_mul(y, y, g_b), r=[ky, "lnp"], w=[ky])
    eng_(lambda e: e.tensor_add(y, y, b_b), r=[ky, "lnp"], w=[ky])


def stage_outproj_ln(cx, mixed, w_out, x_res, ln_g, ln_b, h_out):
    nc, P, sb, c = cx.nc, cx.P, cx.sb, cx.c
    T = cx.T
    m = sb.mark()
    wbf = sb.alloc([128, 16, 2048], BF16, "woutbf")
    wst = [sb.alloc([128, 16, 256], F32, "wst") for _ in range(2)]
    mrow = [sb.alloc([128, 2048], F32, "mrow") for _ in range(2)]
    xrow = [sb.alloc([128, 2048], F32, "xrow") for _ in range(2)]
    mT = [sb.alloc([128, 16, 128], BF16, "mT") for _ in range(2)]
    pre = [sb.alloc([128, 2048], F32, "pre") for _ in range(2)]
    g_b = sb.alloc([128, 2048], F32, "g_b")
    b_b = sb.alloc([128, 2048], F32, "b_b")
    stats = sb.alloc([128, 4, 6], F32, "stats")
    mv = sb.alloc([128, 2], F32, "mv")
    rstd = sb.alloc([128, 1], F32, "rstd")
    psb = cx.ps
    bcast_load(cx, g_b[:], ln_g, "lnp")
    bcast_load(cx, b_b[:], ln_b, "lnp")
    wv = w_out.rearrange("(kc p) n -> p kc n", p=128)
    for j in range(8):
        ws, kws = wst[j % 2], ("wst", j % 2)
        P.dma(ws[:], wv[:, :, j * 256:(j + 1) * 256], w=[kws])
        if j % 2 == 0:
            P.dve(lambda e, ws=ws, j=j: e.tensor_copy(wbf[:, :, j * 256:(j + 1) * 256], ws[:]), r=[kws], w=["wout"])
        else:
            P.act(lambda e, ws=ws, j=j: e.copy(wbf[:, :, j * 256:(j + 1) * 256], ws[:]), r=[kws], w=["wout"])
    nev = 0
    for tt in range(T // 128):
        i2 = tt % 2
        r0 = tt * 128
        km, kx, kmT, kpre = ("mrow", i2), ("xrow", i2), ("mT", i2), ("pre", i2)
        P.dma(mrow[i2][:], mixed[r0:r0 + 128, :], w=[km])
        P.dma(xrow[i2][:], x_res[r0:r0 + 128, :], w=[kx])
        for g in range(4):
            pi = nev % 4
            ps, kp = psb[pi], ("ps", pi)
            for j in range(4):
                kc = g * 4 + j
                P.pe(lambda e, ps=ps, kc=kc, j=j, i2=i2: e.transpose(ps[:, j * 128:(j + 1) * 128],
                                                                 mrow[i2][:, kc * 128:(kc + 1) * 128], c["ident"][:]),
                     r=[km, "ident"], w=[kp])
            evac(P, nev, mT[i2][:, g * 4:(g + 1) * 4, :], ps[:].rearrange("p (j t) -> p j t", j=4), r=[kp], w=[kmT])
            nev += 1
        for cb in range(4):
            pi = 4 + nev % 4
            ps, kp = psb[pi], ("ps", pi)
            for kc in range(16):
                P.pe(lambda e, ps=ps, kc=kc, cb=cb, i2=i2: e.matmul(ps[:], mT[i2][:, kc, :], wbf[:, kc, cb * 512:(cb + 1) * 512],
                                                                  start=(kc == 0), stop=(kc == 15)),
                     r=[kmT, "wout"], w=[kp])
            P.dve(lambda e, ps=ps, cb=cb, i2=i2: e.scalar_tensor_tensor(
                pre[i2][:, cb * 512:(cb + 1) * 512], xrow[i2][:, cb * 512:(cb + 1) * 512], ALPHA, ps[:], ALU.mult, ALU.add),
                r=[kp, kx], w=[kpre])
            nev += 1
        layer_norm_tile(cx, pre[i2][:], xrow[i2][:], g_b[:], b_b[:], stats, mv, rstd, kpre, kx, "lnst")
        P.dma(h_out[r0:r0 + 128, :], xrow[i2][:], r=[kx], q="pool")
    P.barrier()
    sb.reset(m)


def build_masks(cx, kind):
    P, sb = cx.P, cx.sb
    if "maskf" not in cx.c:
        cx.c["maskf"] = sb.alloc([128, 512], F32, "maskf")
    mf = cx.c["maskf"]
    out = []
    for j in range(4):
        mb = sb.alloc([128, 512], BF16, "maskb")
        k = ("mask", kind, j)
        P.pool(lambda e: e.memset(mf[:], 0.0), w=["maskf"])
        if kind == "causal":
            P.pool(lambda e, j=j: e.affine_select(out=mf[:], in_=mf[:], pattern=[[1, 512]], compare_op=ALU.is_ge,
                                                 fill=NEG, base=-128 * j, channel_multiplier=-1), r=["maskf"], w=["maskf"])
        else:
            for hf in range(2):
                P.pool(lambda e, j=j, hf=hf: e.affine_select(
                    out=mf[hf * 64:(hf + 1) * 64, :], in_=mf[hf * 64:(hf + 1) * 64, :], pattern=[[1, 512]],
                    compare_op=ALU.is_ge, fill=NEG, base=-128 * j - 64 * hf, channel_multiplier=0),
                    r=["maskf"], w=["maskf"])
        P.pool(lambda e, mb=mb: e.tensor_copy(mb[:], mf[:]), r=["maskf"], w=[k])
        out.append((mb, k))
    return out


def attn_bufs(cx, nh, dv):
    sb, S = cx.sb, cx.S
    nring = 2 * (S // 128)
    return dict(PT=[sb.alloc([128, 512], BF16, "PT") for _ in range(nring)],
                o_t=[sb.alloc([128, 4, nh * dv], F32, "o_t") for _ in range(2)],
                rec=[sb.alloc([128, 4], F32, "rec") for _ in range(2)])


def attn_core(cx, nh, dv, scale, score_ops, bias_ap, masks, v_ap, out_cb, tag, sbanks=(0, 1, 2), bufs=None):
    P, sb, c = cx.P, cx.sb, cx.c
    S = cx.S
    psb = cx.ps
    nring = 2 * (S // 128)
    PT, o_t, rec = bufs["PT"], bufs["o_t"], bufs["rec"]
    tag = "att"
    st = {"npt": 0, "nsc": 0}
    units = [(qb, h) for qb in range(S // 512) for h in range(nh)]

    def scores(qb, h):
        pts = []
        for kt in range(4 * qb + 4):
            j = kt - 4 * qb
            bi_ = sbanks[st["nsc"] % len(sbanks)]
            pss, kpss = psb[bi_], ("ps", bi_)
            st["nsc"] += 1
            ops = list(score_ops(h, kt, qb))
            if j >= 0:
                mb, km = masks[j]
                ops.append((c["identb"][:], mb[:], [km, "identb"]))
            for i, (lh, rh, ks) in enumerate(ops):
                P.pe(lambda e, pss=pss, lh=lh, rh=rh, i=i, n=len(ops): e.matmul(pss[:], lh, rh, start=(i == 0), stop=(i == n - 1)),
                     r=ks, w=[kpss])
            pt, kpt = PT[st["npt"] % nring], ("PT", tag, st["npt"] % nring)
            st["npt"] += 1
            if bias_ap is not None:
                bap, kb = bias_ap(h, kt)
                P.act(lambda e, pt=pt, pss=pss, bap=bap: e.activation(pt[:], pss[:], AF.Exp, bias=bap, scale=scale),
                      r=[kpss] + kb, w=[kpt])
            else:
                P.act(lambda e, pt=pt, pss=pss: e.activation(pt[:], pss[:], AF.Exp, scale=scale), r=[kpss], w=[kpt])
            pts.append((pt, kpt))
        return pts

    def pv(ui, qb, h, pts):
        ot, kot = o_t[qb % 2], ("o_t", tag, qb % 2)
        if dv + 1 <= 128:
            pso, kpso = psb[4 + ui % 2], [("ps", 4 + ui % 2)]
            acc = [pso[:, tb * 128: tb * 128 + dv + 1] for tb in range(4)]
        else:
            p0, p1 = psb[4 + 2 * (ui % 2)], psb[5 + 2 * (ui % 2)]
            kpso = [("ps", 4 + 2 * (ui % 2)), ("ps", 5 + 2 * (ui % 2))]
            acc = [p0[:, 0:dv + 1], p0[:, 256:256 + dv + 1], p1[:, 0:dv + 1], p1[:, 256:256 + dv + 1]]
        for tb in range(4):
            last = 4 * qb + tb
            for kt in range(last + 1):
                pt, kpt = pts[kt]
                va, kv = v_ap(h, kt)
                P.pe(lambda e, a=acc[tb], pt=pt, tb=tb, va=va, kt=kt, last=last: e.matmul(
                    a, pt[:, tb * 128:(tb + 1) * 128], va, start=(kt == 0), stop=(kt == last)),
                    r=[kpt] + kv, w=kpso)
        rc, krc = rec[ui % 2], ("rec", tag, ui % 2)
        for tb in range(4):
            P.dve(lambda e, rc=rc, a=acc[tb], tb=tb: e.reciprocal(rc[:, tb:tb + 1], a[:, dv:dv + 1]), r=kpso, w=[krc])
        for tb in range(4):
            if tb % 2 == 0:
                P.dve(lambda e, rc=rc, a=acc[tb], tb=tb, h=h, ot=ot: e.tensor_scalar_mul(
                    ot[:, tb, h * dv:(h + 1) * dv], a[:, 0:dv], rc[:, tb:tb + 1]), r=kpso + [krc], w=[kot])
            else:
                P.act(lambda e, rc=rc, a=acc[tb], tb=tb, h=h, ot=ot: e.mul(
                    ot[:, tb, h * dv:(h + 1) * dv], a[:, 0:dv], rc[:, tb:tb + 1]), r=kpso + [krc], w=[kot])
        if h == nh - 1:
            out_cb(qb, ot, kot)

    prev = None
    for ui, (qb, h) in enumerate(units):
        pts = scores(qb, h)
        if prev is not None:
            pv(*prev)
        prev = (ui, qb, h, pts)
    pv(*prev)


def rms_out_cb(cx, col0, g_b, kg, mixed, tag):
    P, sb = cx.P, cx.sb
    junk = sb.alloc([128, 512], F32, "junk")
    ssq = sb.alloc([128, 4], F32, "ssq")
    ybuf = [sb.alloc([128, 4, 512], F32, "ybuf") for _ in range(2)]
    S = cx.S

    def cb(qb, ot, kot, b):
        ks = ("ssq", tag)
        yb, ky = ybuf[qb % 2], ("ybuf", tag, qb % 2)
        for tb in range(4):
            P.act(lambda e, tb=tb: e.activation(junk[:], ot[:, tb, :], AF.Square, accum_out=ssq[:, tb:tb + 1]),
                  r=[kot], w=[ks, ("junk", tag)])
        rstd_from_ssq(P, ssq[:], ssq[:], 512, RMS_EPS, [ks], [ks])
        for tb in range(4):
            P.dve(lambda e, tb=tb, yb=yb: e.scalar_tensor_tensor(yb[:, tb, :], ot[:, tb, :], ssq[:, tb:tb + 1], g_b,
                                                               ALU.mult, ALU.mult), r=[kot, ks, kg], w=[ky])
        r0 = b * S + qb * 512
        P.dma(mixed[r0:r0 + 512, col0:col0 + 512].rearrange("(tb p) n -> p tb n", p=128), yb[:], r=[ky], q="pool")
    return lambda b: (lambda qb, ot, kot: cb(qb, ot, kot, b))


def stage_fox(cx, qkT, fT, u_tok, b_f, out_g, mixed):
    nc, P, sb, c = cx.nc, cx.P, cx.sb, cx.c
    S, NB = cx.S, cx.NB
    nt = S // 128
    m = sb.mark()
    masks = c["masks_causal"]
    qT = sb.alloc([128, 4, S], BF16, "qT")
    kT = sb.alloc([128, 4, S], BF16, "kT")
    vaug = sb.alloc([128, nt, 8, 65], BF16, "vaug")
    vst = [sb.alloc([128, 512], F32, "vst") for _ in range(2)]
    fx = sb.alloc([8, S], F32, "fx")
    fa = sb.alloc([8, S], F32, "fa")
    Fc = sb.alloc([8, S], F32, "Fc")
    ones8 = sb.alloc([8, S], F32, "ones8")
    F8 = sb.alloc([8, S], BF16, "F8")
    bfc = sb.alloc([8, 1], F32, "bfc")
    sel = sb.alloc([8, 8, 128], BF16, "sel")
    nF = sb.alloc([128, nt, 8], F32, "nF")
    g_b = sb.alloc([128, 512], F32, "g_b")
    bcast_load(cx, g_b[:], out_g, "fox_g")
    P.dma(bfc[:], b_f.rearrange("(h o) -> h o", o=1), w=["bfc"])
    P.pool(lambda e: e.memset(ones8[:], 1.0), w=["ones8"])
    P.pool(lambda e: e.memset(vaug[:, :, :, 64:65], 1.0), w=["vones"])
    P.dve(lambda e: e.tensor_copy(sel[:], c["ident"][0:8, 0:8].unsqueeze(2).to_broadcast([8, 8, 128])), r=["ident"], w=["sel"])
    ocb = rms_out_cb(cx, 0, g_b[:], "fox_g", mixed, "fox")
    abufs = attn_bufs(cx, 8, 64)
    for b in range(NB):
        t0 = b * S
        tag = "fox%d" % b
        P.dma(qT[:], qkT[0:512, t0:t0 + S].rearrange("(hp p) t -> p hp t", p=128), w=["qT"])
        P.dma(kT[:], qkT[512:1024, t0:t0 + S].rearrange("(hp p) t -> p hp t", p=128), w=["kT"])
        P.dma(fx[:], fT[0:8, t0:t0 + S], w=["fx"])
        for tt in range(nt):
            vs, kvs = vst[tt % 2], ("vst", tt % 2)
            P.dma(vs[:], u_tok[t0 + tt * 128:t0 + (tt + 1) * 128, 1024:1536], w=[kvs])
            P.pool(lambda e, vs=vs, tt=tt: e.tensor_copy(vaug[:, tt, :, 0:64], vs[:].rearrange("p (h d) -> p h d", h=8)),
                   r=[kvs], w=[("v", tt)])
        P.dve(lambda e: e.tensor_scalar_add(fx[:], fx[:], bfc[:, 0:1]), r=["fx", "bfc"], w=["fx"])
        P.act(lambda e: e.activation(fa[:], fx[:], AF.Abs), r=["fx"], w=["fa"])
        P.act(lambda e: e.activation(fa[:], fa[:], AF.Exp, scale=-1.0), r=["fa"], w=["fa"])
        P.act(lambda e: e.activation(fa[:], fa[:], AF.Ln, bias=1.0), r=["fa"], w=["fa"])
        P.dve(lambda e: e.tensor_scalar_min(fx[:], fx[:], 0.0), r=["fx"], w=["fx"])
        P.dve(lambda e: e.tensor_sub(fx[:], fx[:], fa[:]), r=["fx", "fa"], w=["fx"])
        P.dve(lambda e: e.tensor_tensor_scan(Fc[:], ones8[:], fx[:], 0.0, ALU.mult, ALU.add), r=["fx", "ones8"], w=["Fc"])
        P.act(lambda e: e.mul(F8[:], Fc[:], 8.0), r=["Fc"], w=["F8"])
        for tt in range(nt):
            ps, kp = cx.ps[7], ("ps", 7)
            P.pe(lambda e, tt=tt, ps=ps: e.transpose(ps[:, 0:8], Fc[0:8, tt * 128:(tt + 1) * 128], c["ident"][0:8, 0:8]),
                 r=["Fc", "ident"], w=[kp])
            P.act(lambda e, tt=tt, ps=ps: e.mul(nF[:, tt, :], ps[:, 0:8], -1.0), r=[kp], w=["nF"])

        def score_ops(h, kt, qb):
            hp, base = h // 2, (h % 2) * 64
            return [(kT[base:base + 64, hp, kt * 128:(kt + 1) * 128], qT[base:base + 64, hp, qb * 512:(qb + 1) * 512], ["kT", "qT"]),
                    (sel[0:8, h, :], F8[0:8, qb * 512:(qb + 1) * 512], ["sel", "F8"])]

        def bias_ap(h, kt):
            return nF[:, kt, h:h + 1], ["nF"]

        def v_ap(h, kt):
            return vaug[:, kt, h, :], [("v", kt), "vones"]

        attn_core(cx, 8, 64, 0.125, score_ops, bias_ap, masks, v_ap, ocb(b), tag, bufs=abufs)
    P.barrier()
    sb.reset(m)


def rope_tables(cx, pos_dram, b, cosb, sinb, ifr_b, tmpa, rbufs):
    P, sb = cx.P, cx.sb
    S = cx.S
    nt = S // 128
    posi, posf = rbufs["posi"], rbufs["posf"]
    TWO_PI = 2.0 * np.pi
    P.dma(posi[:], pos_dram[b, :].rearrange("(t p) -> p t", p=128), w=["posi"], slow=True)
    P.dve(lambda e: e.tensor_copy(posf[:], posi[:]), r=["posi"], w=["posf"])
    for tt in range(nt):
        P.dve(lambda e, tt=tt: e.tensor_scalar_mul(tmpa[:, tt, :], ifr_b, posf[:, tt:tt + 1]), r=["posf", "ifr"], w=["ang"])
    ti, tf = rbufs["ti"], rbufs["tf"]
    C1, C2 = 6.28125, 2.0 * np.pi - 6.28125

    def sin_of(out, shift, key):
        P.dve(lambda e: e.tensor_scalar(out, tmpa[:], shift, 1.0 / TWO_PI, ALU.add, ALU.mult), r=["ang"], w=[key])
        P.dve(lambda e: e.tensor_copy(ti[:], out), r=[key], w=["ropei"])
        P.dve(lambda e: e.tensor_copy(tf[:], ti[:]), r=["ropei"], w=["ropef"])
        P.dve(lambda e: e.tensor_scalar_add(out, tmpa[:], shift), r=["ang"], w=[key])
        P.dve(lambda e: e.scalar_tensor_tensor(out, tf[:], -C1, out, ALU.mult, ALU.add), r=["ropef", key], w=[key])
        P.dve(lambda e: e.scalar_tensor_tensor(out, tf[:], -C2, out, ALU.mult, ALU.add), r=["ropef", key], w=[key])
        P.dve(lambda e: e.tensor_scalar(tf[:], out, np.pi, -TWO_PI, ALU.is_gt, ALU.mult), r=[key], w=["ropef"])
        P.dve(lambda e: e.tensor_add(out, out, tf[:]), r=["ropef", key], w=[key])
        P.dve(lambda e: e.tensor_scalar(tf[:], out, -np.pi, TWO_PI, ALU.is_lt, ALU.mult), r=[key], w=["ropef"])
        P.dve(lambda e: e.tensor_add(out, out, tf[:]), r=["ropef", key], w=[key])
        P.dve(lambda e: e.tensor_scalar(out, out, 3.1415925, -3.1415925, ALU.min, ALU.max), r=[key], w=[key])
        P.act(lambda e: e.activation(out, out, AF.Sin), r=[key], w=[key])

    sin_of(sinb[:], 0.0, "sinb")
    sin_of(cosb[:], 0.5 * np.pi, "cosb")


def rope_apply(P, out, x, cos, sin, t1, t2, nh, r, w):
    cb = cos.unsqueeze(1).to_broadcast([128, nh, 32])
    sn = sin.unsqueeze(1).to_broadcast([128, nh, 32])
    x1, x2 = x[:, :, 0:32], x[:, :, 32:64]
    kk = ("ropetmp",)
    P.dve(lambda e: e.tensor_mul(t1, x1, cb), r=r, w=[kk])
    P.dve(lambda e: e.tensor_mul(t2, x2, sn), r=r, w=[kk])
    P.dve(lambda e: e.tensor_sub(out[:, :, 0:32], t1, t2), r=[kk], w=w)
    P.dve(lambda e: e.tensor_mul(t1, x2, cb), r=r + [kk], w=[kk])
    P.dve(lambda e: e.tensor_mul(t2, x1, sn), r=r + [kk], w=[kk])
    P.dve(lambda e: e.tensor_add(out[:, :, 32:64], t1, t2), r=[kk], w=w)


def load_cast_w(cx, dst_bf, src_view, stage, key, eng="dve"):
    P = cx.P
    ks = ("wstage", id(stage))
    P.dma(stage, src_view, w=[ks])
    if eng == "pool":
        P.pool(lambda e: e.tensor_copy(dst_bf, stage), r=[ks], w=[key])
    else:
        P.dve(lambda e: e.tensor_copy(dst_bf, stage), r=[ks], w=[key])


def stage_mla(cx, u_tok, pos, ifr, qg, kvg, w_uq, w_ukv, out_g, mixed):
    nc, P, sb, c = cx.nc, cx.P, cx.sb, cx.c
    S, NB = cx.S, cx.NB
    nt = S // 128
    psb = cx.ps
    m = sb.mark()
    masks = c["masks_chunk64"]
    wq_n = sb.alloc([128, 3, 512], BF16, "wq_n")
    wq_p = sb.alloc([128, 3, 256], BF16, "wq_p")
    wk_n = sb.alloc([128, 512], BF16, "wk_n")
    wk_v = sb.alloc([128, 512], BF16, "wk_v")
    wstg = sb.alloc([128, 3, 768], F32, "wstg")
    P.dma(wstg[:], w_uq.rearrange("(kc p) n -> p kc n", p=128), w=["wstg"])
    v4 = wstg[:].rearrange("p k (h d) -> p k h d", h=4)
    for kc in range(3):
        P.pool(lambda e, kc=kc: e.tensor_copy(wq_n[:, kc, :].rearrange("p (h d) -> p h d", h=4), v4[:, kc, :, 0:128]), r=["wstg"], w=["wq"])
        P.pool(lambda e, kc=kc: e.tensor_copy(wq_p[:, kc, :].rearrange("p (h d) -> p h d", h=4), v4[:, kc, :, 128:192]), r=["wstg"], w=["wq"])
    wstg2 = sb.alloc([128, 1024], F32, "wstg2")
    P.dma(wstg2[:], w_ukv, w=["wstg2"])
    v5 = wstg2[:].rearrange("p (h d) -> p h d", h=4)
    P.pool(lambda e: e.tensor_copy(wk_n[:].rearrange("p (h d) -> p h d", h=4), v5[:, :, 0:128]), r=["wstg2"], w=["wk"])
    P.pool(lambda e: e.tensor_copy(wk_v[:].rearrange("p (h d) -> p h d", h=4), v5[:, :, 128:256]), r=["wstg2"], w=["wk"])
    qg_b = sb.alloc([128, 384], F32, "qg_b")
    kvg_b = sb.alloc([128, 128], F32, "kvg_b")
    g_b = sb.alloc([128, 512], F32, "g_b")
    ifr_b = sb.alloc([128, 32], F32, "ifr_b")
    bcast_load(cx, qg_b[:], qg, "qg")
    bcast_load(cx, kvg_b[:], kvg, "kvg")
    bcast_load(cx, g_b[:], out_g, "mla_g")
    bcast_load(cx, ifr_b[:], ifr, "ifr")
    cosb = sb.alloc([128, nt, 32], F32, "cosb")
    sinb = sb.alloc([128, nt, 32], F32, "sinb")
    ang = sb.alloc([128, nt, 32], F32, "ang")
    cqnT = sb.alloc([128, 3, S], BF16, "cqnT")
    ckvnT = sb.alloc([128, S], BF16, "ckvnT")
    qnT = sb.alloc([128, 4, S], BF16, "qnT")
    qpT = sb.alloc([128, 2, S], BF16, "qpT")
    knT = sb.alloc([128, 4, S], BF16, "knT")
    kpT = sb.alloc([128, S], BF16, "kpT")
    vaug = sb.alloc([128, nt, 4, 129], BF16, "vaug")
    urow = [sb.alloc([128, 576], F32, "urow") for _ in range(2)]
    cn = [sb.alloc([128, 640], F32, "cn") for _ in range(2)]
    qpe = [sb.alloc([128, 256], F32, "qpe") for _ in range(2)]
    junk = sb.alloc([128, 384], F32, "junk")
    ssq = sb.alloc([128, 2], F32, "ssq2")
    t1 = sb.alloc([128, 4, 32], F32, "t1")
    t2 = sb.alloc([128, 4, 32], F32, "t2")
    P.pool(lambda e: e.memset(vaug[:, :, :, 128:129], 1.0), w=["vones"])
    ocb = rms_out_cb(cx, 512, g_b[:], "mla_g", mixed, "mla")
    abufs = attn_bufs(cx, 4, 128)
    rbufs = dict(posi=sb.alloc([128, nt], I32, "posi"), posf=sb.alloc([128, nt], F32, "posf"),
                 ti=sb.alloc([128, nt, 32], I32, "ropei"), tf=sb.alloc([128, nt, 32], F32, "ropef"))
    for b in range(NB):
        t0 = b * S
        tag = "mla%d" % b
        rope_tables(cx, pos, b, cosb, sinb, ifr_b[:], ang, rbufs)
        nev = 0
        for tt in range(nt):
            i2 = tt % 2
            ur, kur = urow[i2], ("urow", i2)
            cnt, kcn = cn[i2], ("cn", i2)
            P.dma(ur[:], u_tok[t0 + tt * 128:t0 + (tt + 1) * 128, C_MLA:C_MLA + 576], w=[kur])
            P.act(lambda e, ur=ur: e.activation(junk[:, 0:384], ur[:, 0:384], AF.Square, accum_out=ssq[:, 0:1]), r=[kur], w=["ssq2", "junk"])
            P.act(lambda e, ur=ur: e.activation(junk[:, 0:128], ur[:, 384:512], AF.Square, accum_out=ssq[:, 1:2]), r=[kur], w=["ssq2", "junk"])
            rstd_from_ssq(P, ssq[:, 0:1], ssq[:, 0:1], 384, RMS_EPS, ["ssq2"], ["ssq2"])
            rstd_from_ssq(P, ssq[:, 1:2], ssq[:, 1:2], 128, RMS_EPS, ["ssq2"], ["ssq2"])
            P.dve(lambda e, ur=ur, cnt=cnt: e.scalar_tensor_tensor(cnt[:, 0:384], ur[:, 0:384], ssq[:, 0:1], qg_b[:], ALU.mult, ALU.mult),
                  r=[kur, "ssq2", "qg"], w=[kcn])
            P.dve(lambda e, ur=ur, cnt=cnt: e.scalar_tensor_tensor(cnt[:, 384:512], ur[:, 384:512], ssq[:, 1:2], kvg_b[:], ALU.mult, ALU.mult),
                  r=[kur, "ssq2", "kvg"], w=[kcn])
            rope_apply(P, cnt[:, 512:576].rearrange("p (h d) -> p h d", h=1), ur[:, 512:576].rearrange("p (h d) -> p h d", h=1),
                       cosb[:, tt, :], sinb[:, tt, :], t1[:, 0:1, :], t2[:, 0:1, :], 1, [kur, "cosb", "sinb"], [kcn])
            P.dve(lambda e, cnt=cnt: e.tensor_copy(cnt[:, 576:640], cnt[:, 512:576]), r=[kcn], w=[kcn])
            pi = 2 + nev % 2
            nev += 1
            ps, kp = psb[pi], ("ps", pi)
            for j in range(4):
                P.pe(lambda e, ps=ps, cnt=cnt, j=j: e.transpose(ps[:, j * 128:(j + 1) * 128], cnt[:, j * 128:(j + 1) * 128], c["ident"][:]),
                     r=[kcn, "ident"], w=[kp])
            P.act(lambda e, ps=ps, tt=tt: e.copy(cqnT[:, :, tt * 128:(tt + 1) * 128], ps[:, 0:384].rearrange("p (j t) -> p j t", j=3)),
                  r=[kp], w=[("cqnT", tt)])
            P.dve(lambda e, ps=ps, tt=tt: e.tensor_copy(ckvnT[:, tt * 128:(tt + 1) * 128], ps[:, 384:512]), r=[kp], w=[("ckvnT", tt)])
            pi = 2 + nev % 2
            nev += 1
            ps, kp = psb[pi], ("ps", pi)
            P.pe(lambda e, ps=ps, cnt=cnt: e.transpose(ps[:, 0:128], cnt[:, 512:640], c["ident"][:]), r=[kcn, "ident"], w=[kp])
            P.act(lambda e, ps=ps, tt=tt: e.copy(kpT[:, tt * 128:(tt + 1) * 128], ps[:, 0:128]), r=[kp], w=[("kpT", tt)])
            pi = 2 + nev % 2
            nev += 1
            ps, kp = psb[pi], ("ps", pi)
            for kc in range(3):
                P.pe(lambda e, ps=ps, kc=kc, tt=tt: e.matmul(ps[:, 0:256], cqnT[:, kc, tt * 128:(tt + 1) * 128], wq_p[:, kc, :],
                                                            start=(kc == 0), stop=(kc == 2)), r=[("cqnT", tt), "wq"], w=[kp])
            qp, kqp = qpe[i2], ("qpe", i2)
            rope_apply(P, qp[:].rearrange("p (h d) -> p h d", h=4), ps[:, 0:256].rearrange("p (h d) -> p h d", h=4),
                       cosb[:, tt, :], sinb[:, tt, :], t1[:], t2[:], 4, [kp, "cosb", "sinb"], [kqp])
            pi = 2 + nev % 2
            nev += 1
            ps, kp = psb[pi], ("ps", pi)
            for j in range(2):
                P.pe(lambda e, ps=ps, qp=qp, j=j: e.transpose(ps[:, j * 128:(j + 1) * 128], qp[:, j * 128:(j + 1) * 128], c["ident"][:]),
                     r=[kqp, "ident"], w=[kp])
            P.act(lambda e, ps=ps, tt=tt: e.copy(qpT[:, :, tt * 128:(tt + 1) * 128], ps[:, 0:256].rearrange("p (j t) -> p j t", j=2)),
                  r=[kp], w=[("qpT", tt)])
            pi = 2 + nev % 2
            nev += 1
            ps, kp = psb[pi], ("ps", pi)
            P.pe(lambda e, ps=ps, tt=tt: e.matmul(ps[:], ckvnT[:, tt * 128:(tt + 1) * 128], wk_v[:], start=True, stop=True),
                 r=[("ckvnT", tt), "wk"], w=[kp])
            P.dve(lambda e, ps=ps, tt=tt: e.tensor_copy(vaug[:, tt, :, 0:128], ps[:].rearrange("p (h d) -> p h d", h=4)),
                  r=[kp], w=[("v", tt)])
        for tb in range(S // 512):
            for h in range(4):
                pi = 2 + nev % 2
                nev += 1
                ps, kp = psb[pi], ("ps", pi)
                for kc in range(3):
                    P.pe(lambda e, ps=ps, kc=kc, h=h, tb=tb: e.matmul(ps[:], wq_n[:, kc, h * 128:(h + 1) * 128], cqnT[:, kc, tb * 512:(tb + 1) * 512],
                                                                  start=(kc == 0), stop=(kc == 2)),
                         r=[("cqnT", tb * 4 + i) for i in range(4)] + ["wq"], w=[kp])
                evac(P, nev, qnT[:, h, tb * 512:(tb + 1) * 512], ps[:], r=[kp], w=[("qnT", tb)])
                pi = 2 + nev % 2
                nev += 1
                ps, kp = psb[pi], ("ps", pi)
                P.pe(lambda e, ps=ps, h=h, tb=tb: e.matmul(ps[:], wk_n[:, h * 128:(h + 1) * 128], ckvnT[:, tb * 512:(tb + 1) * 512],
                                                         start=True, stop=True),
                     r=[("ckvnT", tb * 4 + i) for i in range(4)] + ["wk"], w=[kp])
                evac(P, nev, knT[:, h, tb * 512:(tb + 1) * 512], ps[:], r=[kp], w=[("knT", tb)])

        def score_ops(h, kt, qb):
            base = (h % 2) * 64
            return [(knT[:, h, kt * 128:(kt + 1) * 128], qnT[:, h, qb * 512:(qb + 1) * 512], [("knT", kt // 4), ("qnT", qb)]),
                    (kpT[base:base + 64, kt * 128:(kt + 1) * 128], qpT[base:base + 64, h // 2, qb * 512:(qb + 1) * 512],
                     [("kpT", kt)] + [("qpT", qb * 4 + i) for i in range(4)])]

        def v_ap(h, kt):
            return vaug[:, kt, h, :], [("v", kt), "vones"]

        attn_core(cx, 4, 128, 192 ** -0.5, score_ops, None, masks, v_ap, ocb(b), tag, sbanks=(0, 1), bufs=abufs)
    P.barrier()
    sb.reset(m)


def run_interleaved(gens):
    gens = list(gens)
    while gens:
        for g in list(gens):
            try:
                next(g)
            except StopIteration:
                gens.remove(g)


def dplr_consts(cx):
    P, sb = cx.P, cx.sb
    c = cx.c
    if "MS" in c:
        return
    MS = sb.alloc([128, 128], F32, "MS")
    MI = sb.alloc([128, 128], F32, "MI")
    MZ = sb.alloc([128, 128], F32, "MZ")
    ML = sb.alloc([128, 128], F32, "ML")
    BLK = sb.alloc([128, 128], F32, "BLK")
    for (M, base, cm, pat, lo_zero) in ((MS, -1, -1, 1, "ur"), (MI, 0, -1, 1, "ur"), (MZ, -1, 1, -1, "ll"), (ML, 0, 1, -1, "ll")):
        k = "dplrmask"
        P.pool(lambda e, M=M: e.memset(M[:], 1.0), w=[k])
        P.pool(lambda e, M=M, base=base, cm=cm, pat=pat: e.affine_select(
            out=M[:], in_=M[:], pattern=[[pat, 128]], compare_op=ALU.is_ge, fill=0.0, base=base, channel_multiplier=cm), r=[k], w=[k])
        if lo_zero == "ur":
            P.pool(lambda e, M=M: e.memset(M[0:64, 64:128], 0.0), r=[k], w=[k])
        else:
            P.pool(lambda e, M=M: e.memset(M[64:128, 0:64], 0.0), r=[k], w=[k])
    P.pool(lambda e: e.memset(BLK[:], 0.0), w=["dplrmask"])
    P.pool(lambda e: e.memset(BLK[0:64, 0:64], 1.0), r=["dplrmask"], w=["dplrmask"])
    P.pool(lambda e: e.memset(BLK[64:128, 64:128], 1.0), r=["dplrmask"], w=["dplrmask"])
    c.update(MS=MS, MI=MI, MZ=MZ, ML=ML, BLK=BLK)


def dplr_tile(cx, NH, NK, geo, st, tag, banks=(0, 1, 2)):
    P, c = cx.P, cx.c
    B0, B1, B2 = banks
    psb = {0: cx.ps[B0], 1: cx.ps[B1], 2: cx.ps[B2], 3: cx.ps[B0], 4: cx.ps[B1], 5: cx.ps[B2], 6: cx.ps[B0]}
    bk = {0: B0, 1: B1, 2: B2, 3: B0, 4: B1, 5: B2, 6: B0}
    HP = 128 // NK
    NG = NH // HP
    HG = 512 // 128
    ngr = (NH + HG - 1) // HG
    Xs, Zs, Ps = st["X"], st["Z"], st["P"]
    kX, kZ, kP = [("dX", tag, i) for i in range(2)], [("dZ", tag, i) for i in range(2)], [("dP", tag, i) for i in range(2)]
    identb3 = c["identb"][:].unsqueeze(1).to_broadcast([128, NH, 128])
    P.dve(lambda e: e.tensor_tensor(Ps[0][:], Xs[0][:], identb3, ALU.add), r=[kX[0], "identb"], w=[kP[0]])
    cur = 0
    for lvl in range(1, 6):
        nxt = 1 - cur
        for g in range(ngr):
            hs = list(range(g * HG, min(NH, (g + 1) * HG)))
            n = len(hs)
            if lvl < 5:
                for i, h in enumerate(hs):
                    P.pe(lambda e, i=i, h=h, cur=cur: e.matmul(psb[0][:, i * 128:(i + 1) * 128], Zs[cur][:, h, :], Xs[cur][:, h, :],
                                                             start=True, stop=True), r=[kX[cur], kZ[cur]], w=[("ps", bk[0])])
                P.act(lambda e, g=g, n=n, nxt=nxt: e.copy(Xs[nxt][:, g * HG:g * HG + n, :],
                                                         psb[0][:, 0:n * 128].rearrange("p (h t) -> p h t", h=n)),
                      r=[("ps", bk[0])], w=[kX[nxt]])
            for i, h in enumerate(hs):
                P.pe(lambda e, i=i, h=h, cur=cur: e.matmul(psb[1][:, i * 128:(i + 1) * 128], Xs[cur][:, h, :], Zs[cur][:, h, :],
                                                         start=True, stop=True), r=[kX[cur], kZ[cur]], w=[("ps", bk[1])])
            P.dve(lambda e, g=g, n=n, nxt=nxt: e.tensor_copy(Zs[nxt][:, g * HG:g * HG + n, :],
                                                            psb[1][:, 0:n * 128].rearrange("p (h t) -> p h t", h=n)),
                  r=[("ps", bk[1])], w=[kZ[nxt]])
            for i, h in enumerate(hs):
                P.pe(lambda e, i=i, h=h, cur=cur, nxt=nxt: e.matmul(psb[2][:, i * 128:(i + 1) * 128], Zs[nxt][:, h, :], Ps[cur][:, h, :],
                                                                  start=True, stop=True), r=[kZ[nxt], kP[cur]], w=[("ps", bk[2])])
            P.dve(lambda e, g=g, n=n, cur=cur, nxt=nxt: e.tensor_tensor(
                Ps[nxt][:, g * HG:g * HG + n, :], psb[2][:, 0:n * 128].rearrange("p (h t) -> p h t", h=n),
                Ps[cur][:, g * HG:g * HG + n, :], ALU.add), r=[("ps", bk[2]), kP[cur]], w=[kP[nxt]])
            yield
        cur = nxt
    TT, kTT = Ps[cur], kP[cur]
    ST, STb = st["ST"], st["STb"]
    kST = ("dST", tag)
    NV = NK
    W = NH * NV
    Wb, Ub = st["Wb"], st["Ub"]
    for ch in range(2):
        c0 = ch * 64
        M = 64 + c0
        rows = slice(c0, c0 + 64)
        for h in range(NH):
            g_, hb = h // HP, (h % HP) * NK
            at, kat = geo["AT"](h)
            P.pe(lambda e, h=h, at=at, g_=g_, hb=hb, M=M: e.matmul(psb[3][0:M, h * NV:(h + 1) * NV], at[:, 0:M], STb[:, g_, :],
                                                                start=True, stop=False), r=kat + [kST], w=[("ps", bk[3])])
            ak, kak = geo["AAK"](h)
            v, kv = geo["V"](h)
            P.pe(lambda e, h=h, ak=ak, v=v, M=M, rows=rows: e.matmul(psb[3][0:M, h * NV:(h + 1) * NV], ak[rows, 0:M], v[rows, :],
                                                                  start=False, stop=True), r=kak + kv, w=[("ps", bk[3])])
        kW = ("dW", tag)
        if geo.get("WADD") is not None:
            wa, kwa = geo["WADD"]
            P.dve(lambda e, rows=rows, wa=wa: e.tensor_tensor(Wb[rows, :], psb[3][rows, 0:W], wa[rows, :], ALU.add), r=[("ps", bk[3])] + kwa, w=[kW])
        else:
            P.act(lambda e, rows=rows: e.copy(Wb[rows, :], psb[3][rows, 0:W]), r=[("ps", bk[3])], w=[kW])
        yield
        for h in range(NH):
            P.pe(lambda e, h=h, M=M, rows=rows: e.matmul(psb[4][0:M, h * NV:(h + 1) * NV], TT[rows, h, 0:M], Wb[rows, h * NV:(h + 1) * NV],
                                                      start=True, stop=True), r=[kTT, kW], w=[("ps", bk[4])])
        kU = ("dU", tag)
        P.dve(lambda e, rows=rows: e.tensor_copy(Ub[rows, :], psb[4][rows, 0:W]), r=[("ps", bk[4])], w=[kU])
        yield
        for h in range(NH):
            g_, hb = h // HP, (h % HP) * NK
            rt, krt = geo["RT"](h)
            arb, karb = geo["ARB"](h)
            ark, kark = geo["ARK"](h)
            v, kv = geo["V"](h)
            o = psb[5][0:M, h * NV:(h + 1) * NV]
            P.pe(lambda e, o=o, rt=rt, g_=g_, hb=hb, M=M: e.matmul(o, rt[:, 0:M], STb[:, g_, :], start=True, stop=False),
                 r=krt + [kST], w=[("ps", bk[5])])
            P.pe(lambda e, o=o, arb=arb, h=h, M=M, rows=rows: e.matmul(o, arb[rows, 0:M], Ub[rows, h * NV:(h + 1) * NV], start=False, stop=False),
                 r=karb + [kU], w=[("ps", bk[5])])
            P.pe(lambda e, o=o, ark=ark, v=v, M=M, rows=rows: e.matmul(o, ark[rows, 0:M], v[rows, :], start=False, stop=True),
                 r=kark + kv, w=[("ps", bk[5])])
        yo, kyo = geo["Y"]
        P.act(lambda e, rows=rows: e.copy(yo[rows, :], psb[5][rows, 0:W]), r=[("ps", bk[5])], w=[kyo])
        bd, kbd = geo["BD"]
        kd, kkd = geo["KD"]
        for h in range(NH):
            g_ = h // HP
            v, kv = geo["V"](h)
            o = psb[6][:, h * NV:(h + 1) * NV]
            P.pe(lambda e, o=o, g_=g_, h=h, rows=rows: e.matmul(o, bd[rows, g_ * 128:(g_ + 1) * 128], Ub[rows, h * NV:(h + 1) * NV],
                                                             start=True, stop=False), r=kbd + [kU], w=[("ps", bk[6])])
            P.pe(lambda e, o=o, g_=g_, v=v, rows=rows: e.matmul(o, kd[rows, g_ * 128:(g_ + 1) * 128], v[rows, :], start=False, stop=True),
                 r=kkd + kv, w=[("ps", bk[6])])
        gc_, kgc = geo["GC"](ch)
        for hh in range(HP):
            pr = slice(hh * NK, (hh + 1) * NK)
            src = psb[6][pr, 0:NH * NV].rearrange("p (g x v) -> p g x v", g=NG, x=HP)[:, :, hh, :]
            P.dve(lambda e, pr=pr: e.tensor_tensor(ST[pr, :, :], ST[pr, :, :], gc_[pr, :].unsqueeze(2).to_broadcast([NK, NG, NV]), ALU.mult),
                  r=[kST] + kgc, w=[kST])
            P.dve(lambda e, pr=pr, src=src: e.tensor_tensor(ST[pr, :, :], ST[pr, :, :], src, ALU.add), r=[kST, ("ps", bk[6])], w=[kST])
        P.act(lambda e: e.copy(STb[:], ST[:]), r=[kST], w=[kST])
        yield


def stage_rwkv(cx, u_tok, prm, mixed):
    nc, P, sb, c, psb = cx.nc, cx.P, cx.sb, cx.c, cx.ps
    S, NB = cx.S, cx.NB
    nt = S // 128
    m = sb.mark()
    dplr_consts(cx)
    NH, NK = 8, 64
    f32t = lambda name, w=512: sb.alloc([128, w], F32, name)
    mu_b = f32t("mu_b", 1792)
    bc = {}
    for nm in ("w0", "a0", "k_k", "k_a", "r_k", "ln_g", "ln_b"):
        bc[nm] = f32t(nm + "_b")
        bcast_load(cx, bc[nm][:], prm[nm], "rw_" + nm)
    bcast_load(cx, mu_b[:], prm["mu"], "rw_mu")
    w2b = sb.alloc([64, 512], BF16, "w2b")
    a2b = sb.alloc([128, 512], BF16, "a2b")
    g2b = sb.alloc([128, 512], BF16, "g2b")
    wtmp = f32t("wtmp")
    load_cast_w(cx, w2b[:], prm["w2"], wtmp[0:64, :], "rw_w2")
    wtmp2 = f32t("wtmp2")
    load_cast_w(cx, a2b[64:128, :], prm["a2"], wtmp2[64:128, :], "rw_a2")
    wtmp3 = f32t("wtmp3")
    load_cast_w(cx, g2b[:], prm["g2"], wtmp3[:], "rw_g2")
    Ut = [sb.alloc([128, 1792], F32, "Ut") for _ in range(2)]
    Up = [sb.alloc([128, 1792], F32, "Up") for _ in range(2)]
    xs = sb.alloc([128, 1792], F32, "xs")
    la = f32t("la", 256)
    lT = sb.alloc([128, 256], BF16, "lT")
    names = ["lw", "aa", "gt", "Gs", "eG", "enG", "eGm", "eD", "kx", "sq", "kk", "k2", "bv", "tR", "tK", "tB", "tA", "rk", "yt", "y2"]
    T_ = {n: f32t(n) for n in names}
    small = {n: sb.alloc([128, 8], F32, n) for n in ("ssq", "rn", "bs", "s1", "s2", "mean", "var", "rstd")}
    PB = []
    for _b in range(NB):
        d_ = dict(Kd=sb.alloc([128, 512], BF16, "Kd"), Bd=sb.alloc([128, 512], BF16, "Bd"), Vb=sb.alloc([128, 512], BF16, "Vb"),
                  ART=sb.alloc([128, 8, 2, 128], BF16, "ARTz"), KT=sb.alloc([128, 4, 128], BF16, "KT"), BT=sb.alloc([128, 4, 128], BF16, "BT"),
                  AM3=sb.alloc([128, 8, 384], BF16, "AM3"), gC=[sb.alloc([128, 4], F32, "gC") for _ in range(2)],
                  yt=f32t("ytb"), gt=f32t("gtb"), vk=f32t("vkb"), bs=sb.alloc([128, 8], F32, "bsb"),
                  st=dict(X=[sb.alloc([128, 8, 128], BF16, "dX") for _ in range(2)], Z=[sb.alloc([128, 8, 128], BF16, "dZ") for _ in range(2)],
                          P=[sb.alloc([128, 8, 128], BF16, "dP") for _ in range(2)], ST=sb.alloc([128, 4, 64], F32, "ST"),
                          STb=sb.alloc([128, 4, 64], BF16, "STb"), Wb=sb.alloc([128, 512], BF16, "Wb"), Ub=sb.alloc([128, 512], BF16, "Ub")))
        P.pool(lambda e, A_=d_["ART"]: e.memset(A_[:], 0.0), w=[("ARTz0", _b)])
        PB.append(d_)
    mask4 = sb.alloc([128, 4, 128], F32, "mask4")
    for i, Mk in enumerate(("MS", "MI", "MS", "MI")):
        P.pool(lambda e, i=i, Mk=Mk: e.tensor_copy(mask4[:, i, :], c[Mk][:]), r=["dplrmask"], w=["mask4"])
    MZ4 = c["MZ"][:].unsqueeze(1).to_broadcast([128, 4, 128])
    h3 = lambda t: t.rearrange("p (h d) -> p h d", h=8)
    b3 = lambda t: t.unsqueeze(2).to_broadcast([128, 8, 64])
    if True:
        def tile_prep(b, tt):
            tag = "rw%d" % b
            kST = ("dST", tag)
            pb = PB[b]
            Kd, Bd, Vb, ART, KT, BT, AM3, gC, st = (pb[k] for k in ("Kd", "Bd", "Vb", "ART", "KT", "BT", "AM3", "gC", "st"))
            kgt, kbs, kvk, kyt = ("gt", b), ("bs", b), ("vk", b), ("yt", b)
            if tt == 0:
                P.pool(lambda e: e.memset(st["ST"][:], 0.0), w=[kST])
                P.pool(lambda e: e.memset(st["STb"][:], 0.0), r=[kST], w=[kST])
            i2 = (tt * NB + b) % 2
            r0 = b * S + tt * 128
            U_, Up_ = Ut[i2], Up[i2]
            kU_, kUp = ("Ut", i2), ("Up", i2)
            cs = slice(C_RWKV, C_RWKV + 1792)
            P.dma(U_[:], u_tok[r0:r0 + 128, cs], w=[kU_])
            if tt == 0:
                P.pool(lambda e, Up_=Up_: e.memset(Up_[0:1, :], 0.0), w=[kUp])
                P.dma(Up_[1:128, :], u_tok[r0:r0 + 127, cs], w=[kUp])
            else:
                P.dma(Up_[:], u_tok[r0 - 1:r0 + 127, cs], w=[kUp])
            P.dve(lambda e, U_=U_, Up_=Up_: e.tensor_sub(Up_[:], Up_[:], U_[:]), r=[kU_, kUp], w=[kUp])
            P.dve(lambda e, Up_=Up_: e.tensor_mul(Up_[:], Up_[:], mu_b[:]), r=[kUp, "rw_mu"], w=[kUp])
            P.dve(lambda e, U_=U_, Up_=Up_: e.tensor_add(xs[:], U_[:], Up_[:]), r=[kU_, kUp], w=["xs"])
            r_, k_, v_ = xs[:, 0:512], xs[:, 512:1024], xs[:, 1024:1536]
            P.act(lambda e: e.copy(pb["vk"][:], v_), r=["xs"], w=[kvk])
            P.act(lambda e: e.activation(la[:, 0:64], xs[:, 1536:1600], AF.Tanh), r=["xs"], w=["la"])
            P.act(lambda e: e.copy(la[:, 64:128], xs[:, 1600:1664]), r=["xs"], w=["la"])
            P.act(lambda e: e.activation(la[:, 128:256], xs[:, 1664:1792], AF.Sigmoid), r=["xs"], w=["la"])
            for j in range(2):
                P.pe(lambda e, j=j: e.transpose(psb[7][:, j * 128:(j + 1) * 128], la[:, j * 128:(j + 1) * 128], c["ident"][:]),
                     r=["la", "ident"], w=[("ps", 7)])
            P.act(lambda e: e.copy(lT[:], psb[7][:, 0:256]), r=[("ps", 7)], w=["lT"])
            P.pe(lambda e: e.matmul(psb[3][:], lT[0:64, 0:128], w2b[0:64, :], start=True, stop=True), r=["lT", "rw_w2"], w=[("ps", 3)])
            P.pe(lambda e: e.matmul(psb[4][:], lT[64:128, 0:128], a2b[64:128, :], start=True, stop=True), r=["lT", "rw_a2"], w=[("ps", 4)])
            P.pe(lambda e: e.matmul(psb[5][:], lT[:, 128:256], g2b[:], start=True, stop=True), r=["lT", "rw_g2"], w=[("ps", 5)])
            P.dve(lambda e: e.tensor_add(T_["lw"][:], psb[3][:], bc["w0"][:]), r=[("ps", 3), "rw_w0"], w=["lw"])
            P.act(lambda e: e.activation(T_["lw"][:], T_["lw"][:], AF.Sigmoid), r=["lw"], w=["lw"])
            P.act(lambda e: e.mul(T_["lw"][:], T_["lw"][:], -float(np.exp(-0.5))), r=["lw"], w=["lw"])
            P.dve(lambda e: e.tensor_add(T_["aa"][:], psb[4][:], bc["a0"][:]), r=[("ps", 4), "rw_a0"], w=["aa"])
            P.act(lambda e: e.activation(T_["aa"][:], T_["aa"][:], AF.Sigmoid), r=["aa"], w=["aa"])
            P.act(lambda e: e.copy(pb["gt"][:], psb[5][:]), r=[("ps", 5)], w=[kgt])
            P.pe(lambda e: e.matmul(psb[3][:], c["MI"][:], T_["lw"][:], start=True, stop=True), r=["dplrmask", "lw"], w=[("ps", 3)])
            P.pe(lambda e: e.matmul(psb[4][:], c["BLK"][:], T_["lw"][:], start=True, stop=True), r=["dplrmask", "lw"], w=[("ps", 4)])
            P.act(lambda e: e.copy(T_["Gs"][:], psb[3][:]), r=[("ps", 3)], w=["Gs"])
            P.act(lambda e: e.activation(T_["eG"][:], T_["Gs"][:], AF.Exp), r=["Gs"], w=["eG"])
            P.act(lambda e: e.activation(T_["enG"][:], T_["Gs"][:], AF.Exp, scale=-1.0), r=["Gs"], w=["enG"])
            P.dve(lambda e: e.tensor_sub(T_["eGm"][:], T_["Gs"][:], T_["lw"][:]), r=["Gs", "lw"], w=["eGm"])
            P.act(lambda e: e.activation(T_["eGm"][:], T_["eGm"][:], AF.Exp), r=["eGm"], w=["eGm"])
            P.dve(lambda e: e.tensor_sub(T_["eD"][:], psb[4][:], T_["Gs"][:]), r=[("ps", 4), "Gs"], w=["eD"])
            P.act(lambda e: e.activation(T_["eD"][:], T_["eD"][:], AF.Exp), r=["eD"], w=["eD"])
            P.dve(lambda e: e.tensor_mul(T_["kx"][:], k_, bc["k_k"][:]), r=["xs", "rw_k_k"], w=["kx"])
            P.pool(lambda e: e.tensor_mul(T_["sq"][:], T_["kx"][:], T_["kx"][:]), r=["kx"], w=["sq"])
            P.dve(lambda e: e.tensor_reduce(small["ssq"][:], h3(T_["sq"][:]), AX.X, ALU.add), r=["sq"], w=["ssq"])
            P.dve(lambda e: e.tensor_scalar_add(small["rn"][:], small["ssq"][:], 1e-6), r=["ssq"], w=["rn"])
            P.act(lambda e: e.activation(small["rn"][:], small["rn"][:], AF.Sqrt), r=["rn"], w=["rn"])
            P.dve(lambda e: e.reciprocal(small["rn"][:], small["rn"][:]), r=["rn"], w=["rn"])
            P.dve(lambda e: e.tensor_mul(h3(T_["kk"][:]), h3(T_["kx"][:]), b3(small["rn"][:])), r=["kx", "rn"], w=["kk"])
            P.dve(lambda e: e.scalar_tensor_tensor(T_["k2"][:], T_["aa"][:], -1.0, bc["k_a"][:], ALU.add, ALU.mult), r=["aa", "rw_k_a"], w=["k2"])
            P.dve(lambda e: e.scalar_tensor_tensor(T_["k2"][:], T_["k2"][:], 1.0, k_, ALU.add, ALU.mult), r=["k2", "xs"], w=["k2"])
            P.dve(lambda e: e.tensor_mul(T_["bv"][:], T_["kk"][:], T_["aa"][:]), r=["kk", "aa"], w=["bv"])
            P.pool(lambda e: e.tensor_mul(T_["tR"][:], r_, T_["eG"][:]), r=["xs", "eG"], w=["tR"])
            P.pool(lambda e: e.tensor_mul(T_["tK"][:], T_["k2"][:], T_["enG"][:]), r=["k2", "enG"], w=["tK"])
            P.pool(lambda e: e.tensor_mul(T_["tB"][:], T_["bv"][:], T_["enG"][:]), r=["bv", "enG"], w=["tB"])
            P.dve(lambda e: e.scalar_tensor_tensor(T_["tA"][:], T_["kk"][:], -1.0, T_["eGm"][:], ALU.mult, ALU.mult), r=["kk", "eGm"], w=["tA"])
            kgeo = ("geo", tag)
            P.pool(lambda e: e.tensor_mul(Kd[:], T_["k2"][:], T_["eD"][:]), r=["k2", "eD"], w=[kgeo])
            P.pool(lambda e: e.tensor_mul(Bd[:], T_["bv"][:], T_["eD"][:]), r=["bv", "eD"], w=[kgeo])
            P.act(lambda e: e.copy(Vb[:], v_), r=["xs"], w=[kgeo])
            P.pool(lambda e: e.tensor_mul(T_["rk"][:], r_, T_["k2"][:]), r=["xs", "k2"], w=["rk"])
            P.pool(lambda e: e.tensor_mul(T_["rk"][:], T_["rk"][:], bc["r_k"][:]), r=["rk", "rw_r_k"], w=["rk"])
            P.dve(lambda e: e.tensor_reduce(pb["bs"][:], h3(T_["rk"][:]), AX.X, ALU.add), r=["rk"], w=[kbs])
            for (src, ksrc, dst) in ((T_["tA"], "tA", lambda q: ART[:, q, 0, :]), (T_["tR"], "tR", lambda q: ART[:, q, 1, :]),
                                      (T_["tK"], "tK", lambda q: KT[:, q, :]), (T_["tB"], "tB", lambda q: BT[:, q, :])):
                for q in range(4):
                    P.pe(lambda e, src=src, q=q: e.transpose(psb[7][:, q * 128:(q + 1) * 128], src[:, q * 128:(q + 1) * 128], c["ident"][:]),
                         r=[ksrc, "ident"], w=[("ps", 7)])
                if ksrc in ("tA", "tR"):
                    j = 0 if ksrc == "tA" else 1
                    for x in range(2):
                        pr = slice(x * 64, (x + 1) * 64)
                        dst_ = ART[pr, :, :, :].rearrange("p (q x) a t -> p q x a t", x=2)[:, :, x, j, :]
                        if x == 0:
                            P.act(lambda e, dst_=dst_, pr=pr: e.copy(dst_, psb[7][pr, :].rearrange("p (q t) -> p q t", q=4)), r=[("ps", 7), ("ARTz0", b)], w=[kgeo])
                        else:
                            P.dve(lambda e, dst_=dst_, pr=pr: e.tensor_copy(dst_, psb[7][pr, :].rearrange("p (q t) -> p q t", q=4)), r=[("ps", 7), ("ARTz0", b)], w=[kgeo])
                elif ksrc == "tK":
                    P.dve(lambda e: e.tensor_copy(KT[:], psb[7][:].rearrange("p (q t) -> p q t", q=4)), r=[("ps", 7)], w=[kgeo])
                else:
                    P.dve(lambda e: e.tensor_copy(BT[:], psb[7][:].rearrange("p (q t) -> p q t", q=4)), r=[("ps", 7)], w=[kgeo])
            for q in range(4):
                P.pe(lambda e, q=q: e.transpose(psb[7][:, q * 128:(q + 1) * 128], T_["eG"][:, q * 128:(q + 1) * 128], c["ident"][:]),
                     r=["eG", "ident"], w=[("ps", 7)])
            pv7 = psb[7][:].rearrange("p (q t) -> p q t", q=4)
            P.dve(lambda e: e.tensor_copy(gC[0][:], pv7[:, :, 63]), r=[("ps", 7)], w=[kgeo])
            P.dve(lambda e: e.tensor_copy(gC[1][:], pv7[:, :, 127]), r=[("ps", 7)], w=[kgeo])
            kX0, kZ0 = ("dX", tag, 0), ("dZ", tag, 0)
            for h in range(8):
                q, hb = h // 2, (h % 2) * 64
                pa = psb[h % 2]
                kpa = ("ps", h % 2)
                rhs2 = ART[:, h, :, :].rearrange("p a t -> p (a t)")
                P.pe(lambda e, pa=pa, q=q, rhs2=rhs2: e.matmul(pa[:, 0:256], BT[:, q, :], rhs2, start=True, stop=True), r=[kgeo], w=[kpa])
                P.pe(lambda e, pa=pa, q=q, rhs2=rhs2: e.matmul(pa[:, 256:512], KT[:, q, :], rhs2, start=True, stop=True), r=[kgeo], w=[kpa])
                pav = pa[:].rearrange("p (a t) -> p a t", a=4)
                P.dve(lambda e, h=h, pav=pav: e.tensor_tensor(st["X"][0][:, h, :], pav[:, 0, :], mask4[:, 0, :], ALU.mult), r=[kpa, "mask4"], w=[kX0])
                P.dve(lambda e, h=h, pav=pav: e.tensor_tensor(AM3[:, h, :].rearrange("p (a t) -> p a t", a=3), pav[:, 1:4, :], mask4[:, 1:4, :], ALU.mult),
                      r=[kpa, "mask4"], w=[("AM3", tag)])
                pz = psb[2]
                P.pe(lambda e, q=q, hb=hb, h=h: e.matmul(psb[2][:, (h % 4) * 128:(h % 4 + 1) * 128], ART[:, h, 0, :], BT[:, q, :],
                                                        start=True, stop=True), r=[kgeo], w=[("ps", 2)])
                if h % 4 == 3:
                    g4 = h // 4
                    P.dve(lambda e, g4=g4: e.tensor_tensor(st["Z"][0][:, g4 * 4:(g4 + 1) * 4, :], psb[2][:].rearrange("p (h t) -> p h t", h=4), MZ4, ALU.mult),
                          r=[("ps", 2), "dplrmask"], w=[kZ0])
            geo = dict(
                AT=lambda h: (ART[:, h, 0, :], [kgeo]),
                RT=lambda h: (ART[:, h, 1, :], [kgeo]),
                ARB=lambda h: (AM3[:, h, 0:128], [("AM3", tag)]),
                AAK=lambda h: (AM3[:, h, 128:256], [("AM3", tag)]),
                ARK=lambda h: (AM3[:, h, 256:384], [("AM3", tag)]),
                V=lambda h: (Vb[:, h * 64:(h + 1) * 64], [kgeo]),
                BD=(Bd, [kgeo]), KD=(Kd, [kgeo]), GC=lambda ch: (gC[ch], [kgeo]),
                Y=(pb["yt"], kyt), WADD=None)
            return geo

        def tile_post(b, tt):
            pb = PB[b]
            r0 = b * S + tt * 128
            kgt, kbs, kvk, kyt = ("gt", b), ("bs", b), ("vk", b), ("yt", b)
            yt, y2 = pb["yt"], T_["y2"]
            P.pool(lambda e: e.tensor_mul(y2[:], yt[:], yt[:]), r=[kyt], w=["y2"])
            P.dve(lambda e: e.tensor_reduce(small["s1"][:], h3(yt[:]), AX.X, ALU.add), r=[kyt], w=["s1"])
            P.dve(lambda e: e.tensor_reduce(small["s2"][:], h3(y2[:]), AX.X, ALU.add), r=["y2"], w=["s2"])
            P.dve(lambda e: e.tensor_scalar_mul(small["mean"][:], small["s1"][:], 1.0 / 64), r=["s1"], w=["mean"])
            P.dve(lambda e: e.tensor_mul(small["var"][:], small["mean"][:], small["mean"][:]), r=["mean"], w=["var"])
            P.dve(lambda e: e.scalar_tensor_tensor(small["var"][:], small["s2"][:], 1.0 / 64, small["var"][:], ALU.mult, ALU.subtract), r=["s2", "var"], w=["var"])
            P.dve(lambda e: e.tensor_scalar_add(small["rstd"][:], small["var"][:], 64e-5), r=["var"], w=["rstd"])
            P.act(lambda e: e.activation(small["rstd"][:], small["rstd"][:], AF.Sqrt), r=["rstd"], w=["rstd"])
            P.dve(lambda e: e.reciprocal(small["rstd"][:], small["rstd"][:]), r=["rstd"], w=["rstd"])
            P.dve(lambda e: e.tensor_sub(h3(yt[:]), h3(yt[:]), b3(small["mean"][:])), r=[kyt, "mean"], w=[kyt])
            P.dve(lambda e: e.tensor_mul(h3(yt[:]), h3(yt[:]), b3(small["rstd"][:])), r=[kyt, "rstd"], w=[kyt])
            P.pool(lambda e: e.tensor_mul(yt[:], yt[:], bc["ln_g"][:]), r=[kyt, "rw_ln_g"], w=[kyt])
            P.pool(lambda e: e.tensor_add(yt[:], yt[:], bc["ln_b"][:]), r=[kyt, "rw_ln_b"], w=[kyt])
            P.dve(lambda e: e.tensor_mul(h3(y2[:]), h3(pb["vk"][:]), b3(pb["bs"][:])), r=[kvk, kbs, "y2"], w=["y2"])
            P.dve(lambda e: e.tensor_add(yt[:], yt[:], y2[:]), r=[kyt, "y2"], w=[kyt])
            P.dve(lambda e: e.tensor_mul(y2[:], yt[:], pb["gt"][:]), r=[kyt, kgt], w=["y2"])
            P.dma(mixed[r0:r0 + 128, 1024:1536], y2[:], r=["y2"], q="pool")
    for tt in range(nt):
        geos = [tile_prep(b, tt) for b in range(NB)]
        run_interleaved([dplr_tile(cx, 8, 64, geos[b], PB[b]["st"], "rw%d" % b, banks=(3 * (b % 2), 3 * (b % 2) + 1, 3 * (b % 2) + 2))
                         for b in range(NB)])
        for b in range(NB):
            tile_post(b, tt)
    P.barrier()
    sb.reset(m)


def stage_gdn(cx, u_tok, prm, mixed):
    nc, P, sb, c, psb = cx.nc, cx.P, cx.sb, cx.c, cx.ps
    S, NB = cx.S, cx.NB
    nt = S // 128
    m = sb.mark()
    dplr_consts(cx)
    NH, NK = 4, 128
    cw_b = [sb.alloc([128, 1536], F32, "cw_b") for _ in range(4)]
    for j in range(4):
        bcast_load(cx, cw_b[j][:], prm["conv_w"][j, :], "gd_cw")
    A_b = sb.alloc([128, 4], F32, "A_b")
    dtb_b = sb.alloc([128, 4], F32, "dtb_b")
    ng_b = sb.alloc([128, 4, 128], F32, "ng_b")
    bcast_load(cx, A_b[:], prm["a_log"], "gd_A")
    bcast_load(cx, dtb_b[:], prm["dt_bias"], "gd_dtb")
    for h in range(4):
        bcast_load(cx, ng_b[:, h, :], prm["norm_g"], "gd_ng")
    P.act(lambda e: e.activation(A_b[:], A_b[:], AF.Exp), r=["gd_A"], w=["gd_A"])
    ones = sb.alloc([128, 128], F32, "ones")
    P.pool(lambda e: e.memset(ones[:], 1.0), w=["ones"])
    BIGM = sb.alloc([128, 4, 128], F32, "BIGM")
    for h in range(4):
        P.dve(lambda e, h=h: e.tensor_scalar(BIGM[:, h, :], c["ML"][:], -1.0, 30000.0, ALU.add, ALU.mult), r=["dplrmask"], w=["BIGM"])
    P.dve(lambda e: e.tensor_scalar_mul(BIGM[:], BIGM[:], -1.0), r=["BIGM"], w=["BIGM"])
    MZ4 = c["MZ"][:].unsqueeze(1).to_broadcast([128, 4, 128])
    sh = [sb.alloc([128, 1536], F32, "sh") for _ in range(4)]
    acc = sb.alloc([128, 1536], F32, "acc")
    zab = sb.alloc([128, 520], F32, "zab")
    f4 = lambda n: sb.alloc([128, 4], F32, n)
    sm = {n: f4(n) for n in ("ssq", "rnq", "rnk", "beta", "nbeta", "sp", "g", "gcs", "egc", "edec", "ssqo", "tmp", "tmp2")}
    gC = [f4("gC0"), f4("gC1")]
    diag = sb.alloc([128, 4, 128], F32, "diag")
    D4 = sb.alloc([128, 4, 128], F32, "D4")
    DS4 = sb.alloc([128, 4, 128], F32, "DS4")
    bf = lambda n: sb.alloc([128, 512], BF16, n)
    qn, kn, ta, tr, Kd, Vp = bf("qn"), bf("kn"), bf("ta"), bf("tr"), bf("Kd"), bf("Vp")
    kT, qT, AT, RT = [sb.alloc([128, 4, 128], BF16, n) for n in ("kT", "qT", "AT", "RT")]
    Zt, Art = sb.alloc([128, 4, 128], BF16, "Zt"), sb.alloc([128, 4, 128], BF16, "Art")
    XK, ArT = sb.alloc([128, 4, 128], BF16, "XK"), sb.alloc([128, 4, 128], BF16, "ArT")
    yt = sb.alloc([128, 512], F32, "yt")
    y2 = sb.alloc([128, 512], F32, "y2")
    sz = sb.alloc([128, 512], F32, "sz")
    st = dict(X=[sb.alloc([128, 4, 128], BF16, "dX") for _ in range(2)], Z=[sb.alloc([128, 4, 128], BF16, "dZ") for _ in range(2)],
              P=[sb.alloc([128, 4, 128], BF16, "dP") for _ in range(2)], ST=sb.alloc([128, 4, 128], F32, "ST"),
              STb=sb.alloc([128, 4, 128], BF16, "STb"), Wb=sb.alloc([128, 512], BF16, "Wb"), Ub=sb.alloc([128, 512], BF16, "Ub"))
    h3 = lambda t: t.rearrange("p (h d) -> p h d", h=4)
    b3 = lambda t: t.unsqueeze(2).to_broadcast([128, 4, 128])
    pbf = lambda i: psb[i][:].bitcast(BF16)
    PB = []
    for _b in range(NB):
        PB.append(dict(Kd=bf("Kd"), Vp=bf("Vp"), AT=sb.alloc([128, 4, 128], BF16, "AT"), RT=sb.alloc([128, 4, 128], BF16, "RT"),
                       XK=sb.alloc([128, 4, 128], BF16, "XK"), ArT=sb.alloc([128, 4, 128], BF16, "ArT"), gC=[f4("gC0"), f4("gC1")],
                       yt=sb.alloc([128, 512], F32, "ytb"), sz=sb.alloc([128, 512], F32, "szb"),
                       st=dict(X=[sb.alloc([128, 4, 128], BF16, "dX") for _ in range(2)], Z=[sb.alloc([128, 4, 128], BF16, "dZ") for _ in range(2)],
                               P=[sb.alloc([128, 4, 128], BF16, "dP") for _ in range(2)], ST=sb.alloc([128, 4, 128], F32, "ST"),
                               STb=sb.alloc([128, 4, 128], BF16, "STb"), Wb=sb.alloc([128, 512], BF16, "Wb"), Ub=sb.alloc([128, 512], BF16, "Ub"))))
    if True:
        def tile_prep(b, tt):
            tag = "gd%d" % b
            kST = ("dST", tag)
            pb = PB[b]
            Kd, Vp, AT, RT, XK, ArT, gC, st, yt, sz = (pb[k] for k in ("Kd", "Vp", "AT", "RT", "XK", "ArT", "gC", "st", "yt", "sz"))
            kyt, ksz = ("yt", b), ("sz", b)
            if tt == 0:
                P.pool(lambda e: e.memset(st["ST"][:], 0.0), w=[kST])
                P.pool(lambda e: e.memset(st["STb"][:], 0.0), r=[kST], w=[kST])
            r0 = b * S + tt * 128
            for j in range(4):
                d = 3 - j
                ksh = ("sh", j)
                if tt == 0 and d > 0:
                    P.pool(lambda e, j=j, d=d: e.memset(sh[j][0:d, :], 0.0), w=[ksh])
                    P.dma(sh[j][d:128, :], u_tok[r0:r0 + 128 - d, C_GDN:C_GDN + 1536], w=[ksh])
                else:
                    P.dma(sh[j][:], u_tok[r0 - d:r0 - d + 128, C_GDN:C_GDN + 1536], w=[ksh])
            P.dma(zab[:], u_tok[r0:r0 + 128, C_GDN + 1536:C_GDN + 2056], w=["zab"])
            P.dve(lambda e: e.tensor_mul(acc[:], sh[3][:], cw_b[3][:]), r=[("sh", 3), "gd_cw"], w=["acc"])
            for j in range(3):
                if j == 2:
                    P.dve(lambda e, j=j: e.tensor_mul(sh[j][:], sh[j][:], cw_b[j][:]), r=[("sh", j), "gd_cw"], w=[("sh", j)])
                else:
                    P.pool(lambda e, j=j: e.tensor_mul(sh[j][:], sh[j][:], cw_b[j][:]), r=[("sh", j), "gd_cw"], w=[("sh", j)])
                P.dve(lambda e, j=j: e.tensor_add(acc[:], acc[:], sh[j][:]), r=[("sh", j), "acc"], w=["acc"])
            P.act(lambda e: e.activation(acc[:], acc[:], AF.Silu), r=["acc"], w=["acc"])
            q_, k_, v_ = acc[:, 0:512], acc[:, 512:1024], acc[:, 1024:1536]
            P.pool(lambda e: e.tensor_mul(y2[:], q_, q_), r=["acc"], w=["y2"])
            P.dve(lambda e: e.tensor_reduce(sm["ssq"][:], h3(y2[:]), AX.X, ALU.add), r=["y2"], w=["ssq"])
            P.dve(lambda e: e.tensor_scalar_add(sm["rnq"][:], sm["ssq"][:], 1e-6), r=["ssq"], w=["rnq"])
            P.act(lambda e: e.activation(sm["rnq"][:], sm["rnq"][:], AF.Sqrt), r=["rnq"], w=["rnq"])
            P.dve(lambda e: e.reciprocal(sm["rnq"][:], sm["rnq"][:]), r=["rnq"], w=["rnq"])
            P.dve(lambda e: e.tensor_scalar_mul(sm["rnq"][:], sm["rnq"][:], 128 ** -0.5), r=["rnq"], w=["rnq"])
            P.pool(lambda e: e.tensor_mul(y2[:], k_, k_), r=["acc", "ssq"], w=["y2"])
            P.dve(lambda e: e.tensor_reduce(sm["ssq"][:], h3(y2[:]), AX.X, ALU.add), r=["y2", "rnq"], w=["ssq"])
            P.dve(lambda e: e.tensor_scalar_add(sm["rnk"][:], sm["ssq"][:], 1e-6), r=["ssq"], w=["rnk"])
            P.act(lambda e: e.activation(sm["rnk"][:], sm["rnk"][:], AF.Sqrt), r=["rnk"], w=["rnk"])
            P.dve(lambda e: e.reciprocal(sm["rnk"][:], sm["rnk"][:]), r=["rnk"], w=["rnk"])
            kg = ("gdgeo", tag)
            P.dve(lambda e: e.tensor_mul(h3(qn[:]), h3(q_), b3(sm["rnq"][:])), r=["acc", "rnq"], w=[kg])
            P.dve(lambda e: e.tensor_mul(h3(kn[:]), h3(k_), b3(sm["rnk"][:])), r=["acc", "rnk"], w=[kg])
            P.act(lambda e: e.activation(sm["beta"][:], zab[:, 512:516], AF.Sigmoid), r=["zab"], w=["beta"])
            P.dve(lambda e: e.tensor_scalar_mul(sm["nbeta"][:], sm["beta"][:], -1.0), r=["beta"], w=["nbeta"])
            P.dve(lambda e: e.tensor_add(sm["sp"][:], zab[:, 516:520], dtb_b[:]), r=["zab", "gd_dtb"], w=["sp"])
            P.act(lambda e: e.activation(sm["tmp"][:], sm["sp"][:], AF.Abs), r=["sp"], w=["tmp"])
            P.act(lambda e: e.activation(sm["tmp"][:], sm["tmp"][:], AF.Exp, scale=-1.0), r=["tmp"], w=["tmp"])
            P.act(lambda e: e.activation(sm["tmp"][:], sm["tmp"][:], AF.Ln, bias=1.0), r=["tmp"], w=["tmp"])
            P.dve(lambda e: e.tensor_scalar_max(sm["sp"][:], sm["sp"][:], 0.0), r=["sp"], w=["sp"])
            P.dve(lambda e: e.tensor_add(sm["sp"][:], sm["sp"][:], sm["tmp"][:]), r=["sp", "tmp"], w=["sp"])
            P.dve(lambda e: e.scalar_tensor_tensor(sm["g"][:], sm["sp"][:], -1.0, A_b[:], ALU.mult, ALU.mult), r=["sp", "gd_A"], w=["g"])
            P.pe(lambda e: e.matmul(psb[7][:, 0:4], c["MI"][:], sm["g"][:], start=True, stop=True), r=["dplrmask", "g"], w=[("ps", 7)])
            P.pe(lambda e: e.matmul(psb[7][:, 4:8], c["BLK"][:], sm["g"][:], start=True, stop=True), r=["dplrmask", "g"], w=[("ps", 7)])
            P.act(lambda e: e.copy(sm["gcs"][:], psb[7][:, 0:4]), r=[("ps", 7)], w=["gcs"])
            P.dve(lambda e: e.tensor_sub(sm["edec"][:], psb[7][:, 4:8], sm["gcs"][:]), r=[("ps", 7), "gcs"], w=["edec"])
            P.act(lambda e: e.activation(sm["edec"][:], sm["edec"][:], AF.Exp), r=["edec"], w=["edec"])
            P.act(lambda e: e.activation(sm["egc"][:], sm["gcs"][:], AF.Exp), r=["gcs"], w=["egc"])
            P.pe(lambda e: e.matmul(psb[3][:, 0:4], ones[0:64, :], sm["g"][0:64, :], start=True, stop=True), r=["ones", "g"], w=[("ps", 3)])
            P.pe(lambda e: e.matmul(psb[4][:, 0:4], ones[64:128, :], sm["g"][64:128, :], start=True, stop=True), r=["ones", "g"], w=[("ps", 4)])
            P.act(lambda e: e.activation(gC[0][:], psb[3][:, 0:4], AF.Exp), r=[("ps", 3)], w=[kg])
            P.act(lambda e: e.activation(gC[1][:], psb[4][:, 0:4], AF.Exp), r=[("ps", 4)], w=[kg])
            P.dve(lambda e: e.tensor_mul(h3(tr[:]), h3(qn[:]), b3(sm["egc"][:])), r=[kg, "egc"], w=["tr"])
            P.dve(lambda e: e.tensor_mul(sm["tmp2"][:], sm["nbeta"][:], sm["egc"][:]), r=["nbeta", "egc"], w=["tmp2"])
            P.dve(lambda e: e.tensor_mul(h3(ta[:]), h3(kn[:]), b3(sm["tmp2"][:])), r=[kg, "tmp2"], w=["ta"])
            P.dve(lambda e: e.tensor_mul(h3(Kd[:]), h3(kn[:]), b3(sm["edec"][:])), r=[kg, "edec"], w=[kg])
            P.dve(lambda e: e.tensor_mul(h3(Vp[:]), h3(v_), b3(sm["beta"][:])), r=["acc", "beta"], w=[kg])
            for (src, ksrc, dst) in ((kn, kg, kT), (qn, kg, qT), (ta, "ta", AT), (tr, "tr", RT)):
                for h in range(4):
                    P.pe(lambda e, src=src, h=h: e.transpose(pbf(7)[:, h * 128:(h + 1) * 128], src[:, h * 128:(h + 1) * 128], c["identb"][:]),
                         r=[ksrc, "identb"], w=[("ps", 7)])
                P.act(lambda e, dst=dst: e.copy(dst[:], pbf(7)[:, 0:512].rearrange("p (h t) -> p h t", h=4)), r=[("ps", 7)], w=[kg])
            for h in range(4):
                P.dve(lambda e, h=h: e.tensor_scalar_mul(diag[:, h, :], c["ident"][:], sm["gcs"][:, h:h + 1]), r=["gcs", "ident"], w=["diag"])
            P.pe(lambda e: e.matmul(psb[5][:], ones[:], diag[:].rearrange("p h t -> p (h t)"), start=True, stop=False), r=["ones", "diag"], w=[("ps", 5)])
            P.pe(lambda e: e.matmul(psb[5][:], c["ident"][:], BIGM[:].rearrange("p h t -> p (h t)"), start=False, stop=True), r=["ident", "BIGM"], w=[("ps", 5)])
            for h in range(4):
                P.act(lambda e, h=h: e.activation(D4[:, h, :], psb[5][:, h * 128:(h + 1) * 128], AF.Exp, bias=sm["gcs"][:, h:h + 1], scale=-1.0),
                      r=[("ps", 5), "gcs"], w=["D4"])
            P.pool(lambda e: e.tensor_mul(DS4[:], D4[:], MZ4), r=["D4", "dplrmask"], w=["DS4"])
            for h in range(4):
                P.pe(lambda e, h=h: e.matmul(psb[3][:, h * 128:(h + 1) * 128], kT[:, h, :], kT[:, h, :], start=True, stop=True), r=[kg], w=[("ps", 3)])
            for h in range(4):
                P.pe(lambda e, h=h: e.matmul(psb[4][:, h * 128:(h + 1) * 128], qT[:, h, :], kT[:, h, :], start=True, stop=True), r=[kg], w=[("ps", 4)])
            for h in range(4):
                P.dve(lambda e, h=h: e.scalar_tensor_tensor(Zt[:, h, :], psb[3][:, h * 128:(h + 1) * 128], sm["nbeta"][:, h:h + 1], DS4[:, h, :],
                                                          ALU.mult, ALU.mult), r=[("ps", 3), "nbeta", "DS4"], w=["Zt"])
            P.dve(lambda e: e.tensor_tensor(Art[:], psb[4][:].rearrange("p (h t) -> p h t", h=4), D4[:], ALU.mult), r=[("ps", 4), "D4"], w=["Art"])
            kX0, kZ0 = ("dX", tag, 0), ("dZ", tag, 0)
            P.pool(lambda e: e.tensor_copy(st["Z"][0][:], Zt[:]), r=["Zt"], w=[kZ0])
            for h in range(4):
                P.pe(lambda e, h=h: e.transpose(pbf(7)[:, h * 128:(h + 1) * 128], Zt[:, h, :], c["identb"][:]), r=["Zt", "identb"], w=[("ps", 7)])
            P.act(lambda e: e.copy(XK[:], pbf(7)[:, 0:512].rearrange("p (h t) -> p h t", h=4)), r=[("ps", 7)], w=[kg])
            P.dve(lambda e: e.tensor_copy(st["X"][0][:], pbf(7)[:, 0:512].rearrange("p (h t) -> p h t", h=4)), r=[("ps", 7)], w=[kX0])
            for h in range(4):
                P.pe(lambda e, h=h: e.transpose(pbf(7)[:, h * 128:(h + 1) * 128], Art[:, h, :], c["identb"][:]), r=["Art", "identb"], w=[("ps", 7)])
            P.act(lambda e: e.copy(ArT[:], pbf(7)[:, 0:512].rearrange("p (h t) -> p h t", h=4)), r=[("ps", 7)], w=[kg])
            geo = dict(
                AT=lambda h: (AT[:, h, :], [kg]), RT=lambda h: (RT[:, h, :], [kg]),
                ARB=lambda h: (ArT[:, h, :], [kg]), ARK=lambda h: (ArT[:, h, :], [kg]), AAK=lambda h: (XK[:, h, :], [kg]),
                V=lambda h: (Vp[:, h * 128:(h + 1) * 128], [kg]),
                BD=(Kd, [kg]), KD=(Kd, [kg]), GC=lambda ch: (gC[ch], [kg]), Y=(yt, kyt), WADD=None)
            P.act(lambda e: e.activation(sz[:], zab[:, 0:512], AF.Silu), r=["zab"], w=[ksz])
            return geo

        def tile_post(b, tt):
            pb = PB[b]
            yt, sz = pb["yt"], pb["sz"]
            kyt, ksz = ("yt", b), ("sz", b)
            r0 = b * S + tt * 128
            P.pool(lambda e: e.tensor_mul(y2[:], yt[:], yt[:]), r=[kyt], w=["y2"])
            P.dve(lambda e: e.tensor_reduce(sm["ssqo"][:], h3(y2[:]), AX.X, ALU.add), r=["y2"], w=["ssqo"])
            rstd_from_ssq(P, sm["ssqo"][:], sm["ssqo"][:], 128, RMS_EPS, ["ssqo"], ["ssqo"])
            P.dve(lambda e: e.tensor_mul(h3(yt[:]), h3(yt[:]), b3(sm["ssqo"][:])), r=[kyt, "ssqo"], w=[kyt])
            P.pool(lambda e: e.tensor_mul(yt[:], yt[:], ng_b[:].rearrange("p h d -> p (h d)")), r=[kyt, "gd_ng"], w=[kyt])
            P.dve(lambda e: e.tensor_mul(y2[:], yt[:], sz[:]), r=[kyt, ksz, "ssqo"], w=["y2"])
            P.dma(mixed[r0:r0 + 128, 1536:2048], y2[:], r=["y2"], q="pool")
    for tt in range(nt):
        geos = [tile_prep(b, tt) for b in range(NB)]
        run_interleaved([dplr_tile(cx, 4, 128, geos[b], PB[b]["st"], "gd%d" % b, banks=(3 * (b % 2), 3 * (b % 2) + 1, 3 * (b % 2) + 2))
                         for b in range(NB)])
        for b in range(NB):
            tile_post(b, tt)
    P.barrier()
    sb.reset(m)


MOE_PHASES = "123"
MOE_CAST = "ad"
MOE_CAP = 384


def stage_moe(cx, h1, prm, x_out, xs_d, ys_d):
    nc, P, sb, c, psb = cx.nc, cx.P, cx.sb, cx.c, cx.ps
    T = cx.T
    ntt = T // 128
    CAP = MOE_CAP
    NROW = 32 * CAP
    BIG = 1.0e4
    m = sb.mark()
    route_i = sb.alloc([128, ntt, 2], I32, "route_i")
    route_g = sb.alloc([128, ntt, 2], F32, "route_g")
    m2 = sb.mark()
    Wr = sb.alloc([128, 16, 36], F32, "Wr")
    P.dma(Wr[:, :, 0:4], prm["w_grp"].rearrange("(kc p) n -> p kc n", p=128), w=["Wr"], slow=True)
    P.dma(Wr[:, :, 4:36], prm["w_exp"].rearrange("(kc p) n -> p kc n", p=128), w=["Wr"], slow=True)
    br_b = sb.alloc([128, 36], F32, "br_b")
    bcast_load(cx, br_b[:, 0:4], prm["b_grp"], "br")
    bcast_load(cx, br_b[:, 4:36], prm["b_exp"], "br")
    SUb, eoff, trash = c["SUb"], c["eoff"], c["trash"]
    onesb = sb.alloc([128, 128], BF16, "onesb")
    P.pool(lambda e: e.memset(onesb[:], 1.0), w=["onesb"])
    zf = sb.alloc([128, 2048], F32, "zf")
    P.pool(lambda e: e.memset(zf[:], 0.0), w=["zf"])
    P.dma(ys_d[NROW:NROW + 128, :], zf[:], r=["zf"], w=["ys_d0"])
    carry = sb.alloc([128, 32], F32, "carry")
    P.pool(lambda e: e.memset(carry[:], 0.0), w=["carry"])
    hrow = [sb.alloc([128, 2048], F32, "hrow") for _ in range(2)]
    hbf = [sb.alloc([128, 2048], BF16, "hbf") for _ in range(2)]
    hT = [sb.alloc([128, 16, 128], F32, "hT") for _ in range(2)]
    f = lambda n, w: sb.alloc([128, w], F32, n)
    lg, mg, eg, sg_, pg, gsel, tmp4 = f("lg", 36), f("mg", 1), f("eg", 4), f("sg", 1), f("pg", 1), f("gsel", 4), f("tmp4", 4)
    lem, top8, sel1, sel2, selb_f, pos, tmp32 = f("lem", 32), f("top8", 8), f("sel1", 32), f("sel2", 32), f("selsum", 32), f("pos", 32), f("tmp32", 32)
    selb = sb.alloc([128, 32], BF16, "selb")
    dd, w1, w2, slotf, valid = f("dd", 1), f("w1", 1), f("w2", 1), f("slotf", 2), f("valid", 2)
    zt = sb.alloc([128, 2048], BF16, "zt")
    P.pool(lambda e: e.memset(zt[:], 0.0), w=["zt"])
    rpp = NROW // 128
    for z0 in range(0, rpp, 16):
        P.dma(xs_d.rearrange("(p r) d -> p r d", p=128)[:, z0:z0 + 16, :], zt[:].unsqueeze(1).to_broadcast([128, 16, 2048]), r=["zt"], w=["xs_d"])
    nev = 0
    for tt in range(ntt):
        i2 = tt % 2
        r0 = tt * 128
        kh, khb, khT = ("hrow", i2), ("hbf", i2), ("hT", i2)
        P.dma(hrow[i2][:], h1[r0:r0 + 128, :], w=[kh])
        P.act(lambda e, i2=i2: e.copy(hbf[i2][:], hrow[i2][:]), r=[kh], w=[khb])
        for g in range(4):
            pi = nev % 2
            nev += 1
            for j in range(4):
                kc = g * 4 + j
                P.pe(lambda e, pi=pi, kc=kc, j=j, i2=i2: e.transpose(psb[pi][:, j * 128:(j + 1) * 128], hrow[i2][:, kc * 128:(kc + 1) * 128], c["ident"][:]),
                     r=[kh, "ident"], w=[("ps", pi)])
            evac(P, nev, hT[i2][:, g * 4:(g + 1) * 4, :], psb[pi][:].rearrange("p (j t) -> p j t", j=4), r=[("ps", pi)], w=[khT])
        for kc in range(16):
            P.pe(lambda e, kc=kc, i2=i2: e.matmul(psb[2][:, 0:36], hT[i2][:, kc, :], Wr[:, kc, :], start=(kc == 0), stop=(kc == 15)),
                 r=[khT, "Wr"], w=[("ps", 2)])
        K = "rt"
        P.dve(lambda e: e.tensor_add(lg[:], psb[2][:, 0:36], br_b[:]), r=[("ps", 2), "br"], w=[K])
        P.dve(lambda e: e.tensor_reduce(mg[:], lg[:, 0:4], AX.X, ALU.max), r=[K], w=[K])
        P.dve(lambda e: e.tensor_scalar(gsel[:], lg[:, 0:4], mg[:, 0:1], None, ALU.is_equal), r=[K], w=[K])
        P.dve(lambda e: e.tensor_scalar_mul(tmp4[:, 0:1], mg[:], -1.0), r=[K], w=[K])
        P.act(lambda e: e.activation(eg[:], lg[:, 0:4], AF.Exp, bias=tmp4[:, 0:1], accum_out=sg_[:]), r=[K], w=[K])
        P.dve(lambda e: e.reciprocal(pg[:], sg_[:]), r=[K], w=[K])
        P.dve(lambda e: e.tensor_scalar(tmp4[:], gsel[:], BIG, -BIG, ALU.mult, ALU.add), r=[K], w=[K])
        P.dve(lambda e: e.tensor_add(lem[:].rearrange("p (g x) -> p g x", g=4), lg[:, 4:36].rearrange("p (g x) -> p g x", g=4),
                                    tmp4[:].unsqueeze(2).to_broadcast([128, 4, 8])), r=[K], w=[K])
        P.dve(lambda e: e.max(top8[:], lem[:]), r=[K], w=[K])
        P.dve(lambda e: e.tensor_scalar(sel1[:], lem[:], top8[:, 0:1], None, ALU.is_equal), r=[K], w=[K])
        P.dve(lambda e: e.tensor_scalar(sel2[:], lem[:], top8[:, 1:2], None, ALU.is_equal), r=[K], w=[K])
        P.dve(lambda e: e.tensor_sub(dd[:], top8[:, 1:2], top8[:, 0:1]), r=[K], w=[K])
        P.act(lambda e: e.activation(dd[:], dd[:], AF.Exp), r=[K], w=[K])
        P.dve(lambda e: e.tensor_scalar_add(w1[:], dd[:], 1.0), r=[K], w=[K])
        P.dve(lambda e: e.reciprocal(w1[:], w1[:]), r=[K], w=[K])
        P.dve(lambda e: e.tensor_mul(w2[:], w1[:], dd[:]), r=[K], w=[K])
        P.dve(lambda e: e.tensor_add(selb_f[:], sel1[:], sel2[:]), r=[K], w=[K])
        P.dve(lambda e: e.tensor_copy(selb[:], selb_f[:]), r=[K], w=["selb"])
        P.pe(lambda e: e.matmul(psb[3][:, 0:32], SUb[:], selb[:], start=True, stop=True), r=["SUb", "selb"], w=[("ps", 3)])
        P.pe(lambda e: e.matmul(psb[3][:, 32:64], onesb[:], selb[:], start=True, stop=True), r=["onesb", "selb"], w=[("ps", 3)])
        P.dve(lambda e: e.tensor_add(pos[:], psb[3][:, 0:32], carry[:]), r=[("ps", 3), "carry"], w=[K])
        P.dve(lambda e: e.tensor_add(carry[:], carry[:], psb[3][:, 32:64]), r=[("ps", 3), "carry"], w=["carry"])
        for k_, selk in enumerate((sel1, sel2)):
            P.dve(lambda e, selk=selk: e.tensor_mul(tmp32[:], selk[:], pos[:]), r=[K], w=[K])
            P.dve(lambda e, k_=k_: e.tensor_reduce(slotf[:, k_:k_ + 1], tmp32[:], AX.X, ALU.add), r=[K], w=[K])
            P.dve(lambda e, k_=k_: e.tensor_scalar(valid[:, k_:k_ + 1], slotf[:, k_:k_ + 1], float(CAP) - 0.5, None, ALU.is_lt), r=[K], w=[K])
            P.dve(lambda e, selk=selk: e.tensor_mul(tmp32[:], selk[:], eoff[:]), r=[K, "eoff"], w=[K])
            P.dve(lambda e: e.tensor_reduce(dd[:], tmp32[:], AX.X, ALU.add), r=[K], w=[K])
            P.dve(lambda e, k_=k_: e.tensor_add(slotf[:, k_:k_ + 1], slotf[:, k_:k_ + 1], dd[:]), r=[K], w=[K])
            P.dve(lambda e, k_=k_: e.tensor_sub(slotf[:, k_:k_ + 1], slotf[:, k_:k_ + 1], trash[:]), r=[K, "trash"], w=[K])
            P.dve(lambda e, k_=k_: e.tensor_mul(slotf[:, k_:k_ + 1], slotf[:, k_:k_ + 1], valid[:, k_:k_ + 1]), r=[K], w=[K])
            P.dve(lambda e, k_=k_: e.tensor_add(slotf[:, k_:k_ + 1], slotf[:, k_:k_ + 1], trash[:]), r=[K, "trash"], w=[K])
            wk = w1 if k_ == 0 else w2
            P.dve(lambda e, k_=k_, wk=wk, tt=tt: e.scalar_tensor_tensor(route_g[:, tt, k_:k_ + 1], wk[:], pg[:, 0:1], valid[:, k_:k_ + 1], ALU.mult, ALU.mult),
                  r=[K], w=[("route", tt)])
        P.dve(lambda e, tt=tt: e.tensor_copy(route_i[:, tt, :], slotf[:]), r=[K], w=[("route", tt)])
        for k_ in range(2):
            P.add("pool", lambda e, tt=tt, k_=k_, i2=i2: e.indirect_dma_start(
                out=xs_d[:, :], out_offset=bass.IndirectOffsetOnAxis(ap=route_i[:, tt, k_:k_ + 1], axis=0),
                in_=hbf[i2][:, :], in_offset=None), r=[khb, ("route", tt)], w=["xs_d"], sw=True)
    P.barrier()
    sb.reset(m2)
    stg = [sb.alloc([128, 8, 512], F32, "stg") for _ in range(3)]
    wg = [sb.alloc([128, 16, 512], BF16, "wg") for _ in range(2)]
    wu = [sb.alloc([128, 16, 512], BF16, "wu") for _ in range(2)]
    wd = [sb.alloc([128, 4, 2048], BF16, "wd") for _ in range(2)]
    xrow = [sb.alloc([128, 2048], BF16, "xrowb") for _ in range(2)]
    xsT = sb.alloc([128, 16, CAP], BF16, "xsT")
    HT = sb.alloc([128, 4, CAP], BF16, "HT")
    sgt = [sb.alloc([128, CAP], F32, "sgt") for _ in range(2)]
    yst = [sb.alloc([128, 2048], F32, "yst") for _ in range(2)]
    nst = 0
    ncast = 0
    nps = 0
    ntl = CAP // 128
    pbf = lambda i: psb[i][:].bitcast(BF16)

    def cast(dst, src, r, w):
        nonlocal ncast
        i = {"ad": (ncast % 2) + 1, "d": 2, "dda": (2, 2, 1)[ncast % 3], "pad": ncast % 3}[MOE_CAST]
        ncast += 1
        if i == 0:
            P.pool(lambda e: e.tensor_copy(dst, src), r=r, w=w)
        elif i == 1:
            P.act(lambda e: e.copy(dst, src), r=r, w=w)
        else:
            P.dve(lambda e: e.tensor_copy(dst, src), r=r, w=w)

    for ex in range(32 if "2" in MOE_PHASES else 0):
        e2 = ex % 2
        kw = ("wexp", e2)
        for (dst, src4) in ((wg[e2], prm["w_gate"][ex].rearrange("(kc p) n -> p kc n", p=128)),
                            (wu[e2], prm["w_up"][ex].rearrange("(kc p) n -> p kc n", p=128))):
            for hf in range(2):
                s_, ks = stg[nst % 3], ("stg", nst % 3)
                nst += 1
                P.dma(s_[:], src4[:, hf * 8:(hf + 1) * 8, :], w=[ks])
                cast(dst[:, hf * 8:(hf + 1) * 8, :], s_[:], [ks], [kw])
        wdv = prm["w_down"][ex].rearrange("(fc p) n -> p fc n", p=128)
        for hf in range(2):
            s_, ks = stg[nst % 3], ("stg", nst % 3)
            nst += 1
            sv = s_[:].rearrange("p a b -> p (a b)").rearrange("p (f n) -> p f n", f=2)
            P.dma(sv, wdv[:, hf * 2:(hf + 1) * 2, :], w=[ks])
            cast(wd[e2][:, hf * 2:(hf + 1) * 2, :], sv, [ks], [kw])
        for tl in range(ntl):
            xr, kxr = xrow[tl % 2], ("xrowb", tl % 2)
            P.dma(xr[:], xs_d[ex * CAP + tl * 128: ex * CAP + (tl + 1) * 128, :], r=["xs_d"], w=[kxr])
            for g in range(2):
                pi = nps % 2
                nps += 1
                for j in range(8):
                    kc = g * 8 + j
                    P.pe(lambda e, pi=pi, xr=xr, kc=kc, j=j: e.transpose(pbf(pi)[:, j * 128:(j + 1) * 128], xr[:, kc * 128:(kc + 1) * 128], c["identb"][:]),
                         r=[kxr, "identb"], w=[("ps", pi)])
                evac(P, nps, xsT[:, g * 8:(g + 1) * 8, tl * 128:(tl + 1) * 128], pbf(pi).rearrange("p (j t) -> p j t", j=8), r=[("ps", pi)], w=["xsT"])
        for fc in range(4):
            pg_, pu_ = 2 + (fc % 2) * 2, 3 + (fc % 2) * 2
            for kc in range(16):
                P.pe(lambda e, kc=kc, fc=fc, pg_=pg_, e2=e2: e.matmul(psb[pg_][:, 0:CAP], wg[e2][:, kc, fc * 128:(fc + 1) * 128], xsT[:, kc, :],
                                                                   start=(kc == 0), stop=(kc == 15)), r=[kw, "xsT"], w=[("ps", pg_)])
            for kc in range(16):
                P.pe(lambda e, kc=kc, fc=fc, pu_=pu_, e2=e2: e.matmul(psb[pu_][:, 0:CAP], wu[e2][:, kc, fc * 128:(fc + 1) * 128], xsT[:, kc, :],
                                                                   start=(kc == 0), stop=(kc == 15)), r=[kw, "xsT"], w=[("ps", pu_)])
            sg2, ksg = sgt[fc % 2], ("sgt", fc % 2)
            P.act(lambda e, sg2=sg2, pg_=pg_: e.activation(sg2[:], psb[pg_][:, 0:CAP], AF.Silu), r=[("ps", pg_)], w=[ksg])
            P.dve(lambda e, sg2=sg2, pu_=pu_, fc=fc: e.tensor_tensor(HT[:, fc, :], sg2[:], psb[pu_][:, 0:CAP], ALU.mult), r=[ksg, ("ps", pu_)], w=["HT"])
        for tl in range(ntl):
            ys, kys = yst[tl % 2], ("yst", tl % 2)
            for db in range(4):
                pi = 6 + nps % 2
                nps += 1
                for fc in range(4):
                    P.pe(lambda e, pi=pi, fc=fc, tl=tl, db=db, e2=e2: e.matmul(psb[pi][:], HT[:, fc, tl * 128:(tl + 1) * 128], wd[e2][:, fc, db * 512:(db + 1) * 512],
                                                                            start=(fc == 0), stop=(fc == 3)), r=["HT", kw], w=[("ps", pi)])
                evac(P, nps, ys[:, db * 512:(db + 1) * 512], psb[pi][:], r=[("ps", pi)], w=[kys])
            P.dma(ys_d[ex * CAP + tl * 128: ex * CAP + (tl + 1) * 128, :], ys[:], r=[kys], w=["ys_d"], q="pool")
    P.barrier()
    sb.reset(m2)
    y1 = [sb.alloc([128, 2048], F32, "y1") for _ in range(2)]
    y2 = [sb.alloc([128, 2048], F32, "y2") for _ in range(2)]
    hr = [sb.alloc([128, 2048], F32, "hr") for _ in range(2)]
    pre = sb.alloc([128, 2048], F32, "pre")
    g_b = sb.alloc([128, 2048], F32, "g_b")
    b_b = sb.alloc([128, 2048], F32, "b_b")
    stats = sb.alloc([128, 4, 6], F32, "stats")
    mv = sb.alloc([128, 2], F32, "mv")
    rstd = sb.alloc([128, 1], F32, "rstd")
    bcast_load(cx, g_b[:], prm["ln_g"], "lnp")
    bcast_load(cx, b_b[:], prm["ln_b"], "lnp")
    for i2 in range(2):
        P.pool(lambda e, i2=i2: e.memset(y1[i2][:], 0.0), w=[("y1", i2)])
        P.pool(lambda e, i2=i2: e.memset(y2[i2][:], 0.0), w=[("y2", i2)])
    for tt in range(ntt if "3" in MOE_PHASES else 0):
        i2 = tt % 2
        r0 = tt * 128
        for k_, yy in enumerate((y1, y2)):
            ky = ("y1" if k_ == 0 else "y2", i2)
            P.add("pool", lambda e, tt=tt, k_=k_, yy=yy, i2=i2: e.indirect_dma_start(
                out=yy[i2][:, :], out_offset=None, in_=ys_d[:, :],
                in_offset=bass.IndirectOffsetOnAxis(ap=route_i[:, tt, k_:k_ + 1], axis=0)),
                r=["ys_d", ("route", tt)], w=[ky], sw=True)
        khr = ("hr", i2)
        P.dma(hr[i2][:], h1[r0:r0 + 128, :], w=[khr])
        P.dve(lambda e, i2=i2, tt=tt: e.tensor_scalar_mul(y1[i2][:], y1[i2][:], route_g[:, tt, 0:1]), r=[("y1", i2), ("route", tt)], w=[("y1", i2)])
        P.dve(lambda e, i2=i2, tt=tt: e.scalar_tensor_tensor(y1[i2][:], y2[i2][:], route_g[:, tt, 1:2], y1[i2][:], ALU.mult, ALU.add),
              r=[("y1", i2), ("y2", i2), ("route", tt)], w=[("y1", i2)])
        P.dve(lambda e, i2=i2: e.scalar_tensor_tensor(pre[:], hr[i2][:], ALPHA, y1[i2][:], ALU.mult, ALU.add), r=[khr, ("y1", i2)], w=["pre"])
        layer_norm_tile(cx, pre[:], hr[i2][:], g_b[:], b_b[:], stats, mv, rstd, "pre", khr, "lnst", aff="dve")
        P.dma(x_out[r0:r0 + 128, :], hr[i2][:], r=[khr], q="pool")
    P.barrier()
    sb.reset(m)


PARAM_SHAPES = {
    "w_in": [2048, N_IN], "fox_b_f": [8], "fox_out_g": [512], "mla_q_norm_g": [384], "mla_kv_norm_g": [128],
    "mla_w_uq": [384, 768], "mla_w_ukv": [128, 1024], "mla_out_g": [512], "rwkv_mu": [1792], "rwkv_w0": [512],
    "rwkv_w2": [64, 512], "rwkv_a0": [512], "rwkv_a2": [64, 512], "rwkv_g2": [128, 512], "rwkv_k_k": [512],
    "rwkv_k_a": [512], "rwkv_r_k": [512], "rwkv_ln_g": [512], "rwkv_ln_b": [512], "gdn_conv_w": [4, 1536],
    "gdn_a_log": [4], "gdn_dt_bias": [4], "gdn_norm_g": [128], "w_out": [2048, 2048], "ln1_g": [2048], "ln1_b": [2048],
    "moe_w_grp": [2048, 4], "moe_b_grp": [4], "moe_w_exp": [2048, 32], "moe_b_exp": [32], "moe_w_gate": [32, 2048, 512],
    "moe_w_up": [32, 2048, 512], "moe_w_down": [32, 512, 2048], "ln2_g": [2048], "ln2_b": [2048],
}


def build_program(S, NB, depth):
    nc = bass.Bass("TRN2", target_bir_lowering=False)
    cx = Ctx(nc, S, NB)
    T = cx.T
    x_in = cx.dt("x", [T, 2048], F32, "ExternalInput")
    pos = cx.dt("positions", [NB, S], I32, "ExternalInput")
    ifr = cx.dt("inv_freq", [32], F32, "ExternalInput")
    W = {k: cx.dt(k, [depth] + v, F32, "ExternalInput") for k, v in PARAM_SHAPES.items()}
    y = cx.dt("y", [T, 2048], F32, "ExternalOutput")
    u_tok = cx.dt("u_tok", [T, N_IN], F32)
    qkT = cx.dt("qkT", [1024, T], BF16)
    fT = cx.dt("fT", [8, T], F32)
    mixed = cx.dt("mixed", [T, 2048], F32)
    h1 = cx.dt("h1", [T, 2048], F32)
    xs_d = cx.dt("xs_d", [32 * MOE_CAP + 128, 2048], BF16)
    ys_d = cx.dt("ys_d", [32 * MOE_CAP + 128, 2048], F32)
    xbuf = [cx.dt("xres%d" % i, [T, 2048], F32) for i in range(2)]
    make_consts(cx)
    cur = x_in
    for l in range(depth):
        nxt = y if l == depth - 1 else xbuf[l % 2]
        stage_inproj(cx, cur, W["w_in"][l], u_tok, qkT, fT)
        stage_fox(cx, qkT, fT, u_tok, W["fox_b_f"][l], W["fox_out_g"][l], mixed)
        stage_mla(cx, u_tok, pos, ifr, W["mla_q_norm_g"][l], W["mla_kv_norm_g"][l], W["mla_w_uq"][l], W["mla_w_ukv"][l],
                  W["mla_out_g"][l], mixed)
        stage_rwkv(cx, u_tok, {k: W["rwkv_" + k][l] for k in ("mu", "w0", "w2", "a0", "a2", "g2", "k_k", "k_a", "r_k", "ln_g", "ln_b")}, mixed)
        stage_gdn(cx, u_tok, {k: W["gdn_" + k][l] for k in ("conv_w", "a_log", "dt_bias", "norm_g")}, mixed)
        stage_outproj_ln(cx, mixed, W["w_out"][l], cur, W["ln1_g"][l], W["ln1_b"][l], h1)
        prm = {"w_grp": W["moe_w_grp"][l], "b_grp": W["moe_b_grp"][l], "w_exp": W["moe_w_exp"][l], "b_exp": W["moe_b_exp"][l],
               "w_gate": W["moe_w_gate"][l], "w_up": W["moe_w_up"][l], "w_down": W["moe_w_down"][l], "ln_g": W["ln2_g"][l], "ln_b": W["ln2_b"][l]}
        stage_moe(cx, h1, prm, nxt, xs_d, ys_d)
        cur = nxt
    cx.P.emit()
    return nc, cx


def kernel(**inputs):
    n_cores = 8
    x = np.ascontiguousarray(np.asarray(inputs["x"], dtype=np.float32))
    B, S, Dm = x.shape
    NB = B // n_cores
    depth = int(np.asarray(inputs["w_in"]).shape[0])
    nc, _ = build_program(S, NB, depth)
    positions = np.ascontiguousarray(np.asarray(inputs["positions"], dtype=np.int32))
    inv_freq = (np.float32(10000.0) ** (-np.arange(32, dtype=np.float32) / np.float32(32))).astype(np.float32)
    shared = {k: np.ascontiguousarray(np.asarray(inputs[k], dtype=np.float32)) for k in PARAM_SHAPES}
    in_maps = []
    for c in range(n_cores):
        d = dict(shared)
        d["x"] = x[c * NB:(c + 1) * NB].reshape(NB * S, Dm)
        d["positions"] = positions[c * NB:(c + 1) * NB]
        d["inv_freq"] = inv_freq
        in_maps.append(d)
    res = run_bass_kernel_spmd(nc, in_maps, core_ids=list(range(n_cores)))
    out = np.concatenate([np.asarray(r["y"], dtype=np.float32).reshape(NB, S, Dm) for r in res.results], axis=0)
    return out
```
